# Optimizing a Trainium2 kernel written in Bass

```python
import jax, jax.numpy as jnp
from jax import lax

D_MODEL = 1024
BATCH = 4
SEQ = 4096
DEPTH = 2

GRID_W = 64
CTX_LEN = 256
NORM_EPS = 1e-6
ROPE_THETA = 10000.0
RET_HEADS = 4
RET_DK = 128
RET_DV = 256
RET_CHUNK = 128
SWA_HEADS = 16
SWA_KV_HEADS = 2
SWA_HD = 64
WINDOW = 128
GA_HEADS = 8
GA_KV_HEADS = 2
GA_HD = 128
Q_BLOCK = 128
N_EXPERTS = 16
N_GROUPS = 4
EXPERTS_PER_GROUP = N_EXPERTS // N_GROUPS
TOP_K = 2
D_EXPERT = 512
MOE_BLOCK = 128

RET_QK = RET_HEADS * RET_DK
RET_V = RET_HEADS * RET_DV
SWA_Q = SWA_HEADS * SWA_HD
SWA_KV = SWA_KV_HEADS * SWA_HD
GA_Q = GA_HEADS * GA_HD
GA_KV = GA_KV_HEADS * GA_HD
SPLITS = (RET_QK, RET_QK, RET_V, RET_V, SWA_Q, SWA_KV, SWA_KV, GA_Q, GA_KV, GA_KV, D_MODEL, D_MODEL, D_MODEL)
D_IN = 2 * RET_QK + 2 * RET_V + SWA_Q + 2 * SWA_KV + GA_Q + 2 * GA_KV + 3 * D_MODEL

kernel_name = 'hybrid_retention_swa_gqa_moe_dit'


def rms_normalize(x):
    x32 = x.astype(jnp.float32)
    return (x32 * lax.rsqrt(jnp.mean(x32 * x32, axis=-1, keepdims=True) + NORM_EPS)).astype(x.dtype)


def rms_norm(x, g):
    return rms_normalize(x) * g.astype(x.dtype)


def split_columns(p):
    parts = []
    start = 0
    for width in SPLITS:
        parts.append(p[..., start:start + width])
        start += width
    return parts


def heads(t, n_heads):
    return t.reshape(t.shape[0], t.shape[1], n_heads, -1)


def grid_positions(n_tokens):
    rows = n_tokens // GRID_W
    row = jnp.repeat(jnp.arange(rows, dtype=jnp.int32), GRID_W)
    col = jnp.tile(jnp.arange(GRID_W, dtype=jnp.int32), rows)
    return row, col


def rope_2d(x, row, col):
    hd = x.shape[-1]
    half = hd // 2
    quarter = hd // 4
    freqs = ROPE_THETA ** (-jnp.arange(quarter, dtype=jnp.float32) / quarter)
    parts = []
    for a, pos in enumerate((row, col)):
        ang = pos.astype(jnp.float32)[:, None] * freqs[None, :]
        cos = jnp.cos(ang)[None, :, None, :]
        sin = jnp.sin(ang)[None, :, None, :]
        xa = x[..., a * half:(a + 1) * half].astype(jnp.float32)
        x1, x2 = xa[..., :quarter], xa[..., quarter:]
        parts.append(x1 * cos - x2 * sin)
        parts.append(x1 * sin + x2 * cos)
    return jnp.concatenate(parts, axis=-1).astype(x.dtype)


def retention_chunkwise(q, k, v, log_gamma, state0, strict):
    b, length, h, _ = q.shape
    dv = v.shape[-1]
    n_chunks = length // RET_CHUNK
    idx = jnp.arange(RET_CHUNK, dtype=jnp.float32)
    diff = idx[:, None] - idx[None, :]
    keep = (diff > 0) if strict else (diff >= 0)
    decay_in = jnp.where(keep[None], jnp.exp(jnp.maximum(diff, 0.0)[None] * log_gamma[:, None, None]), 0.0)
    q_decay = jnp.exp((idx[:, None] + 1.0) * log_gamma[None, :])[None, :, :, None]
    k_decay = jnp.exp((RET_CHUNK - 1.0 - idx)[:, None] * log_gamma[None, :])[None, :, :, None]
    chunk_decay = jnp.exp(RET_CHUNK * log_gamma)[None, :, None, None]

    def chunks(t):
        return t.reshape(b, n_chunks, RET_CHUNK, h, t.shape[-1]).swapaxes(0, 1)

    def step(state, qkv):
        qc, kc, vc = qkv
        s = jnp.einsum('bihd,bjhd->bhij', qc, kc) * decay_in
        o = jnp.einsum('bhij,bjhe->bihe', s, vc) + jnp.einsum('bihd,bhde->bihe', qc * q_decay, state)
        state = chunk_decay * state + jnp.einsum('bjhd,bjhe->bhde', kc * k_decay, vc)
        return state, o

    _, o = lax.scan(step, state0, (chunks(q), chunks(k), chunks(v)))
    return o.swapaxes(0, 1).reshape(b, length, h, dv)


def retention_context(q, k, v, lg_f, lg_b):
    n = k.shape[1]
    pos = jnp.arange(n, dtype=jnp.float32)
    diff = pos[:, None] - pos[None, :]
    decay = jnp.where(diff[None] >= 0,
                      jnp.exp(jnp.maximum(diff, 0.0)[None] * lg_f[:, None, None]),
                      jnp.exp(jnp.maximum(-diff, 0.0)[None] * lg_b[:, None, None]))
    s = jnp.einsum('bihd,bjhd->bhij', q, k) * decay
    return jnp.einsum('bhij,bjhe->bihe', s, v)


def retention_context_states(k, v, lg_f, lg_b):
    n = k.shape[1]
    pos = jnp.arange(n, dtype=jnp.float32)
    w_f = jnp.exp((n - 1.0 - pos)[:, None] * lg_f[None, :])
    w_b = jnp.exp(pos[:, None] * lg_b[None, :])
    s_f = jnp.einsum('lh,blhd,blhe->bhde', w_f, k, v).astype(jnp.float32)
    s_b = jnp.einsum('lh,blhd,blhe->bhde', w_b, k, v).astype(jnp.float32)
    return s_f, s_b


def retention_output(o, u):
    b, length, h, dv = o.shape
    y = rms_normalize(o.astype(jnp.float32)) * jax.nn.silu(u.astype(jnp.float32)).reshape(b, length, h, dv)
    return y.reshape(b, length, h * dv).astype(u.dtype)


def window_attention(q, k, v, k_ctx, v_ctx, sink):
    b, length, n_heads, hd = q.shape
    kvh = k.shape[2]
    g = n_heads // kvh
    n_ctx = k_ctx.shape[1]
    nb = length // Q_BLOCK
    band = Q_BLOCK + 2 * WINDOW
    qg = (q * hd ** -0.5).reshape(b, nb, Q_BLOCK, kvh, g, hd).swapaxes(0, 1)
    kp = jnp.pad(k, ((0, 0), (WINDOW, WINDOW), (0, 0), (0, 0)))
    vp = jnp.pad(v, ((0, 0), (WINDOW, WINDOW), (0, 0), (0, 0)))
    sink_g = sink.reshape(kvh, g).astype(jnp.float32)[None, :, :, None, None]
    q_off = jnp.arange(Q_BLOCK, dtype=jnp.int32)
    k_off = jnp.arange(band, dtype=jnp.int32)

    def block(args):
        qb, j = args
        start = j * Q_BLOCK
        kb = lax.dynamic_slice_in_dim(kp, start, band, axis=1)
        vb = lax.dynamic_slice_in_dim(vp, start, band, axis=1)
        qpos = start + q_off
        kpos = start - WINDOW + k_off
        valid = (jnp.abs(qpos[:, None] - kpos[None, :]) <= WINDOW) & (kpos[None, :] >= 0) & (kpos[None, :] < length)
        s_loc = jnp.where(valid, jnp.einsum('bqkgd,bckd->bkgqc', qb, kb).astype(jnp.float32), -jnp.inf)
        s_ctx = jnp.einsum('bqkgd,bckd->bkgqc', qb, k_ctx).astype(jnp.float32)
        s_sink = jnp.broadcast_to(sink_g, s_ctx.shape[:-1] + (1,))
        p = jax.nn.softmax(jnp.concatenate([s_loc, s_ctx, s_sink], axis=-1), axis=-1).astype(v.dtype)
        o = (jnp.einsum('bkgqc,bckd->bqkgd', p[..., :band], vb)
             + jnp.einsum('bkgqc,bckd->bqkgd', p[..., band:band + n_ctx], v_ctx))
        return o.reshape(b, Q_BLOCK, n_heads * hd)

    out = lax.map(block, (qg, jnp.arange(nb, dtype=jnp.int32)))
    return out.swapaxes(0, 1).reshape(b, length, n_heads * hd)


def global_attention(q, k_all, v_all):
    b, length, n_heads, hd = q.shape
    kvh = k_all.shape[2]
    g = n_heads // kvh
    nb = length // Q_BLOCK
    qg = (q * hd ** -0.5).reshape(b, nb, Q_BLOCK, kvh, g, hd).swapaxes(0, 1)

    def block(qb):
        s = jnp.einsum('bqkgd,bckd->bkgqc', qb, k_all).astype(jnp.float32)
        p = jax.nn.softmax(s, axis=-1).astype(v_all.dtype)
        return jnp.einsum('bkgqc,bckd->bqkgd', p, v_all).reshape(b, Q_BLOCK, n_heads * hd)

    out = lax.map(block, qg)
    return out.swapaxes(0, 1).reshape(b, length, n_heads * hd)


def context_attention(q, k, v, sink):
    b, n, n_heads, hd = q.shape
    kvh = k.shape[2]
    g = n_heads // kvh
    qg = (q * hd ** -0.5).reshape(b, n, kvh, g, hd)
    s = jnp.einsum('bqkgd,bckd->bkgqc', qg, k).astype(jnp.float32)
    if sink is not None:
        s_sink = jnp.broadcast_to(sink.reshape(kvh, g).astype(jnp.float32)[None, :, :, None, None], s.shape[:-1] + (1,))
        p = jax.nn.softmax(jnp.concatenate([s, s_sink], axis=-1), axis=-1)[..., :n]
    else:
        p = jax.nn.softmax(s, axis=-1)
    o = jnp.einsum('bkgqc,bckd->bqkgd', p.astype(v.dtype), v)
    return o.reshape(b, n, n_heads * hd)


def merge_branches(o_ret, o_swa, o_ga, a_ret, a_swa, a_ga, w_br_ret, w_br_swa, w_br_ga, w_out):
    y = (jax.nn.sigmoid(a_ret) * (o_ret @ w_br_ret)
         + jax.nn.sigmoid(a_swa) * (o_swa @ w_br_swa)
         + jax.nn.sigmoid(a_ga) * (o_ga @ w_br_ga))
    return y @ w_out


def mixer_sublayer(h, hc, row, col, w_in, ret_logit, sink, g_q, g_k,
                   w_br_ret, w_br_swa, w_br_ga, w_out, need_ctx):
    (q_r, k_r, v_r, u_r, q_s, k_s, v_s, q_a, k_a, v_a, a_r, a_s, a_a) = split_columns(h @ w_in)
    (cq_r, ck_r, cv_r, cu_r, cq_s, ck_s, cv_s, cq_a, ck_a, cv_a, ca_r, ca_s, ca_a) = split_columns(hc @ w_in)
    log_gamma = jax.nn.log_sigmoid(ret_logit.astype(jnp.float32))
    lg_f, lg_b = log_gamma[0], log_gamma[1]
    k_scale = RET_DK ** -0.5

    ckr = heads(ck_r, RET_HEADS) * k_scale
    cvr = heads(cv_r, RET_HEADS)
    s_f, s_b = retention_context_states(ckr, cvr, lg_f, lg_b)
    cks = heads(ck_s, SWA_KV_HEADS)
    cvs = heads(cv_s, SWA_KV_HEADS)
    cka = rms_norm(heads(ck_a, GA_KV_HEADS), g_k)
    cva = heads(cv_a, GA_KV_HEADS)

    qr = rope_2d(heads(q_r, RET_HEADS), row, col)
    kr = rope_2d(heads(k_r, RET_HEADS), row, col) * k_scale
    vr = heads(v_r, RET_HEADS)
    o_f = retention_chunkwise(qr, kr, vr, lg_f, s_f, False)
    o_b = retention_chunkwise(qr[:, ::-1], kr[:, ::-1], vr[:, ::-1], lg_b, s_b, True)[:, ::-1]
    o_ret = retention_output(o_f + o_b, u_r)

    o_swa = window_attention(rope_2d(heads(q_s, SWA_HEADS), row, col),
                             rope_2d(heads(k_s, SWA_KV_HEADS), row, col),
                             heads(v_s, SWA_KV_HEADS), cks, cvs, sink)

    qa = rope_2d(rms_norm(heads(q_a, GA_HEADS), g_q), row, col)
    ka = rope_2d(rms_norm(heads(k_a, GA_KV_HEADS), g_k), row, col)
    k_all = jnp.concatenate([ka, cka], axis=1)
    v_all = jnp.concatenate([heads(v_a, GA_KV_HEADS), cva], axis=1)
    o_ga = global_attention(qa, k_all, v_all)

    y = merge_branches(o_ret, o_swa, o_ga, a_r, a_s, a_a, w_br_ret, w_br_swa, w_br_ga, w_out)
    yc = None
    if need_ctx:
        co_ret = retention_output(retention_context(heads(cq_r, RET_HEADS), ckr, cvr, lg_f, lg_b), cu_r)
        co_swa = context_attention(heads(cq_s, SWA_HEADS), cks, cvs, sink)
        co_ga = context_attention(rms_norm(heads(cq_a, GA_HEADS), g_q), cka, cva, None)
        yc = merge_branches(co_ret, co_swa, co_ga, ca_r, ca_s, ca_a, w_br_ret, w_br_swa, w_br_ga, w_out)
    return y, yc


def moe_ffn(tokens, w_router, b_router, w_gate, w_up, w_down):
    n_tok, d = tokens.shape
    scores = jax.nn.sigmoid(tokens.astype(jnp.float32) @ w_router.astype(jnp.float32))
    biased = (scores + b_router.astype(jnp.float32)).reshape(n_tok, N_GROUPS, EXPERTS_PER_GROUP)
    group_score = lax.top_k(biased, TOP_K)[0].sum(-1)
    group = jnp.argmax(group_score, axis=-1).astype(jnp.int32)
    in_group = jnp.take_along_axis(biased, group[:, None, None], axis=1)[:, 0]
    local = lax.top_k(in_group, TOP_K)[1]
    expert = group[:, None] * EXPERTS_PER_GROUP + local
    weight = jnp.take_along_axis(scores, expert, axis=1)
    weight = weight / jnp.sum(weight, axis=-1, keepdims=True)

    n_assign = n_tok * TOP_K
    flat_e = expert.reshape(-1)
    flat_t = jnp.repeat(jnp.arange(n_tok, dtype=jnp.int32), TOP_K)
    flat_w = weight.reshape(-1)
    order = jnp.argsort(flat_e)
    se, st, sw = flat_e[order], flat_t[order], flat_w[order]
    counts = jax.ops.segment_sum(jnp.ones_like(flat_e), flat_e, num_segments=N_EXPERTS)
    padded = (counts + MOE_BLOCK - 1) // MOE_BLOCK * MOE_BLOCK
    starts = jnp.cumsum(counts) - counts
    pad_ends = jnp.cumsum(padded)
    pad_starts = pad_ends - padded
    dest = pad_starts[se] + jnp.arange(n_assign, dtype=jnp.int32) - starts[se]
    n_blocks = (n_assign + N_EXPERTS * (MOE_BLOCK - 1) + MOE_BLOCK - 1) // MOE_BLOCK
    buf = jnp.zeros((n_blocks * MOE_BLOCK, d), tokens.dtype).at[dest].set(tokens[st])
    block_e = jnp.minimum(jnp.searchsorted(pad_ends, jnp.arange(n_blocks, dtype=jnp.int32) * MOE_BLOCK, side='right'),
                          N_EXPERTS - 1)

    def expert_block(args):
        xb, e = args
        hid = jax.nn.silu(xb @ w_gate[e]) * (xb @ w_up[e])
        return hid @ w_down[e]

    y = lax.map(expert_block, (buf.reshape(n_blocks, MOE_BLOCK, d), block_e)).reshape(-1, d)
    contrib = y[dest] * sw[:, None].astype(y.dtype)
    return jnp.zeros_like(tokens).at[st].add(contrib)


def setup_inputs(seed: int = 0) -> dict:
    key = jax.random.key(seed)
    ks = jax.random.split(key, 24)
    f32 = jnp.float32
    nrm = jax.random.normal

    def w(k, shape, fan_in, scale=1.0):
        return nrm(k, shape, f32) * (scale * fan_in ** -0.5)

    def gain(k, shape):
        return 1.0 + 0.02 * nrm(k, shape, f32)

    decay0 = jnp.log(2.0 ** (5.0 + jnp.arange(RET_HEADS, dtype=f32)) - 1.0)
    return {
        'x': nrm(ks[0], (BATCH, SEQ, D_MODEL), f32),
        'c': nrm(ks[1], (BATCH, D_MODEL), f32),
        'ctx': nrm(ks[2], (BATCH, CTX_LEN, D_MODEL), f32),
        'c_ctx': nrm(ks[3], (D_MODEL,), f32),
        'w_mod': w(ks[4], (DEPTH, D_MODEL, 6 * D_MODEL), D_MODEL, 0.5),
        'b_mod': 0.01 * nrm(ks[5], (DEPTH, 6 * D_MODEL), f32),
        'g_norm1': gain(ks[6], (DEPTH, D_MODEL)),
        'g_norm2': gain(ks[7], (DEPTH, D_MODEL)),
        'w_in': w(ks[8], (DEPTH, D_MODEL, D_IN), D_MODEL),
        'ret_decay_logit': decay0 + 0.1 * nrm(ks[9], (DEPTH, 2, RET_HEADS), f32),
        'swa_sink': 0.5 * nrm(ks[10], (DEPTH, SWA_HEADS), f32),
        'g_qnorm': gain(ks[11], (DEPTH, GA_HD)),
        'g_knorm': gain(ks[12], (DEPTH, GA_HD)),
        'w_br_ret': w(ks[13], (DEPTH, RET_V, D_MODEL), RET_V),
        'w_br_swa': w(ks[14], (DEPTH, SWA_Q, D_MODEL), SWA_Q),
        'w_br_ga': w(ks[15], (DEPTH, GA_Q, D_MODEL), GA_Q),
        'w_out': w(ks[16], (DEPTH, D_MODEL, D_MODEL), D_MODEL),
        'w_router': w(ks[17], (D_MODEL, N_EXPERTS), D_MODEL),
        'b_router': 0.01 * nrm(ks[18], (N_EXPERTS,), f32),
        'w_gate': w(ks[19], (DEPTH, N_EXPERTS, D_MODEL, D_EXPERT), D_MODEL),
        'w_up': w(ks[20], (DEPTH, N_EXPERTS, D_MODEL, D_EXPERT), D_MODEL),
        'w_down': w(ks[21], (DEPTH, N_EXPERTS, D_EXPERT, D_MODEL), D_EXPERT),
        'g_final': gain(ks[22], (D_MODEL,)),
    }


def reference(x, c, ctx, c_ctx, w_mod, b_mod, g_norm1, g_norm2, w_in, ret_decay_logit, swa_sink,
              g_qnorm, g_knorm, w_br_ret, w_br_swa, w_br_ga, w_out, w_router, b_router,
              w_gate, w_up, w_down, g_final):
    b, length, d = x.shape
    row, col = grid_positions(length)
    for l in range(DEPTH):
        need_ctx = l < DEPTH - 1
        mod = (jax.nn.silu(c) @ w_mod[l] + b_mod[l])[:, None, :]
        mod_c = (jax.nn.silu(c_ctx) @ w_mod[l] + b_mod[l])[None, None, :]
        sh1, sc1, gt1, sh2, sc2, gt2 = jnp.split(mod, 6, axis=-1)
        csh1, csc1, cgt1, csh2, csc2, cgt2 = jnp.split(mod_c, 6, axis=-1)

        h = rms_norm(x, g_norm1[l]) * (1.0 + sc1) + sh1
        hc = rms_norm(ctx, g_norm1[l]) * (1.0 + csc1) + csh1
        y, yc = mixer_sublayer(h, hc, row, col, w_in[l], ret_decay_logit[l], swa_sink[l], g_qnorm[l], g_knorm[l],
                               w_br_ret[l], w_br_swa[l], w_br_ga[l], w_out[l], need_ctx)
        x = x + gt1 * y

        h = rms_norm(x, g_norm2[l]) * (1.0 + sc2) + sh2
        if need_ctx:
            ctx = ctx + cgt1 * yc
            hc = rms_norm(ctx, g_norm2[l]) * (1.0 + csc2) + csh2
            tokens = jnp.concatenate([h.reshape(-1, d), hc.reshape(-1, d)], axis=0)
            out = moe_ffn(tokens, w_router, b_router, w_gate[l], w_up[l], w_down[l])
            x = x + gt2 * out[:b * length].reshape(x.shape)
            ctx = ctx + cgt2 * out[b * length:].reshape(ctx.shape)
        else:
            x = x + gt2 * moe_ffn(h.reshape(-1, d), w_router, b_router, w_gate[l], w_up[l], w_down[l]).reshape(x.shape)
    return rms_norm(x, g_final)
```

```python
import numpy as np
import concourse.bass as bass
import concourse.mybir as mybir
from concourse.bass_utils import run_bass_kernel_spmd

F32 = mybir.dt.float32
BF16 = mybir.dt.bfloat16
AF = mybir.ActivationFunctionType
ALU = mybir.AluOpType
AX = mybir.AxisListType

NOWN, NOTH, NCTX = 2048, 2048, 256
NALL = NOWN + NOTH + NCTX
NQ = NOWN + NCTX
D = 1024
EPS = 1e-6
NSLOT = 24
SKIP = set()


class T:
    __slots__ = ("name", "w", "r", "psum")

    def __init__(self, name="", psum=False):
        self.name = name
        self.w = None
        self.r = []
        self.psum = psum


class Op:
    __slots__ = ("eng", "fn", "deps", "id", "is_dma", "slot", "val", "marked", "semval", "cc")

    def __init__(self, eng, fn, is_dma):
        self.eng = eng
        self.fn = fn
        self.deps = []
        self.is_dma = is_dma
        self.slot = None
        self.val = None
        self.marked = False
        self.semval = None
        self.cc = False


class Prog:
    ENGS = ("pe", "act", "dve", "pool", "sp")

    def __init__(self, nc):
        self.nc = nc
        self.ops = []
        self.streams = {e: [] for e in self.ENGS}
        self.ndma = {e: 0 for e in self.ENGS}
        self.dmas = {e: [] for e in self.ENGS}

    def add(self, eng, fn, reads=(), writes=(), dma=False, extra=(), cc=False):
        op = Op(eng, fn, dma or cc)
        op.cc = cc
        op.id = len(self.ops)
        deps = {}
        for t in reads:
            if t.w is not None:
                deps[t.w.id] = t.w
            if t.psum:
                for r in t.r:
                    if r.eng != eng:
                        deps[r.id] = r
        for t in writes:
            if t.w is not None:
                deps[t.w.id] = t.w
            for r in t.r:
                deps[r.id] = r
        for d in extra:
            deps[d.id] = d
        op.deps = list(deps.values())
        for t in reads:
            if not dma:
                t.r = [r for r in t.r if r.is_dma or r.eng != eng]
            t.r.append(op)
        for t in writes:
            t.w = op
            t.r = []
        if cc:
            self.ncc = getattr(self, "ncc", 0) + 1
            op.slot = ("cc", self.ncc - 1)
            op.val = 1
            self.dmas[eng].append(op)
        elif dma:
            i = self.ndma[eng]
            self.ndma[eng] += 1
            op.slot = i % NSLOT
            op.val = 16 * (i // NSLOT + 1)
            self.dmas[eng].append(op)
        self.ops.append(op)
        self.streams[eng].append(op)
        return op

    def barrier(self):
        lasts = []
        for e in self.ENGS:
            if self.streams[e]:
                lasts.append(self.streams[e][-1])
            lasts.extend(self.dmas[e][-NSLOT:])
        for e in self.ENGS:
            self.add(e, lambda eng: eng.nop(), extra=lasts)

    def dma(self, q, out, in_, reads=(), writes=()):
        return self.add(q, lambda e: e.dma_start(out=out, in_=in_), reads, writes, dma=True)

    def mm(self, out, lhsT, rhs, start, stop, reads=(), writes=()):
        return self.add("pe", lambda e: e.matmul(out, lhsT, rhs, start=start, stop=stop), reads, writes)

    def transpose(self, out, in_, ident, reads=(), writes=()):
        return self.add("pe", lambda e: e.transpose(out, in_, ident), reads, writes)

    def act(self, out, in_, func, reads=(), writes=(), bias=None, scale=None):
        kw = {}
        if bias is not None:
            kw["bias"] = bias
        if scale is not None:
            kw["scale"] = scale
        return self.add("act", lambda e: e.activation(out, in_, func, **kw), reads, writes)

    def tt(self, eng, out, in0, in1, op, reads=(), writes=()):
        return self.add(eng, lambda e: e.tensor_tensor(out, in0, in1, op), reads, writes)

    def ts(self, eng, out, in0, s1, s2, op0, op1=None, reads=(), writes=()):
        if op1 is None:
            return self.add(eng, lambda e: e.tensor_scalar(out=out, in0=in0, scalar1=s1, scalar2=None, op0=op0),
                            reads, writes)
        return self.add(eng, lambda e: e.tensor_scalar(out=out, in0=in0, scalar1=s1, scalar2=s2, op0=op0, op1=op1),
                        reads, writes)

    def stt(self, out, in0, scalar, in1, op0, op1, reads=(), writes=()):
        return self.add("dve", lambda e: e.scalar_tensor_tensor(out=out, in0=in0, scalar=scalar, in1=in1,
                                                                op0=op0, op1=op1), reads, writes)

    def copy(self, eng, out, in_, reads=(), writes=()):
        if eng == "act":
            return self.add("act", lambda e: e.copy(out, in_), reads, writes)
        return self.add(eng, lambda e: e.tensor_copy(out, in_), reads, writes)

    def emit(self):
        nc = self.nc
        for op in self.ops:
            for d in op.deps:
                if d.is_dma:
                    continue
                if d.eng == op.eng and not op.is_dma and d.eng == "pe":
                    continue
                d.marked = True
        cnt = {e: 0 for e in self.ENGS}
        for op in self.ops:
            if op.marked and not op.is_dma:
                cnt[op.eng] += 1
                op.semval = cnt[op.eng]
        sems = {e: nc.alloc_semaphore(f"s_{e}") for e in self.ENGS}
        dsems = {e: {i: nc.alloc_semaphore(f"d_{e}_{i}") for i in range(min(NSLOT, self.ndma[e]))}
                 for e in self.ENGS}
        for i in range(getattr(self, "ncc", 0)):
            dsems["pool"][("cc", i)] = nc.alloc_semaphore(f"cc_{i}")
        engobj = {"pe": nc.tensor, "act": nc.scalar, "dve": nc.vector, "pool": nc.gpsimd, "sp": nc.sync}
        with nc.Block() as block:
            def run(ename):
                eng = engobj[ename]
                waited = {}

                def wait(key, sem, val):
                    if waited.get(key, 0) >= val:
                        return
                    waited[key] = val
                    eng.wait_ge(sem, val)

                for op in self.streams[ename]:
                    for d in op.deps:
                        if d.is_dma:
                            wait(("d", d.eng, d.slot), dsems[d.eng][d.slot], d.val)
                        else:
                            if d.eng == ename and not op.is_dma and ename == "pe":
                                continue
                            wait(("c", d.eng), sems[d.eng], d.semval)
                    if op.cc:
                        ins = op.fn(eng)
                        ins.then_inc(dsems[ename][op.slot], 1)
                    elif op.is_dma:
                        if op.val > 16:
                            wait(("d", ename, op.slot), dsems[ename][op.slot], op.val - 16)
                        ins = op.fn(eng)
                        ins.then_inc(dsems[ename][op.slot], 16)
                    else:
                        ins = op.fn(eng)
                        if op.marked:
                            ins.then_inc(sems[ename], 1)
                n = self.ndma[ename]
                for i in range(max(0, n - NSLOT), n):
                    wait(("d", ename, i % NSLOT), dsems[ename][i % NSLOT], 16 * (i // NSLOT + 1))

            @block.tensor
            def _(e):
                run("pe")

            @block.scalar
            def _(e):
                run("act")

            @block.vector
            def _(e):
                run("dve")

            @block.gpsimd
            def _(e):
                run("pool")

            @block.sync
            def _(e):
                run("sp")


class Buf:
    __slots__ = ("ap", "t")

    def __init__(self, ap, name="", psum=False):
        self.ap = ap
        self.t = T(name, psum)


class Arena:
    def __init__(self, nc, nbytes):
        self.t = nc.alloc_sbuf_tensor("arena", [128, nbytes // 2], BF16)
        self.nbytes = nbytes
        self.off = 0

    def alloc(self, shape, dtype, name=""):
        n = 1
        for s in shape[1:]:
            n *= s
        es = 4 if dtype == F32 else 2
        nb = (n * es + 63) // 64 * 64
        assert self.off + nb <= self.nbytes, f"arena overflow {name} {self.off + nb}"
        v = self.t[0:shape[0], self.off // 2:(self.off + n * es) // 2]
        if dtype == F32:
            v = v.bitcast(F32)
        if len(shape) == 3:
            v = v.rearrange("p (a b) -> p a b", a=shape[1])
        self.off += nb
        return Buf(v, name)


SPL = [512, 512, 1024, 1024, 1024, 128, 128, 1024, 256, 256, 1024, 1024, 1024]
OFF = np.concatenate([[0], np.cumsum(SPL)]).astype(int)
O_QR, O_KR, O_VR, O_UR, O_QS, O_KS, O_VS, O_QA, O_KA, O_VA, O_AR, O_AS, O_AA = [int(v) for v in OFF[:13]]


def _perm128():
    f = np.arange(128)
    return np.where(f % 64 < 32, f + 32, f - 32)


def _perm64():
    f = np.arange(128)
    return np.where(f % 32 < 16, f + 16, f - 16)


def fm_units():
    u = []
    ar = np.arange(128)
    for h in range(4):
        u.append(dict(name=f"kr{h}", kind="rope128", cols=O_KR + h * 128 + ar, tok="all"))
    u.append(dict(name="ks", kind="rope64", cols=O_KS + ar, tok="all"))
    u.append(dict(name="ks2", kind="rope64", cols=O_KS + (ar + 64) % 128, tok="all"))
    for h in range(2):
        u.append(dict(name=f"ka{h}", kind="normk", cols=O_KA + h * 128 + ar, tok="all"))
    for h in range(4):
        u.append(dict(name=f"qr{h}", kind="rope128", cols=O_QR + h * 128 + ar, tok="q"))
    for c in range(8):
        u.append(dict(name=f"qs{c}", kind="rope64", cols=O_QS + c * 128 + ar, tok="q"))
    for h in range(8):
        u.append(dict(name=f"qa{h}", kind="normq", cols=O_QA + h * 128 + ar, tok="q"))
    for c in range(8):
        u.append(dict(name=f"ur{c}", kind="silu", cols=O_UR + c * 128 + ar, tok="q"))
    for nm, o in (("ar", O_AR), ("as", O_AS), ("aa", O_AA)):
        for c in range(8):
            u.append(dict(name=f"{nm}{c}", kind="sig", cols=o + c * 128 + ar, tok="q"))
    g, used = 0, 0
    for x in u:
        w = 256 if x["kind"] in ("rope128", "rope64", "normq", "normk") else 128
        if used + w > 512:
            g, used = g + 1, 0
        x["group"], x["c0"] = g, used
        x["c1"] = used + 128 if w == 256 else None
        used += w
    return u, g + 1


FM_UNITS, N_FM_GROUPS = fm_units()
TM_COLS = np.concatenate([O_VR + np.arange(1024), O_VS + np.arange(128), O_VA + np.arange(256)])
N_TM_GROUPS = 3
G_MOD = 0
G_FM = 12
G_TM = G_FM + N_FM_GROUPS
G_MERGE = G_TM + N_TM_GROUPS
N_GROUPS = G_MERGE + 8

SM_BMOD, SM_G1, SM_G2, SM_RET, SM_SINK, SM_GQ, SM_GK = 0, 48, 56, 64, 72, 88, 90
SM_L = 96
SG_C, SG_WR, SG_BR, SG_GF, SG_FLAG = 0, 16, 144, 160, 168
SG_N = 176
C_ID, C_DPOS, C_DNEG, C_MF, C_MB, C_IP1, C_IB, C_ML, C_MR, C_MLB, C_MRB = [i * 128 for i in range(11)]
C_PCF, C_PCB = 11 * 128, 11 * 128 + 1
C_SEL = 11 * 128 + 8
C_N = C_SEL + 2048


def _grp(w):
    n = w.shape[1] // 512
    return np.ascontiguousarray(w.reshape(8, 128, n, 512).transpose(2, 1, 0, 3))


def prep_layer_weights(inp, l):
    w_in = inp["w_in"][l]
    p128, p64 = _perm128(), _perm64()
    cols = np.zeros(N_FM_GROUPS * 512, dtype=np.int64)
    for u in FM_UNITS:
        base = u["group"] * 512
        cols[base + u["c0"]:base + u["c0"] + 128] = u["cols"]
        if u["c1"] is not None:
            pm = p64 if u["kind"] == "rope64" else p128
            cols[base + u["c1"]:base + u["c1"] + 128] = u["cols"][pm]
    tmc = np.concatenate([TM_COLS, np.zeros(1536 - 1408, dtype=np.int64)])
    wcat = np.concatenate([inp["w_mod"][l], w_in[:, cols], w_in[:, tmc], inp["w_br_ret"][l], inp["w_br_swa"][l],
                           inp["w_br_ga"][l], inp["w_out"][l]], axis=1)
    wall = _grp(wcat)
    assert wall.shape[0] == N_GROUPS
    wg = inp["w_gate"][l].reshape(16, 8, 128, 512).transpose(0, 2, 1, 3).reshape(16, 128, 4096)
    wu = inp["w_up"][l].reshape(16, 8, 128, 512).transpose(0, 2, 1, 3).reshape(16, 128, 4096)
    wd = inp["w_down"][l].reshape(16, 4, 128, 1024).transpose(0, 2, 1, 3).reshape(16, 128, 4096)
    we = np.ascontiguousarray(np.stack([wg, wu, wd], axis=1))
    sm = np.zeros((128, SM_L), np.float32)
    sm[:, SM_BMOD:SM_BMOD + 48] = inp["b_mod"][l].reshape(48, 128).T
    sm[:, SM_G1:SM_G1 + 8] = inp["g_norm1"][l].reshape(8, 128).T
    sm[:, SM_G2:SM_G2 + 8] = inp["g_norm2"][l].reshape(8, 128).T
    sm[:, SM_RET:SM_RET + 8] = inp["ret_decay_logit"][l].reshape(1, 8)
    sm[:, SM_SINK:SM_SINK + 16] = inp["swa_sink"][l].reshape(1, 16)
    sm[:, SM_GQ] = inp["g_qnorm"][l]
    sm[:, SM_GQ + 1] = inp["g_qnorm"][l][p128]
    sm[:, SM_GK] = inp["g_knorm"][l]
    sm[:, SM_GK + 1] = inp["g_knorm"][l][p128]
    return wall, we, sm


def make_consts(half):
    c = np.zeros((128, C_N), np.float32)
    m = np.arange(128)[:, None].astype(np.float32)
    n = np.arange(128)[None, :].astype(np.float32)
    c[:, C_ID:C_ID + 128] = np.eye(128)
    c[:, C_DPOS:C_DPOS + 128] = np.maximum(n - m, 0)
    c[:, C_DNEG:C_DNEG + 128] = np.maximum(m - n, 0)
    c[:, C_MF:C_MF + 128] = (n >= m)
    c[:, C_MB:C_MB + 128] = (m > n)
    c[:, C_IP1:C_IP1 + 128] = n + 1 + 0 * m
    c[:, C_IB:C_IB + 128] = 128 - n + 0 * m
    c[:, C_ML:C_ML + 128] = (m >= n)
    c[:, C_MR:C_MR + 128] = (m <= n)
    c[:, C_MLB:C_MLB + 128] = (m >= n) * (1.0 if half == 1 else 0.0)
    c[:, C_MRB:C_MRB + 128] = (m <= n) * (1.0 if half == 0 else 0.0)
    c[:, C_PCF] = 127 - np.arange(128)
    c[:, C_PCB] = np.arange(128)
    for e in range(16):
        c[e, C_SEL + e * 128:C_SEL + (e + 1) * 128] = 1.0
    return c


def make_tabs(half):
    pos_own = half * NOWN + np.arange(NOWN)
    pos_oth = (1 - half) * NOWN + np.arange(NOTH)
    pos = np.concatenate([pos_own, pos_oth])
    row = (pos // 64).astype(np.float32)
    col = (pos % 64).astype(np.float32)
    tabs = np.zeros((4, 128, NALL), np.float32)
    tabs[0, :, 4096:] = 1.0
    tabs[2, :, 4096:] = 1.0
    for ti, hd in ((0, 128), (2, 64)):
        half_d, quarter = hd // 2, hd // 4
        freqs = (np.float32(10000.0) ** (-np.arange(quarter, dtype=np.float32) / np.float32(quarter))).astype(np.float32)
        for f in range(128):
            fl = f % hd
            a = fl // half_d
            j = fl % half_d
            p = row if a == 0 else col
            ang = (p * freqs[j % quarter]).astype(np.float32)
            tabs[ti, f, :4096] = np.cos(ang)
            tabs[ti + 1, f, :4096] = np.sin(ang) * (-1.0 if j < quarter else 1.0)
    return tabs


def make_small_global(inp, b, half):
    sg = np.zeros((128, SG_N), np.float32)
    cT = inp["c"][b].reshape(8, 128).T
    ccT = inp["c_ctx"].reshape(8, 128).T
    sg[:, SG_C:SG_C + 16:2] = cT
    sg[:, SG_C + 1:SG_C + 16:2] = ccT
    sg[:, SG_WR:SG_WR + 128] = inp["w_router"].reshape(8, 128, 16).transpose(1, 0, 2).reshape(128, 128)
    sg[:, SG_BR:SG_BR + 16] = inp["b_router"].reshape(1, 16)
    sg[:, SG_GF:SG_GF + 8] = inp["g_final"].reshape(8, 128).T
    sg[:, SG_FLAG] = 1.0 if half == 0 else 0.0
    sg[:, SG_FLAG + 1] = 1.0 if half == 1 else 0.0
    return sg


TOK_TILES_ALL = [(i * 512, 512, 0) for i in range(8)] + [(4096, 256, 1)]
TOK_TILES_OWN = [(i * 512, 512, 0) for i in range(4)]
CTX_TILE = (4096, 256, 1)


def qpos(t0):
    return t0 if t0 < NOWN else t0 - NOTH


def build(layers, need_ctx_flags, final, dbg=(), stop=99):
    nc = bass.Bass("TRN2", target_bir_lowering=False)
    P = Prog(nc)
    nl = len(layers)

    def din(name, shape, dt=F32):
        return nc.dram_tensor(name, list(shape), dt, kind="ExternalInput").ap()

    def dscr(name, shape, dt=BF16):
        kind = "ExternalOutput" if name in dbg else "Internal"
        return nc.dram_tensor(name, list(shape), dt, kind=kind).ap()

    xall = din("xall", [D, NALL])
    tabs = din("tabs", [4, 128, NALL])
    consts_d = din("consts", [128, C_N])
    sg_d = din("sg", [128, SG_N])
    sm_d = [din(f"sm{l}", [128, SM_L]) for l in range(nl)]
    wall_d = [din(f"wall{l}", [N_GROUPS, 128, 8, 512]) for l in range(nl)]
    we_d = [din(f"we{l}", [16, 3, 128, 4096]) for l in range(nl)]
    if final:
        yout = nc.dram_tensor("yout", [D, NOWN], F32, kind="ExternalOutput").ap()
    else:
        xout = nc.dram_tensor("xout", [D, NQ], F32, kind="ExternalOutput").ap()

    xs_d = dscr("xs", [D, NALL], F32)
    fm_d = {u["name"]: dscr("fm_" + u["name"], [128, NALL]) for u in FM_UNITS}
    vall_d = dscr("vall", [NALL, 1536])
    obr_d = [dscr(f"obr{i}", [D, NQ]) for i in range(3)]
    web_d = dscr("web", [16, 3, 128, 4096])
    T_xs = {}

    def txs(t0):
        return T_xs.setdefault(t0, T(f"xs{t0}"))
    T_fm = {}

    def tfm(name, t0):
        return T_fm.setdefault((name, t0), T(f"fm{name}{t0}"))
    T_vall = {}

    def tvall(sub):
        return T_vall.setdefault(sub, T(f"vall{sub}"))
    T_obr = {}

    def tobr(i, t0):
        return T_obr.setdefault((i, t0), T(f"obr{i}_{t0}"))
    T_web = {}

    def tweb(e, k):
        return T_web.setdefault((e, k), T(f"web{e}_{k}"))

    xs3 = xs_d.rearrange("(kc p) t -> p kc t", p=128)
    if nl > 1:
        xch_src = [nc.dram_tensor(f"xch_src{i}", [D, 512], F32, kind="Internal").ap() for i in range(4)]
        xch_dst = [nc.dram_tensor(f"xch_dst{i}", [2 * D, 512], F32, kind="Internal").ap() for i in range(4)]
        T_xsrc, T_xdst = [T("xsrc") for i in range(4)], [T("xdst") for i in range(4)]
    xall3 = xall.rearrange("(kc p) t -> p kc t", p=128)

    A = Arena(nc, 206 * 1024)
    cst = A.alloc([128, C_N], F32, "cst")
    sg = A.alloc([128, SG_N], F32, "sg")
    ones_bf = A.alloc([128, 128], BF16, "ones")
    id_bf = A.alloc([128, 128], BF16, "idbf")
    mk_bf = A.alloc([128, 4, 512], BF16, "mk")
    PERS_END = None
    ps = [Buf(nc.alloc_psum_tensor(f"ps{i}", [128, 512], F32)[:], f"ps{i}", True) for i in range(8)]

    P.dma("sp", cst.ap, consts_d, writes=[cst.t])
    P.dma("sp", sg.ap, sg_d, writes=[sg.t])
    P.add("dve", lambda e: e.memset(ones_bf.ap, 1.0), writes=[ones_bf.t])
    P.copy("dve", id_bf.ap, cst.ap[:, C_ID:C_ID + 128], reads=[cst.t], writes=[id_bf.t])
    for i, co in enumerate((C_ML, C_MR, C_MLB, C_MRB)):
        for r in range(4):
            P.copy("dve", mk_bf.ap[:, i, r * 128:(r + 1) * 128], cst.ap[:, co:co + 128], reads=[cst.t],
                   writes=[mk_bf.t])
    for (t0, nt, _) in TOK_TILES_ALL:
        P.dma("sp", xs_d[:, t0:t0 + nt], xall[:, t0:t0 + nt], writes=[txs(t0)])
    PERS_END = A.off

    def rsqrt_from_psum(dst, src_ps, n, rd, wr):
        P.ts("dve", dst, src_ps, 1.0 / n, EPS, ALU.mult, ALU.add, reads=rd, writes=wr)
        P.act(dst, dst, AF.Ln, reads=wr, writes=wr)
        P.act(dst, dst, AF.Exp, reads=wr, writes=wr, scale=-0.5)

    psi = [0]

    def ps_next(k=7):
        psi[0] = (psi[0] + 1) % k
        return ps[psi[0]]

    for li, l in enumerate(layers):
        need_ctx = need_ctx_flags[li]
        last = (li == nl - 1)
        qtiles = TOK_TILES_OWN + ([CTX_TILE] if need_ctx else [])
        P.barrier()
        A.off = PERS_END
        sm = A.alloc([128, SM_L], F32, "sm")
        modT = A.alloc([128, 48, 2], F32, "modT")
        A1 = A.alloc([128, 8, 2], F32, "A1")
        A2 = A.alloc([128, 8, 2], F32, "A2")
        silc = A.alloc([128, 16], F32, "silc")
        lg = A.alloc([128, 8], F32, "lg")
        sinkx = A.alloc([128, 16], F32, "sinkx")
        P.dma("sp", sm.ap, sm_d[li], writes=[sm.t])
        P.act(silc.ap, sg.ap[:, SG_C:SG_C + 16], AF.Silu, reads=[sg.t], writes=[silc.t])
        silc3 = silc.ap.rearrange("p (k j) -> p k j", j=2)
        P.act(lg.ap, sm.ap[:, SM_RET:SM_RET + 8], AF.Exp, reads=[sm.t], writes=[lg.t], scale=-1.0)
        P.ts("dve", lg.ap, lg.ap, 1.0, None, ALU.add, reads=[lg.t], writes=[lg.t])
        P.act(lg.ap, lg.ap, AF.Ln, reads=[lg.t], writes=[lg.t])
        P.ts("dve", lg.ap, lg.ap, -1.0, None, ALU.mult, reads=[lg.t], writes=[lg.t])
        P.act(sinkx.ap, sm.ap[:, SM_SINK:SM_SINK + 16], AF.Exp, reads=[sm.t], writes=[sinkx.t])
        PH0_END = A.off
        wst = [A.alloc([128, 8, 512], F32, f"wst{i}") for i in range(2)]
        for g in range(12):
            w = wst[g % 2]
            P.dma("sp", w.ap, wall_d[li][G_MOD + g], writes=[w.t])
            for j in range(4):
                idx = g * 4 + j
                pb = ps_next()
                for kc in range(8):
                    P.mm(pb.ap[:, 0:2], w.ap[:, kc, j * 128:(j + 1) * 128], silc3[:, kc, :], kc == 0, kc == 7,
                         reads=[w.t, silc.t], writes=[pb.t])
                P.ts("dve", modT.ap[:, idx, :], pb.ap[:, 0:2], sm.ap[:, SM_BMOD + idx:SM_BMOD + idx + 1], None,
                     ALU.add, reads=[pb.t, sm.t], writes=[modT.t])
        for j in range(2):
            P.stt(A1.ap[:, :, j], modT.ap[:, 8:16, j], 1.0, sm.ap[:, SM_G1:SM_G1 + 8], ALU.add, ALU.mult,
                  reads=[modT.t, sm.t], writes=[A1.t])
            P.stt(A2.ap[:, :, j], modT.ap[:, 32:40, j], 1.0, sm.ap[:, SM_G2:SM_G2 + 8], ALU.add, ALU.mult,
                  reads=[modT.t, sm.t], writes=[A2.t])

        def SH1(kc, j): return modT.ap[:, 0 + kc, j:j + 1]
        def GT1(kc, j): return modT.ap[:, 16 + kc, j:j + 1]
        def SH2(kc, j): return modT.ap[:, 24 + kc, j:j + 1]
        def GT2(kc, j): return modT.ap[:, 40 + kc, j:j + 1]

        P.barrier()
        A.off = PH0_END
        est = [A.alloc([128, 4096], F32, f"est{i}") for i in range(3)]
        ebf = [A.alloc([128, 4096], BF16, f"ebf{i}") for i in range(3)]
        ci = 0
        for e in range(16):
            for k in range(3):
                s_, b_ = est[ci % 3], ebf[ci % 3]
                P.dma("sp", s_.ap, we_d[li][e, k], writes=[s_.t])
                eng = ("pool", "dve", "act")[ci % 3]
                P.copy(eng, b_.ap, s_.ap, reads=[s_.t], writes=[b_.t])
                P.dma("pool", web_d[e, k], b_.ap, reads=[b_.t], writes=[tweb(e, k)])
                ci += 1
        P.barrier()

        A.off = PH0_END
        hT = A.alloc([128, 8, NALL], BF16, "hT")
        hts = {t0: T(f"hT{t0}") for (t0, _, _) in TOK_TILES_ALL}
        xt = [A.alloc([128, 8, 512], F32, f"xt{i}") for i in range(2)]
        sq = A.alloc([128, 8, 512], BF16, "sq")
        rstd = A.alloc([128, 512], F32, "rstd")
        tmp = [A.alloc([128, 512], F32, f"tmp{i}") for i in range(2)]
        PH1_END = A.off

        def norm_tiles(tiles, Aco, SHf, out_fn, src3=xs3):
            for i, (t0, nt, mj) in enumerate(tiles):
                x_ = xt[i % 2]
                P.dma("sp", x_.ap[:, :, 0:nt], src3[:, :, t0:t0 + nt], reads=[txs(t0)], writes=[x_.t])
                P.act(sq.ap[:, :, 0:nt], x_.ap[:, :, 0:nt], AF.Square, reads=[x_.t], writes=[sq.t])
                pb = ps_next()
                for kc in range(8):
                    P.mm(pb.ap[:, 0:nt], ones_bf.ap, sq.ap[:, kc, 0:nt], kc == 0, kc == 7,
                         reads=[ones_bf.t, sq.t], writes=[pb.t])
                rsqrt_from_psum(rstd.ap[:, 0:nt], pb.ap[:, 0:nt], 1024.0, [pb.t], [rstd.t])
                for kc in range(8):
                    tm_ = tmp[kc % 2]
                    P.tt("dve" if kc % 2 == 0 else "pool", tm_.ap[:, 0:nt], x_.ap[:, kc, 0:nt], rstd.ap[:, 0:nt],
                         ALU.mult, reads=[x_.t, rstd.t], writes=[tm_.t])
                    out_fn(kc, t0, nt, mj, tm_, Aco.ap[:, kc, mj:mj + 1], SHf(kc, mj))

        def out_h1(kc, t0, nt, mj, tm_, a_, b_):
            P.act(hT.ap[:, kc, t0:t0 + nt], tm_.ap[:, 0:nt], AF.Identity, reads=[tm_.t, A1.t, modT.t],
                  writes=[hts[t0]], bias=b_, scale=a_)

        norm_tiles(TOK_TILES_ALL, A1, SH1, out_h1)

        A.off = PH1_END
        wst = [A.alloc([128, 8, 512], F32, f"wst{i}") for i in range(2)]
        wbf = [A.alloc([128, 8, 512], BF16, f"wbf{i}") for i in range(2)]
        tabC = [A.alloc([128, 512], F32, f"tabC{i}") for i in range(2)]
        tabS = [A.alloc([128, 512], F32, f"tabS{i}") for i in range(2)]
        stage = [A.alloc([128, 512], BF16, f"stage{i}") for i in range(3)]
        t1b = [A.alloc([128, 512], F32, f"t1b{i}") for i in range(2)]
        t2b = [A.alloc([128, 512], F32, f"t2b{i}") for i in range(2)]
        sqb = A.alloc([128, 512], BF16, "sqb")
        rs2 = A.alloc([128, 512], F32, "rs2")
        cnt = [0]

        def load_group(gi):
            w, wb = wst[gi % 2], wbf[gi % 2]
            P.dma("sp", w.ap, wall_d[li][gi], writes=[w.t])
            P.copy("pool", wb.ap[:, 0:4, :], w.ap[:, 0:4, :], reads=[w.t], writes=[wb.t])
            P.copy("act", wb.ap[:, 4:8, :], w.ap[:, 4:8, :], reads=[w.t], writes=[wb.t])
            return wb

        for gi in range(N_FM_GROUPS):
            wb = load_group(G_FM + gi)
            for u in [x for x in FM_UNITS if x["group"] == gi]:
                tiles = TOK_TILES_ALL if u["tok"] == "all" else qtiles
                kind = u["kind"]
                dual = u["c1"] is not None
                tsel = 2 if kind == "rope64" else 0
                for (t0, nt, mj) in tiles:
                    k = cnt[0]
                    cnt[0] += 1
                    pa = ps_next()
                    for kc in range(8):
                        P.mm(pa.ap[:, 0:nt], wb.ap[:, kc, u["c0"]:u["c0"] + 128], hT.ap[:, kc, t0:t0 + nt],
                             kc == 0, kc == 7, reads=[wb.t, hts[t0]], writes=[pa.t])
                    if dual:
                        pbk = ps_next()
                        for kc in range(8):
                            P.mm(pbk.ap[:, 0:nt], wb.ap[:, kc, u["c1"]:u["c1"] + 128], hT.ap[:, kc, t0:t0 + nt],
                                 kc == 0, kc == 7, reads=[wb.t, hts[t0]], writes=[pbk.t])
                        tc_, ts_ = tabC[k % 2], tabS[k % 2]
                        P.dma("sp", tc_.ap[:, 0:nt], tabs[tsel, :, t0:t0 + nt], writes=[tc_.t])
                        P.dma("sp", ts_.ap[:, 0:nt], tabs[tsel + 1, :, t0:t0 + nt], writes=[ts_.t])
                    st = stage[k % 3]
                    o_ = st.ap[:, 0:nt]
                    if kind in ("rope128", "rope64"):
                        a_, b_ = t1b[k % 2], t2b[k % 2]
                        P.tt("dve", a_.ap[:, 0:nt], pa.ap[:, 0:nt], tc_.ap[:, 0:nt], ALU.mult,
                             reads=[pa.t, tc_.t], writes=[a_.t])
                        P.tt("dve", b_.ap[:, 0:nt], pbk.ap[:, 0:nt], ts_.ap[:, 0:nt], ALU.mult,
                             reads=[pbk.t, ts_.t], writes=[b_.t])
                        P.tt("pool", o_, a_.ap[:, 0:nt], b_.ap[:, 0:nt], ALU.add, reads=[a_.t, b_.t], writes=[st.t])
                    elif kind in ("normq", "normk"):
                        gco = SM_GQ if kind == "normq" else SM_GK
                        a_, b_ = t1b[k % 2], t2b[k % 2]
                        P.act(sqb.ap[:, 0:nt], pa.ap[:, 0:nt], AF.Square, reads=[pa.t], writes=[sqb.t])
                        pc = ps_next()
                        P.mm(pc.ap[:, 0:nt], ones_bf.ap, sqb.ap[:, 0:nt], True, True, reads=[ones_bf.t, sqb.t],
                             writes=[pc.t])
                        rsqrt_from_psum(rs2.ap[:, 0:nt], pc.ap[:, 0:nt], 128.0, [pc.t], [rs2.t])
                        P.stt(a_.ap[:, 0:nt], pa.ap[:, 0:nt], sm.ap[:, gco:gco + 1], tc_.ap[:, 0:nt], ALU.mult,
                              ALU.mult, reads=[pa.t, tc_.t, sm.t], writes=[a_.t])
                        P.stt(b_.ap[:, 0:nt], pbk.ap[:, 0:nt], sm.ap[:, gco + 1:gco + 2], ts_.ap[:, 0:nt], ALU.mult,
                              ALU.mult, reads=[pbk.t, ts_.t, sm.t], writes=[b_.t])
                        P.tt("pool", a_.ap[:, 0:nt], a_.ap[:, 0:nt], b_.ap[:, 0:nt], ALU.add, reads=[a_.t, b_.t],
                             writes=[a_.t])
                        P.tt("pool", o_, a_.ap[:, 0:nt], rs2.ap[:, 0:nt], ALU.mult, reads=[a_.t, rs2.t],
                             writes=[st.t])
                    elif kind == "silu":
                        P.act(o_, pa.ap[:, 0:nt], AF.Silu, reads=[pa.t], writes=[st.t])
                    else:
                        P.act(o_, pa.ap[:, 0:nt], AF.Sigmoid, reads=[pa.t], writes=[st.t])
                    P.dma("pool", fm_d[u["name"]][:, t0:t0 + nt], o_, reads=[st.t], writes=[tfm(u["name"], t0)])
        for gi in range(N_TM_GROUPS):
            wb = load_group(G_TM + gi)
            for sub in range(NALL // 128):
                t0 = sub * 128
                tile0 = (t0 // 512) * 512
                k = cnt[0]
                cnt[0] += 1
                pa = ps_next()
                for kc in range(8):
                    P.mm(pa.ap, hT.ap[:, kc, t0:t0 + 128], wb.ap[:, kc, :], kc == 0, kc == 7,
                         reads=[wb.t, hts[tile0]], writes=[pa.t])
                st = stage[k % 3]
                P.copy("act" if k % 2 == 0 else "dve", st.ap, pa.ap, reads=[pa.t], writes=[st.t])
                P.dma("pool", vall_d[t0:t0 + 128, gi * 512:(gi + 1) * 512], st.ap, reads=[st.t],
                      writes=[tvall(sub)])
        P.barrier()
        if stop <= 2:
            continue
        A.off = PH0_END
        KSCALE = 128.0 ** -0.5
        KT = A.alloc([128, NALL], BF16, "KT")
        QT = A.alloc([128, NALL], BF16, "QT")
        Vr = A.alloc([128, 34, 256], BF16, "Vr")
        Ur = A.alloc([128, 2, NALL], BF16, "Ur")
        Kf = A.alloc([128, 34, 128], BF16, "Kf")
        Kb = A.alloc([128, 34, 128], BF16, "Kb")
        SFb = A.alloc([128, 18, 256], BF16, "SFb")
        SBb = A.alloc([128, 18, 256], BF16, "SBb")
        Dm = A.alloc([128, 512], BF16, "Dm")
        qdf = A.alloc([128, 512], F32, "qdf")
        qdb = A.alloc([128, 512], F32, "qdb")
        e1 = A.alloc([128, 128], F32, "e1")
        e2 = A.alloc([128, 128], F32, "e2")
        kd = A.alloc([128, 4], F32, "kd")
        S = A.alloc([128, 256], F32, "S")
        S0f = A.alloc([128, 256], F32, "S0f")
        S0b = A.alloc([128, 256], F32, "S0b")
        Pm = A.alloc([128, 512], BF16, "Pm")
        Qf = A.alloc([128, 512], BF16, "Qf")
        Qb = A.alloc([128, 512], BF16, "Qb")
        sq2 = A.alloc([128, 2, 512], BF16, "sq2")
        rs3 = A.alloc([128, 512], F32, "rs3")
        to_ = [A.alloc([128, 512], F32, f"to{i}") for i in range(2)]
        stg = [A.alloc([128, 512], BF16, f"stg{i}") for i in range(2)]
        flg = sg.ap[:, SG_FLAG:SG_FLAG + 2]
        for h in range(4):
            lgf, lgb = lg.ap[:, h:h + 1], lg.ap[:, 4 + h:5 + h]
            P.act(e1.ap, cst.ap[:, C_DPOS:C_DPOS + 128], AF.Exp, reads=[cst.t, lg.t], writes=[e1.t], scale=lgf)
            P.tt("dve", e1.ap, e1.ap, cst.ap[:, C_MF:C_MF + 128], ALU.mult, reads=[e1.t, cst.t], writes=[e1.t])
            P.act(e2.ap, cst.ap[:, C_DNEG:C_DNEG + 128], AF.Exp, reads=[cst.t, lg.t], writes=[e2.t], scale=lgb)
            P.tt("dve", e2.ap, e2.ap, cst.ap[:, C_MB:C_MB + 128], ALU.mult, reads=[e2.t, cst.t], writes=[e2.t])
            P.tt("dve", e1.ap, e1.ap, e2.ap, ALU.add, reads=[e1.t, e2.t], writes=[e1.t])
            for r in range(4):
                P.ts("dve", Dm.ap[:, r * 128:(r + 1) * 128], e1.ap, KSCALE, None, ALU.mult, reads=[e1.t],
                     writes=[Dm.t])
                P.act(qdf.ap[:, r * 128:(r + 1) * 128], cst.ap[:, C_IP1:C_IP1 + 128], AF.Exp, reads=[cst.t, lg.t],
                      writes=[qdf.t], scale=lgf)
                P.act(qdb.ap[:, r * 128:(r + 1) * 128], cst.ap[:, C_IB:C_IB + 128], AF.Exp, reads=[cst.t, lg.t],
                      writes=[qdb.t], scale=lgb)
            P.act(kd.ap[:, 0:1], cst.ap[:, C_PCF:C_PCF + 1], AF.Exp, reads=[cst.t, lg.t], writes=[kd.t], scale=lgf)
            P.act(kd.ap[:, 1:2], cst.ap[:, C_PCB:C_PCB + 1], AF.Exp, reads=[cst.t, lg.t], writes=[kd.t], scale=lgb)
            P.ts("dve", kd.ap[:, 0:2], kd.ap[:, 0:2], KSCALE, None, ALU.mult, reads=[kd.t], writes=[kd.t])
            P.act(kd.ap[:, 2:3], lgf, AF.Exp, reads=[lg.t], writes=[kd.t], scale=128.0)
            P.act(kd.ap[:, 3:4], lgb, AF.Exp, reads=[lg.t], writes=[kd.t], scale=128.0)
            P.dma("sp", KT.ap, fm_d[f"kr{h}"], writes=[KT.t])
            P.dma("sp", QT.ap[:, 0:NOWN], fm_d[f"qr{h}"][:, 0:NOWN], writes=[QT.t])
            if need_ctx:
                P.dma("sp", QT.ap[:, 4096:NALL], fm_d[f"qr{h}"][:, 4096:NALL], writes=[QT.t])
            vsrc = vall_d[:, h * 256:(h + 1) * 256].rearrange("(t p) c -> p t c", p=128)
            for tq in range(0, 34, 4):
                P.dma("sp", Vr.ap[:, tq:min(tq + 4, 34), :], vsrc[:, tq:min(tq + 4, 34), :], writes=[Vr.t])
            for j in range(2):
                P.dma("sp", Ur.ap[:, j, 0:NOWN], fm_d[f"ur{2 * h + j}"][:, 0:NOWN], writes=[Ur.t])
                if need_ctx:
                    P.dma("sp", Ur.ap[:, j, 4096:NALL], fm_d[f"ur{2 * h + j}"][:, 4096:NALL], writes=[Ur.t])
            for c0 in (range(0, 32 if 'trlast' in SKIP else 34, 4) if 'tr' not in SKIP else []):
                n = min(4, 34 - c0)
                pb_ = ps_next()
                for c in range(n):
                    P.mm(pb_.ap[:, c * 128:(c + 1) * 128], KT.ap[:, (c0 + c) * 128:(c0 + c + 1) * 128], id_bf.ap,
                         True, True, reads=[KT.t, id_bf.t], writes=[pb_.t])
                P.ts("dve", Kf.ap[:, c0:c0 + n, :], pb_.ap[:, 0:n * 128].rearrange("p (a b) -> p a b", a=n),
                     kd.ap[:, 0:1], None, ALU.mult, reads=[pb_.t, kd.t], writes=[Kf.t])
                P.act(Kb.ap[:, c0:c0 + n, :], pb_.ap[:, 0:n * 128].rearrange("p (a b) -> p a b", a=n), AF.Identity,
                      reads=[pb_.t, kd.t], writes=[Kb.t], scale=kd.ap[:, 1:2])

            def upd(Kx, c, cdcol, first):
                if 'upd' in SKIP:
                    return
                pb_ = ps_next()
                P.mm(pb_.ap[:, 0:256], Kx.ap[:, c, :], Vr.ap[:, c, :], True, True, reads=[Kx.t, Vr.t],
                     writes=[pb_.t])
                if first:
                    P.copy("dve", S.ap, pb_.ap[:, 0:256], reads=[pb_.t], writes=[S.t])
                else:
                    P.stt(S.ap, S.ap, kd.ap[:, cdcol:cdcol + 1], pb_.ap[:, 0:256], ALU.mult, ALU.add,
                          reads=[S.t, kd.t, pb_.t], writes=[S.t])

            def snap(dst, idx):
                if 'upd' in SKIP:
                    return
                P.copy("act", dst.ap[:, idx, :], S.ap, reads=[S.t], writes=[dst.t])

            upd(Kf, 32, 2, True)
            snap(SFb, 17)
            upd(Kf, 33, 2, False)
            P.copy("dve", S0f.ap, S.ap, reads=[S.t], writes=[S0f.t])
            for c in range(16, 32):
                upd(Kf, c, 2, False)
            P.tt("dve", S.ap, S.ap, S0f.ap, ALU.subtract, reads=[S.t, S0f.t], writes=[S.t])
            P.stt(S.ap, S.ap, flg[:, 1:2], S0f.ap, ALU.mult, ALU.add, reads=[S.t, S0f.t, sg.t], writes=[S.t])
            for c in range(0, 16):
                snap(SFb, c)
                if c < 15:
                    upd(Kf, c, 2, False)
            upd(Kb, 33, 3, True)
            snap(SBb, 16)
            upd(Kb, 32, 3, False)
            P.copy("dve", S0b.ap, S.ap, reads=[S.t], writes=[S0b.t])
            for c in range(31, 15, -1):
                upd(Kb, c, 3, False)
            P.tt("dve", S.ap, S.ap, S0b.ap, ALU.subtract, reads=[S.t, S0b.t], writes=[S.t])
            P.stt(S.ap, S.ap, flg[:, 0:1], S0b.ap, ALU.mult, ALU.add, reads=[S.t, S0b.t, sg.t], writes=[S.t])
            for c in range(15, -1, -1):
                snap(SBb, c)
                if c > 0:
                    upd(Kb, c, 3, False)
            for (t0, nt, mj) in (qtiles if 'out' not in SKIP else []):
                ncx = nt // 128
                pS = ps_next()
                for j in range(ncx):
                    sl = slice(t0 + j * 128, t0 + (j + 1) * 128)
                    P.mm(pS.ap[:, j * 128:(j + 1) * 128], KT.ap[:, sl], QT.ap[:, sl], True, True,
                         reads=[KT.t, QT.t], writes=[pS.t])
                P.tt("dve", Pm.ap[:, 0:nt], pS.ap[:, 0:nt], Dm.ap[:, 0:nt], ALU.mult, reads=[pS.t, Dm.t],
                     writes=[Pm.t])
                P.tt("pool", Qf.ap[:, 0:nt], QT.ap[:, t0:t0 + nt], qdf.ap[:, 0:nt], ALU.mult, reads=[QT.t, qdf.t],
                     writes=[Qf.t])
                P.tt("pool", Qb.ap[:, 0:nt], QT.ap[:, t0:t0 + nt], qdb.ap[:, 0:nt], ALU.mult, reads=[QT.t, qdb.t],
                     writes=[Qb.t])
                pO = [ps_next(), ps_next()]
                for dj in range(2):
                    dsl = slice(dj * 128, (dj + 1) * 128)
                    for j in range(ncx):
                        c = t0 // 128 + j
                        csl = slice(j * 128, (j + 1) * 128)
                        if c < 16:
                            sf, sb = c, c
                        elif c == 32:
                            sf, sb = None, 16
                        else:
                            sf, sb = 17, None
                        terms = [(Vr.ap[:, c, dsl], Pm.ap[:, csl], [Vr.t, Pm.t])]
                        if sf is not None:
                            terms.append((SFb.ap[:, sf, dsl], Qf.ap[:, csl], [SFb.t, Qf.t]))
                        if sb is not None:
                            terms.append((SBb.ap[:, sb, dsl], Qb.ap[:, csl], [SBb.t, Qb.t]))
                        for ti, (l_, r_, rd) in enumerate(terms):
                            P.mm(pO[dj].ap[:, csl], l_, r_, ti == 0, ti == len(terms) - 1, reads=rd,
                                 writes=[pO[dj].t])
                    P.act(sq2.ap[:, dj, 0:nt], pO[dj].ap[:, 0:nt], AF.Square, reads=[pO[dj].t], writes=[sq2.t])
                pN = ps_next()
                for dj in range(2):
                    P.mm(pN.ap[:, 0:nt], ones_bf.ap, sq2.ap[:, dj, 0:nt], dj == 0, dj == 1,
                         reads=[ones_bf.t, sq2.t], writes=[pN.t])
                rsqrt_from_psum(rs3.ap[:, 0:nt], pN.ap[:, 0:nt], 256.0, [pN.t], [rs3.t])
                for dj in range(2):
                    P.tt("dve", to_[dj].ap[:, 0:nt], pO[dj].ap[:, 0:nt], rs3.ap[:, 0:nt], ALU.mult,
                         reads=[pO[dj].t, rs3.t], writes=[to_[dj].t])
                    P.tt("pool", stg[dj].ap[:, 0:nt], to_[dj].ap[:, 0:nt], Ur.ap[:, dj, t0:t0 + nt], ALU.mult,
                         reads=[to_[dj].t, Ur.t], writes=[stg[dj].t])
                    r0 = (2 * h + dj) * 128
                    P.dma("pool", obr_d[0][r0:r0 + 128, qpos(t0):qpos(t0) + nt], stg[dj].ap[:, 0:nt],
                          reads=[stg[dj].t], writes=[tobr(0, (h, dj, t0))])
        P.barrier()
        if stop <= 3:
            continue
        A.off = PH0_END
        KSa = A.alloc([128, NALL], BF16, "KSa")
        KSb = A.alloc([128, NALL], BF16, "KSb")
        QS = A.alloc([128, 4, NALL], BF16, "QS")
        Vs = A.alloc([128, 34, 64], BF16, "Vs")
        Pt = [A.alloc([128, 512], BF16, f"Pt{i}") for i in range(3)]
        sk = A.alloc([64, 512], F32, "sk")
        den = A.alloc([128, 512], F32, "den")
        ostg = [A.alloc([128, 512], BF16, f"ostg{i}") for i in range(2)]
        P.dma("sp", KSa.ap, fm_d["ks"], writes=[KSa.t])
        P.dma("sp", KSb.ap, fm_d["ks2"], writes=[KSb.t])
        obr1 = obr_d[1].rearrange("(c p) t -> p c t", p=128)
        oc = 0
        for g in range(2):
            for i in range(4):
                P.dma("sp", QS.ap[:, i, 0:NOWN], fm_d[f"qs{g * 4 + i}"][:, 0:NOWN], writes=[QS.t])
                if need_ctx:
                    P.dma("sp", QS.ap[:, i, 4096:NALL], fm_d[f"qs{g * 4 + i}"][:, 4096:NALL], writes=[QS.t])
            vsrc = vall_d[:, 1024 + g * 64:1024 + (g + 1) * 64].rearrange("(t p) c -> p t c", p=128)
            for tq in range(0, 34, 4):
                P.dma("sp", Vs.ap[:, tq:min(tq + 4, 34), :], vsrc[:, tq:min(tq + 4, 34), :], writes=[Vs.t])
            for par in range(2):
                pb0 = par * 64
                Ksrc = KSa if g == par else KSb
                for i in range(4):
                    hq = g * 8 + par + 2 * i
                    P.act(sk.ap[:, i * 128:(i + 1) * 128], cst.ap[0:64, C_DPOS:C_DPOS + 128], AF.Identity,
                          reads=[cst.t, sinkx.t], writes=[sk.t], bias=sinkx.ap[0:64, hq:hq + 1], scale=0.0)
                blocks = [(jb * 128, jb) for jb in range(16)] + ([(4096, 16), (4224, 17)] if need_ctx else [])
                for (t0, jb) in blocks:
                    if jb < 16:
                        kts = [((jb - 1) * 128, 0) if jb > 0 else (2048 + 15 * 128, 2), (jb * 128, None),
                               ((jb + 1) * 128, 1) if jb < 15 else (2048, 3), (4096, None), (4224, None)]
                    else:
                        kts = [(4096, None), (4224, None)]
                    pO, pD = ps[5], ps[6]
                    rhs = QS.ap[pb0:pb0 + 64, :, t0:t0 + 128]
                    for ki, (k0, mi) in enumerate(kts):
                        pS = ps_next(5)
                        P.mm(pS.ap.rearrange("p (a b) -> p a b", a=4), Ksrc.ap[pb0:pb0 + 64, k0:k0 + 128], rhs,
                             True, True, reads=[Ksrc.t, QS.t], writes=[pS.t])
                        pt = Pt[oc % 3]
                        oc += 1
                        P.act(pt.ap, pS.ap, AF.Exp, reads=[pS.t], writes=[pt.t], scale=0.125)
                        if mi is not None:
                            P.tt("pool", pt.ap, pt.ap, mk_bf.ap[:, mi, :], ALU.mult, reads=[pt.t, mk_bf.t],
                                 writes=[pt.t])
                        P.mm(pO.ap[0:64, :], Vs.ap[:, k0 // 128, :], pt.ap, ki == 0, ki == len(kts) - 1,
                             reads=[Vs.t, pt.t], writes=[pO.t])
                        P.mm(pD.ap[0:64, :], ones_bf.ap[:, 0:64], pt.ap, ki == 0, ki == len(kts) - 1,
                             reads=[ones_bf.t, pt.t], writes=[pD.t])
                    P.tt("dve", den.ap[0:64, :], pD.ap[0:64, :], sk.ap, ALU.add, reads=[pD.t, sk.t], writes=[den.t])
                    P.act(den.ap[0:64, :], den.ap[0:64, :], AF.Ln, reads=[den.t], writes=[den.t])
                    P.act(den.ap[0:64, :], den.ap[0:64, :], AF.Exp, reads=[den.t], writes=[den.t], scale=-1.0)
                    os_ = ostg[jb % 2]
                    P.tt("dve", os_.ap[0:64, :], pO.ap[0:64, :], den.ap[0:64, :], ALU.mult, reads=[pO.t, den.t],
                         writes=[os_.t])
                    q0 = qpos(t0)
                    P.dma("pool", obr1[pb0:pb0 + 64, g * 4:g * 4 + 4, q0:q0 + 128],
                          os_.ap[0:64, :].rearrange("p (a b) -> p a b", a=4), reads=[os_.t],
                          writes=[tobr(1, (g, par, t0))])
        P.barrier()
        if stop <= 4:
            continue
        A.off = PH0_END
        KA = A.alloc([128, NALL], BF16, "KA")
        VA = A.alloc([128, 34, 128], BF16, "VA")
        QA = [A.alloc([128, NALL], BF16, f"QA{i}") for i in range(2)]
        Pt = [A.alloc([128, 512], BF16, f"Pt{i}") for i in range(3)]
        den = A.alloc([128, 512], F32, "den")
        ostg = [A.alloc([128, 512], BF16, f"ostg{i}") for i in range(2)]
        GSCALE = 128.0 ** -0.5
        oc = 0
        for g in range(2):
            P.dma("sp", KA.ap, fm_d[f"ka{g}"], writes=[KA.t])
            vsrc = vall_d[:, 1152 + g * 128:1152 + (g + 1) * 128].rearrange("(t p) c -> p t c", p=128)
            for tq in range(0, 34, 4):
                P.dma("sp", VA.ap[:, tq:min(tq + 4, 34), :], vsrc[:, tq:min(tq + 4, 34), :], writes=[VA.t])
            for hh in range(4):
                h = g * 4 + hh
                Q_ = QA[h % 2]
                P.dma("sp", Q_.ap[:, 0:NOWN], fm_d[f"qa{h}"][:, 0:NOWN], writes=[Q_.t])
                if need_ctx:
                    P.dma("sp", Q_.ap[:, 4096:NALL], fm_d[f"qa{h}"][:, 4096:NALL], writes=[Q_.t])
                for (t0, nt, mj) in qtiles:
                    kts = list(range(34)) if t0 < 4096 else [32, 33]
                    pO, pD = ps[5], ps[6]
                    for ki, kt in enumerate(kts):
                        pS = ps_next(5)
                        P.mm(pS.ap[:, 0:nt], KA.ap[:, kt * 128:(kt + 1) * 128], Q_.ap[:, t0:t0 + nt], True, True,
                             reads=[KA.t, Q_.t], writes=[pS.t])
                        pt = Pt[oc % 3]
                        oc += 1
                        P.act(pt.ap[:, 0:nt], pS.ap[:, 0:nt], AF.Exp, reads=[pS.t], writes=[pt.t], scale=GSCALE)
                        P.mm(pO.ap[:, 0:nt], VA.ap[:, kt, :], pt.ap[:, 0:nt], ki == 0, ki == len(kts) - 1,
                             reads=[VA.t, pt.t], writes=[pO.t])
                        P.mm(pD.ap[:, 0:nt], ones_bf.ap, pt.ap[:, 0:nt], ki == 0, ki == len(kts) - 1,
                             reads=[ones_bf.t, pt.t], writes=[pD.t])
                    P.act(den.ap[:, 0:nt], pD.ap[:, 0:nt], AF.Ln, reads=[pD.t], writes=[den.t])
                    P.act(den.ap[:, 0:nt], den.ap[:, 0:nt], AF.Exp, reads=[den.t], writes=[den.t], scale=-1.0)
                    os_ = ostg[oc % 2]
                    P.tt("dve", os_.ap[:, 0:nt], pO.ap[:, 0:nt], den.ap[:, 0:nt], ALU.mult, reads=[pO.t, den.t],
                         writes=[os_.t])
                    q0 = qpos(t0)
                    P.dma("pool", obr_d[2][h * 128:(h + 1) * 128, q0:q0 + nt], os_.ap[:, 0:nt], reads=[os_.t],
                          writes=[tobr(2, (h, t0))])
        P.barrier()
        if stop <= 5:
            continue
        A.off = PH0_END
        wm = [A.alloc([128, 8, 1024], BF16, f"wm{i}") for i in range(4)]
        wst = [A.alloc([128, 8, 512], F32, f"wst{i}") for i in range(2)]
        ob = [A.alloc([128, 8, 512], BF16, f"ob{i}") for i in range(3)]
        gb = [A.alloc([128, 8, 512], BF16, f"gb{i}") for i in range(3)]
        ypre = A.alloc([128, 8, 512], BF16, "ypre")
        xtl = A.alloc([128, 8, 512], F32, "xtl")
        ya = [A.alloc([128, 512], F32, f"ya{i}") for i in range(2)]
        tb = [A.alloc([128, 512], F32, f"tb{i}") for i in range(2)]
        for gi in range(8):
            w = wst[gi % 2]
            P.dma("sp", w.ap, wall_d[li][G_MERGE + gi], writes=[w.t])
            dst = wm[gi // 2].ap[:, :, (gi % 2) * 512:(gi % 2 + 1) * 512]
            P.copy("pool", dst[:, 0:4, :], w.ap[:, 0:4, :], reads=[w.t], writes=[wm[gi // 2].t])
            P.copy("act", dst[:, 4:8, :], w.ap[:, 4:8, :], reads=[w.t], writes=[wm[gi // 2].t])
        gnames = ("ar", "as", "aa")
        for (t0, nt, mj) in qtiles:
            q0 = qpos(t0)
            for b_ in range(3):
                P.dma("sp", ob[b_].ap[:, :, 0:nt],
                      obr_d[b_].rearrange("(kc p) t -> p kc t", p=128)[:, :, q0:q0 + nt], writes=[ob[b_].t])
                for c in range(8):
                    P.dma("sp", gb[b_].ap[:, c, 0:nt], fm_d[f"{gnames[b_]}{c}"][:, t0:t0 + nt], writes=[gb[b_].t])
            P.dma("sp", xtl.ap[:, :, 0:nt], xs3[:, :, t0:t0 + nt], reads=[txs(t0)], writes=[xtl.t])
            for dc in range(8):
                dsl = slice(dc * 128, (dc + 1) * 128)
                y_ = ya[dc % 2]
                for b_ in range(3):
                    pa = ps_next()
                    for kc in range(8):
                        P.mm(pa.ap[:, 0:nt], wm[b_].ap[:, kc, dsl], ob[b_].ap[:, kc, 0:nt], kc == 0, kc == 7,
                             reads=[wm[b_].t, ob[b_].t], writes=[pa.t])
                    if b_ == 0:
                        P.tt("dve", y_.ap[:, 0:nt], pa.ap[:, 0:nt], gb[0].ap[:, dc, 0:nt], ALU.mult,
                             reads=[pa.t, gb[0].t], writes=[y_.t])
                    else:
                        t_ = tb[b_ % 2]
                        P.tt("dve", t_.ap[:, 0:nt], pa.ap[:, 0:nt], gb[b_].ap[:, dc, 0:nt], ALU.mult,
                             reads=[pa.t, gb[b_].t], writes=[t_.t])
                        if b_ == 1:
                            P.tt("pool", y_.ap[:, 0:nt], y_.ap[:, 0:nt], t_.ap[:, 0:nt], ALU.add,
                                 reads=[y_.t, t_.t], writes=[y_.t])
                        else:
                            P.tt("pool", ypre.ap[:, dc, 0:nt], y_.ap[:, 0:nt], t_.ap[:, 0:nt], ALU.add,
                                 reads=[y_.t, t_.t], writes=[ypre.t])
            for dc in range(8):
                dsl = slice(dc * 128, (dc + 1) * 128)
                py = ps_next()
                for kc in range(8):
                    P.mm(py.ap[:, 0:nt], wm[3].ap[:, kc, dsl], ypre.ap[:, kc, 0:nt], kc == 0, kc == 7,
                         reads=[wm[3].t, ypre.t], writes=[py.t])
                P.stt(xtl.ap[:, dc, 0:nt], py.ap[:, 0:nt], GT1(dc, mj), xtl.ap[:, dc, 0:nt], ALU.mult, ALU.add,
                      reads=[py.t, modT.t, xtl.t], writes=[xtl.t])
            P.dma("pool", xs3[:, :, t0:t0 + nt], xtl.ap[:, :, 0:nt], reads=[xtl.t], writes=[txs(t0)])
        P.barrier()
        if stop <= 6:
            continue
        A.off = PH0_END
        h2T = A.alloc([128, 8, NQ], BF16, "h2T")
        xres = A.alloc([128, 8, NQ], F32, "xres")
        WT = A.alloc([16, NQ], F32, "WT")
        M0 = A.off
        h2f = A.alloc([128, 8, 512], F32, "h2f")
        sq = A.alloc([128, 8, 512], BF16, "sq")
        rstd = A.alloc([128, 512], F32, "rstd")
        tmp = [A.alloc([128, 512], F32, f"tmp{i}") for i in range(2)]
        rt = {nm: A.alloc([128, 16], F32, "rt_" + nm) for nm in ("s", "bz", "eq", "msk", "ch", "ws", "wts")}
        rs = {nm: A.alloc([128, 4], F32, "rs_" + nm) for nm in ("m1", "m2", "gs", "gsel", "gm", "dn")}

        def v3(b_):
            return b_.ap.rearrange("p (g k) -> p g k", k=4)

        def bc(b_):
            return b_.ap[:, 0:4].unsqueeze(2).to_broadcast([128, 4, 4])
        mtiles = qtiles
        for i, (t0, nt, mj) in enumerate(mtiles):
            q0 = qpos(t0)
            P.dma("sp", xres.ap[:, :, q0:q0 + nt], xs3[:, :, t0:t0 + nt], reads=[txs(t0)], writes=[xres.t])
            P.act(sq.ap[:, :, 0:nt], xres.ap[:, :, q0:q0 + nt], AF.Square, reads=[xres.t], writes=[sq.t])
            pb = ps_next()
            for kc in range(8):
                P.mm(pb.ap[:, 0:nt], ones_bf.ap, sq.ap[:, kc, 0:nt], kc == 0, kc == 7, reads=[ones_bf.t, sq.t],
                     writes=[pb.t])
            rsqrt_from_psum(rstd.ap[:, 0:nt], pb.ap[:, 0:nt], 1024.0, [pb.t], [rstd.t])
            for kc in range(8):
                tm_ = tmp[kc % 2]
                P.tt("dve", tm_.ap[:, 0:nt], xres.ap[:, kc, q0:q0 + nt], rstd.ap[:, 0:nt], ALU.mult,
                     reads=[xres.t, rstd.t], writes=[tm_.t])
                P.act(h2f.ap[:, kc, 0:nt], tm_.ap[:, 0:nt], AF.Identity, reads=[tm_.t, A2.t, modT.t], writes=[h2f.t],
                      bias=SH2(kc, mj), scale=A2.ap[:, kc, mj:mj + 1])
                P.copy("pool", h2T.ap[:, kc, q0:q0 + nt], h2f.ap[:, kc, 0:nt], reads=[h2f.t], writes=[h2T.t])
            for sub in range(nt // 128):
                ssl = slice(sub * 128, (sub + 1) * 128)
                pr = ps_next()
                for kc in range(8):
                    P.mm(pr.ap[:, 0:16], h2f.ap[:, kc, ssl], sg.ap[:, SG_WR + kc * 16:SG_WR + (kc + 1) * 16],
                         kc == 0, kc == 7, reads=[h2f.t, sg.t], writes=[pr.t])
                s_, bz, eq, msk, ch, ws, wts = [rt[n] for n in ("s", "bz", "eq", "msk", "ch", "ws", "wts")]
                m1, m2, gs, gsel, gm, dn = [rs[n] for n in ("m1", "m2", "gs", "gsel", "gm", "dn")]
                P.act(s_.ap, pr.ap[:, 0:16], AF.Sigmoid, reads=[pr.t], writes=[s_.t])
                P.tt("dve", bz.ap, s_.ap, sg.ap[:, SG_BR:SG_BR + 16], ALU.add, reads=[s_.t, sg.t], writes=[bz.t])
                P.add("dve", lambda e, o=m1.ap, i_=v3(bz): e.tensor_reduce(out=o, in_=i_, axis=AX.X, op=ALU.max),
                      reads=[bz.t], writes=[m1.t])
                P.tt("dve", v3(eq), v3(bz), bc(m1), ALU.is_equal, reads=[bz.t, m1.t], writes=[eq.t])
                P.stt(msk.ap, eq.ap, -1e9, bz.ap, ALU.mult, ALU.add, reads=[eq.t, bz.t], writes=[msk.t])
                P.add("dve", lambda e, o=m2.ap, i_=v3(msk): e.tensor_reduce(out=o, in_=i_, axis=AX.X, op=ALU.max),
                      reads=[msk.t], writes=[m2.t])
                P.tt("dve", gs.ap, m1.ap, m2.ap, ALU.add, reads=[m1.t, m2.t], writes=[gs.t])
                P.add("dve", lambda e, o=gm.ap[:, 0:1], i_=gs.ap: e.tensor_reduce(out=o, in_=i_, axis=AX.X,
                                                                                 op=ALU.max),
                      reads=[gs.t], writes=[gm.t])
                P.ts("dve", gsel.ap, gs.ap, gm.ap[:, 0:1], None, ALU.is_equal, reads=[gs.t, gm.t], writes=[gsel.t])
                P.tt("dve", v3(ch), v3(bz), bc(m2), ALU.is_ge, reads=[bz.t, m2.t], writes=[ch.t])
                P.tt("dve", v3(ch), v3(ch), bc(gsel), ALU.mult, reads=[ch.t, gsel.t], writes=[ch.t])
                P.tt("dve", ws.ap, s_.ap, ch.ap, ALU.mult, reads=[s_.t, ch.t], writes=[ws.t])
                P.add("dve", lambda e, o=dn.ap[:, 0:1], i_=ws.ap: e.tensor_reduce(out=o, in_=i_, axis=AX.X,
                                                                                 op=ALU.add),
                      reads=[ws.t], writes=[dn.t])
                P.add("dve", lambda e, o=dn.ap[:, 1:2], i_=dn.ap[:, 0:1]: e.reciprocal(o, i_), reads=[dn.t],
                      writes=[dn.t])
                P.ts("dve", wts.ap, ws.ap, dn.ap[:, 1:2], None, ALU.mult, reads=[ws.t, dn.t], writes=[wts.t])
                pT = ps_next()
                P.transpose(pT.ap[0:16, 0:128], wts.ap, cst.ap[:, C_ID:C_ID + 128], reads=[wts.t, cst.t],
                            writes=[pT.t])
                P.copy("act", WT.ap[0:16, q0 + sub * 128:q0 + (sub + 1) * 128], pT.ap[0:16, 0:128], reads=[pT.t],
                       writes=[WT.t])
        P.barrier()
        A.off = M0
        ew = [[A.alloc([128, 4096], BF16, f"ew{i}_{k}") for k in range(3)] for i in range(2)]
        wbc = A.alloc([128, 512], F32, "wbc")
        sG = [A.alloc([128, 512], F32, f"sG{i}") for i in range(2)]
        tG = [A.alloc([128, 512], F32, f"tG{i}") for i in range(2)]
        hid = A.alloc([128, 4, 512], BF16, "hid")
        for e in range(16):
            wg_, wu_, wd_ = ew[e % 2]
            P.dma("sp", wg_.ap, web_d[e, 0], reads=[tweb(e, 0)], writes=[wg_.t])
            P.dma("sp", wu_.ap, web_d[e, 1], reads=[tweb(e, 1)], writes=[wu_.t])
            P.dma("sp", wd_.ap, web_d[e, 2], reads=[tweb(e, 2)], writes=[wd_.t])
            wg3 = wg_.ap.rearrange("p (a b) -> p a b", a=8)
            wu3 = wu_.ap.rearrange("p (a b) -> p a b", a=8)
            wd3 = wd_.ap.rearrange("p (a b) -> p a b", a=4)
            for (t0, nt, mj) in mtiles:
                q0 = qpos(t0)
                pw = ps_next()
                P.mm(pw.ap[:, 0:nt], cst.ap[0:16, C_SEL + e * 128:C_SEL + (e + 1) * 128], WT.ap[0:16, q0:q0 + nt],
                     True, True, reads=[cst.t, WT.t], writes=[pw.t])
                P.copy("act", wbc.ap[:, 0:nt], pw.ap[:, 0:nt], reads=[pw.t], writes=[wbc.t])
                for hc in range(4):
                    hsl = slice(hc * 128, (hc + 1) * 128)
                    pg, pu = ps_next(), ps_next()
                    for kc in range(8):
                        P.mm(pg.ap[:, 0:nt], wg3[:, kc, hsl], h2T.ap[:, kc, q0:q0 + nt], kc == 0, kc == 7,
                             reads=[wg_.t, h2T.t], writes=[pg.t])
                    for kc in range(8):
                        P.mm(pu.ap[:, 0:nt], wu3[:, kc, hsl], h2T.ap[:, kc, q0:q0 + nt], kc == 0, kc == 7,
                             reads=[wu_.t, h2T.t], writes=[pu.t])
                    sg_, tg_ = sG[hc % 2], tG[hc % 2]
                    P.act(sg_.ap[:, 0:nt], pg.ap[:, 0:nt], AF.Silu, reads=[pg.t], writes=[sg_.t])
                    P.tt("dve", tg_.ap[:, 0:nt], sg_.ap[:, 0:nt], pu.ap[:, 0:nt], ALU.mult, reads=[sg_.t, pu.t],
                         writes=[tg_.t])
                    P.tt("pool", hid.ap[:, hc, 0:nt], tg_.ap[:, 0:nt], wbc.ap[:, 0:nt], ALU.mult,
                         reads=[tg_.t, wbc.t], writes=[hid.t])
                for dc in range(8):
                    dsl = slice(dc * 128, (dc + 1) * 128)
                    py = ps_next()
                    for hc in range(4):
                        P.mm(py.ap[:, 0:nt], wd3[:, hc, dsl], hid.ap[:, hc, 0:nt], hc == 0, hc == 3,
                             reads=[wd_.t, hid.t], writes=[py.t])
                    P.stt(xres.ap[:, dc, q0:q0 + nt], py.ap[:, 0:nt], GT2(dc, mj), xres.ap[:, dc, q0:q0 + nt],
                          ALU.mult, ALU.add, reads=[py.t, modT.t, xres.t], writes=[xres.t])
        if not (last and final):
            for (t0, nt, mj) in mtiles:
                q0 = qpos(t0)
                P.dma("pool", xs3[:, :, t0:t0 + nt], xres.ap[:, :, q0:q0 + nt], reads=[xres.t], writes=[txs(t0)])
        if not last:
            for i, (t0, nt, mj) in enumerate(TOK_TILES_OWN):
                P.dma("pool", xch_src[i].rearrange("(kc p) t -> p kc t", p=128), xres.ap[:, :, t0:t0 + nt],
                      reads=[xres.t], writes=[T_xsrc[i]])
            P.barrier()
            for i in range(4):
                P.add("pool", lambda e, i=i: e.collective_compute("AllGather", ALU.bypass,
                                                                  replica_groups=[[0, 1], [2, 3], [4, 5], [6, 7]],
                                                                  ins=[xch_src[i]], outs=[xch_dst[i]]),
                      reads=[T_xsrc[i]], writes=[T_xdst[i]], cc=True)
            P.barrier()
            A.off = M0
            xa = [A.alloc([128, 8, 512], F32, f"xa{i}") for i in range(2)]
            xb_ = [A.alloc([128, 8, 512], F32, f"xb{i}") for i in range(2)]
            for i, (t0, nt, mj) in enumerate(TOK_TILES_OWN):
                d3 = xch_dst[i].rearrange("(r kc p) t -> r p kc t", r=2, p=128)
                a_, b_ = xa[i % 2], xb_[i % 2]
                P.dma("sp", a_.ap, d3[0], reads=[T_xdst[i]], writes=[a_.t])
                P.dma("sp", b_.ap, d3[1], reads=[T_xdst[i]], writes=[b_.t])
                P.ts("dve", a_.ap, a_.ap, sg.ap[:, SG_FLAG + 1:SG_FLAG + 2], None, ALU.mult, reads=[a_.t, sg.t],
                     writes=[a_.t])
                P.stt(a_.ap, b_.ap, sg.ap[:, SG_FLAG:SG_FLAG + 1], a_.ap, ALU.mult, ALU.add,
                      reads=[a_.t, b_.t, sg.t], writes=[a_.t])
                P.dma("pool", xs3[:, :, NOWN + t0:NOWN + t0 + nt], a_.ap, reads=[a_.t], writes=[txs(NOWN + t0)])
        if last and not final:
            xout3 = xout.rearrange("(kc p) t -> p kc t", p=128)
            for (t0, nt, mj) in mtiles:
                q0 = qpos(t0)
                P.dma("pool", xout3[:, :, q0:q0 + nt], xres.ap[:, :, q0:q0 + nt], reads=[xres.t])
        if last and final:
            P.barrier()
            A.off = M0
            h2f = A.alloc([128, 8, 512], F32, "h2f")
            sq = A.alloc([128, 8, 512], BF16, "sq")
            rstd = A.alloc([128, 512], F32, "rstd")
            tmp = [A.alloc([128, 512], F32, f"tmp{i}") for i in range(2)]
            yout3 = yout.rearrange("(kc p) t -> p kc t", p=128)
            for (t0, nt, mj) in TOK_TILES_OWN:
                P.act(sq.ap[:, :, 0:nt], xres.ap[:, :, t0:t0 + nt], AF.Square, reads=[xres.t], writes=[sq.t])
                pb = ps_next()
                for kc in range(8):
                    P.mm(pb.ap[:, 0:nt], ones_bf.ap, sq.ap[:, kc, 0:nt], kc == 0, kc == 7, reads=[ones_bf.t, sq.t],
                         writes=[pb.t])
                rsqrt_from_psum(rstd.ap[:, 0:nt], pb.ap[:, 0:nt], 1024.0, [pb.t], [rstd.t])
                for kc in range(8):
                    tm_ = tmp[kc % 2]
                    P.tt("dve", tm_.ap[:, 0:nt], xres.ap[:, kc, t0:t0 + nt], rstd.ap[:, 0:nt], ALU.mult,
                         reads=[xres.t, rstd.t], writes=[tm_.t])
                    P.act(h2f.ap[:, kc, 0:nt], tm_.ap[:, 0:nt], AF.Identity, reads=[tm_.t, sg.t], writes=[h2f.t],
                          scale=sg.ap[:, SG_GF + kc:SG_GF + kc + 1])
                P.dma("pool", yout3[:, :, t0:t0 + nt], h2f.ap[:, :, 0:nt], reads=[h2f.t])
    P.emit()
    return nc


_PROGS = {}


def _prog(key, *args, **kw):
    if key not in _PROGS:
        _PROGS[key] = build(*args, **kw)
    return _PROGS[key]


def kernel(**inp):
    inp = {k: np.asarray(v) for k, v in inp.items()}
    x, ctx = inp["x"], inp["ctx"]
    B = x.shape[0]
    cores = [(b, h) for b in range(B) for h in range(2)]
    consts = [make_consts(h) for h in range(2)]
    tabs = [make_tabs(h) for h in range(2)]
    w0 = prep_layer_weights(inp, 0)
    w1 = prep_layer_weights(inp, 1)
    maps = []
    for (b, h) in cores:
        xo = x[b, h * NOWN:(h + 1) * NOWN].T
        xt = x[b, (1 - h) * NOWN:(2 - h) * NOWN].T
        xall = np.ascontiguousarray(np.concatenate([xo, xt, ctx[b].T], axis=1))
        maps.append(dict(xall=xall, tabs=tabs[h], consts=consts[h], sg=make_small_global(inp, b, h),
                         sm0=w0[2], wall0=w0[0], we0=w0[1], sm1=w1[2], wall1=w1[0], we1=w1[1]))
    nc = _prog("fused", [0, 1], [True, False], True)
    r = run_bass_kernel_spmd(nc, maps, core_ids=list(range(len(cores)))).results
    out = np.zeros((B, 2 * NOWN, D), np.float32)
    for i, (b, h) in enumerate(cores):
        out[b, h * NOWN:(h + 1) * NOWN] = np.asarray(r[i]["yout"]).T
    return out
```

```python
import numpy as np
import concourse.bass as bass
import concourse.mybir as mybir
from concourse.bass_utils import run_bass_kernel_spmd

F32 = mybir.dt.float32
BF16 = mybir.dt.bfloat16
AF = mybir.ActivationFunctionType
ALU = mybir.AluOpType
AX = mybir.AxisListType

NOWN, NOTH, NCTX = 2048, 2048, 256
NALL = NOWN + NOTH + NCTX
NQ = NOWN + NCTX
D = 1024
EPS = 1e-6
NSLOT = 24
SKIP = set()


class T:
    __slots__ = ("name", "w", "r", "psum")

    def __init__(self, name="", psum=False):
        self.name = name
        self.w = None
        self.r = []
        self.psum = psum


class Op:
    __slots__ = ("eng", "fn", "deps", "id", "is_dma", "slot", "val", "marked", "semval", "cc")

    def __init__(self, eng, fn, is_dma):
        self.eng = eng
        self.fn = fn
        self.deps = []
        self.is_dma = is_dma
        self.slot = None
        self.val = None
        self.marked = False
        self.semval = None
        self.cc = False


class Prog:
    ENGS = ("pe", "act", "dve", "pool", "sp")

    def __init__(self, nc):
        self.nc = nc
        self.ops = []
        self.streams = {e: [] for e in self.ENGS}
        self.ndma = {e: 0 for e in self.ENGS}
        self.dmas = {e: [] for e in self.ENGS}

    def add(self, eng, fn, reads=(), writes=(), dma=False, extra=(), cc=False):
        op = Op(eng, fn, dma or cc)
        op.cc = cc
        op.id = len(self.ops)
        deps = {}
        for t in reads:
            if t.w is not None:
                deps[t.w.id] = t.w
            if t.psum:
                for r in t.r:
                    if r.eng != eng:
                        deps[r.id] = r
        for t in writes:
            if t.w is not None:
                deps[t.w.id] = t.w
            for r in t.r:
                deps[r.id] = r
        for d in extra:
            deps[d.id] = d
        op.deps = list(deps.values())
        for t in reads:
            if not dma:
                t.r = [r for r in t.r if r.is_dma or r.eng != eng]
            t.r.append(op)
        for t in writes:
            t.w = op
            t.r = []
        if cc:
            self.ncc = getattr(self, "ncc", 0) + 1
            op.slot = ("cc", self.ncc - 1)
            op.val = 1
            self.dmas[eng].append(op)
        elif dma:
            i = self.ndma[eng]
            self.ndma[eng] += 1
            op.slot = i % NSLOT
            op.val = 16 * (i // NSLOT + 1)
            self.dmas[eng].append(op)
        self.ops.append(op)
        self.streams[eng].append(op)
        return op

    def barrier(self):
        lasts = []
        for e in self.ENGS:
            if self.streams[e]:
                lasts.append(self.streams[e][-1])
            lasts.extend(self.dmas[e][-NSLOT:])
        for e in self.ENGS:
            self.add(e, lambda eng: eng.nop(), extra=lasts)

    def dma(self, q, out, in_, reads=(), writes=()):
        return self.add(q, lambda e: e.dma_start(out=out, in_=in_), reads, writes, dma=True)

    def mm(self, out, lhsT, rhs, start, stop, reads=(), writes=()):
        return self.add("pe", lambda e: e.matmul(out, lhsT, rhs, start=start, stop=stop), reads, writes)

    def transpose(self, out, in_, ident, reads=(), writes=()):
        return self.add("pe", lambda e: e.transpose(out, in_, ident), reads, writes)

    def act(self, out, in_, func, reads=(), writes=(), bias=None, scale=None):
        kw = {}
        if bias is not None:
            kw["bias"] = bias
        if scale is not None:
            kw["scale"] = scale
        return self.add("act", lambda e: e.activation(out, in_, func, **kw), reads, writes)

    def tt(self, eng, out, in0, in1, op, reads=(), writes=()):
        return self.add(eng, lambda e: e.tensor_tensor(out, in0, in1, op), reads, writes)

    def ts(self, eng, out, in0, s1, s2, op0, op1=None, reads=(), writes=()):
        if op1 is None:
            return self.add(eng, lambda e: e.tensor_scalar(out=out, in0=in0, scalar1=s1, scalar2=None, op0=op0),
                            reads, writes)
        return self.add(eng, lambda e: e.tensor_scalar(out=out, in0=in0, scalar1=s1, scalar2=s2, op0=op0, op1=op1),
                        reads, writes)

    def stt(self, out, in0, scalar, in1, op0, op1, reads=(), writes=()):
        return self.add("dve", lambda e: e.scalar_tensor_tensor(out=out, in0=in0, scalar=scalar, in1=in1,
                                                                op0=op0, op1=op1), reads, writes)

    def copy(self, eng, out, in_, reads=(), writes=()):
        if eng == "act":
            return self.add("act", lambda e: e.copy(out, in_), reads, writes)
        return self.add(eng, lambda e: e.tensor_copy(out, in_), reads, writes)

    def emit(self):
        nc = self.nc
        for op in self.ops:
            for d in op.deps:
                if d.is_dma:
                    continue
                if d.eng == op.eng and not op.is_dma and d.eng == "pe":
                    continue
                d.marked = True
        cnt = {e: 0 for e in self.ENGS}
        for op in self.ops:
            if op.marked and not op.is_dma:
                cnt[op.eng] += 1
                op.semval = cnt[op.eng]
        sems = {e: nc.alloc_semaphore(f"s_{e}") for e in self.ENGS}
        dsems = {e: {i: nc.alloc_semaphore(f"d_{e}_{i}") for i in range(min(NSLOT, self.ndma[e]))}
                 for e in self.ENGS}
        for i in range(getattr(self, "ncc", 0)):
            dsems["pool"][("cc", i)] = nc.alloc_semaphore(f"cc_{i}")
        engobj = {"pe": nc.tensor, "act": nc.scalar, "dve": nc.vector, "pool": nc.gpsimd, "sp": nc.sync}
        with nc.Block() as block:
            def run(ename):
                eng = engobj[ename]
                waited = {}

                def wait(key, sem, val):
                    if waited.get(key, 0) >= val:
                        return
                    waited[key] = val
                    eng.wait_ge(sem, val)

                for op in self.streams[ename]:
                    for d in op.deps:
                        if d.is_dma:
                            wait(("d", d.eng, d.slot), dsems[d.eng][d.slot], d.val)
                        else:
                            if d.eng == ename and not op.is_dma and ename == "pe":
                                continue
                            wait(("c", d.eng), sems[d.eng], d.semval)
                    if op.cc:
                        ins = op.fn(eng)
                        ins.then_inc(dsems[ename][op.slot], 1)
                    elif op.is_dma:
                        if op.val > 16:
                            wait(("d", ename, op.slot), dsems[ename][op.slot], op.val - 16)
                        ins = op.fn(eng)
                        ins.then_inc(dsems[ename][op.slot], 16)
                    else:
                        ins = op.fn(eng)
                        if op.marked:
                            ins.then_inc(sems[ename], 1)
                n = self.ndma[ename]
                for i in range(max(0, n - NSLOT), n):
                    wait(("d", ename, i % NSLOT), dsems[ename][i % NSLOT], 16 * (i // NSLOT + 1))

            @block.tensor
            def _(e):
                run("pe")

            @block.scalar
            def _(e):
                run("act")

            @block.vector
            def _(e):
                run("dve")

            @block.gpsimd
            def _(e):
                run("pool")

            @block.sync
            def _(e):
                run("sp")


class Buf:
    __slots__ = ("ap", "t")

    def __init__(self, ap, name="", psum=False):
        self.ap = ap
        self.t = T(name, psum)


class Arena:
    def __init__(self, nc, nbytes):
        self.t = nc.alloc_sbuf_tensor("arena", [128, nbytes // 2], BF16)
        self.nbytes = nbytes
        self.off = 0

    def alloc(self, shape, dtype, name=""):
        n = 1
        for s in shape[1:]:
            n *= s
        es = 4 if dtype == F32 else 2
        nb = (n * es + 63) // 64 * 64
        assert self.off + nb <= self.nbytes, f"arena overflow {name} {self.off + nb}"
        v = self.t[0:shape[0], self.off // 2:(self.off + n * es) // 2]
        if dtype == F32:
            v = v.bitcast(F32)
        if len(shape) == 3:
            v = v.rearrange("p (a b) -> p a b", a=shape[1])
        self.off += nb
        return Buf(v, name)


SPL = [512, 512, 1024, 1024, 1024, 128, 128, 1024, 256, 256, 1024, 1024, 1024]
OFF = np.concatenate([[0], np.cumsum(SPL)]).astype(int)
O_QR, O_KR, O_VR, O_UR, O_QS, O_KS, O_VS, O_QA, O_KA, O_VA, O_AR, O_AS, O_AA = [int(v) for v in OFF[:13]]


def _perm128():
    f = np.arange(128)
    return np.where(f % 64 < 32, f + 32, f - 32)


def _perm64():
    f = np.arange(128)
    return np.where(f % 32 < 16, f + 16, f - 16)


def fm_units():
    u = []
    ar = np.arange(128)
    for h in range(4):
        u.append(dict(name=f"kr{h}", kind="rope128", cols=O_KR + h * 128 + ar, tok="all"))
    u.append(dict(name="ks", kind="rope64", cols=O_KS + ar, tok="all"))
    u.append(dict(name="ks2", kind="rope64", cols=O_KS + (ar + 64) % 128, tok="all"))
    for h in range(2):
        u.append(dict(name=f"ka{h}", kind="normk", cols=O_KA + h * 128 + ar, tok="all"))
    for h in range(4):
        u.append(dict(name=f"qr{h}", kind="rope128", cols=O_QR + h * 128 + ar, tok="q"))
    for c in range(8):
        u.append(dict(name=f"qs{c}", kind="rope64", cols=O_QS + c * 128 + ar, tok="q"))
    for h in range(8):
        u.append(dict(name=f"qa{h}", kind="normq", cols=O_QA + h * 128 + ar, tok="q"))
    for c in range(8):
        u.append(dict(name=f"ur{c}", kind="silu", cols=O_UR + c * 128 + ar, tok="q"))
    for nm, o in (("ar", O_AR), ("as", O_AS), ("aa", O_AA)):
        for c in range(8):
            u.append(dict(name=f"{nm}{c}", kind="sig", cols=o + c * 128 + ar, tok="q"))
    g, used = 0, 0
    for x in u:
        w = 256 if x["kind"] in ("rope128", "rope64", "normq", "normk") else 128
        if used + w > 512:
            g, used = g + 1, 0
        x["group"], x["c0"] = g, used
        x["c1"] = used + 128 if w == 256 else None
        used += w
    return u, g + 1


FM_UNITS, N_FM_GROUPS = fm_units()
TM_COLS = np.concatenate([O_VR + np.arange(1024), O_VS + np.arange(128), O_VA + np.arange(256)])
N_TM_GROUPS = 3
G_MOD = 0
G_FM = 12
G_TM = G_FM + N_FM_GROUPS
G_MERGE = G_TM + N_TM_GROUPS
N_GROUPS = G_MERGE + 8

SM_BMOD, SM_G1, SM_G2, SM_RET, SM_SINK, SM_GQ, SM_GK = 0, 48, 56, 64, 72, 88, 90
SM_L = 96
SG_C, SG_WR, SG_BR, SG_GF, SG_FLAG = 0, 16, 144, 160, 168
SG_N = 176
C_ID, C_DPOS, C_DNEG, C_MF, C_MB, C_IP1, C_IB, C_ML, C_MR, C_MLB, C_MRB = [i * 128 for i in range(11)]
C_PCF, C_PCB = 11 * 128, 11 * 128 + 1
C_SEL = 11 * 128 + 8
C_N = C_SEL + 2048


def _grp(w):
    n = w.shape[1] // 512
    return np.ascontiguousarray(w.reshape(8, 128, n, 512).transpose(2, 1, 0, 3))


def prep_layer_weights(inp, l):
    w_in = inp["w_in"][l]
    p128, p64 = _perm128(), _perm64()
    cols = np.zeros(N_FM_GROUPS * 512, dtype=np.int64)
    for u in FM_UNITS:
        base = u["group"] * 512
        cols[base + u["c0"]:base + u["c0"] + 128] = u["cols"]
        if u["c1"] is not None:
            pm = p64 if u["kind"] == "rope64" else p128
            cols[base + u["c1"]:base + u["c1"] + 128] = u["cols"][pm]
    tmc = np.concatenate([TM_COLS, np.zeros(1536 - 1408, dtype=np.int64)])
    wcat = np.concatenate([inp["w_mod"][l], w_in[:, cols], w_in[:, tmc], inp["w_br_ret"][l], inp["w_br_swa"][l],
                           inp["w_br_ga"][l], inp["w_out"][l]], axis=1)
    wall = _grp(wcat)
    assert wall.shape[0] == N_GROUPS
    wg = inp["w_gate"][l].reshape(16, 8, 128, 512).transpose(0, 2, 1, 3).reshape(16, 128, 4096)
    wu = inp["w_up"][l].reshape(16, 8, 128, 512).transpose(0, 2, 1, 3).reshape(16, 128, 4096)
    wd = inp["w_down"][l].reshape(16, 4, 128, 1024).transpose(0, 2, 1, 3).reshape(16, 128, 4096)
    we = np.ascontiguousarray(np.stack([wg, wu, wd], axis=1))
    sm = np.zeros((128, SM_L), np.float32)
    sm[:, SM_BMOD:SM_BMOD + 48] = inp["b_mod"][l].reshape(48, 128).T
    sm[:, SM_G1:SM_G1 + 8] = inp["g_norm1"][l].reshape(8, 128).T
    sm[:, SM_G2:SM_G2 + 8] = inp["g_norm2"][l].reshape(8, 128).T
    sm[:, SM_RET:SM_RET + 8] = inp["ret_decay_logit"][l].reshape(1, 8)
    sm[:, SM_SINK:SM_SINK + 16] = inp["swa_sink"][l].reshape(1, 16)
    sm[:, SM_GQ] = inp["g_qnorm"][l]
    sm[:, SM_GQ + 1] = inp["g_qnorm"][l][p128]
    sm[:, SM_GK] = inp["g_knorm"][l]
    sm[:, SM_GK + 1] = inp["g_knorm"][l][p128]
    return wall, we, sm


def make_consts(half):
    c = np.zeros((128, C_N), np.float32)
    m = np.arange(128)[:, None].astype(np.float32)
    n = np.arange(128)[None, :].astype(np.float32)
    c[:, C_ID:C_ID + 128] = np.eye(128)
    c[:, C_DPOS:C_DPOS + 128] = np.maximum(n - m, 0)
    c[:, C_DNEG:C_DNEG + 128] = np.maximum(m - n, 0)
    c[:, C_MF:C_MF + 128] = (n >= m)
    c[:, C_MB:C_MB + 128] = (m > n)
    c[:, C_IP1:C_IP1 + 128] = n + 1 + 0 * m
    c[:, C_IB:C_IB + 128] = 128 - n + 0 * m
    c[:, C_ML:C_ML + 128] = (m >= n)
    c[:, C_MR:C_MR + 128] = (m <= n)
    c[:, C_MLB:C_MLB + 128] = (m >= n) * (1.0 if half == 1 else 0.0)
    c[:, C_MRB:C_MRB + 128] = (m <= n) * (1.0 if half == 0 else 0.0)
    c[:, C_PCF] = 127 - np.arange(128)
    c[:, C_PCB] = np.arange(128)
    for e in range(16):
        c[e, C_SEL + e * 128:C_SEL + (e + 1) * 128] = 1.0
    return c


def make_tabs(half):
    pos_own = half * NOWN + np.arange(NOWN)
    pos_oth = (1 - half) * NOWN + np.arange(NOTH)
    pos = np.concatenate([pos_own, pos_oth])
    row = (pos // 64).astype(np.float32)
    col = (pos % 64).astype(np.float32)
    tabs = np.zeros((4, 128, NALL), np.float32)
    tabs[0, :, 4096:] = 1.0
    tabs[2, :, 4096:] = 1.0
    for ti, hd in ((0, 128), (2, 64)):
        half_d, quarter = hd // 2, hd // 4
        freqs = (np.float32(10000.0) ** (-np.arange(quarter, dtype=np.float32) / np.float32(quarter))).astype(np.float32)
        for f in range(128):
            fl = f % hd
            a = fl // half_d
            j = fl % half_d
            p = row if a == 0 else col
            ang = (p * freqs[j % quarter]).astype(np.float32)
            tabs[ti, f, :4096] = np.cos(ang)
            tabs[ti + 1, f, :4096] = np.sin(ang) * (-1.0 if j < quarter else 1.0)
    return tabs


def make_small_global(inp, b, half):
    sg = np.zeros((128, SG_N), np.float32)
    cT = inp["c"][b].reshape(8, 128).T
    ccT = inp["c_ctx"].reshape(8, 128).T
    sg[:, SG_C:SG_C + 16:2] = cT
    sg[:, SG_C + 1:SG_C + 16:2] = ccT
    sg[:, SG_WR:SG_WR + 128] = inp["w_router"].reshape(8, 128, 16).transpose(1, 0, 2).reshape(128, 128)
    sg[:, SG_BR:SG_BR + 16] = inp["b_router"].reshape(1, 16)
    sg[:, SG_GF:SG_GF + 8] = inp["g_final"].reshape(8, 128).T
    sg[:, SG_FLAG] = 1.0 if half == 0 else 0.0
    sg[:, SG_FLAG + 1] = 1.0 if half == 1 else 0.0
    return sg


TOK_TILES_ALL = [(i * 512, 512, 0) for i in range(8)] + [(4096, 256, 1)]
TOK_TILES_OWN = [(i * 512, 512, 0) for i in range(4)]
CTX_TILE = (4096, 256, 1)


def qpos(t0):
    return t0 if t0 < NOWN else t0 - NOTH


def build(layers, need_ctx_flags, final, dbg=(), stop=99):
    nc = bass.Bass("TRN2", target_bir_lowering=False)
    P = Prog(nc)
    nl = len(layers)

    def din(name, shape, dt=F32):
        return nc.dram_tensor(name, list(shape), dt, kind="ExternalInput").ap()

    def dscr(name, shape, dt=BF16):
        kind = "ExternalOutput" if name in dbg else "Internal"
        return nc.dram_tensor(name, list(shape), dt, kind=kind).ap()

    xall = din("xall", [D, NALL])
    tabs = din("tabs", [4, 128, NALL])
    consts_d = din("consts", [128, C_N])
    sg_d = din("sg", [128, SG_N])
    sm_d = [din(f"sm{l}", [128, SM_L]) for l in range(nl)]
    wall_d = [din(f"wall{l}", [N_GROUPS, 128, 8, 512]) for l in range(nl)]
    we_d = [din(f"we{l}", [16, 3, 128, 4096]) for l in range(nl)]
    if final:
        yout = nc.dram_tensor("yout", [D, NOWN], F32, kind="ExternalOutput").ap()
    else:
        xout = nc.dram_tensor("xout", [D, NQ], F32, kind="ExternalOutput").ap()

    xs_d = dscr("xs", [D, NALL], F32)
    fm_d = {u["name"]: dscr("fm_" + u["name"], [128, NALL]) for u in FM_UNITS}
    vall_d = dscr("vall", [NALL, 1536])
    obr_d = [dscr(f"obr{i}", [D, NQ]) for i in range(3)]
    web_d = dscr("web", [16, 3, 128, 4096])
    T_xs = {}

    def txs(t0):
        return T_xs.setdefault(t0, T(f"xs{t0}"))
    T_fm = {}

    def tfm(name, t0):
        return T_fm.setdefault((name, t0), T(f"fm{name}{t0}"))
    T_vall = {}

    def tvall(sub):
        return T_vall.setdefault(sub, T(f"vall{sub}"))
    T_obr = {}

    def tobr(i, t0):
        return T_obr.setdefault((i, t0), T(f"obr{i}_{t0}"))
    T_web = {}

    def tweb(e, k):
        return T_web.setdefault((e, k), T(f"web{e}_{k}"))

    xs3 = xs_d.rearrange("(kc p) t -> p kc t", p=128)
    if nl > 1:
        xch_src = [nc.dram_tensor(f"xch_src{i}", [D, 512], F32, kind="Internal").ap() for i in range(4)]
        xch_dst = [nc.dram_tensor(f"xch_dst{i}", [2 * D, 512], F32, kind="Internal").ap() for i in range(4)]
        T_xsrc, T_xdst = [T("xsrc") for i in range(4)], [T("xdst") for i in range(4)]
    xall3 = xall.rearrange("(kc p) t -> p kc t", p=128)

    A = Arena(nc, 206 * 1024)
    cst = A.alloc([128, C_N], F32, "cst")
    sg = A.alloc([128, SG_N], F32, "sg")
    ones_bf = A.alloc([128, 128], BF16, "ones")
    id_bf = A.alloc([128, 128], BF16, "idbf")
    mk_bf = A.alloc([128, 4, 512], BF16, "mk")
    PERS_END = None
    ps = [Buf(nc.alloc_psum_tensor(f"ps{i}", [128, 512], F32)[:], f"ps{i}", True) for i in range(8)]

    P.dma("sp", cst.ap, consts_d, writes=[cst.t])
    P.dma("sp", sg.ap, sg_d, writes=[sg.t])
    P.add("dve", lambda e: e.memset(ones_bf.ap, 1.0), writes=[ones_bf.t])
    P.copy("dve", id_bf.ap, cst.ap[:, C_ID:C_ID + 128], reads=[cst.t], writes=[id_bf.t])
    for i, co in enumerate((C_ML, C_MR, C_MLB, C_MRB)):
        for r in range(4):
            P.copy("dve", mk_bf.ap[:, i, r * 128:(r + 1) * 128], cst.ap[:, co:co + 128], reads=[cst.t],
                   writes=[mk_bf.t])
    for (t0, nt, _) in TOK_TILES_ALL:
        P.dma("sp", xs_d[:, t0:t0 + nt], xall[:, t0:t0 + nt], writes=[txs(t0)])
    PERS_END = A.off

    def rsqrt_from_psum(dst, src_ps, n, rd, wr):
        P.ts("dve", dst, src_ps, 1.0 / n, EPS, ALU.mult, ALU.add, reads=rd, writes=wr)
        P.act(dst, dst, AF.Ln, reads=wr, writes=wr)
        P.act(dst, dst, AF.Exp, reads=wr, writes=wr, scale=-0.5)

    psi = [0]

    def ps_next(k=7):
        psi[0] = (psi[0] + 1) % k
        return ps[psi[0]]

    for li, l in enumerate(layers):
        need_ctx = need_ctx_flags[li]
        last = (li == nl - 1)
        qtiles = TOK_TILES_OWN + ([CTX_TILE] if need_ctx else [])
        P.barrier()
        A.off = PERS_END
        sm = A.alloc([128, SM_L], F32, "sm")
        modT = A.alloc([128, 48, 2], F32, "modT")
        A1 = A.alloc([128, 8, 2], F32, "A1")
        A2 = A.alloc([128, 8, 2], F32, "A2")
        silc = A.alloc([128, 16], F32, "silc")
        lg = A.alloc([128, 8], F32, "lg")
        sinkx = A.alloc([128, 16], F32, "sinkx")
        P.dma("sp", sm.ap, sm_d[li], writes=[sm.t])
        P.act(silc.ap, sg.ap[:, SG_C:SG_C + 16], AF.Silu, reads=[sg.t], writes=[silc.t])
        silc3 = silc.ap.rearrange("p (k j) -> p k j", j=2)
        P.act(lg.ap, sm.ap[:, SM_RET:SM_RET + 8], AF.Exp, reads=[sm.t], writes=[lg.t], scale=-1.0)
        P.ts("dve", lg.ap, lg.ap, 1.0, None, ALU.add, reads=[lg.t], writes=[lg.t])
        P.act(lg.ap, lg.ap, AF.Ln, reads=[lg.t], writes=[lg.t])
        P.ts("dve", lg.ap, lg.ap, -1.0, None, ALU.mult, reads=[lg.t], writes=[lg.t])
        P.act(sinkx.ap, sm.ap[:, SM_SINK:SM_SINK + 16], AF.Exp, reads=[sm.t], writes=[sinkx.t])
        PH0_END = A.off
        wst = [A.alloc([128, 8, 512], F32, f"wst{i}") for i in range(2)]
        for g in range(12):
            w = wst[g % 2]
            P.dma("sp", w.ap, wall_d[li][G_MOD + g], writes=[w.t])
            for j in range(4):
                idx = g * 4 + j
                pb = ps_next()
                for kc in range(8):
                    P.mm(pb.ap[:, 0:2], w.ap[:, kc, j * 128:(j + 1) * 128], silc3[:, kc, :], kc == 0, kc == 7,
                         reads=[w.t, silc.t], writes=[pb.t])
                P.ts("dve", modT.ap[:, idx, :], pb.ap[:, 0:2], sm.ap[:, SM_BMOD + idx:SM_BMOD + idx + 1], None,
                     ALU.add, reads=[pb.t, sm.t], writes=[modT.t])
        for j in range(2):
            P.stt(A1.ap[:, :, j], modT.ap[:, 8:16, j], 1.0, sm.ap[:, SM_G1:SM_G1 + 8], ALU.add, ALU.mult,
                  reads=[modT.t, sm.t], writes=[A1.t])
            P.stt(A2.ap[:, :, j], modT.ap[:, 32:40, j], 1.0, sm.ap[:, SM_G2:SM_G2 + 8], ALU.add, ALU.mult,
                  reads=[modT.t, sm.t], writes=[A2.t])

        def SH1(kc, j): return modT.ap[:, 0 + kc, j:j + 1]
        def GT1(kc, j): return modT.ap[:, 16 + kc, j:j + 1]
        def SH2(kc, j): return modT.ap[:, 24 + kc, j:j + 1]
        def GT2(kc, j): return modT.ap[:, 40 + kc, j:j + 1]

        P.barrier()
        A.off = PH0_END
        est = [A.alloc([128, 4096], F32, f"est{i}") for i in range(3)]
        ebf = [A.alloc([128, 4096], BF16, f"ebf{i}") for i in range(3)]
        ci = 0
        for e in range(16):
            for k in range(3):
                s_, b_ = est[ci % 3], ebf[ci % 3]
                P.dma("sp", s_.ap, we_d[li][e, k], writes=[s_.t])
                eng = ("pool", "dve", "act")[ci % 3]
                P.copy(eng, b_.ap, s_.ap, reads=[s_.t], writes=[b_.t])
                P.dma("pool", web_d[e, k], b_.ap, reads=[b_.t], writes=[tweb(e, k)])
                ci += 1
        P.barrier()

        A.off = PH0_END
        hT = A.alloc([128, 8, NALL], BF16, "hT")
        hts = {t0: T(f"hT{t0}") for (t0, _, _) in TOK_TILES_ALL}
        xt = [A.alloc([128, 8, 512], F32, f"xt{i}") for i in range(2)]
        sq = A.alloc([128, 8, 512], BF16, "sq")
        rstd = A.alloc([128, 512], F32, "rstd")
        tmp = [A.alloc([128, 512], F32, f"tmp{i}") for i in range(2)]
        PH1_END = A.off

        def norm_tiles(tiles, Aco, SHf, out_fn, src3=xs3):
            for i, (t0, nt, mj) in enumerate(tiles):
                x_ = xt[i % 2]
                P.dma("sp", x_.ap[:, :, 0:nt], src3[:, :, t0:t0 + nt], reads=[txs(t0)], writes=[x_.t])
                P.act(sq.ap[:, :, 0:nt], x_.ap[:, :, 0:nt], AF.Square, reads=[x_.t], writes=[sq.t])
                pb = ps_next()
                for kc in range(8):
                    P.mm(pb.ap[:, 0:nt], ones_bf.ap, sq.ap[:, kc, 0:nt], kc == 0, kc == 7,
                         reads=[ones_bf.t, sq.t], writes=[pb.t])
                rsqrt_from_psum(rstd.ap[:, 0:nt], pb.ap[:, 0:nt], 1024.0, [pb.t], [rstd.t])
                for kc in range(8):
                    tm_ = tmp[kc % 2]
                    P.tt("dve" if kc % 2 == 0 else "pool", tm_.ap[:, 0:nt], x_.ap[:, kc, 0:nt], rstd.ap[:, 0:nt],
                         ALU.mult, reads=[x_.t, rstd.t], writes=[tm_.t])
                    out_fn(kc, t0, nt, mj, tm_, Aco.ap[:, kc, mj:mj + 1], SHf(kc, mj))

        def out_h1(kc, t0, nt, mj, tm_, a_, b_):
            P.act(hT.ap[:, kc, t0:t0 + nt], tm_.ap[:, 0:nt], AF.Identity, reads=[tm_.t, A1.t, modT.t],
                  writes=[hts[t0]], bias=b_, scale=a_)

        norm_tiles(TOK_TILES_ALL, A1, SH1, out_h1)

        A.off = PH1_END
        wst = [A.alloc([128, 8, 512], F32, f"wst{i}") for i in range(2)]
        wbf = [A.alloc([128, 8, 512], BF16, f"wbf{i}") for i in range(2)]
        tabC = [A.alloc([128, 512], F32, f"tabC{i}") for i in range(2)]
        tabS = [A.alloc([128, 512], F32, f"tabS{i}") for i in range(2)]
        stage = [A.alloc([128, 512], BF16, f"stage{i}") for i in range(3)]
        t1b = [A.alloc([128, 512], F32, f"t1b{i}") for i in range(2)]
        t2b = [A.alloc([128, 512], F32, f"t2b{i}") for i in range(2)]
        sqb = A.alloc([128, 512], BF16, "sqb")
        rs2 = A.alloc([128, 512], F32, "rs2")
        cnt = [0]

        def load_group(gi):
            w, wb = wst[gi % 2], wbf[gi % 2]
            P.dma("sp", w.ap, wall_d[li][gi], writes=[w.t])
            P.copy("pool", wb.ap[:, 0:4, :], w.ap[:, 0:4, :], reads=[w.t], writes=[wb.t])
            P.copy("act", wb.ap[:, 4:8, :], w.ap[:, 4:8, :], reads=[w.t], writes=[wb.t])
            return wb

        for gi in range(N_FM_GROUPS):
            wb = load_group(G_FM + gi)
            for u in [x for x in FM_UNITS if x["group"] == gi]:
                tiles = TOK_TILES_ALL if u["tok"] == "all" else qtiles
                kind = u["kind"]
                dual = u["c1"] is not None
                tsel = 2 if kind == "rope64" else 0
                for (t0, nt, mj) in tiles:
                    k = cnt[0]
                    cnt[0] += 1
                    pa = ps_next()
                    for kc in range(8):
                        P.mm(pa.ap[:, 0:nt], wb.ap[:, kc, u["c0"]:u["c0"] + 128], hT.ap[:, kc, t0:t0 + nt],
                             kc == 0, kc == 7, reads=[wb.t, hts[t0]], writes=[pa.t])
                    if dual:
                        pbk = ps_next()
                        for kc in range(8):
                            P.mm(pbk.ap[:, 0:nt], wb.ap[:, kc, u["c1"]:u["c1"] + 128], hT.ap[:, kc, t0:t0 + nt],
                                 kc == 0, kc == 7, reads=[wb.t, hts[t0]], writes=[pbk.t])
                        tc_, ts_ = tabC[k % 2], tabS[k % 2]
                        P.dma("sp", tc_.ap[:, 0:nt], tabs[tsel, :, t0:t0 + nt], writes=[tc_.t])
                        P.dma("sp", ts_.ap[:, 0:nt], tabs[tsel + 1, :, t0:t0 + nt], writes=[ts_.t])
                    st = stage[k % 3]
                    o_ = st.ap[:, 0:nt]
                    if kind in ("rope128", "rope64"):
                        a_, b_ = t1b[k % 2], t2b[k % 2]
                        P.tt("dve", a_.ap[:, 0:nt], pa.ap[:, 0:nt], tc_.ap[:, 0:nt], ALU.mult,
                             reads=[pa.t, tc_.t], writes=[a_.t])
                        P.tt("dve", b_.ap[:, 0:nt], pbk.ap[:, 0:nt], ts_.ap[:, 0:nt], ALU.mult,
                             reads=[pbk.t, ts_.t], writes=[b_.t])
                        P.tt("pool", o_, a_.ap[:, 0:nt], b_.ap[:, 0:nt], ALU.add, reads=[a_.t, b_.t], writes=[st.t])
                    elif kind in ("normq", "normk"):
                        gco = SM_GQ if kind == "normq" else SM_GK
                        a_, b_ = t1b[k % 2], t2b[k % 2]
                        P.act(sqb.ap[:, 0:nt], pa.ap[:, 0:nt], AF.Square, reads=[pa.t], writes=[sqb.t])
                        pc = ps_next()
                        P.mm(pc.ap[:, 0:nt], ones_bf.ap, sqb.ap[:, 0:nt], True, True, reads=[ones_bf.t, sqb.t],
                             writes=[pc.t])
                        rsqrt_from_psum(rs2.ap[:, 0:nt], pc.ap[:, 0:nt], 128.0, [pc.t], [rs2.t])
                        P.stt(a_.ap[:, 0:nt], pa.ap[:, 0:nt], sm.ap[:, gco:gco + 1], tc_.ap[:, 0:nt], ALU.mult,
                              ALU.mult, reads=[pa.t, tc_.t, sm.t], writes=[a_.t])
                        P.stt(b_.ap[:, 0:nt], pbk.ap[:, 0:nt], sm.ap[:, gco + 1:gco + 2], ts_.ap[:, 0:nt], ALU.mult,
                              ALU.mult, reads=[pbk.t, ts_.t, sm.t], writes=[b_.t])
                        P.tt("pool", a_.ap[:, 0:nt], a_.ap[:, 0:nt], b_.ap[:, 0:nt], ALU.add, reads=[a_.t, b_.t],
                             writes=[a_.t])
                        P.tt("pool", o_, a_.ap[:, 0:nt], rs2.ap[:, 0:nt], ALU.mult, reads=[a_.t, rs2.t],
                             writes=[st.t])
                    elif kind == "silu":
                        P.act(o_, pa.ap[:, 0:nt], AF.Silu, reads=[pa.t], writes=[st.t])
                    else:
                        P.act(o_, pa.ap[:, 0:nt], AF.Sigmoid, reads=[pa.t], writes=[st.t])
                    P.dma("pool", fm_d[u["name"]][:, t0:t0 + nt], o_, reads=[st.t], writes=[tfm(u["name"], t0)])
        for gi in range(N_TM_GROUPS):
            wb = load_group(G_TM + gi)
            for sub in range(NALL // 128):
                t0 = sub * 128
                tile0 = (t0 // 512) * 512
                k = cnt[0]
                cnt[0] += 1
                pa = ps_next()
                for kc in range(8):
                    P.mm(pa.ap, hT.ap[:, kc, t0:t0 + 128], wb.ap[:, kc, :], kc == 0, kc == 7,
                         reads=[wb.t, hts[tile0]], writes=[pa.t])
                st = stage[k % 3]
                P.copy("act" if k % 2 == 0 else "dve", st.ap, pa.ap, reads=[pa.t], writes=[st.t])
                P.dma("pool", vall_d[t0:t0 + 128, gi * 512:(gi + 1) * 512], st.ap, reads=[st.t],
                      writes=[tvall(sub)])
        P.barrier()
        if stop <= 2:
            continue
        A.off = PH0_END
        KSCALE = 128.0 ** -0.5
        KT = A.alloc([128, NALL], BF16, "KT")
        QT = A.alloc([128, NALL], BF16, "QT")
        Vr = A.alloc([128, 34, 256], BF16, "Vr")
        Ur = A.alloc([128, 2, NALL], BF16, "Ur")
        Kf = A.alloc([128, 34, 128], BF16, "Kf")
        Kb = A.alloc([128, 34, 128], BF16, "Kb")
        SFb = A.alloc([128, 18, 256], BF16, "SFb")
        SBb = A.alloc([128, 18, 256], BF16, "SBb")
        Dm = A.alloc([128, 512], BF16, "Dm")
        qdf = A.alloc([128, 512], F32, "qdf")
        qdb = A.alloc([128, 512], F32, "qdb")
        e1 = A.alloc([128, 128], F32, "e1")
        e2 = A.alloc([128, 128], F32, "e2")
        kd = A.alloc([128, 4], F32, "kd")
        S = A.alloc([128, 256], F32, "S")
        S0f = A.alloc([128, 256], F32, "S0f")
        S0b = A.alloc([128, 256], F32, "S0b")
        Pm = A.alloc([128, 512], BF16, "Pm")
        Qf = A.alloc([128, 512], BF16, "Qf")
        Qb = A.alloc([128, 512], BF16, "Qb")
        sq2 = A.alloc([128, 2, 512], BF16, "sq2")
        rs3 = A.alloc([128, 512], F32, "rs3")
        to_ = [A.alloc([128, 512], F32, f"to{i}") for i in range(2)]
        stg = [A.alloc([128, 512], BF16, f"stg{i}") for i in range(2)]
        flg = sg.ap[:, SG_FLAG:SG_FLAG + 2]
        for h in range(4):
            lgf, lgb = lg.ap[:, h:h + 1], lg.ap[:, 4 + h:5 + h]
            P.act(e1.ap, cst.ap[:, C_DPOS:C_DPOS + 128], AF.Exp, reads=[cst.t, lg.t], writes=[e1.t], scale=lgf)
            P.tt("dve", e1.ap, e1.ap, cst.ap[:, C_MF:C_MF + 128], ALU.mult, reads=[e1.t, cst.t], writes=[e1.t])
            P.act(e2.ap, cst.ap[:, C_DNEG:C_DNEG + 128], AF.Exp, reads=[cst.t, lg.t], writes=[e2.t], scale=lgb)
            P.tt("dve", e2.ap, e2.ap, cst.ap[:, C_MB:C_MB + 128], ALU.mult, reads=[e2.t, cst.t], writes=[e2.t])
            P.tt("dve", e1.ap, e1.ap, e2.ap, ALU.add, reads=[e1.t, e2.t], writes=[e1.t])
            for r in range(4):
                P.ts("dve", Dm.ap[:, r * 128:(r + 1) * 128], e1.ap, KSCALE, None, ALU.mult, reads=[e1.t],
                     writes=[Dm.t])
                P.act(qdf.ap[:, r * 128:(r + 1) * 128], cst.ap[:, C_IP1:C_IP1 + 128], AF.Exp, reads=[cst.t, lg.t],
                      writes=[qdf.t], scale=lgf)
                P.act(qdb.ap[:, r * 128:(r + 1) * 128], cst.ap[:, C_IB:C_IB + 128], AF.Exp, reads=[cst.t, lg.t],
                      writes=[qdb.t], scale=lgb)
            P.act(kd.ap[:, 0:1], cst.ap[:, C_PCF:C_PCF + 1], AF.Exp, reads=[cst.t, lg.t], writes=[kd.t], scale=lgf)
            P.act(kd.ap[:, 1:2], cst.ap[:, C_PCB:C_PCB + 1], AF.Exp, reads=[cst.t, lg.t], writes=[kd.t], scale=lgb)
            P.ts("dve", kd.ap[:, 0:2], kd.ap[:, 0:2], KSCALE, None, ALU.mult, reads=[kd.t], writes=[kd.t])
            P.act(kd.ap[:, 2:3], lgf, AF.Exp, reads=[lg.t], writes=[kd.t], scale=128.0)
            P.act(kd.ap[:, 3:4], lgb, AF.Exp, reads=[lg.t], writes=[kd.t], scale=128.0)
            P.dma("sp", KT.ap, fm_d[f"kr{h}"], writes=[KT.t])
            P.dma("sp", QT.ap[:, 0:NOWN], fm_d[f"qr{h}"][:, 0:NOWN], writes=[QT.t])
            if need_ctx:
                P.dma("sp", QT.ap[:, 4096:NALL], fm_d[f"qr{h}"][:, 4096:NALL], writes=[QT.t])
            vsrc = vall_d[:, h * 256:(h + 1) * 256].rearrange("(t p) c -> p t c", p=128)
            for tq in range(0, 34, 4):
                P.dma("sp", Vr.ap[:, tq:min(tq + 4, 34), :], vsrc[:, tq:min(tq + 4, 34), :], writes=[Vr.t])
            for j in range(2):
                P.dma("sp", Ur.ap[:, j, 0:NOWN], fm_d[f"ur{2 * h + j}"][:, 0:NOWN], writes=[Ur.t])
                if need_ctx:
                    P.dma("sp", Ur.ap[:, j, 4096:NALL], fm_d[f"ur{2 * h + j}"][:, 4096:NALL], writes=[Ur.t])
            for c0 in (range(0, 32 if 'trlast' in SKIP else 34, 4) if 'tr' not in SKIP else []):
                n = min(4, 34 - c0)
                pb_ = ps_next()
                for c in range(n):
                    P.mm(pb_.ap[:, c * 128:(c + 1) * 128], KT.ap[:, (c0 + c) * 128:(c0 + c + 1) * 128], id_bf.ap,
                         True, True, reads=[KT.t, id_bf.t], writes=[pb_.t])
                P.ts("dve", Kf.ap[:, c0:c0 + n, :], pb_.ap[:, 0:n * 128].rearrange("p (a b) -> p a b", a=n),
                     kd.ap[:, 0:1], None, ALU.mult, reads=[pb_.t, kd.t], writes=[Kf.t])
                P.act(Kb.ap[:, c0:c0 + n, :], pb_.ap[:, 0:n * 128].rearrange("p (a b) -> p a b", a=n), AF.Identity,
                      reads=[pb_.t, kd.t], writes=[Kb.t], scale=kd.ap[:, 1:2])

            def upd(Kx, c, cdcol, first):
                if 'upd' in SKIP:
                    return
                pb_ = ps_next()
                P.mm(pb_.ap[:, 0:256], Kx.ap[:, c, :], Vr.ap[:, c, :], True, True, reads=[Kx.t, Vr.t],
                     writes=[pb_.t])
                if first:
                    P.copy("dve", S.ap, pb_.ap[:, 0:256], reads=[pb_.t], writes=[S.t])
                else:
                    P.stt(S.ap, S.ap, kd.ap[:, cdcol:cdcol + 1], pb_.ap[:, 0:256], ALU.mult, ALU.add,
                          reads=[S.t, kd.t, pb_.t], writes=[S.t])

            def snap(dst, idx):
                if 'upd' in SKIP:
                    return
                P.copy("act", dst.ap[:, idx, :], S.ap, reads=[S.t], writes=[dst.t])

            upd(Kf, 32, 2, True)
            snap(SFb, 17)
            upd(Kf, 33, 2, False)
            P.copy("dve", S0f.ap, S.ap, reads=[S.t], writes=[S0f.t])
            for c in range(16, 32):
                upd(Kf, c, 2, False)
            P.tt("dve", S.ap, S.ap, S0f.ap, ALU.subtract, reads=[S.t, S0f.t], writes=[S.t])
            P.stt(S.ap, S.ap, flg[:, 1:2], S0f.ap, ALU.mult, ALU.add, reads=[S.t, S0f.t, sg.t], writes=[S.t])
            for c in range(0, 16):
                snap(SFb, c)
                if c < 15:
                    upd(Kf, c, 2, False)
            upd(Kb, 33, 3, True)
            snap(SBb, 16)
            upd(Kb, 32, 3, False)
            P.copy("dve", S0b.ap, S.ap, reads=[S.t], writes=[S0b.t])
            for c in range(31, 15, -1):
                upd(Kb, c, 3, False)
            P.tt("dve", S.ap, S.ap, S0b.ap, ALU.subtract, reads=[S.t, S0b.t], writes=[S.t])
            P.stt(S.ap, S.ap, flg[:, 0:1], S0b.ap, ALU.mult, ALU.add, reads=[S.t, S0b.t, sg.t], writes=[S.t])
            for c in range(15, -1, -1):
                snap(SBb, c)
                if c > 0:
                    upd(Kb, c, 3, False)
            for (t0, nt, mj) in (qtiles if 'out' not in SKIP else []):
                ncx = nt // 128
                pS = ps_next()
                for j in range(ncx):
                    sl = slice(t0 + j * 128, t0 + (j + 1) * 128)
                    P.mm(pS.ap[:, j * 128:(j + 1) * 128], KT.ap[:, sl], QT.ap[:, sl], True, True,
                         reads=[KT.t, QT.t], writes=[pS.t])
                P.tt("dve", Pm.ap[:, 0:nt], pS.ap[:, 0:nt], Dm.ap[:, 0:nt], ALU.mult, reads=[pS.t, Dm.t],
                     writes=[Pm.t])
                P.tt("pool", Qf.ap[:, 0:nt], QT.ap[:, t0:t0 + nt], qdf.ap[:, 0:nt], ALU.mult, reads=[QT.t, qdf.t],
                     writes=[Qf.t])
                P.tt("pool", Qb.ap[:, 0:nt], QT.ap[:, t0:t0 + nt], qdb.ap[:, 0:nt], ALU.mult, reads=[QT.t, qdb.t],
                     writes=[Qb.t])
                pO = [ps_next(), ps_next()]
                for dj in range(2):
                    dsl = slice(dj * 128, (dj + 1) * 128)
                    for j in range(ncx):
                        c = t0 // 128 + j
                        csl = slice(j * 128, (j + 1) * 128)
                        if c < 16:
                            sf, sb = c, c
                        elif c == 32:
                            sf, sb = None, 16
                        else:
                            sf, sb = 17, None
                        terms = [(Vr.ap[:, c, dsl], Pm.ap[:, csl], [Vr.t, Pm.t])]
                        if sf is not None:
                            terms.append((SFb.ap[:, sf, dsl], Qf.ap[:, csl], [SFb.t, Qf.t]))
                        if sb is not None:
                            terms.append((SBb.ap[:, sb, dsl], Qb.ap[:, csl], [SBb.t, Qb.t]))
                        for ti, (l_, r_, rd) in enumerate(terms):
                            P.mm(pO[dj].ap[:, csl], l_, r_, ti == 0, ti == len(terms) - 1, reads=rd,
                                 writes=[pO[dj].t])
                    P.act(sq2.ap[:, dj, 0:nt], pO[dj].ap[:, 0:nt], AF.Square, reads=[pO[dj].t], writes=[sq2.t])
                pN = ps_next()
                for dj in range(2):
                    P.mm(pN.ap[:, 0:nt], ones_bf.ap, sq2.ap[:, dj, 0:nt], dj == 0, dj == 1,
                         reads=[ones_bf.t, sq2.t], writes=[pN.t])
                rsqrt_from_psum(rs3.ap[:, 0:nt], pN.ap[:, 0:nt], 256.0, [pN.t], [rs3.t])
                for dj in range(2):
                    P.tt("dve", to_[dj].ap[:, 0:nt], pO[dj].ap[:, 0:nt], rs3.ap[:, 0:nt], ALU.mult,
                         reads=[pO[dj].t, rs3.t], writes=[to_[dj].t])
                    P.tt("pool", stg[dj].ap[:, 0:nt], to_[dj].ap[:, 0:nt], Ur.ap[:, dj, t0:t0 + nt], ALU.mult,
                         reads=[to_[dj].t, Ur.t], writes=[stg[dj].t])
                    r0 = (2 * h + dj) * 128
                    P.dma("pool", obr_d[0][r0:r0 + 128, qpos(t0):qpos(t0) + nt], stg[dj].ap[:, 0:nt],
                          reads=[stg[dj].t], writes=[tobr(0, (h, dj, t0))])
        P.barrier()
        if stop <= 3:
            continue
        def run_attn(groups, Pt, depth=2):
            items = [(gi, ki) for gi, g in enumerate(groups) for ki in range(g["n"])]
            pts = {}
            sidx = [0]

            def SE(idx):
                gi, ki = items[idx]
                g = groups[gi]
                pS = ps[sidx[0] % 4]
                sidx[0] += 1
                g["S"](ki, pS)
                pt = Pt[idx % len(Pt)]
                g["E"](ki, pS, pt)
                pts[idx] = pt
            for idx in range(min(depth, len(items))):
                SE(idx)
            for idx in range(len(items)):
                if idx + depth < len(items):
                    SE(idx + depth)
                gi, ki = items[idx]
                g = groups[gi]
                pO, pD = (ps[4], ps[5]) if gi % 2 == 0 else (ps[6], ps[7])
                g["PV"](ki, pts.pop(idx), pO, pD, ki == 0, ki == g["n"] - 1)
                if ki == g["n"] - 1:
                    g["epi"](pO, pD)

        A.off = PH0_END
        KSa = A.alloc([128, NALL], BF16, "KSa")
        KSb = A.alloc([128, NALL], BF16, "KSb")
        QS = A.alloc([128, 4, NALL], BF16, "QS")
        Vs = A.alloc([128, 34, 64], BF16, "Vs")
        Pt = [A.alloc([128, 512], BF16, f"Pt{i}") for i in range(4)]
        sk = [A.alloc([64, 512], F32, f"sk{i}") for i in range(2)]
        den = [A.alloc([128, 512], F32, f"den{i}") for i in range(2)]
        ostg = [A.alloc([128, 512], BF16, f"ostg{i}") for i in range(2)]
        P.dma("sp", KSa.ap, fm_d["ks"], writes=[KSa.t])
        P.dma("sp", KSb.ap, fm_d["ks2"], writes=[KSb.t])
        obr1 = obr_d[1].rearrange("(c p) t -> p c t", p=128)
        gcount = 0
        for g in range(2):
            for i in range(4):
                P.dma("sp", QS.ap[:, i, 0:NOWN], fm_d[f"qs{g * 4 + i}"][:, 0:NOWN], writes=[QS.t])
                if need_ctx:
                    P.dma("sp", QS.ap[:, i, 4096:NALL], fm_d[f"qs{g * 4 + i}"][:, 4096:NALL], writes=[QS.t])
            vsrc = vall_d[:, 1024 + g * 64:1024 + (g + 1) * 64].rearrange("(t p) c -> p t c", p=128)
            for tq in range(0, 34, 4):
                P.dma("sp", Vs.ap[:, tq:min(tq + 4, 34), :], vsrc[:, tq:min(tq + 4, 34), :], writes=[Vs.t])
            groups = []
            for par in range(2):
                pb0 = par * 64
                Ksrc = KSa if g == par else KSb
                sk_ = sk[par]
                for i in range(4):
                    hq = g * 8 + par + 2 * i
                    P.act(sk_.ap[:, i * 128:(i + 1) * 128], cst.ap[0:64, C_DPOS:C_DPOS + 128], AF.Identity,
                          reads=[cst.t, sinkx.t], writes=[sk_.t], bias=sinkx.ap[0:64, hq:hq + 1], scale=0.0)
                blocks = [(jb * 128, jb) for jb in range(16)] + ([(4096, 16), (4224, 17)] if need_ctx else [])
                for (t0, jb) in blocks:
                    if jb < 16:
                        kts = [((jb - 1) * 128, 0) if jb > 0 else (2048 + 15 * 128, 2), (jb * 128, None),
                               ((jb + 1) * 128, 1) if jb < 15 else (2048, 3), (4096, None), (4224, None)]
                    else:
                        kts = [(4096, None), (4224, None)]

                    def S(ki, pS, kts=kts, pb0=pb0, Ksrc=Ksrc, t0=t0):
                        k0 = kts[ki][0]
                        P.mm(pS.ap.rearrange("p (a b) -> p a b", a=4), Ksrc.ap[pb0:pb0 + 64, k0:k0 + 128],
                             QS.ap[pb0:pb0 + 64, :, t0:t0 + 128], True, True, reads=[Ksrc.t, QS.t], writes=[pS.t])

                    def E(ki, pS, pt, kts=kts):
                        P.act(pt.ap, pS.ap, AF.Exp, reads=[pS.t], writes=[pt.t], scale=0.125)
                        mi = kts[ki][1]
                        if mi is not None:
                            P.tt("pool", pt.ap, pt.ap, mk_bf.ap[:, mi, :], ALU.mult, reads=[pt.t, mk_bf.t],
                                 writes=[pt.t])

                    def PV(ki, pt, pO, pD, first, lastk, kts=kts):
                        k0 = kts[ki][0]
                        P.mm(pO.ap[0:64, :], Vs.ap[:, k0 // 128, :], pt.ap, first, lastk, reads=[Vs.t, pt.t],
                             writes=[pO.t])
                        P.mm(pD.ap[0:64, :], ones_bf.ap[:, 0:64], pt.ap, first, lastk, reads=[ones_bf.t, pt.t],
                             writes=[pD.t])

                    def epi(pO, pD, t0=t0, pb0=pb0, g=g, sk_=sk_, gc=gcount):
                        dn_, os_ = den[gc % 2], ostg[gc % 2]
                        P.tt("dve", dn_.ap[0:64, :], pD.ap[0:64, :], sk_.ap, ALU.add, reads=[pD.t, sk_.t],
                             writes=[dn_.t])
                        P.act(dn_.ap[0:64, :], dn_.ap[0:64, :], AF.Ln, reads=[dn_.t], writes=[dn_.t])
                        P.act(dn_.ap[0:64, :], dn_.ap[0:64, :], AF.Exp, reads=[dn_.t], writes=[dn_.t], scale=-1.0)
                        P.tt("dve", os_.ap[0:64, :], pO.ap[0:64, :], dn_.ap[0:64, :], ALU.mult,
                             reads=[pO.t, dn_.t], writes=[os_.t])
                        q0 = qpos(t0)
                        P.dma("pool", obr1[pb0:pb0 + 64, g * 4:g * 4 + 4, q0:q0 + 128],
                              os_.ap[0:64, :].rearrange("p (a b) -> p a b", a=4), reads=[os_.t],
                              writes=[tobr(1, (g, pb0, t0))])
                    groups.append(dict(n=len(kts), S=S, E=E, PV=PV, epi=epi))
                    gcount += 1
            run_attn(groups, Pt)
        P.barrier()
        if stop <= 4:
            continue
        A.off = PH0_END
        KA = A.alloc([128, NALL], BF16, "KA")
        VA = A.alloc([128, 34, 128], BF16, "VA")
        QA = [A.alloc([128, NALL], BF16, f"QA{i}") for i in range(4)]
        Pt = [A.alloc([128, 512], BF16, f"Pt{i}") for i in range(4)]
        den = [A.alloc([128, 512], F32, f"den{i}") for i in range(2)]
        ostg = [A.alloc([128, 512], BF16, f"ostg{i}") for i in range(2)]
        GSCALE = 128.0 ** -0.5
        gcount = 0
        for g in range(2):
            P.dma("sp", KA.ap, fm_d[f"ka{g}"], writes=[KA.t])
            vsrc = vall_d[:, 1152 + g * 128:1152 + (g + 1) * 128].rearrange("(t p) c -> p t c", p=128)
            for tq in range(0, 34, 4):
                P.dma("sp", VA.ap[:, tq:min(tq + 4, 34), :], vsrc[:, tq:min(tq + 4, 34), :], writes=[VA.t])
            groups = []
            for hh in range(4):
                h = g * 4 + hh
                Q_ = QA[hh]
                P.dma("sp", Q_.ap[:, 0:NOWN], fm_d[f"qa{h}"][:, 0:NOWN], writes=[Q_.t])
                if need_ctx:
                    P.dma("sp", Q_.ap[:, 4096:NALL], fm_d[f"qa{h}"][:, 4096:NALL], writes=[Q_.t])
                for (t0, nt, mj) in qtiles:
                    kts = list(range(34)) if t0 < 4096 else [32, 33]

                    def S(ki, pS, kts=kts, Q_=Q_, t0=t0, nt=nt):
                        kt = kts[ki]
                        P.mm(pS.ap[:, 0:nt], KA.ap[:, kt * 128:(kt + 1) * 128], Q_.ap[:, t0:t0 + nt], True, True,
                             reads=[KA.t, Q_.t], writes=[pS.t])

                    def E(ki, pS, pt, nt=nt):
                        P.act(pt.ap[:, 0:nt], pS.ap[:, 0:nt], AF.Exp, reads=[pS.t], writes=[pt.t], scale=GSCALE)

                    def PV(ki, pt, pO, pD, first, lastk, kts=kts, nt=nt):
                        kt = kts[ki]
                        P.mm(pO.ap[:, 0:nt], VA.ap[:, kt, :], pt.ap[:, 0:nt], first, lastk, reads=[VA.t, pt.t],
                             writes=[pO.t])
                        P.mm(pD.ap[:, 0:nt], ones_bf.ap, pt.ap[:, 0:nt], first, lastk, reads=[ones_bf.t, pt.t],
                             writes=[pD.t])

                    def epi(pO, pD, t0=t0, nt=nt, h=h, gc=gcount):
                        dn_, os_ = den[gc % 2], ostg[gc % 2]
                        P.act(dn_.ap[:, 0:nt], pD.ap[:, 0:nt], AF.Ln, reads=[pD.t], writes=[dn_.t])
                        P.act(dn_.ap[:, 0:nt], dn_.ap[:, 0:nt], AF.Exp, reads=[dn_.t], writes=[dn_.t], scale=-1.0)
                        P.tt("dve", os_.ap[:, 0:nt], pO.ap[:, 0:nt], dn_.ap[:, 0:nt], ALU.mult,
                             reads=[pO.t, dn_.t], writes=[os_.t])
                        q0 = qpos(t0)
                        P.dma("pool", obr_d[2][h * 128:(h + 1) * 128, q0:q0 + nt], os_.ap[:, 0:nt], reads=[os_.t],
                              writes=[tobr(2, (h, t0))])
                    groups.append(dict(n=len(kts), S=S, E=E, PV=PV, epi=epi))
                    gcount += 1
            run_attn(groups, Pt)
        P.barrier()
        if stop <= 5:
            continue
        A.off = PH0_END
        wm = [A.alloc([128, 8, 1024], BF16, f"wm{i}") for i in range(4)]
        wst = [A.alloc([128, 8, 512], F32, f"wst{i}") for i in range(2)]
        ob = [A.alloc([128, 8, 512], BF16, f"ob{i}") for i in range(3)]
        gb = [A.alloc([128, 8, 512], BF16, f"gb{i}") for i in range(3)]
        ypre = A.alloc([128, 8, 512], BF16, "ypre")
        xtl = A.alloc([128, 8, 512], F32, "xtl")
        ya = [A.alloc([128, 512], F32, f"ya{i}") for i in range(2)]
        tb = [A.alloc([128, 512], F32, f"tb{i}") for i in range(2)]
        for gi in range(8):
            w = wst[gi % 2]
            P.dma("sp", w.ap, wall_d[li][G_MERGE + gi], writes=[w.t])
            dst = wm[gi // 2].ap[:, :, (gi % 2) * 512:(gi % 2 + 1) * 512]
            P.copy("pool", dst[:, 0:4, :], w.ap[:, 0:4, :], reads=[w.t], writes=[wm[gi // 2].t])
            P.copy("act", dst[:, 4:8, :], w.ap[:, 4:8, :], reads=[w.t], writes=[wm[gi // 2].t])
        gnames = ("ar", "as", "aa")
        for (t0, nt, mj) in qtiles:
            q0 = qpos(t0)
            for b_ in range(3):
                P.dma("sp", ob[b_].ap[:, :, 0:nt],
                      obr_d[b_].rearrange("(kc p) t -> p kc t", p=128)[:, :, q0:q0 + nt], writes=[ob[b_].t])
                for c in range(8):
                    P.dma("sp", gb[b_].ap[:, c, 0:nt], fm_d[f"{gnames[b_]}{c}"][:, t0:t0 + nt], writes=[gb[b_].t])
            P.dma("sp", xtl.ap[:, :, 0:nt], xs3[:, :, t0:t0 + nt], reads=[txs(t0)], writes=[xtl.t])
            for dc in range(8):
                dsl = slice(dc * 128, (dc + 1) * 128)
                y_ = ya[dc % 2]
                for b_ in range(3):
                    pa = ps_next()
                    for kc in range(8):
                        P.mm(pa.ap[:, 0:nt], wm[b_].ap[:, kc, dsl], ob[b_].ap[:, kc, 0:nt], kc == 0, kc == 7,
                             reads=[wm[b_].t, ob[b_].t], writes=[pa.t])
                    if b_ == 0:
                        P.tt("dve", y_.ap[:, 0:nt], pa.ap[:, 0:nt], gb[0].ap[:, dc, 0:nt], ALU.mult,
                             reads=[pa.t, gb[0].t], writes=[y_.t])
                    else:
                        t_ = tb[b_ % 2]
                        P.tt("dve", t_.ap[:, 0:nt], pa.ap[:, 0:nt], gb[b_].ap[:, dc, 0:nt], ALU.mult,
                             reads=[pa.t, gb[b_].t], writes=[t_.t])
                        if b_ == 1:
                            P.tt("pool", y_.ap[:, 0:nt], y_.ap[:, 0:nt], t_.ap[:, 0:nt], ALU.add,
                                 reads=[y_.t, t_.t], writes=[y_.t])
                        else:
                            P.tt("pool", ypre.ap[:, dc, 0:nt], y_.ap[:, 0:nt], t_.ap[:, 0:nt], ALU.add,
                                 reads=[y_.t, t_.t], writes=[ypre.t])
            for dc in range(8):
                dsl = slice(dc * 128, (dc + 1) * 128)
                py = ps_next()
                for kc in range(8):
                    P.mm(py.ap[:, 0:nt], wm[3].ap[:, kc, dsl], ypre.ap[:, kc, 0:nt], kc == 0, kc == 7,
                         reads=[wm[3].t, ypre.t], writes=[py.t])
                P.stt(xtl.ap[:, dc, 0:nt], py.ap[:, 0:nt], GT1(dc, mj), xtl.ap[:, dc, 0:nt], ALU.mult, ALU.add,
                      reads=[py.t, modT.t, xtl.t], writes=[xtl.t])
            P.dma("pool", xs3[:, :, t0:t0 + nt], xtl.ap[:, :, 0:nt], reads=[xtl.t], writes=[txs(t0)])
        P.barrier()
        if stop <= 6:
            continue
        A.off = PH0_END
        h2T = A.alloc([128, 8, NQ], BF16, "h2T")
        xres = A.alloc([128, 8, NQ], F32, "xres")
        WT = A.alloc([16, NQ], F32, "WT")
        M0 = A.off
        h2f = A.alloc([128, 8, 512], F32, "h2f")
        sq = A.alloc([128, 8, 512], BF16, "sq")
        rstd = A.alloc([128, 512], F32, "rstd")
        tmp = [A.alloc([128, 512], F32, f"tmp{i}") for i in range(2)]
        rt = {nm: A.alloc([128, 16], F32, "rt_" + nm) for nm in ("s", "bz", "eq", "msk", "ch", "ws", "wts")}
        rs = {nm: A.alloc([128, 4], F32, "rs_" + nm) for nm in ("m1", "m2", "gs", "gsel", "gm", "dn")}

        def v3(b_):
            return b_.ap.rearrange("p (g k) -> p g k", k=4)

        def bc(b_):
            return b_.ap[:, 0:4].unsqueeze(2).to_broadcast([128, 4, 4])
        mtiles = qtiles
        for i, (t0, nt, mj) in enumerate(mtiles):
            q0 = qpos(t0)
            P.dma("sp", xres.ap[:, :, q0:q0 + nt], xs3[:, :, t0:t0 + nt], reads=[txs(t0)], writes=[xres.t])
            P.act(sq.ap[:, :, 0:nt], xres.ap[:, :, q0:q0 + nt], AF.Square, reads=[xres.t], writes=[sq.t])
            pb = ps_next()
            for kc in range(8):
                P.mm(pb.ap[:, 0:nt], ones_bf.ap, sq.ap[:, kc, 0:nt], kc == 0, kc == 7, reads=[ones_bf.t, sq.t],
                     writes=[pb.t])
            rsqrt_from_psum(rstd.ap[:, 0:nt], pb.ap[:, 0:nt], 1024.0, [pb.t], [rstd.t])
            for kc in range(8):
                tm_ = tmp[kc % 2]
                P.tt("dve", tm_.ap[:, 0:nt], xres.ap[:, kc, q0:q0 + nt], rstd.ap[:, 0:nt], ALU.mult,
                     reads=[xres.t, rstd.t], writes=[tm_.t])
                P.act(h2f.ap[:, kc, 0:nt], tm_.ap[:, 0:nt], AF.Identity, reads=[tm_.t, A2.t, modT.t], writes=[h2f.t],
                      bias=SH2(kc, mj), scale=A2.ap[:, kc, mj:mj + 1])
                P.copy("pool", h2T.ap[:, kc, q0:q0 + nt], h2f.ap[:, kc, 0:nt], reads=[h2f.t], writes=[h2T.t])
            for sub in range(nt // 128):
                ssl = slice(sub * 128, (sub + 1) * 128)
                pr = ps_next()
                for kc in range(8):
                    P.mm(pr.ap[:, 0:16], h2f.ap[:, kc, ssl], sg.ap[:, SG_WR + kc * 16:SG_WR + (kc + 1) * 16],
                         kc == 0, kc == 7, reads=[h2f.t, sg.t], writes=[pr.t])
                s_, bz, eq, msk, ch, ws, wts = [rt[n] for n in ("s", "bz", "eq", "msk", "ch", "ws", "wts")]
                m1, m2, gs, gsel, gm, dn = [rs[n] for n in ("m1", "m2", "gs", "gsel", "gm", "dn")]
                P.act(s_.ap, pr.ap[:, 0:16], AF.Sigmoid, reads=[pr.t], writes=[s_.t])
                P.tt("dve", bz.ap, s_.ap, sg.ap[:, SG_BR:SG_BR + 16], ALU.add, reads=[s_.t, sg.t], writes=[bz.t])
                P.add("dve", lambda e, o=m1.ap, i_=v3(bz): e.tensor_reduce(out=o, in_=i_, axis=AX.X, op=ALU.max),
                      reads=[bz.t], writes=[m1.t])
                P.tt("dve", v3(eq), v3(bz), bc(m1), ALU.is_equal, reads=[bz.t, m1.t], writes=[eq.t])
                P.stt(msk.ap, eq.ap, -1e9, bz.ap, ALU.mult, ALU.add, reads=[eq.t, bz.t], writes=[msk.t])
                P.add("dve", lambda e, o=m2.ap, i_=v3(msk): e.tensor_reduce(out=o, in_=i_, axis=AX.X, op=ALU.max),
                      reads=[msk.t], writes=[m2.t])
                P.tt("dve", gs.ap, m1.ap, m2.ap, ALU.add, reads=[m1.t, m2.t], writes=[gs.t])
                P.add("dve", lambda e, o=gm.ap[:, 0:1], i_=gs.ap: e.tensor_reduce(out=o, in_=i_, axis=AX.X,
                                                                                 op=ALU.max),
                      reads=[gs.t], writes=[gm.t])
                P.ts("dve", gsel.ap, gs.ap, gm.ap[:, 0:1], None, ALU.is_equal, reads=[gs.t, gm.t], writes=[gsel.t])
                P.tt("dve", v3(ch), v3(bz), bc(m2), ALU.is_ge, reads=[bz.t, m2.t], writes=[ch.t])
                P.tt("dve", v3(ch), v3(ch), bc(gsel), ALU.mult, reads=[ch.t, gsel.t], writes=[ch.t])
                P.tt("dve", ws.ap, s_.ap, ch.ap, ALU.mult, reads=[s_.t, ch.t], writes=[ws.t])
                P.add("dve", lambda e, o=dn.ap[:, 0:1], i_=ws.ap: e.tensor_reduce(out=o, in_=i_, axis=AX.X,
                                                                                 op=ALU.add),
                      reads=[ws.t], writes=[dn.t])
                P.add("dve", lambda e, o=dn.ap[:, 1:2], i_=dn.ap[:, 0:1]: e.reciprocal(o, i_), reads=[dn.t],
                      writes=[dn.t])
                P.ts("dve", wts.ap, ws.ap, dn.ap[:, 1:2], None, ALU.mult, reads=[ws.t, dn.t], writes=[wts.t])
                pT = ps_next()
                P.transpose(pT.ap[0:16, 0:128], wts.ap, cst.ap[:, C_ID:C_ID + 128], reads=[wts.t, cst.t],
                            writes=[pT.t])
                P.copy("act", WT.ap[0:16, q0 + sub * 128:q0 + (sub + 1) * 128], pT.ap[0:16, 0:128], reads=[pT.t],
                       writes=[WT.t])
        P.barrier()
        A.off = M0
        ew = [[A.alloc([128, 4096], BF16, f"ew{i}_{k}") for k in range(3)] for i in range(2)]
        wbc = A.alloc([128, 512], F32, "wbc")
        sG = [A.alloc([128, 512], F32, f"sG{i}") for i in range(2)]
        tG = [A.alloc([128, 512], F32, f"tG{i}") for i in range(2)]
        hid = A.alloc([128, 4, 512], BF16, "hid")
        for e in range(16):
            wg_, wu_, wd_ = ew[e % 2]
            P.dma("sp", wg_.ap, web_d[e, 0], reads=[tweb(e, 0)], writes=[wg_.t])
            P.dma("sp", wu_.ap, web_d[e, 1], reads=[tweb(e, 1)], writes=[wu_.t])
            P.dma("sp", wd_.ap, web_d[e, 2], reads=[tweb(e, 2)], writes=[wd_.t])
            wg3 = wg_.ap.rearrange("p (a b) -> p a b", a=8)
            wu3 = wu_.ap.rearrange("p (a b) -> p a b", a=8)
            wd3 = wd_.ap.rearrange("p (a b) -> p a b", a=4)
            for (t0, nt, mj) in mtiles:
                q0 = qpos(t0)
                pw = ps_next()
                P.mm(pw.ap[:, 0:nt], cst.ap[0:16, C_SEL + e * 128:C_SEL + (e + 1) * 128], WT.ap[0:16, q0:q0 + nt],
                     True, True, reads=[cst.t, WT.t], writes=[pw.t])
                P.copy("act", wbc.ap[:, 0:nt], pw.ap[:, 0:nt], reads=[pw.t], writes=[wbc.t])
                for hc in range(4):
                    hsl = slice(hc * 128, (hc + 1) * 128)
                    pg, pu = ps_next(), ps_next()
                    for kc in range(8):
                        P.mm(pg.ap[:, 0:nt], wg3[:, kc, hsl], h2T.ap[:, kc, q0:q0 + nt], kc == 0, kc == 7,
                             reads=[wg_.t, h2T.t], writes=[pg.t])
                    for kc in range(8):
                        P.mm(pu.ap[:, 0:nt], wu3[:, kc, hsl], h2T.ap[:, kc, q0:q0 + nt], kc == 0, kc == 7,
                             reads=[wu_.t, h2T.t], writes=[pu.t])
                    sg_, tg_ = sG[hc % 2], tG[hc % 2]
                    P.act(sg_.ap[:, 0:nt], pg.ap[:, 0:nt], AF.Silu, reads=[pg.t], writes=[sg_.t])
                    P.tt("dve", tg_.ap[:, 0:nt], sg_.ap[:, 0:nt], pu.ap[:, 0:nt], ALU.mult, reads=[sg_.t, pu.t],
                         writes=[tg_.t])
                    P.tt("pool", hid.ap[:, hc, 0:nt], tg_.ap[:, 0:nt], wbc.ap[:, 0:nt], ALU.mult,
                         reads=[tg_.t, wbc.t], writes=[hid.t])
                for dc in range(8):
                    dsl = slice(dc * 128, (dc + 1) * 128)
                    py = ps_next()
                    for hc in range(4):
                        P.mm(py.ap[:, 0:nt], wd3[:, hc, dsl], hid.ap[:, hc, 0:nt], hc == 0, hc == 3,
                             reads=[wd_.t, hid.t], writes=[py.t])
                    P.stt(xres.ap[:, dc, q0:q0 + nt], py.ap[:, 0:nt], GT2(dc, mj), xres.ap[:, dc, q0:q0 + nt],
                          ALU.mult, ALU.add, reads=[py.t, modT.t, xres.t], writes=[xres.t])
        if not (last and final):
            for (t0, nt, mj) in mtiles:
                q0 = qpos(t0)
                P.dma("pool", xs3[:, :, t0:t0 + nt], xres.ap[:, :, q0:q0 + nt], reads=[xres.t], writes=[txs(t0)])
        if not last:
            for i, (t0, nt, mj) in enumerate(TOK_TILES_OWN):
                P.dma("pool", xch_src[i].rearrange("(kc p) t -> p kc t", p=128), xres.ap[:, :, t0:t0 + nt],
                      reads=[xres.t], writes=[T_xsrc[i]])
            P.barrier()
            for i in range(4):
                P.add("pool", lambda e, i=i: e.collective_compute("AllGather", ALU.bypass,
                                                                  replica_groups=[[0, 1], [2, 3], [4, 5], [6, 7]],
                                                                  ins=[xch_src[i]], outs=[xch_dst[i]]),
                      reads=[T_xsrc[i]], writes=[T_xdst[i]], cc=True)
            P.barrier()
            A.off = M0
            xa = [A.alloc([128, 8, 512], F32, f"xa{i}") for i in range(2)]
            xb_ = [A.alloc([128, 8, 512], F32, f"xb{i}") for i in range(2)]
            for i, (t0, nt, mj) in enumerate(TOK_TILES_OWN):
                d3 = xch_dst[i].rearrange("(r kc p) t -> r p kc t", r=2, p=128)
                a_, b_ = xa[i % 2], xb_[i % 2]
                P.dma("sp", a_.ap, d3[0], reads=[T_xdst[i]], writes=[a_.t])
                P.dma("sp", b_.ap, d3[1], reads=[T_xdst[i]], writes=[b_.t])
                P.ts("dve", a_.ap, a_.ap, sg.ap[:, SG_FLAG + 1:SG_FLAG + 2], None, ALU.mult, reads=[a_.t, sg.t],
                     writes=[a_.t])
                P.stt(a_.ap, b_.ap, sg.ap[:, SG_FLAG:SG_FLAG + 1], a_.ap, ALU.mult, ALU.add,
                      reads=[a_.t, b_.t, sg.t], writes=[a_.t])
                P.dma("pool", xs3[:, :, NOWN + t0:NOWN + t0 + nt], a_.ap, reads=[a_.t], writes=[txs(NOWN + t0)])
        if last and not final:
            xout3 = xout.rearrange("(kc p) t -> p kc t", p=128)
            for (t0, nt, mj) in mtiles:
                q0 = qpos(t0)
                P.dma("pool", xout3[:, :, q0:q0 + nt], xres.ap[:, :, q0:q0 + nt], reads=[xres.t])
        if last and final:
            P.barrier()
            A.off = M0
            h2f = A.alloc([128, 8, 512], F32, "h2f")
            sq = A.alloc([128, 8, 512], BF16, "sq")
            rstd = A.alloc([128, 512], F32, "rstd")
            tmp = [A.alloc([128, 512], F32, f"tmp{i}") for i in range(2)]
            yout3 = yout.rearrange("(kc p) t -> p kc t", p=128)
            for (t0, nt, mj) in TOK_TILES_OWN:
                P.act(sq.ap[:, :, 0:nt], xres.ap[:, :, t0:t0 + nt], AF.Square, reads=[xres.t], writes=[sq.t])
                pb = ps_next()
                for kc in range(8):
                    P.mm(pb.ap[:, 0:nt], ones_bf.ap, sq.ap[:, kc, 0:nt], kc == 0, kc == 7, reads=[ones_bf.t, sq.t],
                         writes=[pb.t])
                rsqrt_from_psum(rstd.ap[:, 0:nt], pb.ap[:, 0:nt], 1024.0, [pb.t], [rstd.t])
                for kc in range(8):
                    tm_ = tmp[kc % 2]
                    P.tt("dve", tm_.ap[:, 0:nt], xres.ap[:, kc, t0:t0 + nt], rstd.ap[:, 0:nt], ALU.mult,
                         reads=[xres.t, rstd.t], writes=[tm_.t])
                    P.act(h2f.ap[:, kc, 0:nt], tm_.ap[:, 0:nt], AF.Identity, reads=[tm_.t, sg.t], writes=[h2f.t],
                          scale=sg.ap[:, SG_GF + kc:SG_GF + kc + 1])
                P.dma("pool", yout3[:, :, t0:t0 + nt], h2f.ap[:, :, 0:nt], reads=[h2f.t])
    P.emit()
    return nc


_PROGS = {}


def _prog(key, *args, **kw):
    if key not in _PROGS:
        _PROGS[key] = build(*args, **kw)
    return _PROGS[key]


def kernel(**inp):
    inp = {k: np.asarray(v) for k, v in inp.items()}
    x, ctx = inp["x"], inp["ctx"]
    B = x.shape[0]
    cores = [(b, h) for b in range(B) for h in range(2)]
    consts = [make_consts(h) for h in range(2)]
    tabs = [make_tabs(h) for h in range(2)]
    w0 = prep_layer_weights(inp, 0)
    w1 = prep_layer_weights(inp, 1)
    maps = []
    for (b, h) in cores:
        xo = x[b, h * NOWN:(h + 1) * NOWN].T
        xt = x[b, (1 - h) * NOWN:(2 - h) * NOWN].T
        xall = np.ascontiguousarray(np.concatenate([xo, xt, ctx[b].T], axis=1))
        maps.append(dict(xall=xall, tabs=tabs[h], consts=consts[h], sg=make_small_global(inp, b, h),
                         sm0=w0[2], wall0=w0[0], we0=w0[1], sm1=w1[2], wall1=w1[0], we1=w1[1]))
    nc = _prog("fused", [0, 1], [True, False], True)
    r = run_bass_kernel_spmd(nc, maps, core_ids=list(range(len(cores)))).results
    out = np.zeros((B, 2 * NOWN, D), np.float32)
    for i, (b, h) in enumerate(cores):
        out[b, h * NOWN:(h + 1) * NOWN] = np.asarray(r[i]["yout"]).T
    return out
```

```python
import numpy as np
import concourse.bass as bass
import concourse.mybir as mybir
from concourse.bass_utils import run_bass_kernel_spmd

F32 = mybir.dt.float32
BF16 = mybir.dt.bfloat16
AF = mybir.ActivationFunctionType
ALU = mybir.AluOpType
AX = mybir.AxisListType

NOWN, NOTH, NCTX = 2048, 2048, 256
NALL = NOWN + NOTH + NCTX
NQ = NOWN + NCTX
D = 1024
EPS = 1e-6
NSLOT = 24
SKIP = set()


class T:
    __slots__ = ("name", "w", "r", "psum")

    def __init__(self, name="", psum=False):
        self.name = name
        self.w = None
        self.r = []
        self.psum = psum


class Op:
    __slots__ = ("eng", "fn", "deps", "id", "is_dma", "slot", "val", "marked", "semval", "cc")

    def __init__(self, eng, fn, is_dma):
        self.eng = eng
        self.fn = fn
        self.deps = []
        self.is_dma = is_dma
        self.slot = None
        self.val = None
        self.marked = False
        self.semval = None
        self.cc = False


class Prog:
    ENGS = ("pe", "act", "dve", "pool", "sp")

    def __init__(self, nc):
        self.nc = nc
        self.ops = []
        self.streams = {e: [] for e in self.ENGS}
        self.ndma = {e: 0 for e in self.ENGS}
        self.dmas = {e: [] for e in self.ENGS}

    def add(self, eng, fn, reads=(), writes=(), dma=False, extra=(), cc=False):
        op = Op(eng, fn, dma or cc)
        op.cc = cc
        op.id = len(self.ops)
        deps = {}
        for t in reads:
            if t.w is not None:
                deps[t.w.id] = t.w
            if t.psum:
                for r in t.r:
                    if r.eng != eng:
                        deps[r.id] = r
        for t in writes:
            if t.w is not None:
                deps[t.w.id] = t.w
            for r in t.r:
                deps[r.id] = r
        for d in extra:
            deps[d.id] = d
        op.deps = list(deps.values())
        for t in reads:
            if not dma:
                t.r = [r for r in t.r if r.is_dma or r.eng != eng]
            t.r.append(op)
        for t in writes:
            t.w = op
            t.r = []
        if cc:
            self.ncc = getattr(self, "ncc", 0) + 1
            op.slot = ("cc", self.ncc - 1)
            op.val = 1
            self.dmas[eng].append(op)
        elif dma:
            i = self.ndma[eng]
            self.ndma[eng] += 1
            op.slot = i % NSLOT
            op.val = 16 * (i // NSLOT + 1)
            self.dmas[eng].append(op)
        self.ops.append(op)
        self.streams[eng].append(op)
        return op

    def barrier(self):
        lasts = []
        for e in self.ENGS:
            if self.streams[e]:
                lasts.append(self.streams[e][-1])
            lasts.extend(self.dmas[e][-NSLOT:])
        for e in self.ENGS:
            self.add(e, lambda eng: eng.nop(), extra=lasts)

    def dma(self, q, out, in_, reads=(), writes=()):
        return self.add(q, lambda e: e.dma_start(out=out, in_=in_), reads, writes, dma=True)

    def mm(self, out, lhsT, rhs, start, stop, reads=(), writes=()):
        return self.add("pe", lambda e: e.matmul(out, lhsT, rhs, start=start, stop=stop), reads, writes)

    def transpose(self, out, in_, ident, reads=(), writes=()):
        return self.add("pe", lambda e: e.transpose(out, in_, ident), reads, writes)

    def act(self, out, in_, func, reads=(), writes=(), bias=None, scale=None):
        kw = {}
        if bias is not None:
            kw["bias"] = bias
        if scale is not None:
            kw["scale"] = scale
        return self.add("act", lambda e: e.activation(out, in_, func, **kw), reads, writes)

    def tt(self, eng, out, in0, in1, op, reads=(), writes=()):
        return self.add(eng, lambda e: e.tensor_tensor(out, in0, in1, op), reads, writes)

    def ts(self, eng, out, in0, s1, s2, op0, op1=None, reads=(), writes=()):
        if op1 is None:
            return self.add(eng, lambda e: e.tensor_scalar(out=out, in0=in0, scalar1=s1, scalar2=None, op0=op0),
                            reads, writes)
        return self.add(eng, lambda e: e.tensor_scalar(out=out, in0=in0, scalar1=s1, scalar2=s2, op0=op0, op1=op1),
                        reads, writes)

    def stt(self, out, in0, scalar, in1, op0, op1, reads=(), writes=()):
        return self.add("dve", lambda e: e.scalar_tensor_tensor(out=out, in0=in0, scalar=scalar, in1=in1,
                                                                op0=op0, op1=op1), reads, writes)

    def copy(self, eng, out, in_, reads=(), writes=()):
        if eng == "act":
            return self.add("act", lambda e: e.copy(out, in_), reads, writes)
        return self.add(eng, lambda e: e.tensor_copy(out, in_), reads, writes)

    def emit(self):
        nc = self.nc
        for op in self.ops:
            for d in op.deps:
                if d.is_dma:
                    continue
                if d.eng == op.eng and not op.is_dma and d.eng == "pe":
                    continue
                d.marked = True
        cnt = {e: 0 for e in self.ENGS}
        for op in self.ops:
            if op.marked and not op.is_dma:
                cnt[op.eng] += 1
                op.semval = cnt[op.eng]
        sems = {e: nc.alloc_semaphore(f"s_{e}") for e in self.ENGS}
        dsems = {e: {i: nc.alloc_semaphore(f"d_{e}_{i}") for i in range(min(NSLOT, self.ndma[e]))}
                 for e in self.ENGS}
        for i in range(getattr(self, "ncc", 0)):
            dsems["pool"][("cc", i)] = nc.alloc_semaphore(f"cc_{i}")
        engobj = {"pe": nc.tensor, "act": nc.scalar, "dve": nc.vector, "pool": nc.gpsimd, "sp": nc.sync}
        with nc.Block() as block:
            def run(ename):
                eng = engobj[ename]
                waited = {}

                def wait(key, sem, val):
                    if waited.get(key, 0) >= val:
                        return
                    waited[key] = val
                    eng.wait_ge(sem, val)

                for op in self.streams[ename]:
                    for d in op.deps:
                        if d.is_dma:
                            wait(("d", d.eng, d.slot), dsems[d.eng][d.slot], d.val)
                        else:
                            if d.eng == ename and not op.is_dma and ename == "pe":
                                continue
                            wait(("c", d.eng), sems[d.eng], d.semval)
                    if op.cc:
                        ins = op.fn(eng)
                        ins.then_inc(dsems[ename][op.slot], 1)
                    elif op.is_dma:
                        if op.val > 16:
                            wait(("d", ename, op.slot), dsems[ename][op.slot], op.val - 16)
                        ins = op.fn(eng)
                        ins.then_inc(dsems[ename][op.slot], 16)
                    else:
                        ins = op.fn(eng)
                        if op.marked:
                            ins.then_inc(sems[ename], 1)
                n = self.ndma[ename]
                for i in range(max(0, n - NSLOT), n):
                    wait(("d", ename, i % NSLOT), dsems[ename][i % NSLOT], 16 * (i // NSLOT + 1))

            @block.tensor
            def _(e):
                run("pe")

            @block.scalar
            def _(e):
                run("act")

            @block.vector
            def _(e):
                run("dve")

            @block.gpsimd
            def _(e):
                run("pool")

            @block.sync
            def _(e):
                run("sp")


class Buf:
    __slots__ = ("ap", "t")

    def __init__(self, ap, name="", psum=False):
        self.ap = ap
        self.t = T(name, psum)


class Arena:
    def __init__(self, nc, nbytes):
        self.t = nc.alloc_sbuf_tensor("arena", [128, nbytes // 2], BF16)
        self.nbytes = nbytes
        self.off = 0

    def alloc(self, shape, dtype, name=""):
        n = 1
        for s in shape[1:]:
            n *= s
        es = 4 if dtype == F32 else 2
        nb = (n * es + 63) // 64 * 64
        assert self.off + nb <= self.nbytes, f"arena overflow {name} {self.off + nb}"
        v = self.t[0:shape[0], self.off // 2:(self.off + n * es) // 2]
        if dtype == F32:
            v = v.bitcast(F32)
        if len(shape) == 3:
            v = v.rearrange("p (a b) -> p a b", a=shape[1])
        self.off += nb
        return Buf(v, name)


SPL = [512, 512, 1024, 1024, 1024, 128, 128, 1024, 256, 256, 1024, 1024, 1024]
OFF = np.concatenate([[0], np.cumsum(SPL)]).astype(int)
O_QR, O_KR, O_VR, O_UR, O_QS, O_KS, O_VS, O_QA, O_KA, O_VA, O_AR, O_AS, O_AA = [int(v) for v in OFF[:13]]


def _perm128():
    f = np.arange(128)
    return np.where(f % 64 < 32, f + 32, f - 32)


def _perm64():
    f = np.arange(128)
    return np.where(f % 32 < 16, f + 16, f - 16)


def fm_units():
    u = []
    ar = np.arange(128)
    for h in range(4):
        u.append(dict(name=f"kr{h}", kind="rope128", cols=O_KR + h * 128 + ar, tok="all"))
    u.append(dict(name="ks", kind="rope64", cols=O_KS + ar, tok="all"))
    u.append(dict(name="ks2", kind="rope64", cols=O_KS + (ar + 64) % 128, tok="all"))
    for h in range(2):
        u.append(dict(name=f"ka{h}", kind="normk", cols=O_KA + h * 128 + ar, tok="all"))
    for h in range(4):
        u.append(dict(name=f"qr{h}", kind="rope128", cols=O_QR + h * 128 + ar, tok="q"))
    for c in range(8):
        u.append(dict(name=f"qs{c}", kind="rope64", cols=O_QS + c * 128 + ar, tok="q"))
    for h in range(8):
        u.append(dict(name=f"qa{h}", kind="normq", cols=O_QA + h * 128 + ar, tok="q"))
    for c in range(8):
        u.append(dict(name=f"ur{c}", kind="silu", cols=O_UR + c * 128 + ar, tok="q"))
    for nm, o in (("ar", O_AR), ("as", O_AS), ("aa", O_AA)):
        for c in range(8):
            u.append(dict(name=f"{nm}{c}", kind="sig", cols=o + c * 128 + ar, tok="q"))
    g, used = 0, 0
    for x in u:
        w = 256 if x["kind"] in ("rope128", "rope64", "normq", "normk") else 128
        if used + w > 512:
            g, used = g + 1, 0
        x["group"], x["c0"] = g, used
        x["c1"] = used + 128 if w == 256 else None
        used += w
    return u, g + 1


FM_UNITS, N_FM_GROUPS = fm_units()
TM_COLS = np.concatenate([O_VR + np.arange(1024), O_VS + np.arange(128), O_VA + np.arange(256)])
N_TM_GROUPS = 3
G_MOD = 0
G_FM = 12
G_TM = G_FM + N_FM_GROUPS
G_MERGE = G_TM + N_TM_GROUPS
N_GROUPS = G_MERGE + 8

SM_BMOD, SM_G1, SM_G2, SM_RET, SM_SINK, SM_GQ, SM_GK = 0, 48, 56, 64, 72, 88, 90
SM_L = 96
SG_C, SG_WR, SG_BR, SG_GF, SG_FLAG = 0, 16, 144, 160, 168
SG_N = 176
C_ID, C_DPOS, C_DNEG, C_MF, C_MB, C_IP1, C_IB, C_ML, C_MR, C_MLB, C_MRB = [i * 128 for i in range(11)]
C_PCF, C_PCB = 11 * 128, 11 * 128 + 1
C_SEL = 11 * 128 + 8
C_N = C_SEL + 2048


def _grp(w):
    n = w.shape[1] // 512
    return np.ascontiguousarray(w.reshape(8, 128, n, 512).transpose(2, 1, 0, 3))


def prep_layer_weights(inp, l):
    w_in = inp["w_in"][l]
    p128, p64 = _perm128(), _perm64()
    cols = np.zeros(N_FM_GROUPS * 512, dtype=np.int64)
    for u in FM_UNITS:
        base = u["group"] * 512
        cols[base + u["c0"]:base + u["c0"] + 128] = u["cols"]
        if u["c1"] is not None:
            pm = p64 if u["kind"] == "rope64" else p128
            cols[base + u["c1"]:base + u["c1"] + 128] = u["cols"][pm]
    tmc = np.concatenate([TM_COLS, np.zeros(1536 - 1408, dtype=np.int64)])
    wcat = np.concatenate([inp["w_mod"][l], w_in[:, cols], w_in[:, tmc], inp["w_br_ret"][l], inp["w_br_swa"][l],
                           inp["w_br_ga"][l], inp["w_out"][l]], axis=1)
    wall = _grp(wcat)
    assert wall.shape[0] == N_GROUPS
    wg = inp["w_gate"][l].reshape(16, 8, 128, 512).transpose(0, 2, 1, 3).reshape(16, 128, 4096)
    wu = inp["w_up"][l].reshape(16, 8, 128, 512).transpose(0, 2, 1, 3).reshape(16, 128, 4096)
    wd = inp["w_down"][l].reshape(16, 4, 128, 1024).transpose(0, 2, 1, 3).reshape(16, 128, 4096)
    we = np.ascontiguousarray(np.stack([wg, wu, wd], axis=1))
    sm = np.zeros((128, SM_L), np.float32)
    sm[:, SM_BMOD:SM_BMOD + 48] = inp["b_mod"][l].reshape(48, 128).T
    sm[:, SM_G1:SM_G1 + 8] = inp["g_norm1"][l].reshape(8, 128).T
    sm[:, SM_G2:SM_G2 + 8] = inp["g_norm2"][l].reshape(8, 128).T
    sm[:, SM_RET:SM_RET + 8] = inp["ret_decay_logit"][l].reshape(1, 8)
    sm[:, SM_SINK:SM_SINK + 16] = inp["swa_sink"][l].reshape(1, 16)
    sm[:, SM_GQ] = inp["g_qnorm"][l]
    sm[:, SM_GQ + 1] = inp["g_qnorm"][l][p128]
    sm[:, SM_GK] = inp["g_knorm"][l]
    sm[:, SM_GK + 1] = inp["g_knorm"][l][p128]
    return wall, we, sm


def make_consts(half):
    c = np.zeros((128, C_N), np.float32)
    m = np.arange(128)[:, None].astype(np.float32)
    n = np.arange(128)[None, :].astype(np.float32)
    c[:, C_ID:C_ID + 128] = np.eye(128)
    c[:, C_DPOS:C_DPOS + 128] = np.maximum(n - m, 0)
    c[:, C_DNEG:C_DNEG + 128] = np.maximum(m - n, 0)
    c[:, C_MF:C_MF + 128] = (n >= m)
    c[:, C_MB:C_MB + 128] = (m > n)
    c[:, C_IP1:C_IP1 + 128] = n + 1 + 0 * m
    c[:, C_IB:C_IB + 128] = 128 - n + 0 * m
    c[:, C_ML:C_ML + 128] = (m >= n)
    c[:, C_MR:C_MR + 128] = (m <= n)
    c[:, C_MLB:C_MLB + 128] = (m >= n) * (1.0 if half == 1 else 0.0)
    c[:, C_MRB:C_MRB + 128] = (m <= n) * (1.0 if half == 0 else 0.0)
    c[:, C_PCF] = 127 - np.arange(128)
    c[:, C_PCB] = np.arange(128)
    for e in range(16):
        c[e, C_SEL + e * 128:C_SEL + (e + 1) * 128] = 1.0
    return c


def make_tabs(half):
    pos_own = half * NOWN + np.arange(NOWN)
    pos_oth = (1 - half) * NOWN + np.arange(NOTH)
    pos = np.concatenate([pos_own, pos_oth])
    row = (pos // 64).astype(np.float32)
    col = (pos % 64).astype(np.float32)
    tabs = np.zeros((4, 128, NALL), np.float32)
    tabs[0, :, 4096:] = 1.0
    tabs[2, :, 4096:] = 1.0
    for ti, hd in ((0, 128), (2, 64)):
        half_d, quarter = hd // 2, hd // 4
        freqs = (np.float32(10000.0) ** (-np.arange(quarter, dtype=np.float32) / np.float32(quarter))).astype(np.float32)
        for f in range(128):
            fl = f % hd
            a = fl // half_d
            j = fl % half_d
            p = row if a == 0 else col
            ang = (p * freqs[j % quarter]).astype(np.float32)
            tabs[ti, f, :4096] = np.cos(ang)
            tabs[ti + 1, f, :4096] = np.sin(ang) * (-1.0 if j < quarter else 1.0)
    return tabs


def make_small_global(inp, b, half):
    sg = np.zeros((128, SG_N), np.float32)
    cT = inp["c"][b].reshape(8, 128).T
    ccT = inp["c_ctx"].reshape(8, 128).T
    sg[:, SG_C:SG_C + 16:2] = cT
    sg[:, SG_C + 1:SG_C + 16:2] = ccT
    sg[:, SG_WR:SG_WR + 128] = inp["w_router"].reshape(8, 128, 16).transpose(1, 0, 2).reshape(128, 128)
    sg[:, SG_BR:SG_BR + 16] = inp["b_router"].reshape(1, 16)
    sg[:, SG_GF:SG_GF + 8] = inp["g_final"].reshape(8, 128).T
    sg[:, SG_FLAG] = 1.0 if half == 0 else 0.0
    sg[:, SG_FLAG + 1] = 1.0 if half == 1 else 0.0
    return sg


TOK_TILES_ALL = [(i * 512, 512, 0) for i in range(8)] + [(4096, 256, 1)]
TOK_TILES_OWN = [(i * 512, 512, 0) for i in range(4)]
CTX_TILE = (4096, 256, 1)


def qpos(t0):
    return t0 if t0 < NOWN else t0 - NOTH


def build(layers, need_ctx_flags, final, dbg=(), stop=99):
    nc = bass.Bass("TRN2", target_bir_lowering=False)
    P = Prog(nc)
    nl = len(layers)

    def din(name, shape, dt=F32):
        return nc.dram_tensor(name, list(shape), dt, kind="ExternalInput").ap()

    def dscr(name, shape, dt=BF16):
        kind = "ExternalOutput" if name in dbg else "Internal"
        return nc.dram_tensor(name, list(shape), dt, kind=kind).ap()

    xall = din("xall", [D, NALL])
    tabs = din("tabs", [4, 128, NALL])
    consts_d = din("consts", [128, C_N])
    sg_d = din("sg", [128, SG_N])
    sm_d = [din(f"sm{l}", [128, SM_L]) for l in range(nl)]
    wall_d = [din(f"wall{l}", [N_GROUPS, 128, 8, 512]) for l in range(nl)]
    we_d = [din(f"we{l}", [16, 3, 128, 4096]) for l in range(nl)]
    if final:
        yout = nc.dram_tensor("yout", [D, NOWN], F32, kind="ExternalOutput").ap()
    else:
        xout = nc.dram_tensor("xout", [D, NQ], F32, kind="ExternalOutput").ap()

    xs_d = dscr("xs", [D, NALL], F32)
    fm_d = {u["name"]: dscr("fm_" + u["name"], [128, NALL]) for u in FM_UNITS}
    vall_d = dscr("vall", [NALL, 1536])
    obr_d = [dscr(f"obr{i}", [D, NQ]) for i in range(3)]
    web_d = dscr("web", [16, 3, 128, 4096])
    T_xs = {}

    def txs(t0):
        return T_xs.setdefault(t0, T(f"xs{t0}"))
    T_fm = {}

    def tfm(name, t0):
        return T_fm.setdefault((name, t0), T(f"fm{name}{t0}"))
    T_vall = {}

    def tvall(sub):
        return T_vall.setdefault(sub, T(f"vall{sub}"))
    T_obr = {}

    def tobr(i, t0):
        return T_obr.setdefault((i, t0), T(f"obr{i}_{t0}"))
    T_web = {}

    def tweb(e, k):
        return T_web.setdefault((e, k), T(f"web{e}_{k}"))

    xs3 = xs_d.rearrange("(kc p) t -> p kc t", p=128)
    if nl > 1:
        xch_src = [nc.dram_tensor(f"xch_src{i}", [D, 512], F32, kind="Internal").ap() for i in range(4)]
        xch_dst = [nc.dram_tensor(f"xch_dst{i}", [2 * D, 512], F32, kind="Internal").ap() for i in range(4)]
        T_xsrc, T_xdst = [T("xsrc") for i in range(4)], [T("xdst") for i in range(4)]
    xall3 = xall.rearrange("(kc p) t -> p kc t", p=128)

    A = Arena(nc, 206 * 1024)
    cst = A.alloc([128, C_N], F32, "cst")
    sg = A.alloc([128, SG_N], F32, "sg")
    ones_bf = A.alloc([128, 128], BF16, "ones")
    id_bf = A.alloc([128, 128], BF16, "idbf")
    mk_bf = A.alloc([128, 4, 512], BF16, "mk")
    PERS_END = None
    ps = [Buf(nc.alloc_psum_tensor(f"ps{i}", [128, 512], F32)[:], f"ps{i}", True) for i in range(8)]

    P.dma("sp", cst.ap, consts_d, writes=[cst.t])
    P.dma("sp", sg.ap, sg_d, writes=[sg.t])
    P.add("dve", lambda e: e.memset(ones_bf.ap, 1.0), writes=[ones_bf.t])
    P.copy("dve", id_bf.ap, cst.ap[:, C_ID:C_ID + 128], reads=[cst.t], writes=[id_bf.t])
    for i, co in enumerate((C_ML, C_MR, C_MLB, C_MRB)):
        for r in range(4):
            P.copy("dve", mk_bf.ap[:, i, r * 128:(r + 1) * 128], cst.ap[:, co:co + 128], reads=[cst.t],
                   writes=[mk_bf.t])
    for (t0, nt, _) in TOK_TILES_ALL:
        P.dma("sp", xs_d[:, t0:t0 + nt], xall[:, t0:t0 + nt], writes=[txs(t0)])
    PERS_END = A.off

    def rsqrt_from_psum(dst, src_ps, n, rd, wr):
        P.ts("dve", dst, src_ps, 1.0 / n, EPS, ALU.mult, ALU.add, reads=rd, writes=wr)
        P.act(dst, dst, AF.Ln, reads=wr, writes=wr)
        P.act(dst, dst, AF.Exp, reads=wr, writes=wr, scale=-0.5)

    psi = [0]

    def ps_next(k=7):
        psi[0] = (psi[0] + 1) % k
        return ps[psi[0]]

    for li, l in enumerate(layers):
        need_ctx = need_ctx_flags[li]
        last = (li == nl - 1)
        qtiles = TOK_TILES_OWN + ([CTX_TILE] if need_ctx else [])
        P.barrier()
        A.off = PERS_END
        sm = A.alloc([128, SM_L], F32, "sm")
        modT = A.alloc([128, 48, 2], F32, "modT")
        A1 = A.alloc([128, 8, 2], F32, "A1")
        A2 = A.alloc([128, 8, 2], F32, "A2")
        silc = A.alloc([128, 16], F32, "silc")
        lg = A.alloc([128, 8], F32, "lg")
        sinkx = A.alloc([128, 16], F32, "sinkx")
        P.dma("sp", sm.ap, sm_d[li], writes=[sm.t])
        P.act(silc.ap, sg.ap[:, SG_C:SG_C + 16], AF.Silu, reads=[sg.t], writes=[silc.t])
        silc3 = silc.ap.rearrange("p (k j) -> p k j", j=2)
        P.act(lg.ap, sm.ap[:, SM_RET:SM_RET + 8], AF.Exp, reads=[sm.t], writes=[lg.t], scale=-1.0)
        P.ts("dve", lg.ap, lg.ap, 1.0, None, ALU.add, reads=[lg.t], writes=[lg.t])
        P.act(lg.ap, lg.ap, AF.Ln, reads=[lg.t], writes=[lg.t])
        P.ts("dve", lg.ap, lg.ap, -1.0, None, ALU.mult, reads=[lg.t], writes=[lg.t])
        P.act(sinkx.ap, sm.ap[:, SM_SINK:SM_SINK + 16], AF.Exp, reads=[sm.t], writes=[sinkx.t])
        PH0_END = A.off
        wst = [A.alloc([128, 8, 512], F32, f"wst{i}") for i in range(2)]
        for g in range(12):
            w = wst[g % 2]
            P.dma("sp", w.ap, wall_d[li][G_MOD + g], writes=[w.t])
            for j in range(4):
                idx = g * 4 + j
                pb = ps_next()
                for kc in range(8):
                    P.mm(pb.ap[:, 0:2], w.ap[:, kc, j * 128:(j + 1) * 128], silc3[:, kc, :], kc == 0, kc == 7,
                         reads=[w.t, silc.t], writes=[pb.t])
                P.ts("dve", modT.ap[:, idx, :], pb.ap[:, 0:2], sm.ap[:, SM_BMOD + idx:SM_BMOD + idx + 1], None,
                     ALU.add, reads=[pb.t, sm.t], writes=[modT.t])
        for j in range(2):
            P.stt(A1.ap[:, :, j], modT.ap[:, 8:16, j], 1.0, sm.ap[:, SM_G1:SM_G1 + 8], ALU.add, ALU.mult,
                  reads=[modT.t, sm.t], writes=[A1.t])
            P.stt(A2.ap[:, :, j], modT.ap[:, 32:40, j], 1.0, sm.ap[:, SM_G2:SM_G2 + 8], ALU.add, ALU.mult,
                  reads=[modT.t, sm.t], writes=[A2.t])

        def SH1(kc, j): return modT.ap[:, 0 + kc, j:j + 1]
        def GT1(kc, j): return modT.ap[:, 16 + kc, j:j + 1]
        def SH2(kc, j): return modT.ap[:, 24 + kc, j:j + 1]
        def GT2(kc, j): return modT.ap[:, 40 + kc, j:j + 1]

        P.barrier()
        A.off = PH0_END
        hT = A.alloc([128, 8, NALL], BF16, "hT")
        hts = {t0: T(f"hT{t0}") for (t0, _, _) in TOK_TILES_ALL}
        xt = [A.alloc([128, 8, 512], F32, f"xt{i}") for i in range(2)]
        sq = A.alloc([128, 8, 512], BF16, "sq")
        rstd = A.alloc([128, 512], F32, "rstd")
        tmp = [A.alloc([128, 512], F32, f"tmp{i}") for i in range(2)]
        PH1_END = A.off

        def norm_tiles(tiles, Aco, SHf, out_fn, src3=xs3):
            for i, (t0, nt, mj) in enumerate(tiles):
                x_ = xt[i % 2]
                P.dma("sp", x_.ap[:, :, 0:nt], src3[:, :, t0:t0 + nt], reads=[txs(t0)], writes=[x_.t])
                P.act(sq.ap[:, :, 0:nt], x_.ap[:, :, 0:nt], AF.Square, reads=[x_.t], writes=[sq.t])
                pb = ps_next()
                for kc in range(8):
                    P.mm(pb.ap[:, 0:nt], ones_bf.ap, sq.ap[:, kc, 0:nt], kc == 0, kc == 7,
                         reads=[ones_bf.t, sq.t], writes=[pb.t])
                rsqrt_from_psum(rstd.ap[:, 0:nt], pb.ap[:, 0:nt], 1024.0, [pb.t], [rstd.t])
                for kc in range(8):
                    tm_ = tmp[kc % 2]
                    P.tt("dve" if kc % 2 == 0 else "pool", tm_.ap[:, 0:nt], x_.ap[:, kc, 0:nt], rstd.ap[:, 0:nt],
                         ALU.mult, reads=[x_.t, rstd.t], writes=[tm_.t])
                    out_fn(kc, t0, nt, mj, tm_, Aco.ap[:, kc, mj:mj + 1], SHf(kc, mj))

        def out_h1(kc, t0, nt, mj, tm_, a_, b_):
            P.act(hT.ap[:, kc, t0:t0 + nt], tm_.ap[:, 0:nt], AF.Identity, reads=[tm_.t, A1.t, modT.t],
                  writes=[hts[t0]], bias=b_, scale=a_)

        norm_tiles(TOK_TILES_ALL, A1, SH1, out_h1)

        A.off = PH1_END
        wbf = [A.alloc([128, 8, 512], BF16, f"wbf{i}") for i in range(3)]
        tabC = [A.alloc([128, 512], F32, f"tabC{i}") for i in range(2)]
        tabS = [A.alloc([128, 512], F32, f"tabS{i}") for i in range(2)]
        stage = [A.alloc([128, 512], BF16, f"stage{i}") for i in range(3)]
        t1b = [A.alloc([128, 512], F32, f"t1b{i}") for i in range(2)]
        t2b = [A.alloc([128, 512], F32, f"t2b{i}") for i in range(2)]
        sqb = A.alloc([128, 512], BF16, "sqb")
        rs2 = A.alloc([128, 512], F32, "rs2")
        cnt = [0]

        def load_group(gi):
            wb = wbf[gi % 3]
            P.dma("pool", wb.ap, wall_d[li][gi], writes=[wb.t])
            return wb

        nxt = load_group(G_FM)
        for gi in range(N_FM_GROUPS):
            wb = nxt
            nxt = load_group(G_FM + gi + 1)
            for u in [x for x in FM_UNITS if x["group"] == gi]:
                tiles = TOK_TILES_ALL if u["tok"] == "all" else qtiles
                kind = u["kind"]
                dual = u["c1"] is not None
                tsel = 2 if kind == "rope64" else 0
                for (t0, nt, mj) in tiles:
                    k = cnt[0]
                    cnt[0] += 1
                    pa = ps_next()
                    for kc in range(8):
                        P.mm(pa.ap[:, 0:nt], wb.ap[:, kc, u["c0"]:u["c0"] + 128], hT.ap[:, kc, t0:t0 + nt],
                             kc == 0, kc == 7, reads=[wb.t, hts[t0]], writes=[pa.t])
                    if dual:
                        pbk = ps_next()
                        for kc in range(8):
                            P.mm(pbk.ap[:, 0:nt], wb.ap[:, kc, u["c1"]:u["c1"] + 128], hT.ap[:, kc, t0:t0 + nt],
                                 kc == 0, kc == 7, reads=[wb.t, hts[t0]], writes=[pbk.t])
                        tc_, ts_ = tabC[k % 2], tabS[k % 2]
                        P.dma("sp", tc_.ap[:, 0:nt], tabs[tsel, :, t0:t0 + nt], writes=[tc_.t])
                        P.dma("sp", ts_.ap[:, 0:nt], tabs[tsel + 1, :, t0:t0 + nt], writes=[ts_.t])
                    st = stage[k % 3]
                    o_ = st.ap[:, 0:nt]
                    if kind in ("rope128", "rope64"):
                        a_, b_ = t1b[k % 2], t2b[k % 2]
                        P.tt("dve", a_.ap[:, 0:nt], pa.ap[:, 0:nt], tc_.ap[:, 0:nt], ALU.mult,
                             reads=[pa.t, tc_.t], writes=[a_.t])
                        P.tt("dve", b_.ap[:, 0:nt], pbk.ap[:, 0:nt], ts_.ap[:, 0:nt], ALU.mult,
                             reads=[pbk.t, ts_.t], writes=[b_.t])
                        P.tt("pool", o_, a_.ap[:, 0:nt], b_.ap[:, 0:nt], ALU.add, reads=[a_.t, b_.t], writes=[st.t])
                    elif kind in ("normq", "normk"):
                        gco = SM_GQ if kind == "normq" else SM_GK
                        a_, b_ = t1b[k % 2], t2b[k % 2]
                        P.act(sqb.ap[:, 0:nt], pa.ap[:, 0:nt], AF.Square, reads=[pa.t], writes=[sqb.t])
                        pc = ps_next()
                        P.mm(pc.ap[:, 0:nt], ones_bf.ap, sqb.ap[:, 0:nt], True, True, reads=[ones_bf.t, sqb.t],
                             writes=[pc.t])
                        rsqrt_from_psum(rs2.ap[:, 0:nt], pc.ap[:, 0:nt], 128.0, [pc.t], [rs2.t])
                        P.stt(a_.ap[:, 0:nt], pa.ap[:, 0:nt], sm.ap[:, gco:gco + 1], tc_.ap[:, 0:nt], ALU.mult,
                              ALU.mult, reads=[pa.t, tc_.t, sm.t], writes=[a_.t])
                        P.stt(b_.ap[:, 0:nt], pbk.ap[:, 0:nt], sm.ap[:, gco + 1:gco + 2], ts_.ap[:, 0:nt], ALU.mult,
                              ALU.mult, reads=[pbk.t, ts_.t, sm.t], writes=[b_.t])
                        P.tt("pool", a_.ap[:, 0:nt], a_.ap[:, 0:nt], b_.ap[:, 0:nt], ALU.add, reads=[a_.t, b_.t],
                             writes=[a_.t])
                        P.tt("pool", o_, a_.ap[:, 0:nt], rs2.ap[:, 0:nt], ALU.mult, reads=[a_.t, rs2.t],
                             writes=[st.t])
                    elif kind == "silu":
                        P.act(o_, pa.ap[:, 0:nt], AF.Silu, reads=[pa.t], writes=[st.t])
                    else:
                        P.act(o_, pa.ap[:, 0:nt], AF.Sigmoid, reads=[pa.t], writes=[st.t])
                    P.dma("pool", fm_d[u["name"]][:, t0:t0 + nt], o_, reads=[st.t], writes=[tfm(u["name"], t0)])
        for gi in range(N_TM_GROUPS):
            wb = nxt
            if gi + 1 < N_TM_GROUPS:
                nxt = load_group(G_TM + gi + 1)
            for sub in range(NALL // 128):
                t0 = sub * 128
                tile0 = (t0 // 512) * 512
                k = cnt[0]
                cnt[0] += 1
                pa = ps_next()
                for kc in range(8):
                    P.mm(pa.ap, hT.ap[:, kc, t0:t0 + 128], wb.ap[:, kc, :], kc == 0, kc == 7,
                         reads=[wb.t, hts[tile0]], writes=[pa.t])
                st = stage[k % 3]
                P.copy("act" if k % 2 == 0 else "dve", st.ap, pa.ap, reads=[pa.t], writes=[st.t])
                P.dma("pool", vall_d[t0:t0 + 128, gi * 512:(gi + 1) * 512], st.ap, reads=[st.t],
                      writes=[tvall(sub)])
        P.barrier()
        if stop <= 2:
            continue
        A.off = PH0_END
        KSCALE = 128.0 ** -0.5
        KT = A.alloc([128, NALL], BF16, "KT")
        QT = A.alloc([128, NALL], BF16, "QT")
        Vr = A.alloc([128, 34, 256], BF16, "Vr")
        Ur = A.alloc([128, 2, NALL], BF16, "Ur")
        Kf = A.alloc([128, 34, 128], BF16, "Kf")
        Kb = A.alloc([128, 34, 128], BF16, "Kb")
        SFb = A.alloc([128, 18, 256], BF16, "SFb")
        SBb = A.alloc([128, 18, 256], BF16, "SBb")
        Dm = A.alloc([128, 512], BF16, "Dm")
        qdf = A.alloc([128, 512], F32, "qdf")
        qdb = A.alloc([128, 512], F32, "qdb")
        e1 = A.alloc([128, 128], F32, "e1")
        e2 = A.alloc([128, 128], F32, "e2")
        kd = A.alloc([128, 4], F32, "kd")
        S = A.alloc([128, 256], F32, "S")
        S0f = A.alloc([128, 256], F32, "S0f")
        S0b = A.alloc([128, 256], F32, "S0b")
        Pm = A.alloc([128, 512], BF16, "Pm")
        Qf = A.alloc([128, 512], BF16, "Qf")
        Qb = A.alloc([128, 512], BF16, "Qb")
        sq2 = A.alloc([128, 2, 512], BF16, "sq2")
        rs3 = A.alloc([128, 512], F32, "rs3")
        to_ = [A.alloc([128, 512], F32, f"to{i}") for i in range(2)]
        stg = [A.alloc([128, 512], BF16, f"stg{i}") for i in range(2)]
        flg = sg.ap[:, SG_FLAG:SG_FLAG + 2]
        for h in range(4):
            lgf, lgb = lg.ap[:, h:h + 1], lg.ap[:, 4 + h:5 + h]
            P.act(e1.ap, cst.ap[:, C_DPOS:C_DPOS + 128], AF.Exp, reads=[cst.t, lg.t], writes=[e1.t], scale=lgf)
            P.tt("dve", e1.ap, e1.ap, cst.ap[:, C_MF:C_MF + 128], ALU.mult, reads=[e1.t, cst.t], writes=[e1.t])
            P.act(e2.ap, cst.ap[:, C_DNEG:C_DNEG + 128], AF.Exp, reads=[cst.t, lg.t], writes=[e2.t], scale=lgb)
            P.tt("dve", e2.ap, e2.ap, cst.ap[:, C_MB:C_MB + 128], ALU.mult, reads=[e2.t, cst.t], writes=[e2.t])
            P.tt("dve", e1.ap, e1.ap, e2.ap, ALU.add, reads=[e1.t, e2.t], writes=[e1.t])
            for r in range(4):
                P.ts("dve", Dm.ap[:, r * 128:(r + 1) * 128], e1.ap, KSCALE, None, ALU.mult, reads=[e1.t],
                     writes=[Dm.t])
                P.act(qdf.ap[:, r * 128:(r + 1) * 128], cst.ap[:, C_IP1:C_IP1 + 128], AF.Exp, reads=[cst.t, lg.t],
                      writes=[qdf.t], scale=lgf)
                P.act(qdb.ap[:, r * 128:(r + 1) * 128], cst.ap[:, C_IB:C_IB + 128], AF.Exp, reads=[cst.t, lg.t],
                      writes=[qdb.t], scale=lgb)
            P.act(kd.ap[:, 0:1], cst.ap[:, C_PCF:C_PCF + 1], AF.Exp, reads=[cst.t, lg.t], writes=[kd.t], scale=lgf)
            P.act(kd.ap[:, 1:2], cst.ap[:, C_PCB:C_PCB + 1], AF.Exp, reads=[cst.t, lg.t], writes=[kd.t], scale=lgb)
            P.ts("dve", kd.ap[:, 0:2], kd.ap[:, 0:2], KSCALE, None, ALU.mult, reads=[kd.t], writes=[kd.t])
            P.act(kd.ap[:, 2:3], lgf, AF.Exp, reads=[lg.t], writes=[kd.t], scale=128.0)
            P.act(kd.ap[:, 3:4], lgb, AF.Exp, reads=[lg.t], writes=[kd.t], scale=128.0)
            P.dma("sp", KT.ap, fm_d[f"kr{h}"], writes=[KT.t])
            P.dma("sp", QT.ap[:, 0:NOWN], fm_d[f"qr{h}"][:, 0:NOWN], writes=[QT.t])
            if need_ctx:
                P.dma("sp", QT.ap[:, 4096:NALL], fm_d[f"qr{h}"][:, 4096:NALL], writes=[QT.t])
            vsrc = vall_d[:, h * 256:(h + 1) * 256].rearrange("(t p) c -> p t c", p=128)
            for tq in range(0, 34, 4):
                P.dma("sp", Vr.ap[:, tq:min(tq + 4, 34), :], vsrc[:, tq:min(tq + 4, 34), :], writes=[Vr.t])
            for j in range(2):
                P.dma("sp", Ur.ap[:, j, 0:NOWN], fm_d[f"ur{2 * h + j}"][:, 0:NOWN], writes=[Ur.t])
                if need_ctx:
                    P.dma("sp", Ur.ap[:, j, 4096:NALL], fm_d[f"ur{2 * h + j}"][:, 4096:NALL], writes=[Ur.t])
            for c0 in (range(0, 32 if 'trlast' in SKIP else 34, 4) if 'tr' not in SKIP else []):
                n = min(4, 34 - c0)
                pb_ = ps_next()
                for c in range(n):
                    P.mm(pb_.ap[:, c * 128:(c + 1) * 128], KT.ap[:, (c0 + c) * 128:(c0 + c + 1) * 128], id_bf.ap,
                         True, True, reads=[KT.t, id_bf.t], writes=[pb_.t])
                P.ts("dve", Kf.ap[:, c0:c0 + n, :], pb_.ap[:, 0:n * 128].rearrange("p (a b) -> p a b", a=n),
                     kd.ap[:, 0:1], None, ALU.mult, reads=[pb_.t, kd.t], writes=[Kf.t])
                P.act(Kb.ap[:, c0:c0 + n, :], pb_.ap[:, 0:n * 128].rearrange("p (a b) -> p a b", a=n), AF.Identity,
                      reads=[pb_.t, kd.t], writes=[Kb.t], scale=kd.ap[:, 1:2])

            def upd(Kx, c, cdcol, first):
                if 'upd' in SKIP:
                    return
                pb_ = ps_next()
                P.mm(pb_.ap[:, 0:256], Kx.ap[:, c, :], Vr.ap[:, c, :], True, True, reads=[Kx.t, Vr.t],
                     writes=[pb_.t])
                if first:
                    P.copy("dve", S.ap, pb_.ap[:, 0:256], reads=[pb_.t], writes=[S.t])
                else:
                    P.stt(S.ap, S.ap, kd.ap[:, cdcol:cdcol + 1], pb_.ap[:, 0:256], ALU.mult, ALU.add,
                          reads=[S.t, kd.t, pb_.t], writes=[S.t])

            def snap(dst, idx):
                if 'upd' in SKIP:
                    return
                P.copy("act", dst.ap[:, idx, :], S.ap, reads=[S.t], writes=[dst.t])

            upd(Kf, 32, 2, True)
            snap(SFb, 17)
            upd(Kf, 33, 2, False)
            P.copy("dve", S0f.ap, S.ap, reads=[S.t], writes=[S0f.t])
            for c in range(16, 32):
                upd(Kf, c, 2, False)
            P.tt("dve", S.ap, S.ap, S0f.ap, ALU.subtract, reads=[S.t, S0f.t], writes=[S.t])
            P.stt(S.ap, S.ap, flg[:, 1:2], S0f.ap, ALU.mult, ALU.add, reads=[S.t, S0f.t, sg.t], writes=[S.t])
            for c in range(0, 16):
                snap(SFb, c)
                if c < 15:
                    upd(Kf, c, 2, False)
            upd(Kb, 33, 3, True)
            snap(SBb, 16)
            upd(Kb, 32, 3, False)
            P.copy("dve", S0b.ap, S.ap, reads=[S.t], writes=[S0b.t])
            for c in range(31, 15, -1):
                upd(Kb, c, 3, False)
            P.tt("dve", S.ap, S.ap, S0b.ap, ALU.subtract, reads=[S.t, S0b.t], writes=[S.t])
            P.stt(S.ap, S.ap, flg[:, 0:1], S0b.ap, ALU.mult, ALU.add, reads=[S.t, S0b.t, sg.t], writes=[S.t])
            for c in range(15, -1, -1):
                snap(SBb, c)
                if c > 0:
                    upd(Kb, c, 3, False)
            for (t0, nt, mj) in (qtiles if 'out' not in SKIP else []):
                ncx = nt // 128
                pS = ps_next()
                for j in range(ncx):
                    sl = slice(t0 + j * 128, t0 + (j + 1) * 128)
                    P.mm(pS.ap[:, j * 128:(j + 1) * 128], KT.ap[:, sl], QT.ap[:, sl], True, True,
                         reads=[KT.t, QT.t], writes=[pS.t])
                P.tt("dve", Pm.ap[:, 0:nt], pS.ap[:, 0:nt], Dm.ap[:, 0:nt], ALU.mult, reads=[pS.t, Dm.t],
                     writes=[Pm.t])
                P.tt("pool", Qf.ap[:, 0:nt], QT.ap[:, t0:t0 + nt], qdf.ap[:, 0:nt], ALU.mult, reads=[QT.t, qdf.t],
                     writes=[Qf.t])
                P.tt("pool", Qb.ap[:, 0:nt], QT.ap[:, t0:t0 + nt], qdb.ap[:, 0:nt], ALU.mult, reads=[QT.t, qdb.t],
                     writes=[Qb.t])
                pO = [ps_next(), ps_next()]
                for dj in range(2):
                    dsl = slice(dj * 128, (dj + 1) * 128)
                    for j in range(ncx):
                        c = t0 // 128 + j
                        csl = slice(j * 128, (j + 1) * 128)
                        if c < 16:
                            sf, sb = c, c
                        elif c == 32:
                            sf, sb = None, 16
                        else:
                            sf, sb = 17, None
                        terms = [(Vr.ap[:, c, dsl], Pm.ap[:, csl], [Vr.t, Pm.t])]
                        if sf is not None:
                            terms.append((SFb.ap[:, sf, dsl], Qf.ap[:, csl], [SFb.t, Qf.t]))
                        if sb is not None:
                            terms.append((SBb.ap[:, sb, dsl], Qb.ap[:, csl], [SBb.t, Qb.t]))
                        for ti, (l_, r_, rd) in enumerate(terms):
                            P.mm(pO[dj].ap[:, csl], l_, r_, ti == 0, ti == len(terms) - 1, reads=rd,
                                 writes=[pO[dj].t])
                    P.act(sq2.ap[:, dj, 0:nt], pO[dj].ap[:, 0:nt], AF.Square, reads=[pO[dj].t], writes=[sq2.t])
                pN = ps_next()
                for dj in range(2):
                    P.mm(pN.ap[:, 0:nt], ones_bf.ap, sq2.ap[:, dj, 0:nt], dj == 0, dj == 1,
                         reads=[ones_bf.t, sq2.t], writes=[pN.t])
                rsqrt_from_psum(rs3.ap[:, 0:nt], pN.ap[:, 0:nt], 256.0, [pN.t], [rs3.t])
                for dj in range(2):
                    P.tt("dve", to_[dj].ap[:, 0:nt], pO[dj].ap[:, 0:nt], rs3.ap[:, 0:nt], ALU.mult,
                         reads=[pO[dj].t, rs3.t], writes=[to_[dj].t])
                    P.tt("pool", stg[dj].ap[:, 0:nt], to_[dj].ap[:, 0:nt], Ur.ap[:, dj, t0:t0 + nt], ALU.mult,
                         reads=[to_[dj].t, Ur.t], writes=[stg[dj].t])
                    r0 = (2 * h + dj) * 128
                    P.dma("pool", obr_d[0][r0:r0 + 128, qpos(t0):qpos(t0) + nt], stg[dj].ap[:, 0:nt],
                          reads=[stg[dj].t], writes=[tobr(0, (h, dj, t0))])
        P.barrier()
        if stop <= 3:
            continue
        def run_attn(groups, Pt, depth=2):
            items = [(gi, ki) for gi, g in enumerate(groups) for ki in range(g["n"])]
            pts = {}
            sidx = [0]

            def SE(idx):
                gi, ki = items[idx]
                g = groups[gi]
                pS = ps[sidx[0] % 4]
                sidx[0] += 1
                g["S"](ki, pS)
                pt = Pt[idx % len(Pt)]
                g["E"](ki, pS, pt)
                pts[idx] = pt
            for idx in range(min(depth, len(items))):
                SE(idx)
            for idx in range(len(items)):
                if idx + depth < len(items):
                    SE(idx + depth)
                gi, ki = items[idx]
                g = groups[gi]
                pO, pD = (ps[4], ps[5]) if gi % 2 == 0 else (ps[6], ps[7])
                g["PV"](ki, pts.pop(idx), pO, pD, ki == 0, ki == g["n"] - 1)
                if ki == g["n"] - 1:
                    g["epi"](pO, pD)

        A.off = PH0_END
        KSa = A.alloc([128, NALL], BF16, "KSa")
        KSb = A.alloc([128, NALL], BF16, "KSb")
        QS = A.alloc([128, 4, NALL], BF16, "QS")
        Vs = A.alloc([128, 34, 64], BF16, "Vs")
        Pt = [A.alloc([128, 512], BF16, f"Pt{i}") for i in range(4)]
        sk = [A.alloc([64, 512], F32, f"sk{i}") for i in range(2)]
        den = [A.alloc([128, 512], F32, f"den{i}") for i in range(2)]
        ostg = [A.alloc([128, 512], BF16, f"ostg{i}") for i in range(2)]
        P.dma("sp", KSa.ap, fm_d["ks"], writes=[KSa.t])
        P.dma("sp", KSb.ap, fm_d["ks2"], writes=[KSb.t])
        obr1 = obr_d[1].rearrange("(c p) t -> p c t", p=128)
        gcount = 0
        for g in range(2):
            for i in range(4):
                P.dma("sp", QS.ap[:, i, 0:NOWN], fm_d[f"qs{g * 4 + i}"][:, 0:NOWN], writes=[QS.t])
                if need_ctx:
                    P.dma("sp", QS.ap[:, i, 4096:NALL], fm_d[f"qs{g * 4 + i}"][:, 4096:NALL], writes=[QS.t])
            vsrc = vall_d[:, 1024 + g * 64:1024 + (g + 1) * 64].rearrange("(t p) c -> p t c", p=128)
            for tq in range(0, 34, 4):
                P.dma("sp", Vs.ap[:, tq:min(tq + 4, 34), :], vsrc[:, tq:min(tq + 4, 34), :], writes=[Vs.t])
            groups = []
            for par in range(2):
                pb0 = par * 64
                Ksrc = KSa if g == par else KSb
                sk_ = sk[par]
                for i in range(4):
                    hq = g * 8 + par + 2 * i
                    P.act(sk_.ap[:, i * 128:(i + 1) * 128], cst.ap[0:64, C_DPOS:C_DPOS + 128], AF.Identity,
                          reads=[cst.t, sinkx.t], writes=[sk_.t], bias=sinkx.ap[0:64, hq:hq + 1], scale=0.0)
                blocks = [(jb * 128, jb) for jb in range(16)] + ([(4096, 16), (4224, 17)] if need_ctx else [])
                for (t0, jb) in blocks:
                    if jb < 16:
                        kts = [((jb - 1) * 128, 0) if jb > 0 else (2048 + 15 * 128, 2), (jb * 128, None),
                               ((jb + 1) * 128, 1) if jb < 15 else (2048, 3), (4096, None), (4224, None)]
                    else:
                        kts = [(4096, None), (4224, None)]

                    def S(ki, pS, kts=kts, pb0=pb0, Ksrc=Ksrc, t0=t0):
                        k0 = kts[ki][0]
                        P.mm(pS.ap.rearrange("p (a b) -> p a b", a=4), Ksrc.ap[pb0:pb0 + 64, k0:k0 + 128],
                             QS.ap[pb0:pb0 + 64, :, t0:t0 + 128], True, True, reads=[Ksrc.t, QS.t], writes=[pS.t])

                    def E(ki, pS, pt, kts=kts):
                        P.act(pt.ap, pS.ap, AF.Exp, reads=[pS.t], writes=[pt.t], scale=0.125)
                        mi = kts[ki][1]
                        if mi is not None:
                            P.tt("pool", pt.ap, pt.ap, mk_bf.ap[:, mi, :], ALU.mult, reads=[pt.t, mk_bf.t],
                                 writes=[pt.t])

                    def PV(ki, pt, pO, pD, first, lastk, kts=kts):
                        k0 = kts[ki][0]
                        P.mm(pO.ap[0:64, :], Vs.ap[:, k0 // 128, :], pt.ap, first, lastk, reads=[Vs.t, pt.t],
                             writes=[pO.t])
                        P.mm(pD.ap[0:64, :], ones_bf.ap[:, 0:64], pt.ap, first, lastk, reads=[ones_bf.t, pt.t],
                             writes=[pD.t])

                    def epi(pO, pD, t0=t0, pb0=pb0, g=g, sk_=sk_, gc=gcount):
                        dn_, os_ = den[gc % 2], ostg[gc % 2]
                        P.tt("dve", dn_.ap[0:64, :], pD.ap[0:64, :], sk_.ap, ALU.add, reads=[pD.t, sk_.t],
                             writes=[dn_.t])
                        P.act(dn_.ap[0:64, :], dn_.ap[0:64, :], AF.Ln, reads=[dn_.t], writes=[dn_.t])
                        P.act(dn_.ap[0:64, :], dn_.ap[0:64, :], AF.Exp, reads=[dn_.t], writes=[dn_.t], scale=-1.0)
                        P.tt("dve", os_.ap[0:64, :], pO.ap[0:64, :], dn_.ap[0:64, :], ALU.mult,
                             reads=[pO.t, dn_.t], writes=[os_.t])
                        q0 = qpos(t0)
                        P.dma("pool", obr1[pb0:pb0 + 64, g * 4:g * 4 + 4, q0:q0 + 128],
                              os_.ap[0:64, :].rearrange("p (a b) -> p a b", a=4), reads=[os_.t],
                              writes=[tobr(1, (g, pb0, t0))])
                    groups.append(dict(n=len(kts), S=S, E=E, PV=PV, epi=epi))
                    gcount += 1
            run_attn(groups, Pt)
        P.barrier()
        if stop <= 4:
            continue
        A.off = PH0_END
        KA = A.alloc([128, NALL], BF16, "KA")
        VA = A.alloc([128, 34, 128], BF16, "VA")
        QA = [A.alloc([128, NALL], BF16, f"QA{i}") for i in range(4)]
        Pt = [A.alloc([128, 512], BF16, f"Pt{i}") for i in range(4)]
        den = [A.alloc([128, 512], F32, f"den{i}") for i in range(2)]
        ostg = [A.alloc([128, 512], BF16, f"ostg{i}") for i in range(2)]
        GSCALE = 128.0 ** -0.5
        gcount = 0
        for g in range(2):
            P.dma("sp", KA.ap, fm_d[f"ka{g}"], writes=[KA.t])
            vsrc = vall_d[:, 1152 + g * 128:1152 + (g + 1) * 128].rearrange("(t p) c -> p t c", p=128)
            for tq in range(0, 34, 4):
                P.dma("sp", VA.ap[:, tq:min(tq + 4, 34), :], vsrc[:, tq:min(tq + 4, 34), :], writes=[VA.t])
            groups = []
            for hh in range(4):
                h = g * 4 + hh
                Q_ = QA[hh]
                P.dma("sp", Q_.ap[:, 0:NOWN], fm_d[f"qa{h}"][:, 0:NOWN], writes=[Q_.t])
                if need_ctx:
                    P.dma("sp", Q_.ap[:, 4096:NALL], fm_d[f"qa{h}"][:, 4096:NALL], writes=[Q_.t])
                for (t0, nt, mj) in qtiles:
                    kts = list(range(34)) if t0 < 4096 else [32, 33]

                    def S(ki, pS, kts=kts, Q_=Q_, t0=t0, nt=nt):
                        kt = kts[ki]
                        P.mm(pS.ap[:, 0:nt], KA.ap[:, kt * 128:(kt + 1) * 128], Q_.ap[:, t0:t0 + nt], True, True,
                             reads=[KA.t, Q_.t], writes=[pS.t])

                    def E(ki, pS, pt, nt=nt):
                        P.act(pt.ap[:, 0:nt], pS.ap[:, 0:nt], AF.Exp, reads=[pS.t], writes=[pt.t], scale=GSCALE)

                    def PV(ki, pt, pO, pD, first, lastk, kts=kts, nt=nt):
                        kt = kts[ki]
                        P.mm(pO.ap[:, 0:nt], VA.ap[:, kt, :], pt.ap[:, 0:nt], first, lastk, reads=[VA.t, pt.t],
                             writes=[pO.t])
                        P.mm(pD.ap[:, 0:nt], ones_bf.ap, pt.ap[:, 0:nt], first, lastk, reads=[ones_bf.t, pt.t],
                             writes=[pD.t])

                    def epi(pO, pD, t0=t0, nt=nt, h=h, gc=gcount):
                        dn_, os_ = den[gc % 2], ostg[gc % 2]
                        P.act(dn_.ap[:, 0:nt], pD.ap[:, 0:nt], AF.Ln, reads=[pD.t], writes=[dn_.t])
                        P.act(dn_.ap[:, 0:nt], dn_.ap[:, 0:nt], AF.Exp, reads=[dn_.t], writes=[dn_.t], scale=-1.0)
                        P.tt("dve", os_.ap[:, 0:nt], pO.ap[:, 0:nt], dn_.ap[:, 0:nt], ALU.mult,
                             reads=[pO.t, dn_.t], writes=[os_.t])
                        q0 = qpos(t0)
                        P.dma("pool", obr_d[2][h * 128:(h + 1) * 128, q0:q0 + nt], os_.ap[:, 0:nt], reads=[os_.t],
                              writes=[tobr(2, (h, t0))])
                    groups.append(dict(n=len(kts), S=S, E=E, PV=PV, epi=epi))
                    gcount += 1
            run_attn(groups, Pt)
        P.barrier()
        if stop <= 5:
            continue
        A.off = PH0_END
        wm = [A.alloc([128, 8, 1024], BF16, f"wm{i}") for i in range(4)]
        ob = [A.alloc([128, 8, 512], BF16, f"ob{i}") for i in range(3)]
        gb = [A.alloc([128, 8, 512], BF16, f"gb{i}") for i in range(3)]
        ypre = A.alloc([128, 8, 512], BF16, "ypre")
        xtl = A.alloc([128, 8, 512], F32, "xtl")
        ya = [A.alloc([128, 512], F32, f"ya{i}") for i in range(2)]
        tb = [A.alloc([128, 512], F32, f"tb{i}") for i in range(2)]
        for gi in range(8):
            dst = wm[gi // 2].ap[:, :, (gi % 2) * 512:(gi % 2 + 1) * 512]
            P.dma("pool", dst, wall_d[li][G_MERGE + gi], writes=[wm[gi // 2].t])
        gnames = ("ar", "as", "aa")
        for (t0, nt, mj) in qtiles:
            q0 = qpos(t0)
            for b_ in range(3):
                P.dma("sp", ob[b_].ap[:, :, 0:nt],
                      obr_d[b_].rearrange("(kc p) t -> p kc t", p=128)[:, :, q0:q0 + nt], writes=[ob[b_].t])
                for c in range(8):
                    P.dma("sp", gb[b_].ap[:, c, 0:nt], fm_d[f"{gnames[b_]}{c}"][:, t0:t0 + nt], writes=[gb[b_].t])
            P.dma("sp", xtl.ap[:, :, 0:nt], xs3[:, :, t0:t0 + nt], reads=[txs(t0)], writes=[xtl.t])
            for dc in range(8):
                dsl = slice(dc * 128, (dc + 1) * 128)
                y_ = ya[dc % 2]
                for b_ in range(3):
                    pa = ps_next()
                    for kc in range(8):
                        P.mm(pa.ap[:, 0:nt], wm[b_].ap[:, kc, dsl], ob[b_].ap[:, kc, 0:nt], kc == 0, kc == 7,
                             reads=[wm[b_].t, ob[b_].t], writes=[pa.t])
                    if b_ == 0:
                        P.tt("dve", y_.ap[:, 0:nt], pa.ap[:, 0:nt], gb[0].ap[:, dc, 0:nt], ALU.mult,
                             reads=[pa.t, gb[0].t], writes=[y_.t])
                    else:
                        t_ = tb[b_ % 2]
                        P.tt("dve", t_.ap[:, 0:nt], pa.ap[:, 0:nt], gb[b_].ap[:, dc, 0:nt], ALU.mult,
                             reads=[pa.t, gb[b_].t], writes=[t_.t])
                        if b_ == 1:
                            P.tt("pool", y_.ap[:, 0:nt], y_.ap[:, 0:nt], t_.ap[:, 0:nt], ALU.add,
                                 reads=[y_.t, t_.t], writes=[y_.t])
                        else:
                            P.tt("pool", ypre.ap[:, dc, 0:nt], y_.ap[:, 0:nt], t_.ap[:, 0:nt], ALU.add,
                                 reads=[y_.t, t_.t], writes=[ypre.t])
            for dc in range(8):
                dsl = slice(dc * 128, (dc + 1) * 128)
                py = ps_next()
                for kc in range(8):
                    P.mm(py.ap[:, 0:nt], wm[3].ap[:, kc, dsl], ypre.ap[:, kc, 0:nt], kc == 0, kc == 7,
                         reads=[wm[3].t, ypre.t], writes=[py.t])
                P.stt(xtl.ap[:, dc, 0:nt], py.ap[:, 0:nt], GT1(dc, mj), xtl.ap[:, dc, 0:nt], ALU.mult, ALU.add,
                      reads=[py.t, modT.t, xtl.t], writes=[xtl.t])
            P.dma("pool", xs3[:, :, t0:t0 + nt], xtl.ap[:, :, 0:nt], reads=[xtl.t], writes=[txs(t0)])
        P.barrier()
        if stop <= 6:
            continue
        A.off = PH0_END
        h2T = A.alloc([128, 8, NQ], BF16, "h2T")
        xres = A.alloc([128, 8, NQ], F32, "xres")
        WT = A.alloc([16, NQ], F32, "WT")
        M0 = A.off
        h2f = A.alloc([128, 8, 512], F32, "h2f")
        sq = A.alloc([128, 8, 512], BF16, "sq")
        rstd = A.alloc([128, 512], F32, "rstd")
        tmp = [A.alloc([128, 512], F32, f"tmp{i}") for i in range(2)]
        rt = {nm: A.alloc([128, 16], F32, "rt_" + nm) for nm in ("s", "bz", "eq", "msk", "ch", "ws", "wts")}
        rs = {nm: A.alloc([128, 4], F32, "rs_" + nm) for nm in ("m1", "m2", "gs", "gsel", "gm", "dn")}

        def v3(b_):
            return b_.ap.rearrange("p (g k) -> p g k", k=4)

        def bc(b_):
            return b_.ap[:, 0:4].unsqueeze(2).to_broadcast([128, 4, 4])
        mtiles = qtiles
        for i, (t0, nt, mj) in enumerate(mtiles):
            q0 = qpos(t0)
            P.dma("sp", xres.ap[:, :, q0:q0 + nt], xs3[:, :, t0:t0 + nt], reads=[txs(t0)], writes=[xres.t])
            P.act(sq.ap[:, :, 0:nt], xres.ap[:, :, q0:q0 + nt], AF.Square, reads=[xres.t], writes=[sq.t])
            pb = ps_next()
            for kc in range(8):
                P.mm(pb.ap[:, 0:nt], ones_bf.ap, sq.ap[:, kc, 0:nt], kc == 0, kc == 7, reads=[ones_bf.t, sq.t],
                     writes=[pb.t])
            rsqrt_from_psum(rstd.ap[:, 0:nt], pb.ap[:, 0:nt], 1024.0, [pb.t], [rstd.t])
            for kc in range(8):
                tm_ = tmp[kc % 2]
                P.tt("dve", tm_.ap[:, 0:nt], xres.ap[:, kc, q0:q0 + nt], rstd.ap[:, 0:nt], ALU.mult,
                     reads=[xres.t, rstd.t], writes=[tm_.t])
                P.act(h2f.ap[:, kc, 0:nt], tm_.ap[:, 0:nt], AF.Identity, reads=[tm_.t, A2.t, modT.t], writes=[h2f.t],
                      bias=SH2(kc, mj), scale=A2.ap[:, kc, mj:mj + 1])
                P.copy("pool", h2T.ap[:, kc, q0:q0 + nt], h2f.ap[:, kc, 0:nt], reads=[h2f.t], writes=[h2T.t])
            for sub in range(nt // 128):
                ssl = slice(sub * 128, (sub + 1) * 128)
                pr = ps_next()
                for kc in range(8):
                    P.mm(pr.ap[:, 0:16], h2f.ap[:, kc, ssl], sg.ap[:, SG_WR + kc * 16:SG_WR + (kc + 1) * 16],
                         kc == 0, kc == 7, reads=[h2f.t, sg.t], writes=[pr.t])
                s_, bz, eq, msk, ch, ws, wts = [rt[n] for n in ("s", "bz", "eq", "msk", "ch", "ws", "wts")]
                m1, m2, gs, gsel, gm, dn = [rs[n] for n in ("m1", "m2", "gs", "gsel", "gm", "dn")]
                P.act(s_.ap, pr.ap[:, 0:16], AF.Sigmoid, reads=[pr.t], writes=[s_.t])
                P.tt("dve", bz.ap, s_.ap, sg.ap[:, SG_BR:SG_BR + 16], ALU.add, reads=[s_.t, sg.t], writes=[bz.t])
                P.add("dve", lambda e, o=m1.ap, i_=v3(bz): e.tensor_reduce(out=o, in_=i_, axis=AX.X, op=ALU.max),
                      reads=[bz.t], writes=[m1.t])
                P.tt("dve", v3(eq), v3(bz), bc(m1), ALU.is_equal, reads=[bz.t, m1.t], writes=[eq.t])
                P.stt(msk.ap, eq.ap, -1e9, bz.ap, ALU.mult, ALU.add, reads=[eq.t, bz.t], writes=[msk.t])
                P.add("dve", lambda e, o=m2.ap, i_=v3(msk): e.tensor_reduce(out=o, in_=i_, axis=AX.X, op=ALU.max),
                      reads=[msk.t], writes=[m2.t])
                P.tt("dve", gs.ap, m1.ap, m2.ap, ALU.add, reads=[m1.t, m2.t], writes=[gs.t])
                P.add("dve", lambda e, o=gm.ap[:, 0:1], i_=gs.ap: e.tensor_reduce(out=o, in_=i_, axis=AX.X,
                                                                                 op=ALU.max),
                      reads=[gs.t], writes=[gm.t])
                P.ts("dve", gsel.ap, gs.ap, gm.ap[:, 0:1], None, ALU.is_equal, reads=[gs.t, gm.t], writes=[gsel.t])
                P.tt("dve", v3(ch), v3(bz), bc(m2), ALU.is_ge, reads=[bz.t, m2.t], writes=[ch.t])
                P.tt("dve", v3(ch), v3(ch), bc(gsel), ALU.mult, reads=[ch.t, gsel.t], writes=[ch.t])
                P.tt("dve", ws.ap, s_.ap, ch.ap, ALU.mult, reads=[s_.t, ch.t], writes=[ws.t])
                P.add("dve", lambda e, o=dn.ap[:, 0:1], i_=ws.ap: e.tensor_reduce(out=o, in_=i_, axis=AX.X,
                                                                                 op=ALU.add),
                      reads=[ws.t], writes=[dn.t])
                P.add("dve", lambda e, o=dn.ap[:, 1:2], i_=dn.ap[:, 0:1]: e.reciprocal(o, i_), reads=[dn.t],
                      writes=[dn.t])
                P.ts("dve", wts.ap, ws.ap, dn.ap[:, 1:2], None, ALU.mult, reads=[ws.t, dn.t], writes=[wts.t])
                pT = ps_next()
                P.transpose(pT.ap[0:16, 0:128], wts.ap, cst.ap[:, C_ID:C_ID + 128], reads=[wts.t, cst.t],
                            writes=[pT.t])
                P.copy("act", WT.ap[0:16, q0 + sub * 128:q0 + (sub + 1) * 128], pT.ap[0:16, 0:128], reads=[pT.t],
                       writes=[WT.t])
        P.barrier()
        A.off = M0
        ew = [[A.alloc([128, 4096], BF16, f"ew{i}_{k}") for k in range(3)] for i in range(2)]
        wbc = A.alloc([128, 512], F32, "wbc")
        sG = [A.alloc([128, 512], F32, f"sG{i}") for i in range(2)]
        tG = [A.alloc([128, 512], F32, f"tG{i}") for i in range(2)]
        hid = A.alloc([128, 4, 512], BF16, "hid")
        def load_expert(e):
            for k in range(3):
                P.dma("pool", ew[e % 2][k].ap, we_d[li][e, k], writes=[ew[e % 2][k].t])

        load_expert(0)
        for e in range(16):
            wg_, wu_, wd_ = ew[e % 2]
            if e + 1 < 16:
                load_expert(e + 1)
            wg3 = wg_.ap.rearrange("p (a b) -> p a b", a=8)
            wu3 = wu_.ap.rearrange("p (a b) -> p a b", a=8)
            wd3 = wd_.ap.rearrange("p (a b) -> p a b", a=4)
            for (t0, nt, mj) in mtiles:
                q0 = qpos(t0)
                pw = ps_next()
                P.mm(pw.ap[:, 0:nt], cst.ap[0:16, C_SEL + e * 128:C_SEL + (e + 1) * 128], WT.ap[0:16, q0:q0 + nt],
                     True, True, reads=[cst.t, WT.t], writes=[pw.t])
                P.copy("act", wbc.ap[:, 0:nt], pw.ap[:, 0:nt], reads=[pw.t], writes=[wbc.t])
                for hc in range(4):
                    hsl = slice(hc * 128, (hc + 1) * 128)
                    pg, pu = ps_next(), ps_next()
                    for kc in range(8):
                        P.mm(pg.ap[:, 0:nt], wg3[:, kc, hsl], h2T.ap[:, kc, q0:q0 + nt], kc == 0, kc == 7,
                             reads=[wg_.t, h2T.t], writes=[pg.t])
                    for kc in range(8):
                        P.mm(pu.ap[:, 0:nt], wu3[:, kc, hsl], h2T.ap[:, kc, q0:q0 + nt], kc == 0, kc == 7,
                             reads=[wu_.t, h2T.t], writes=[pu.t])
                    sg_, tg_ = sG[hc % 2], tG[hc % 2]
                    P.act(sg_.ap[:, 0:nt], pg.ap[:, 0:nt], AF.Silu, reads=[pg.t], writes=[sg_.t])
                    P.tt("dve", tg_.ap[:, 0:nt], sg_.ap[:, 0:nt], pu.ap[:, 0:nt], ALU.mult, reads=[sg_.t, pu.t],
                         writes=[tg_.t])
                    P.tt("pool", hid.ap[:, hc, 0:nt], tg_.ap[:, 0:nt], wbc.ap[:, 0:nt], ALU.mult,
                         reads=[tg_.t, wbc.t], writes=[hid.t])
                for dc in range(8):
                    dsl = slice(dc * 128, (dc + 1) * 128)
                    py = ps_next()
                    for hc in range(4):
                        P.mm(py.ap[:, 0:nt], wd3[:, hc, dsl], hid.ap[:, hc, 0:nt], hc == 0, hc == 3,
                             reads=[wd_.t, hid.t], writes=[py.t])
                    P.stt(xres.ap[:, dc, q0:q0 + nt], py.ap[:, 0:nt], GT2(dc, mj), xres.ap[:, dc, q0:q0 + nt],
                          ALU.mult, ALU.add, reads=[py.t, modT.t, xres.t], writes=[xres.t])
        if not (last and final):
            for (t0, nt, mj) in mtiles:
                q0 = qpos(t0)
                P.dma("pool", xs3[:, :, t0:t0 + nt], xres.ap[:, :, q0:q0 + nt], reads=[xres.t], writes=[txs(t0)])
        if not last:
            for i, (t0, nt, mj) in enumerate(TOK_TILES_OWN):
                P.dma("pool", xch_src[i].rearrange("(kc p) t -> p kc t", p=128), xres.ap[:, :, t0:t0 + nt],
                      reads=[xres.t], writes=[T_xsrc[i]])
            P.barrier()
            for i in range(4):
                P.add("pool", lambda e, i=i: e.collective_compute("AllGather", ALU.bypass,
                                                                  replica_groups=[[0, 1], [2, 3], [4, 5], [6, 7]],
                                                                  ins=[xch_src[i]], outs=[xch_dst[i]]),
                      reads=[T_xsrc[i]], writes=[T_xdst[i]], cc=True)
            P.barrier()
            A.off = M0
            xa = [A.alloc([128, 8, 512], F32, f"xa{i}") for i in range(2)]
            xb_ = [A.alloc([128, 8, 512], F32, f"xb{i}") for i in range(2)]
            for i, (t0, nt, mj) in enumerate(TOK_TILES_OWN):
                d3 = xch_dst[i].rearrange("(r kc p) t -> r p kc t", r=2, p=128)
                a_, b_ = xa[i % 2], xb_[i % 2]
                P.dma("sp", a_.ap, d3[0], reads=[T_xdst[i]], writes=[a_.t])
                P.dma("sp", b_.ap, d3[1], reads=[T_xdst[i]], writes=[b_.t])
                P.ts("dve", a_.ap, a_.ap, sg.ap[:, SG_FLAG + 1:SG_FLAG + 2], None, ALU.mult, reads=[a_.t, sg.t],
                     writes=[a_.t])
                P.stt(a_.ap, b_.ap, sg.ap[:, SG_FLAG:SG_FLAG + 1], a_.ap, ALU.mult, ALU.add,
                      reads=[a_.t, b_.t, sg.t], writes=[a_.t])
                P.dma("pool", xs3[:, :, NOWN + t0:NOWN + t0 + nt], a_.ap, reads=[a_.t], writes=[txs(NOWN + t0)])
        if last and not final:
            xout3 = xout.rearrange("(kc p) t -> p kc t", p=128)
            for (t0, nt, mj) in mtiles:
                q0 = qpos(t0)
                P.dma("pool", xout3[:, :, q0:q0 + nt], xres.ap[:, :, q0:q0 + nt], reads=[xres.t])
        if last and final:
            P.barrier()
            A.off = M0
            h2f = A.alloc([128, 8, 512], F32, "h2f")
            sq = A.alloc([128, 8, 512], BF16, "sq")
            rstd = A.alloc([128, 512], F32, "rstd")
            tmp = [A.alloc([128, 512], F32, f"tmp{i}") for i in range(2)]
            yout3 = yout.rearrange("(kc p) t -> p kc t", p=128)
            for (t0, nt, mj) in TOK_TILES_OWN:
                P.act(sq.ap[:, :, 0:nt], xres.ap[:, :, t0:t0 + nt], AF.Square, reads=[xres.t], writes=[sq.t])
                pb = ps_next()
                for kc in range(8):
                    P.mm(pb.ap[:, 0:nt], ones_bf.ap, sq.ap[:, kc, 0:nt], kc == 0, kc == 7, reads=[ones_bf.t, sq.t],
                         writes=[pb.t])
                rsqrt_from_psum(rstd.ap[:, 0:nt], pb.ap[:, 0:nt], 1024.0, [pb.t], [rstd.t])
                for kc in range(8):
                    tm_ = tmp[kc % 2]
                    P.tt("dve", tm_.ap[:, 0:nt], xres.ap[:, kc, t0:t0 + nt], rstd.ap[:, 0:nt], ALU.mult,
                         reads=[xres.t, rstd.t], writes=[tm_.t])
                    P.act(h2f.ap[:, kc, 0:nt], tm_.ap[:, 0:nt], AF.Identity, reads=[tm_.t, sg.t], writes=[h2f.t],
                          scale=sg.ap[:, SG_GF + kc:SG_GF + kc + 1])
                P.dma("pool", yout3[:, :, t0:t0 + nt], h2f.ap[:, :, 0:nt], reads=[h2f.t])
    P.emit()
    return nc


_PROGS = {}


def _prog(key, *args, **kw):
    if key not in _PROGS:
        _PROGS[key] = build(*args, **kw)
    return _PROGS[key]


def kernel(**inp):
    inp = {k: np.asarray(v) for k, v in inp.items()}
    x, ctx = inp["x"], inp["ctx"]
    B = x.shape[0]
    cores = [(b, h) for b in range(B) for h in range(2)]
    consts = [make_consts(h) for h in range(2)]
    tabs = [make_tabs(h) for h in range(2)]
    w0 = prep_layer_weights(inp, 0)
    w1 = prep_layer_weights(inp, 1)
    maps = []
    for (b, h) in cores:
        xo = x[b, h * NOWN:(h + 1) * NOWN].T
        xt = x[b, (1 - h) * NOWN:(2 - h) * NOWN].T
        xall = np.ascontiguousarray(np.concatenate([xo, xt, ctx[b].T], axis=1))
        maps.append(dict(xall=xall, tabs=tabs[h], consts=consts[h], sg=make_small_global(inp, b, h),
                         sm0=w0[2], wall0=w0[0], we0=w0[1], sm1=w1[2], wall1=w1[0], we1=w1[1]))
    nc = _prog("fused", [0, 1], [True, False], True)
    r = run_bass_kernel_spmd(nc, maps, core_ids=list(range(len(cores)))).results
    out = np.zeros((B, 2 * NOWN, D), np.float32)
    for i, (b, h) in enumerate(cores):
        out[b, h * NOWN:(h + 1) * NOWN] = np.asarray(r[i]["yout"]).T
    return out
```

```python
import numpy as np
import concourse.bass as bass
import concourse.mybir as mybir
from concourse.bass_utils import run_bass_kernel_spmd

F32 = mybir.dt.float32
BF16 = mybir.dt.bfloat16
AF = mybir.ActivationFunctionType
ALU = mybir.AluOpType
AX = mybir.AxisListType

NOWN, NOTH, NCTX = 2048, 2048, 256
NALL = NOWN + NOTH + NCTX
NQ = NOWN + NCTX
D = 1024
EPS = 1e-6
NSLOT = 24
SKIP = set()


class T:
    __slots__ = ("name", "w", "r", "psum")

    def __init__(self, name="", psum=False):
        self.name = name
        self.w = None
        self.r = []
        self.psum = psum


class Op:
    __slots__ = ("eng", "fn", "deps", "id", "is_dma", "slot", "val", "marked", "semval", "cc")

    def __init__(self, eng, fn, is_dma):
        self.eng = eng
        self.fn = fn
        self.deps = []
        self.is_dma = is_dma
        self.slot = None
        self.val = None
        self.marked = False
        self.semval = None
        self.cc = False


class Prog:
    ENGS = ("pe", "act", "dve", "pool", "sp")

    def __init__(self, nc):
        self.nc = nc
        self.ops = []
        self.streams = {e: [] for e in self.ENGS}
        self.ndma = {e: 0 for e in self.ENGS}
        self.dmas = {e: [] for e in self.ENGS}

    def add(self, eng, fn, reads=(), writes=(), dma=False, extra=(), cc=False):
        op = Op(eng, fn, dma or cc)
        op.cc = cc
        op.id = len(self.ops)
        deps = {}
        for t in reads:
            if t.w is not None:
                deps[t.w.id] = t.w
            if t.psum:
                for r in t.r:
                    if r.eng != eng:
                        deps[r.id] = r
        for t in writes:
            if t.w is not None:
                deps[t.w.id] = t.w
            for r in t.r:
                deps[r.id] = r
        for d in extra:
            deps[d.id] = d
        op.deps = list(deps.values())
        for t in reads:
            if not dma:
                t.r = [r for r in t.r if r.is_dma or r.eng != eng]
            t.r.append(op)
        for t in writes:
            t.w = op
            t.r = []
        if cc:
            self.ncc = getattr(self, "ncc", 0) + 1
            op.slot = ("cc", self.ncc - 1)
            op.val = 1
            self.dmas[eng].append(op)
        elif dma:
            i = self.ndma[eng]
            self.ndma[eng] += 1
            op.slot = i % NSLOT
            op.val = 16 * (i // NSLOT + 1)
            self.dmas[eng].append(op)
        self.ops.append(op)
        self.streams[eng].append(op)
        return op

    def barrier(self):
        lasts = []
        for e in self.ENGS:
            if self.streams[e]:
                lasts.append(self.streams[e][-1])
            lasts.extend(self.dmas[e][-NSLOT:])
        for e in self.ENGS:
            self.add(e, lambda eng: eng.nop(), extra=lasts)

    def dma(self, q, out, in_, reads=(), writes=()):
        return self.add(q, lambda e: e.dma_start(out=out, in_=in_), reads, writes, dma=True)

    def mm(self, out, lhsT, rhs, start, stop, reads=(), writes=()):
        return self.add("pe", lambda e: e.matmul(out, lhsT, rhs, start=start, stop=stop), reads, writes)

    def transpose(self, out, in_, ident, reads=(), writes=()):
        return self.add("pe", lambda e: e.transpose(out, in_, ident), reads, writes)

    def act(self, out, in_, func, reads=(), writes=(), bias=None, scale=None):
        kw = {}
        if bias is not None:
            kw["bias"] = bias
        if scale is not None:
            kw["scale"] = scale
        return self.add("act", lambda e: e.activation(out, in_, func, **kw), reads, writes)

    def tt(self, eng, out, in0, in1, op, reads=(), writes=()):
        return self.add(eng, lambda e: e.tensor_tensor(out, in0, in1, op), reads, writes)

    def ts(self, eng, out, in0, s1, s2, op0, op1=None, reads=(), writes=()):
        if op1 is None:
            return self.add(eng, lambda e: e.tensor_scalar(out=out, in0=in0, scalar1=s1, scalar2=None, op0=op0),
                            reads, writes)
        return self.add(eng, lambda e: e.tensor_scalar(out=out, in0=in0, scalar1=s1, scalar2=s2, op0=op0, op1=op1),
                        reads, writes)

    def stt(self, out, in0, scalar, in1, op0, op1, reads=(), writes=()):
        return self.add("dve", lambda e: e.scalar_tensor_tensor(out=out, in0=in0, scalar=scalar, in1=in1,
                                                                op0=op0, op1=op1), reads, writes)

    def copy(self, eng, out, in_, reads=(), writes=()):
        if eng == "act":
            return self.add("act", lambda e: e.copy(out, in_), reads, writes)
        return self.add(eng, lambda e: e.tensor_copy(out, in_), reads, writes)

    def emit(self):
        nc = self.nc
        for op in self.ops:
            for d in op.deps:
                if d.is_dma:
                    continue
                if d.eng == op.eng and not op.is_dma and d.eng == "pe":
                    continue
                d.marked = True
        cnt = {e: 0 for e in self.ENGS}
        for op in self.ops:
            if op.marked and not op.is_dma:
                cnt[op.eng] += 1
                op.semval = cnt[op.eng]
        sems = {e: nc.alloc_semaphore(f"s_{e}") for e in self.ENGS}
        dsems = {e: {i: nc.alloc_semaphore(f"d_{e}_{i}") for i in range(min(NSLOT, self.ndma[e]))}
                 for e in self.ENGS}
        for i in range(getattr(self, "ncc", 0)):
            dsems["pool"][("cc", i)] = nc.alloc_semaphore(f"cc_{i}")
        engobj = {"pe": nc.tensor, "act": nc.scalar, "dve": nc.vector, "pool": nc.gpsimd, "sp": nc.sync}
        with nc.Block() as block:
            def run(ename):
                eng = engobj[ename]
                waited = {}

                def wait(key, sem, val):
                    if waited.get(key, 0) >= val:
                        return
                    waited[key] = val
                    eng.wait_ge(sem, val)

                for op in self.streams[ename]:
                    for d in op.deps:
                        if d.is_dma:
                            wait(("d", d.eng, d.slot), dsems[d.eng][d.slot], d.val)
                        else:
                            if d.eng == ename and not op.is_dma and ename == "pe":
                                continue
                            wait(("c", d.eng), sems[d.eng], d.semval)
                    if op.cc:
                        ins = op.fn(eng)
                        ins.then_inc(dsems[ename][op.slot], 1)
                    elif op.is_dma:
                        if op.val > 16:
                            wait(("d", ename, op.slot), dsems[ename][op.slot], op.val - 16)
                        ins = op.fn(eng)
                        ins.then_inc(dsems[ename][op.slot], 16)
                    else:
                        ins = op.fn(eng)
                        if op.marked:
                            ins.then_inc(sems[ename], 1)
                n = self.ndma[ename]
                for i in range(max(0, n - NSLOT), n):
                    wait(("d", ename, i % NSLOT), dsems[ename][i % NSLOT], 16 * (i // NSLOT + 1))

            @block.tensor
            def _(e):
                run("pe")

            @block.scalar
            def _(e):
                run("act")

            @block.vector
            def _(e):
                run("dve")

            @block.gpsimd
            def _(e):
                run("pool")

            @block.sync
            def _(e):
                run("sp")


class Buf:
    __slots__ = ("ap", "t")

    def __init__(self, ap, name="", psum=False):
        self.ap = ap
        self.t = T(name, psum)


class Arena:
    def __init__(self, nc, nbytes):
        self.t = nc.alloc_sbuf_tensor("arena", [128, nbytes // 2], BF16)
        self.nbytes = nbytes
        self.off = 0

    def alloc(self, shape, dtype, name=""):
        n = 1
        for s in shape[1:]:
            n *= s
        es = 4 if dtype == F32 else 2
        nb = (n * es + 63) // 64 * 64
        assert self.off + nb <= self.nbytes, f"arena overflow {name} {self.off + nb}"
        v = self.t[0:shape[0], self.off // 2:(self.off + n * es) // 2]
        if dtype == F32:
            v = v.bitcast(F32)
        if len(shape) == 3:
            v = v.rearrange("p (a b) -> p a b", a=shape[1])
        self.off += nb
        return Buf(v, name)


SPL = [512, 512, 1024, 1024, 1024, 128, 128, 1024, 256, 256, 1024, 1024, 1024]
OFF = np.concatenate([[0], np.cumsum(SPL)]).astype(int)
O_QR, O_KR, O_VR, O_UR, O_QS, O_KS, O_VS, O_QA, O_KA, O_VA, O_AR, O_AS, O_AA = [int(v) for v in OFF[:13]]


def _perm128():
    f = np.arange(128)
    return np.where(f % 64 < 32, f + 32, f - 32)


def _perm64():
    f = np.arange(128)
    return np.where(f % 32 < 16, f + 16, f - 16)


def fm_units():
    u = []
    ar = np.arange(128)
    for h in range(4):
        u.append(dict(name=f"kr{h}", kind="rope128", cols=O_KR + h * 128 + ar, tok="all"))
    u.append(dict(name="ks", kind="rope64", cols=O_KS + ar, tok="all"))
    u.append(dict(name="ks2", kind="rope64", cols=O_KS + (ar + 64) % 128, tok="all"))
    for h in range(2):
        u.append(dict(name=f"ka{h}", kind="normk", cols=O_KA + h * 128 + ar, tok="all"))
    for h in range(4):
        u.append(dict(name=f"qr{h}", kind="rope128", cols=O_QR + h * 128 + ar, tok="q"))
    for c in range(8):
        u.append(dict(name=f"qs{c}", kind="rope64", cols=O_QS + c * 128 + ar, tok="q"))
    for h in range(8):
        u.append(dict(name=f"qa{h}", kind="normq", cols=O_QA + h * 128 + ar, tok="q"))
    for c in range(8):
        u.append(dict(name=f"ur{c}", kind="silu", cols=O_UR + c * 128 + ar, tok="q"))
    for nm, o in (("ar", O_AR), ("as", O_AS), ("aa", O_AA)):
        for c in range(8):
            u.append(dict(name=f"{nm}{c}", kind="sig", cols=o + c * 128 + ar, tok="q"))
    g, used = 0, 0
    for x in u:
        w = 256 if x["kind"] in ("rope128", "rope64", "normq", "normk") else 128
        if used + w > 512:
            g, used = g + 1, 0
        x["group"], x["c0"] = g, used
        x["c1"] = used + 128 if w == 256 else None
        used += w
    return u, g + 1


FM_UNITS, N_FM_GROUPS = fm_units()
TM_COLS = np.concatenate([O_VR + np.arange(1024), O_VS + np.arange(128), O_VA + np.arange(256)])
N_TM_GROUPS = 3
G_MOD = 0
G_FM = 12
G_TM = G_FM + N_FM_GROUPS
G_MERGE = G_TM + N_TM_GROUPS
N_GROUPS = G_MERGE + 8

SM_BMOD, SM_G1, SM_G2, SM_RET, SM_SINK, SM_GQ, SM_GK = 0, 48, 56, 64, 72, 88, 90
SM_L = 96
SG_C, SG_WR, SG_BR, SG_GF, SG_FLAG = 0, 16, 144, 160, 168
SG_N = 176
C_ID, C_DPOS, C_DNEG, C_MF, C_MB, C_IP1, C_IB, C_ML, C_MR, C_MLB, C_MRB = [i * 128 for i in range(11)]
C_PCF, C_PCB = 11 * 128, 11 * 128 + 1
C_SEL = 11 * 128 + 8
C_N = C_SEL + 2048


def _grp(w):
    n = w.shape[1] // 512
    return np.ascontiguousarray(w.reshape(8, 128, n, 512).transpose(2, 1, 0, 3))


def prep_layer_weights(inp, l):
    w_in = inp["w_in"][l]
    p128, p64 = _perm128(), _perm64()
    cols = np.zeros(N_FM_GROUPS * 512, dtype=np.int64)
    for u in FM_UNITS:
        base = u["group"] * 512
        cols[base + u["c0"]:base + u["c0"] + 128] = u["cols"]
        if u["c1"] is not None:
            pm = p64 if u["kind"] == "rope64" else p128
            cols[base + u["c1"]:base + u["c1"] + 128] = u["cols"][pm]
    tmc = np.concatenate([TM_COLS, np.zeros(1536 - 1408, dtype=np.int64)])
    wcat = np.concatenate([inp["w_mod"][l], w_in[:, cols], w_in[:, tmc], inp["w_br_ret"][l], inp["w_br_swa"][l],
                           inp["w_br_ga"][l], inp["w_out"][l]], axis=1)
    wall = _grp(wcat)
    assert wall.shape[0] == N_GROUPS
    wg = inp["w_gate"][l].reshape(16, 8, 128, 512).transpose(0, 2, 1, 3).reshape(16, 128, 4096)
    wu = inp["w_up"][l].reshape(16, 8, 128, 512).transpose(0, 2, 1, 3).reshape(16, 128, 4096)
    wd = inp["w_down"][l].reshape(16, 4, 128, 1024).transpose(0, 2, 1, 3).reshape(16, 128, 4096)
    we = np.ascontiguousarray(np.stack([wg, wu, wd], axis=1))
    sm = np.zeros((128, SM_L), np.float32)
    sm[:, SM_BMOD:SM_BMOD + 48] = inp["b_mod"][l].reshape(48, 128).T
    sm[:, SM_G1:SM_G1 + 8] = inp["g_norm1"][l].reshape(8, 128).T
    sm[:, SM_G2:SM_G2 + 8] = inp["g_norm2"][l].reshape(8, 128).T
    sm[:, SM_RET:SM_RET + 8] = inp["ret_decay_logit"][l].reshape(1, 8)
    sm[:, SM_SINK:SM_SINK + 16] = inp["swa_sink"][l].reshape(1, 16)
    sm[:, SM_GQ] = inp["g_qnorm"][l]
    sm[:, SM_GQ + 1] = inp["g_qnorm"][l][p128]
    sm[:, SM_GK] = inp["g_knorm"][l]
    sm[:, SM_GK + 1] = inp["g_knorm"][l][p128]
    return wall, we, sm


def make_consts(half):
    c = np.zeros((128, C_N), np.float32)
    m = np.arange(128)[:, None].astype(np.float32)
    n = np.arange(128)[None, :].astype(np.float32)
    c[:, C_ID:C_ID + 128] = np.eye(128)
    c[:, C_DPOS:C_DPOS + 128] = np.maximum(n - m, 0)
    c[:, C_DNEG:C_DNEG + 128] = np.maximum(m - n, 0)
    c[:, C_MF:C_MF + 128] = (n >= m)
    c[:, C_MB:C_MB + 128] = (m > n)
    c[:, C_IP1:C_IP1 + 128] = n + 1 + 0 * m
    c[:, C_IB:C_IB + 128] = 128 - n + 0 * m
    c[:, C_ML:C_ML + 128] = (m >= n)
    c[:, C_MR:C_MR + 128] = (m <= n)
    c[:, C_MLB:C_MLB + 128] = (m >= n) * (1.0 if half == 1 else 0.0)
    c[:, C_MRB:C_MRB + 128] = (m <= n) * (1.0 if half == 0 else 0.0)
    c[:, C_PCF] = 127 - np.arange(128)
    c[:, C_PCB] = np.arange(128)
    for e in range(16):
        c[e, C_SEL + e * 128:C_SEL + (e + 1) * 128] = 1.0
    return c


def make_tabs(half):
    pos_own = half * NOWN + np.arange(NOWN)
    pos_oth = (1 - half) * NOWN + np.arange(NOTH)
    pos = np.concatenate([pos_own, pos_oth])
    row = (pos // 64).astype(np.float32)
    col = (pos % 64).astype(np.float32)
    tabs = np.zeros((4, 128, NALL), np.float32)
    tabs[0, :, 4096:] = 1.0
    tabs[2, :, 4096:] = 1.0
    for ti, hd in ((0, 128), (2, 64)):
        half_d, quarter = hd // 2, hd // 4
        freqs = (np.float32(10000.0) ** (-np.arange(quarter, dtype=np.float32) / np.float32(quarter))).astype(np.float32)
        for f in range(128):
            fl = f % hd
            a = fl // half_d
            j = fl % half_d
            p = row if a == 0 else col
            ang = (p * freqs[j % quarter]).astype(np.float32)
            tabs[ti, f, :4096] = np.cos(ang)
            tabs[ti + 1, f, :4096] = np.sin(ang) * (-1.0 if j < quarter else 1.0)
    return tabs


def make_small_global(inp, b, half):
    sg = np.zeros((128, SG_N), np.float32)
    cT = inp["c"][b].reshape(8, 128).T
    ccT = inp["c_ctx"].reshape(8, 128).T
    sg[:, SG_C:SG_C + 16:2] = cT
    sg[:, SG_C + 1:SG_C + 16:2] = ccT
    sg[:, SG_WR:SG_WR + 128] = inp["w_router"].reshape(8, 128, 16).transpose(1, 0, 2).reshape(128, 128)
    sg[:, SG_BR:SG_BR + 16] = inp["b_router"].reshape(1, 16)
    sg[:, SG_GF:SG_GF + 8] = inp["g_final"].reshape(8, 128).T
    sg[:, SG_FLAG] = 1.0 if half == 0 else 0.0
    sg[:, SG_FLAG + 1] = 1.0 if half == 1 else 0.0
    return sg


TOK_TILES_ALL = [(i * 512, 512, 0) for i in range(8)] + [(4096, 256, 1)]
TOK_TILES_OWN = [(i * 512, 512, 0) for i in range(4)]
CTX_TILE = (4096, 256, 1)


def qpos(t0):
    return t0 if t0 < NOWN else t0 - NOTH


def build(layers, need_ctx_flags, final, dbg=(), stop=99):
    nc = bass.Bass("TRN2", target_bir_lowering=False)
    P = Prog(nc)
    nl = len(layers)

    def din(name, shape, dt=F32):
        return nc.dram_tensor(name, list(shape), dt, kind="ExternalInput").ap()

    def dscr(name, shape, dt=BF16):
        kind = "ExternalOutput" if name in dbg else "Internal"
        return nc.dram_tensor(name, list(shape), dt, kind=kind).ap()

    xall = din("xall", [D, NALL])
    tabs = din("tabs", [4, 128, NALL])
    consts_d = din("consts", [128, C_N])
    sg_d = din("sg", [128, SG_N])
    sm_d = [din(f"sm{l}", [128, SM_L]) for l in range(nl)]
    wall_d = [din(f"wall{l}", [N_GROUPS, 128, 8, 512]) for l in range(nl)]
    we_d = [din(f"we{l}", [16, 3, 128, 4096]) for l in range(nl)]
    if final:
        yout = nc.dram_tensor("yout", [D, NOWN], F32, kind="ExternalOutput").ap()
    else:
        xout = nc.dram_tensor("xout", [D, NQ], F32, kind="ExternalOutput").ap()

    xs_d = dscr("xs", [D, NALL], F32)
    fm_d = {u["name"]: dscr("fm_" + u["name"], [128, NALL]) for u in FM_UNITS}
    vall_d = dscr("vall", [NALL, 1536])
    obr_d = [dscr(f"obr{i}", [D, NQ]) for i in range(3)]
    web_d = dscr("web", [16, 3, 128, 4096])
    T_xs = {}

    def txs(t0):
        return T_xs.setdefault(t0, T(f"xs{t0}"))
    T_fm = {}

    def tfm(name, t0):
        return T_fm.setdefault((name, t0), T(f"fm{name}{t0}"))
    T_vall = {}

    def tvall(sub):
        return T_vall.setdefault(sub, T(f"vall{sub}"))
    T_obr = {}

    def tobr(i, t0):
        return T_obr.setdefault((i, t0), T(f"obr{i}_{t0}"))
    T_web = {}

    def tweb(e, k):
        return T_web.setdefault((e, k), T(f"web{e}_{k}"))

    xs3 = xs_d.rearrange("(kc p) t -> p kc t", p=128)
    if nl > 1:
        xch_src = [nc.dram_tensor(f"xch_src{i}", [D, 512], F32, kind="Internal").ap() for i in range(4)]
        xch_dst = [nc.dram_tensor(f"xch_dst{i}", [2 * D, 512], F32, kind="Internal").ap() for i in range(4)]
        T_xsrc, T_xdst = [T("xsrc") for i in range(4)], [T("xdst") for i in range(4)]
    xall3 = xall.rearrange("(kc p) t -> p kc t", p=128)

    A = Arena(nc, 206 * 1024)
    cst = A.alloc([128, C_N], F32, "cst")
    sg = A.alloc([128, SG_N], F32, "sg")
    ones_bf = A.alloc([128, 128], BF16, "ones")
    id_bf = A.alloc([128, 128], BF16, "idbf")
    mk_bf = A.alloc([128, 4, 512], BF16, "mk")
    PERS_END = None
    ps = [Buf(nc.alloc_psum_tensor(f"ps{i}", [128, 512], F32)[:], f"ps{i}", True) for i in range(8)]

    P.dma("sp", cst.ap, consts_d, writes=[cst.t])
    P.dma("sp", sg.ap, sg_d, writes=[sg.t])
    P.add("dve", lambda e: e.memset(ones_bf.ap, 1.0), writes=[ones_bf.t])
    P.copy("dve", id_bf.ap, cst.ap[:, C_ID:C_ID + 128], reads=[cst.t], writes=[id_bf.t])
    for i, co in enumerate((C_ML, C_MR, C_MLB, C_MRB)):
        for r in range(4):
            P.copy("dve", mk_bf.ap[:, i, r * 128:(r + 1) * 128], cst.ap[:, co:co + 128], reads=[cst.t],
                   writes=[mk_bf.t])
    for (t0, nt, _) in TOK_TILES_ALL:
        P.dma("sp", xs_d[:, t0:t0 + nt], xall[:, t0:t0 + nt], writes=[txs(t0)])
    PERS_END = A.off

    def rsqrt_from_psum(dst, src_ps, n, rd, wr):
        P.ts("dve", dst, src_ps, 1.0 / n, EPS, ALU.mult, ALU.add, reads=rd, writes=wr)
        P.act(dst, dst, AF.Ln, reads=wr, writes=wr)
        P.act(dst, dst, AF.Exp, reads=wr, writes=wr, scale=-0.5)

    psi = [0]

    def ps_next(k=8):
        psi[0] = (psi[0] + 1) % k
        return ps[psi[0]]

    for li, l in enumerate(layers):
        need_ctx = need_ctx_flags[li]
        last = (li == nl - 1)
        qtiles = TOK_TILES_OWN + ([CTX_TILE] if need_ctx else [])
        P.barrier()
        A.off = PERS_END
        sm = A.alloc([128, SM_L], F32, "sm")
        modT = A.alloc([128, 48, 2], F32, "modT")
        A1 = A.alloc([128, 8, 2], F32, "A1")
        A2 = A.alloc([128, 8, 2], F32, "A2")
        silc = A.alloc([128, 16], F32, "silc")
        lg = A.alloc([128, 8], F32, "lg")
        sinkx = A.alloc([128, 16], F32, "sinkx")
        P.dma("sp", sm.ap, sm_d[li], writes=[sm.t])
        P.act(silc.ap, sg.ap[:, SG_C:SG_C + 16], AF.Silu, reads=[sg.t], writes=[silc.t])
        silc3 = silc.ap.rearrange("p (k j) -> p k j", j=2)
        P.act(lg.ap, sm.ap[:, SM_RET:SM_RET + 8], AF.Exp, reads=[sm.t], writes=[lg.t], scale=-1.0)
        P.ts("dve", lg.ap, lg.ap, 1.0, None, ALU.add, reads=[lg.t], writes=[lg.t])
        P.act(lg.ap, lg.ap, AF.Ln, reads=[lg.t], writes=[lg.t])
        P.ts("dve", lg.ap, lg.ap, -1.0, None, ALU.mult, reads=[lg.t], writes=[lg.t])
        P.act(sinkx.ap, sm.ap[:, SM_SINK:SM_SINK + 16], AF.Exp, reads=[sm.t], writes=[sinkx.t])
        PH0_END = A.off
        wst = [A.alloc([128, 8, 512], F32, f"wst{i}") for i in range(2)]
        for g in range(12):
            w = wst[g % 2]
            P.dma("sp", w.ap, wall_d[li][G_MOD + g], writes=[w.t])
            for j in range(4):
                idx = g * 4 + j
                pb = ps_next()
                for kc in range(8):
                    P.mm(pb.ap[:, 0:2], w.ap[:, kc, j * 128:(j + 1) * 128], silc3[:, kc, :], kc == 0, kc == 7,
                         reads=[w.t, silc.t], writes=[pb.t])
                P.ts("dve", modT.ap[:, idx, :], pb.ap[:, 0:2], sm.ap[:, SM_BMOD + idx:SM_BMOD + idx + 1], None,
                     ALU.add, reads=[pb.t, sm.t], writes=[modT.t])
        for j in range(2):
            P.stt(A1.ap[:, :, j], modT.ap[:, 8:16, j], 1.0, sm.ap[:, SM_G1:SM_G1 + 8], ALU.add, ALU.mult,
                  reads=[modT.t, sm.t], writes=[A1.t])
            P.stt(A2.ap[:, :, j], modT.ap[:, 32:40, j], 1.0, sm.ap[:, SM_G2:SM_G2 + 8], ALU.add, ALU.mult,
                  reads=[modT.t, sm.t], writes=[A2.t])

        def SH1(kc, j): return modT.ap[:, 0 + kc, j:j + 1]
        def GT1(kc, j): return modT.ap[:, 16 + kc, j:j + 1]
        def SH2(kc, j): return modT.ap[:, 24 + kc, j:j + 1]
        def GT2(kc, j): return modT.ap[:, 40 + kc, j:j + 1]

        P.barrier()
        A.off = PH0_END
        hT = A.alloc([128, 8, NALL], BF16, "hT")
        hts = {t0: T(f"hT{t0}") for (t0, _, _) in TOK_TILES_ALL}
        xt = [A.alloc([128, 8, 512], F32, f"xt{i}") for i in range(2)]
        sq = A.alloc([128, 8, 512], BF16, "sq")
        rstd = A.alloc([128, 512], F32, "rstd")
        tmp = [A.alloc([128, 512], F32, f"tmp{i}") for i in range(2)]
        PH1_END = A.off

        def norm_tiles(tiles, Aco, SHf, out_fn, src3=xs3):
            for i, (t0, nt, mj) in enumerate(tiles):
                x_ = xt[i % 2]
                P.dma("sp", x_.ap[:, :, 0:nt], src3[:, :, t0:t0 + nt], reads=[txs(t0)], writes=[x_.t])
                P.act(sq.ap[:, :, 0:nt], x_.ap[:, :, 0:nt], AF.Square, reads=[x_.t], writes=[sq.t])
                pb = ps_next()
                for kc in range(8):
                    P.mm(pb.ap[:, 0:nt], ones_bf.ap, sq.ap[:, kc, 0:nt], kc == 0, kc == 7,
                         reads=[ones_bf.t, sq.t], writes=[pb.t])
                rsqrt_from_psum(rstd.ap[:, 0:nt], pb.ap[:, 0:nt], 1024.0, [pb.t], [rstd.t])
                for kc in range(8):
                    tm_ = tmp[kc % 2]
                    P.tt("dve" if kc % 2 == 0 else "pool", tm_.ap[:, 0:nt], x_.ap[:, kc, 0:nt], rstd.ap[:, 0:nt],
                         ALU.mult, reads=[x_.t, rstd.t], writes=[tm_.t])
                    out_fn(kc, t0, nt, mj, tm_, Aco.ap[:, kc, mj:mj + 1], SHf(kc, mj))

        def out_h1(kc, t0, nt, mj, tm_, a_, b_):
            P.act(hT.ap[:, kc, t0:t0 + nt], tm_.ap[:, 0:nt], AF.Identity, reads=[tm_.t, A1.t, modT.t],
                  writes=[hts[t0]], bias=b_, scale=a_)

        norm_tiles(TOK_TILES_ALL, A1, SH1, out_h1)

        A.off = PH1_END
        wbf = [A.alloc([128, 8, 512], BF16, f"wbf{i}") for i in range(3)]
        tabC = [A.alloc([128, 512], F32, f"tabC{i}") for i in range(2)]
        tabS = [A.alloc([128, 512], F32, f"tabS{i}") for i in range(2)]
        stage = [A.alloc([128, 512], BF16, f"stage{i}") for i in range(3)]
        t1b = [A.alloc([128, 512], F32, f"t1b{i}") for i in range(2)]
        t2b = [A.alloc([128, 512], F32, f"t2b{i}") for i in range(2)]
        sqb = A.alloc([128, 512], BF16, "sqb")
        rs2 = A.alloc([128, 512], F32, "rs2")
        cnt = [0]

        def load_group(gi):
            wb = wbf[gi % 3]
            P.dma("pool", wb.ap, wall_d[li][gi], writes=[wb.t])
            return wb

        nxt = load_group(G_FM)
        for gi in range(N_FM_GROUPS):
            wb = nxt
            nxt = load_group(G_FM + gi + 1)
            for u in [x for x in FM_UNITS if x["group"] == gi]:
                tiles = TOK_TILES_ALL if u["tok"] == "all" else qtiles
                kind = u["kind"]
                dual = u["c1"] is not None
                tsel = 2 if kind == "rope64" else 0
                for (t0, nt, mj) in tiles:
                    k = cnt[0]
                    cnt[0] += 1
                    pa = ps_next()
                    for kc in range(8):
                        P.mm(pa.ap[:, 0:nt], wb.ap[:, kc, u["c0"]:u["c0"] + 128], hT.ap[:, kc, t0:t0 + nt],
                             kc == 0, kc == 7, reads=[wb.t, hts[t0]], writes=[pa.t])
                    if dual:
                        pbk = ps_next()
                        for kc in range(8):
                            P.mm(pbk.ap[:, 0:nt], wb.ap[:, kc, u["c1"]:u["c1"] + 128], hT.ap[:, kc, t0:t0 + nt],
                                 kc == 0, kc == 7, reads=[wb.t, hts[t0]], writes=[pbk.t])
                        tc_, ts_ = tabC[k % 2], tabS[k % 2]
                        P.dma("sp", tc_.ap[:, 0:nt], tabs[tsel, :, t0:t0 + nt], writes=[tc_.t])
                        P.dma("sp", ts_.ap[:, 0:nt], tabs[tsel + 1, :, t0:t0 + nt], writes=[ts_.t])
                    st = stage[k % 3]
                    o_ = st.ap[:, 0:nt]
                    if kind in ("rope128", "rope64"):
                        a_, b_ = t1b[k % 2], t2b[k % 2]
                        P.tt("dve", a_.ap[:, 0:nt], pa.ap[:, 0:nt], tc_.ap[:, 0:nt], ALU.mult,
                             reads=[pa.t, tc_.t], writes=[a_.t])
                        P.tt("dve", b_.ap[:, 0:nt], pbk.ap[:, 0:nt], ts_.ap[:, 0:nt], ALU.mult,
                             reads=[pbk.t, ts_.t], writes=[b_.t])
                        P.tt("pool", o_, a_.ap[:, 0:nt], b_.ap[:, 0:nt], ALU.add, reads=[a_.t, b_.t], writes=[st.t])
                    elif kind in ("normq", "normk"):
                        gco = SM_GQ if kind == "normq" else SM_GK
                        a_, b_ = t1b[k % 2], t2b[k % 2]
                        P.act(sqb.ap[:, 0:nt], pa.ap[:, 0:nt], AF.Square, reads=[pa.t], writes=[sqb.t])
                        pc = ps_next()
                        P.mm(pc.ap[:, 0:nt], ones_bf.ap, sqb.ap[:, 0:nt], True, True, reads=[ones_bf.t, sqb.t],
                             writes=[pc.t])
                        rsqrt_from_psum(rs2.ap[:, 0:nt], pc.ap[:, 0:nt], 128.0, [pc.t], [rs2.t])
                        P.stt(a_.ap[:, 0:nt], pa.ap[:, 0:nt], sm.ap[:, gco:gco + 1], tc_.ap[:, 0:nt], ALU.mult,
                              ALU.mult, reads=[pa.t, tc_.t, sm.t], writes=[a_.t])
                        P.stt(b_.ap[:, 0:nt], pbk.ap[:, 0:nt], sm.ap[:, gco + 1:gco + 2], ts_.ap[:, 0:nt], ALU.mult,
                              ALU.mult, reads=[pbk.t, ts_.t, sm.t], writes=[b_.t])
                        P.tt("pool", a_.ap[:, 0:nt], a_.ap[:, 0:nt], b_.ap[:, 0:nt], ALU.add, reads=[a_.t, b_.t],
                             writes=[a_.t])
                        P.tt("pool", o_, a_.ap[:, 0:nt], rs2.ap[:, 0:nt], ALU.mult, reads=[a_.t, rs2.t],
                             writes=[st.t])
                    elif kind == "silu":
                        P.act(o_, pa.ap[:, 0:nt], AF.Silu, reads=[pa.t], writes=[st.t])
                    else:
                        P.act(o_, pa.ap[:, 0:nt], AF.Sigmoid, reads=[pa.t], writes=[st.t])
                    P.dma("pool", fm_d[u["name"]][:, t0:t0 + nt], o_, reads=[st.t], writes=[tfm(u["name"], t0)])
        for gi in range(N_TM_GROUPS):
            wb = nxt
            if gi + 1 < N_TM_GROUPS:
                nxt = load_group(G_TM + gi + 1)
            for sub in range(NALL // 128):
                t0 = sub * 128
                tile0 = (t0 // 512) * 512
                k = cnt[0]
                cnt[0] += 1
                pa = ps_next()
                for kc in range(8):
                    P.mm(pa.ap, hT.ap[:, kc, t0:t0 + 128], wb.ap[:, kc, :], kc == 0, kc == 7,
                         reads=[wb.t, hts[tile0]], writes=[pa.t])
                st = stage[k % 3]
                P.copy("act" if k % 2 == 0 else "dve", st.ap, pa.ap, reads=[pa.t], writes=[st.t])
                P.dma("pool", vall_d[t0:t0 + 128, gi * 512:(gi + 1) * 512], st.ap, reads=[st.t],
                      writes=[tvall(sub)])
        P.barrier()
        if stop <= 2:
            continue
        A.off = PH0_END
        KSCALE = 128.0 ** -0.5
        KT = A.alloc([128, NALL], BF16, "KT")
        QT = A.alloc([128, NALL], BF16, "QT")
        Vr = A.alloc([128, 34, 256], BF16, "Vr")
        Ur = A.alloc([128, 2, NALL], BF16, "Ur")
        Kf = A.alloc([128, 34, 128], BF16, "Kf")
        Kb = A.alloc([128, 34, 128], BF16, "Kb")
        SFb = A.alloc([128, 18, 256], BF16, "SFb")
        SBb = A.alloc([128, 18, 256], BF16, "SBb")
        Dm = A.alloc([128, 512], BF16, "Dm")
        qdf = A.alloc([128, 512], F32, "qdf")
        qdb = A.alloc([128, 512], F32, "qdb")
        e1 = A.alloc([128, 128], F32, "e1")
        e2 = A.alloc([128, 128], F32, "e2")
        kd = A.alloc([128, 4], F32, "kd")
        S = A.alloc([128, 256], F32, "S")
        S0f = A.alloc([128, 256], F32, "S0f")
        S0b = A.alloc([128, 256], F32, "S0b")
        Pm = A.alloc([128, 512], BF16, "Pm")
        Qf = A.alloc([128, 512], BF16, "Qf")
        Qb = A.alloc([128, 512], BF16, "Qb")
        sq2 = A.alloc([128, 2, 512], BF16, "sq2")
        rs3 = A.alloc([128, 512], F32, "rs3")
        to_ = [A.alloc([128, 512], F32, f"to{i}") for i in range(2)]
        stg = [A.alloc([128, 512], BF16, f"stg{i}") for i in range(2)]
        flg = sg.ap[:, SG_FLAG:SG_FLAG + 2]
        for h in range(4):
            lgf, lgb = lg.ap[:, h:h + 1], lg.ap[:, 4 + h:5 + h]
            P.act(e1.ap, cst.ap[:, C_DPOS:C_DPOS + 128], AF.Exp, reads=[cst.t, lg.t], writes=[e1.t], scale=lgf)
            P.tt("dve", e1.ap, e1.ap, cst.ap[:, C_MF:C_MF + 128], ALU.mult, reads=[e1.t, cst.t], writes=[e1.t])
            P.act(e2.ap, cst.ap[:, C_DNEG:C_DNEG + 128], AF.Exp, reads=[cst.t, lg.t], writes=[e2.t], scale=lgb)
            P.tt("dve", e2.ap, e2.ap, cst.ap[:, C_MB:C_MB + 128], ALU.mult, reads=[e2.t, cst.t], writes=[e2.t])
            P.tt("dve", e1.ap, e1.ap, e2.ap, ALU.add, reads=[e1.t, e2.t], writes=[e1.t])
            for r in range(4):
                P.ts("dve", Dm.ap[:, r * 128:(r + 1) * 128], e1.ap, KSCALE, None, ALU.mult, reads=[e1.t],
                     writes=[Dm.t])
                P.act(qdf.ap[:, r * 128:(r + 1) * 128], cst.ap[:, C_IP1:C_IP1 + 128], AF.Exp, reads=[cst.t, lg.t],
                      writes=[qdf.t], scale=lgf)
                P.act(qdb.ap[:, r * 128:(r + 1) * 128], cst.ap[:, C_IB:C_IB + 128], AF.Exp, reads=[cst.t, lg.t],
                      writes=[qdb.t], scale=lgb)
            P.act(kd.ap[:, 0:1], cst.ap[:, C_PCF:C_PCF + 1], AF.Exp, reads=[cst.t, lg.t], writes=[kd.t], scale=lgf)
            P.act(kd.ap[:, 1:2], cst.ap[:, C_PCB:C_PCB + 1], AF.Exp, reads=[cst.t, lg.t], writes=[kd.t], scale=lgb)
            P.ts("dve", kd.ap[:, 0:2], kd.ap[:, 0:2], KSCALE, None, ALU.mult, reads=[kd.t], writes=[kd.t])
            P.act(kd.ap[:, 2:3], lgf, AF.Exp, reads=[lg.t], writes=[kd.t], scale=128.0)
            P.act(kd.ap[:, 3:4], lgb, AF.Exp, reads=[lg.t], writes=[kd.t], scale=128.0)
            P.dma("sp", KT.ap, fm_d[f"kr{h}"], writes=[KT.t])
            P.dma("sp", QT.ap[:, 0:NOWN], fm_d[f"qr{h}"][:, 0:NOWN], writes=[QT.t])
            if need_ctx:
                P.dma("sp", QT.ap[:, 4096:NALL], fm_d[f"qr{h}"][:, 4096:NALL], writes=[QT.t])
            vsrc = vall_d[:, h * 256:(h + 1) * 256].rearrange("(t p) c -> p t c", p=128)
            for tq in range(0, 34, 4):
                P.dma("sp", Vr.ap[:, tq:min(tq + 4, 34), :], vsrc[:, tq:min(tq + 4, 34), :], writes=[Vr.t])
            for j in range(2):
                P.dma("sp", Ur.ap[:, j, 0:NOWN], fm_d[f"ur{2 * h + j}"][:, 0:NOWN], writes=[Ur.t])
                if need_ctx:
                    P.dma("sp", Ur.ap[:, j, 4096:NALL], fm_d[f"ur{2 * h + j}"][:, 4096:NALL], writes=[Ur.t])
            for c0 in (range(0, 32 if 'trlast' in SKIP else 34, 4) if 'tr' not in SKIP else []):
                n = min(4, 34 - c0)
                pb_ = ps_next()
                for c in range(n):
                    P.mm(pb_.ap[:, c * 128:(c + 1) * 128], KT.ap[:, (c0 + c) * 128:(c0 + c + 1) * 128], id_bf.ap,
                         True, True, reads=[KT.t, id_bf.t], writes=[pb_.t])
                P.ts("dve", Kf.ap[:, c0:c0 + n, :], pb_.ap[:, 0:n * 128].rearrange("p (a b) -> p a b", a=n),
                     kd.ap[:, 0:1], None, ALU.mult, reads=[pb_.t, kd.t], writes=[Kf.t])
                P.act(Kb.ap[:, c0:c0 + n, :], pb_.ap[:, 0:n * 128].rearrange("p (a b) -> p a b", a=n), AF.Identity,
                      reads=[pb_.t, kd.t], writes=[Kb.t], scale=kd.ap[:, 1:2])

            def upd(Kx, c, cdcol, first):
                if 'upd' in SKIP:
                    return
                pb_ = ps_next()
                P.mm(pb_.ap[:, 0:256], Kx.ap[:, c, :], Vr.ap[:, c, :], True, True, reads=[Kx.t, Vr.t],
                     writes=[pb_.t])
                if first:
                    P.copy("dve", S.ap, pb_.ap[:, 0:256], reads=[pb_.t], writes=[S.t])
                else:
                    P.stt(S.ap, S.ap, kd.ap[:, cdcol:cdcol + 1], pb_.ap[:, 0:256], ALU.mult, ALU.add,
                          reads=[S.t, kd.t, pb_.t], writes=[S.t])

            def snap(dst, idx):
                if 'upd' in SKIP:
                    return
                P.copy("act", dst.ap[:, idx, :], S.ap, reads=[S.t], writes=[dst.t])

            upd(Kf, 32, 2, True)
            snap(SFb, 17)
            upd(Kf, 33, 2, False)
            P.copy("dve", S0f.ap, S.ap, reads=[S.t], writes=[S0f.t])
            for c in range(16, 32):
                upd(Kf, c, 2, False)
            P.tt("dve", S.ap, S.ap, S0f.ap, ALU.subtract, reads=[S.t, S0f.t], writes=[S.t])
            P.stt(S.ap, S.ap, flg[:, 1:2], S0f.ap, ALU.mult, ALU.add, reads=[S.t, S0f.t, sg.t], writes=[S.t])
            for c in range(0, 16):
                snap(SFb, c)
                if c < 15:
                    upd(Kf, c, 2, False)
            upd(Kb, 33, 3, True)
            snap(SBb, 16)
            upd(Kb, 32, 3, False)
            P.copy("dve", S0b.ap, S.ap, reads=[S.t], writes=[S0b.t])
            for c in range(31, 15, -1):
                upd(Kb, c, 3, False)
            P.tt("dve", S.ap, S.ap, S0b.ap, ALU.subtract, reads=[S.t, S0b.t], writes=[S.t])
            P.stt(S.ap, S.ap, flg[:, 0:1], S0b.ap, ALU.mult, ALU.add, reads=[S.t, S0b.t, sg.t], writes=[S.t])
            for c in range(15, -1, -1):
                snap(SBb, c)
                if c > 0:
                    upd(Kb, c, 3, False)
            for (t0, nt, mj) in (qtiles if 'out' not in SKIP else []):
                ncx = nt // 128
                pS = ps_next()
                for j in range(ncx):
                    sl = slice(t0 + j * 128, t0 + (j + 1) * 128)
                    P.mm(pS.ap[:, j * 128:(j + 1) * 128], KT.ap[:, sl], QT.ap[:, sl], True, True,
                         reads=[KT.t, QT.t], writes=[pS.t])
                P.tt("dve", Pm.ap[:, 0:nt], pS.ap[:, 0:nt], Dm.ap[:, 0:nt], ALU.mult, reads=[pS.t, Dm.t],
                     writes=[Pm.t])
                P.tt("pool", Qf.ap[:, 0:nt], QT.ap[:, t0:t0 + nt], qdf.ap[:, 0:nt], ALU.mult, reads=[QT.t, qdf.t],
                     writes=[Qf.t])
                P.tt("pool", Qb.ap[:, 0:nt], QT.ap[:, t0:t0 + nt], qdb.ap[:, 0:nt], ALU.mult, reads=[QT.t, qdb.t],
                     writes=[Qb.t])
                pO = [ps_next(), ps_next()]
                for dj in range(2):
                    dsl = slice(dj * 128, (dj + 1) * 128)
                    for j in range(ncx):
                        c = t0 // 128 + j
                        csl = slice(j * 128, (j + 1) * 128)
                        if c < 16:
                            sf, sb = c, c
                        elif c == 32:
                            sf, sb = None, 16
                        else:
                            sf, sb = 17, None
                        terms = [(Vr.ap[:, c, dsl], Pm.ap[:, csl], [Vr.t, Pm.t])]
                        if sf is not None:
                            terms.append((SFb.ap[:, sf, dsl], Qf.ap[:, csl], [SFb.t, Qf.t]))
                        if sb is not None:
                            terms.append((SBb.ap[:, sb, dsl], Qb.ap[:, csl], [SBb.t, Qb.t]))
                        for ti, (l_, r_, rd) in enumerate(terms):
                            P.mm(pO[dj].ap[:, csl], l_, r_, ti == 0, ti == len(terms) - 1, reads=rd,
                                 writes=[pO[dj].t])
                    P.act(sq2.ap[:, dj, 0:nt], pO[dj].ap[:, 0:nt], AF.Square, reads=[pO[dj].t], writes=[sq2.t])
                pN = ps_next()
                for dj in range(2):
                    P.mm(pN.ap[:, 0:nt], ones_bf.ap, sq2.ap[:, dj, 0:nt], dj == 0, dj == 1,
                         reads=[ones_bf.t, sq2.t], writes=[pN.t])
                rsqrt_from_psum(rs3.ap[:, 0:nt], pN.ap[:, 0:nt], 256.0, [pN.t], [rs3.t])
                for dj in range(2):
                    P.tt("dve", to_[dj].ap[:, 0:nt], pO[dj].ap[:, 0:nt], rs3.ap[:, 0:nt], ALU.mult,
                         reads=[pO[dj].t, rs3.t], writes=[to_[dj].t])
                    P.tt("pool", stg[dj].ap[:, 0:nt], to_[dj].ap[:, 0:nt], Ur.ap[:, dj, t0:t0 + nt], ALU.mult,
                         reads=[to_[dj].t, Ur.t], writes=[stg[dj].t])
                    r0 = (2 * h + dj) * 128
                    P.dma("sp", obr_d[0][r0:r0 + 128, qpos(t0):qpos(t0) + nt], stg[dj].ap[:, 0:nt],
                          reads=[stg[dj].t], writes=[tobr(0, (h, dj, t0))])
        P.barrier()
        if stop <= 3:
            continue
        def run_attn(groups, Pt, depth=2):
            items = [(gi, ki) for gi, g in enumerate(groups) for ki in range(g["n"])]
            pts = {}
            sidx = [0]

            def SE(idx):
                gi, ki = items[idx]
                g = groups[gi]
                pS = ps[sidx[0] % 4]
                sidx[0] += 1
                g["S"](ki, pS)
                pt = Pt[idx % len(Pt)]
                g["E"](ki, pS, pt)
                pts[idx] = pt
            for idx in range(min(depth, len(items))):
                SE(idx)
            for idx in range(len(items)):
                if idx + depth < len(items):
                    SE(idx + depth)
                gi, ki = items[idx]
                g = groups[gi]
                pO, pD = (ps[4], ps[5]) if gi % 2 == 0 else (ps[6], ps[7])
                g["PV"](ki, pts.pop(idx), pO, pD, ki == 0, ki == g["n"] - 1)
                if ki == g["n"] - 1:
                    g["epi"](pO, pD)

        A.off = PH0_END
        KSa = A.alloc([128, NALL], BF16, "KSa")
        KSb = A.alloc([128, NALL], BF16, "KSb")
        QS = A.alloc([128, 4, NALL], BF16, "QS")
        Vs = A.alloc([128, 34, 64], BF16, "Vs")
        Pt = [A.alloc([128, 512], BF16, f"Pt{i}") for i in range(4)]
        sk = [A.alloc([64, 512], F32, f"sk{i}") for i in range(2)]
        den = [A.alloc([128, 512], F32, f"den{i}") for i in range(2)]
        ostg = [A.alloc([128, 512], BF16, f"ostg{i}") for i in range(2)]
        P.dma("sp", KSa.ap, fm_d["ks"], writes=[KSa.t])
        P.dma("sp", KSb.ap, fm_d["ks2"], writes=[KSb.t])
        obr1 = obr_d[1].rearrange("(c p) t -> p c t", p=128)
        gcount = 0
        for g in range(2):
            for i in range(4):
                P.dma("sp", QS.ap[:, i, 0:NOWN], fm_d[f"qs{g * 4 + i}"][:, 0:NOWN], writes=[QS.t])
                if need_ctx:
                    P.dma("sp", QS.ap[:, i, 4096:NALL], fm_d[f"qs{g * 4 + i}"][:, 4096:NALL], writes=[QS.t])
            vsrc = vall_d[:, 1024 + g * 64:1024 + (g + 1) * 64].rearrange("(t p) c -> p t c", p=128)
            for tq in range(0, 34, 4):
                P.dma("sp", Vs.ap[:, tq:min(tq + 4, 34), :], vsrc[:, tq:min(tq + 4, 34), :], writes=[Vs.t])
            groups = []
            for par in range(2):
                pb0 = par * 64
                Ksrc = KSa if g == par else KSb
                sk_ = sk[par]
                for i in range(4):
                    hq = g * 8 + par + 2 * i
                    P.act(sk_.ap[:, i * 128:(i + 1) * 128], cst.ap[0:64, C_DPOS:C_DPOS + 128], AF.Identity,
                          reads=[cst.t, sinkx.t], writes=[sk_.t], bias=sinkx.ap[0:64, hq:hq + 1], scale=0.0)
                blocks = [(jb * 128, jb) for jb in range(16)] + ([(4096, 16), (4224, 17)] if need_ctx else [])
                for (t0, jb) in blocks:
                    if jb < 16:
                        kts = [((jb - 1) * 128, 0) if jb > 0 else (2048 + 15 * 128, 2), (jb * 128, None),
                               ((jb + 1) * 128, 1) if jb < 15 else (2048, 3), (4096, None), (4224, None)]
                    else:
                        kts = [(4096, None), (4224, None)]

                    def S(ki, pS, kts=kts, pb0=pb0, Ksrc=Ksrc, t0=t0):
                        k0 = kts[ki][0]
                        P.mm(pS.ap.rearrange("p (a b) -> p a b", a=4), Ksrc.ap[pb0:pb0 + 64, k0:k0 + 128],
                             QS.ap[pb0:pb0 + 64, :, t0:t0 + 128], True, True, reads=[Ksrc.t, QS.t], writes=[pS.t])

                    def E(ki, pS, pt, kts=kts):
                        P.act(pt.ap, pS.ap, AF.Exp, reads=[pS.t], writes=[pt.t], scale=0.125)
                        mi = kts[ki][1]
                        if mi is not None:
                            P.tt("pool", pt.ap, pt.ap, mk_bf.ap[:, mi, :], ALU.mult, reads=[pt.t, mk_bf.t],
                                 writes=[pt.t])

                    def PV(ki, pt, pO, pD, first, lastk, kts=kts):
                        k0 = kts[ki][0]
                        P.mm(pO.ap[0:64, :], Vs.ap[:, k0 // 128, :], pt.ap, first, lastk, reads=[Vs.t, pt.t],
                             writes=[pO.t])
                        P.mm(pD.ap[0:64, :], ones_bf.ap[:, 0:64], pt.ap, first, lastk, reads=[ones_bf.t, pt.t],
                             writes=[pD.t])

                    def epi(pO, pD, t0=t0, pb0=pb0, g=g, sk_=sk_, gc=gcount):
                        dn_, os_ = den[gc % 2], ostg[gc % 2]
                        P.tt("dve", dn_.ap[0:64, :], pD.ap[0:64, :], sk_.ap, ALU.add, reads=[pD.t, sk_.t],
                             writes=[dn_.t])
                        P.act(dn_.ap[0:64, :], dn_.ap[0:64, :], AF.Ln, reads=[dn_.t], writes=[dn_.t])
                        P.act(dn_.ap[0:64, :], dn_.ap[0:64, :], AF.Exp, reads=[dn_.t], writes=[dn_.t], scale=-1.0)
                        P.tt("dve", os_.ap[0:64, :], pO.ap[0:64, :], dn_.ap[0:64, :], ALU.mult,
                             reads=[pO.t, dn_.t], writes=[os_.t])
                        q0 = qpos(t0)
                        P.dma("sp", obr1[pb0:pb0 + 64, g * 4:g * 4 + 4, q0:q0 + 128],
                              os_.ap[0:64, :].rearrange("p (a b) -> p a b", a=4), reads=[os_.t],
                              writes=[tobr(1, (g, pb0, t0))])
                    groups.append(dict(n=len(kts), S=S, E=E, PV=PV, epi=epi))
                    gcount += 1
            run_attn(groups, Pt)
        P.barrier()
        if stop <= 4:
            continue
        A.off = PH0_END
        KA = A.alloc([128, NALL], BF16, "KA")
        VA = A.alloc([128, 34, 128], BF16, "VA")
        QA = [A.alloc([128, NALL], BF16, f"QA{i}") for i in range(4)]
        Pt = [A.alloc([128, 512], BF16, f"Pt{i}") for i in range(4)]
        den = [A.alloc([128, 512], F32, f"den{i}") for i in range(2)]
        ostg = [A.alloc([128, 512], BF16, f"ostg{i}") for i in range(2)]
        GSCALE = 128.0 ** -0.5
        gcount = 0
        for g in range(2):
            P.dma("sp", KA.ap, fm_d[f"ka{g}"], writes=[KA.t])
            vsrc = vall_d[:, 1152 + g * 128:1152 + (g + 1) * 128].rearrange("(t p) c -> p t c", p=128)
            for tq in range(0, 34, 4):
                P.dma("sp", VA.ap[:, tq:min(tq + 4, 34), :], vsrc[:, tq:min(tq + 4, 34), :], writes=[VA.t])
            groups = []
            for hh in range(4):
                h = g * 4 + hh
                Q_ = QA[hh]
                P.dma("sp", Q_.ap[:, 0:NOWN], fm_d[f"qa{h}"][:, 0:NOWN], writes=[Q_.t])
                if need_ctx:
                    P.dma("sp", Q_.ap[:, 4096:NALL], fm_d[f"qa{h}"][:, 4096:NALL], writes=[Q_.t])
                for (t0, nt, mj) in qtiles:
                    kts = list(range(34)) if t0 < 4096 else [32, 33]

                    def S(ki, pS, kts=kts, Q_=Q_, t0=t0, nt=nt):
                        kt = kts[ki]
                        P.mm(pS.ap[:, 0:nt], KA.ap[:, kt * 128:(kt + 1) * 128], Q_.ap[:, t0:t0 + nt], True, True,
                             reads=[KA.t, Q_.t], writes=[pS.t])

                    def E(ki, pS, pt, nt=nt):
                        P.act(pt.ap[:, 0:nt], pS.ap[:, 0:nt], AF.Exp, reads=[pS.t], writes=[pt.t], scale=GSCALE)

                    def PV(ki, pt, pO, pD, first, lastk, kts=kts, nt=nt):
                        kt = kts[ki]
                        P.mm(pO.ap[:, 0:nt], VA.ap[:, kt, :], pt.ap[:, 0:nt], first, lastk, reads=[VA.t, pt.t],
                             writes=[pO.t])
                        P.mm(pD.ap[:, 0:nt], ones_bf.ap, pt.ap[:, 0:nt], first, lastk, reads=[ones_bf.t, pt.t],
                             writes=[pD.t])

                    def epi(pO, pD, t0=t0, nt=nt, h=h, gc=gcount):
                        dn_, os_ = den[gc % 2], ostg[gc % 2]
                        P.act(dn_.ap[:, 0:nt], pD.ap[:, 0:nt], AF.Ln, reads=[pD.t], writes=[dn_.t])
                        P.act(dn_.ap[:, 0:nt], dn_.ap[:, 0:nt], AF.Exp, reads=[dn_.t], writes=[dn_.t], scale=-1.0)
                        P.tt("dve", os_.ap[:, 0:nt], pO.ap[:, 0:nt], dn_.ap[:, 0:nt], ALU.mult,
                             reads=[pO.t, dn_.t], writes=[os_.t])
                        q0 = qpos(t0)
                        P.dma("sp", obr_d[2][h * 128:(h + 1) * 128, q0:q0 + nt], os_.ap[:, 0:nt], reads=[os_.t],
                              writes=[tobr(2, (h, t0))])
                    groups.append(dict(n=len(kts), S=S, E=E, PV=PV, epi=epi))
                    gcount += 1
            run_attn(groups, Pt)
        P.barrier()
        if stop <= 5:
            continue
        A.off = PH0_END
        wm = [A.alloc([128, 8, 1024], BF16, f"wm{i}") for i in range(4)]
        ob = [A.alloc([128, 8, 512], BF16, f"ob{i}") for i in range(3)]
        gb = [A.alloc([128, 8, 512], BF16, f"gb{i}") for i in range(3)]
        ypre = A.alloc([128, 8, 512], BF16, "ypre")
        xtl = A.alloc([128, 8, 512], F32, "xtl")
        ya = [A.alloc([128, 512], F32, f"ya{i}") for i in range(2)]
        tb = [A.alloc([128, 512], F32, f"tb{i}") for i in range(2)]
        for gi in range(8):
            dst = wm[gi // 2].ap[:, :, (gi % 2) * 512:(gi % 2 + 1) * 512]
            P.dma("pool", dst, wall_d[li][G_MERGE + gi], writes=[wm[gi // 2].t])
        gnames = ("ar", "as", "aa")
        for (t0, nt, mj) in qtiles:
            q0 = qpos(t0)
            for b_ in range(3):
                P.dma("sp", ob[b_].ap[:, :, 0:nt],
                      obr_d[b_].rearrange("(kc p) t -> p kc t", p=128)[:, :, q0:q0 + nt], writes=[ob[b_].t])
                for c in range(8):
                    P.dma("sp", gb[b_].ap[:, c, 0:nt], fm_d[f"{gnames[b_]}{c}"][:, t0:t0 + nt], writes=[gb[b_].t])
            P.dma("sp", xtl.ap[:, :, 0:nt], xs3[:, :, t0:t0 + nt], reads=[txs(t0)], writes=[xtl.t])
            for dc in range(8):
                dsl = slice(dc * 128, (dc + 1) * 128)
                y_ = ya[dc % 2]
                for b_ in range(3):
                    pa = ps_next()
                    for kc in range(8):
                        P.mm(pa.ap[:, 0:nt], wm[b_].ap[:, kc, dsl], ob[b_].ap[:, kc, 0:nt], kc == 0, kc == 7,
                             reads=[wm[b_].t, ob[b_].t], writes=[pa.t])
                    if b_ == 0:
                        P.tt("dve", y_.ap[:, 0:nt], pa.ap[:, 0:nt], gb[0].ap[:, dc, 0:nt], ALU.mult,
                             reads=[pa.t, gb[0].t], writes=[y_.t])
                    else:
                        t_ = tb[b_ % 2]
                        P.tt("dve", t_.ap[:, 0:nt], pa.ap[:, 0:nt], gb[b_].ap[:, dc, 0:nt], ALU.mult,
                             reads=[pa.t, gb[b_].t], writes=[t_.t])
                        if b_ == 1:
                            P.tt("pool", y_.ap[:, 0:nt], y_.ap[:, 0:nt], t_.ap[:, 0:nt], ALU.add,
                                 reads=[y_.t, t_.t], writes=[y_.t])
                        else:
                            P.tt("pool", ypre.ap[:, dc, 0:nt], y_.ap[:, 0:nt], t_.ap[:, 0:nt], ALU.add,
                                 reads=[y_.t, t_.t], writes=[ypre.t])
            for dc in range(8):
                dsl = slice(dc * 128, (dc + 1) * 128)
                py = ps_next()
                for kc in range(8):
                    P.mm(py.ap[:, 0:nt], wm[3].ap[:, kc, dsl], ypre.ap[:, kc, 0:nt], kc == 0, kc == 7,
                         reads=[wm[3].t, ypre.t], writes=[py.t])
                P.stt(xtl.ap[:, dc, 0:nt], py.ap[:, 0:nt], GT1(dc, mj), xtl.ap[:, dc, 0:nt], ALU.mult, ALU.add,
                      reads=[py.t, modT.t, xtl.t], writes=[xtl.t])
            P.dma("pool", xs3[:, :, t0:t0 + nt], xtl.ap[:, :, 0:nt], reads=[xtl.t], writes=[txs(t0)])
        P.barrier()
        if stop <= 6:
            continue
        A.off = PH0_END
        h2T = A.alloc([128, 8, NQ], BF16, "h2T")
        xres = A.alloc([128, 8, NQ], F32, "xres")
        WT = A.alloc([16, NQ], F32, "WT")
        M0 = A.off
        h2f = A.alloc([128, 8, 512], F32, "h2f")
        sq = A.alloc([128, 8, 512], BF16, "sq")
        rstd = A.alloc([128, 512], F32, "rstd")
        tmp = [A.alloc([128, 512], F32, f"tmp{i}") for i in range(2)]
        rt = {nm: A.alloc([128, 16], F32, "rt_" + nm) for nm in ("s", "bz", "eq", "msk", "ch", "ws", "wts")}
        rs = {nm: A.alloc([128, 4], F32, "rs_" + nm) for nm in ("m1", "m2", "gs", "gsel", "gm", "dn")}

        def v3(b_):
            return b_.ap.rearrange("p (g k) -> p g k", k=4)

        def bc(b_):
            return b_.ap[:, 0:4].unsqueeze(2).to_broadcast([128, 4, 4])
        mtiles = qtiles
        for i, (t0, nt, mj) in enumerate(mtiles):
            q0 = qpos(t0)
            P.dma("sp", xres.ap[:, :, q0:q0 + nt], xs3[:, :, t0:t0 + nt], reads=[txs(t0)], writes=[xres.t])
            P.act(sq.ap[:, :, 0:nt], xres.ap[:, :, q0:q0 + nt], AF.Square, reads=[xres.t], writes=[sq.t])
            pb = ps_next()
            for kc in range(8):
                P.mm(pb.ap[:, 0:nt], ones_bf.ap, sq.ap[:, kc, 0:nt], kc == 0, kc == 7, reads=[ones_bf.t, sq.t],
                     writes=[pb.t])
            rsqrt_from_psum(rstd.ap[:, 0:nt], pb.ap[:, 0:nt], 1024.0, [pb.t], [rstd.t])
            for kc in range(8):
                tm_ = tmp[kc % 2]
                P.tt("dve", tm_.ap[:, 0:nt], xres.ap[:, kc, q0:q0 + nt], rstd.ap[:, 0:nt], ALU.mult,
                     reads=[xres.t, rstd.t], writes=[tm_.t])
                P.act(h2f.ap[:, kc, 0:nt], tm_.ap[:, 0:nt], AF.Identity, reads=[tm_.t, A2.t, modT.t], writes=[h2f.t],
                      bias=SH2(kc, mj), scale=A2.ap[:, kc, mj:mj + 1])
                P.copy("pool", h2T.ap[:, kc, q0:q0 + nt], h2f.ap[:, kc, 0:nt], reads=[h2f.t], writes=[h2T.t])
            for sub in range(nt // 128):
                ssl = slice(sub * 128, (sub + 1) * 128)
                pr = ps_next()
                for kc in range(8):
                    P.mm(pr.ap[:, 0:16], h2f.ap[:, kc, ssl], sg.ap[:, SG_WR + kc * 16:SG_WR + (kc + 1) * 16],
                         kc == 0, kc == 7, reads=[h2f.t, sg.t], writes=[pr.t])
                s_, bz, eq, msk, ch, ws, wts = [rt[n] for n in ("s", "bz", "eq", "msk", "ch", "ws", "wts")]
                m1, m2, gs, gsel, gm, dn = [rs[n] for n in ("m1", "m2", "gs", "gsel", "gm", "dn")]
                P.act(s_.ap, pr.ap[:, 0:16], AF.Sigmoid, reads=[pr.t], writes=[s_.t])
                P.tt("dve", bz.ap, s_.ap, sg.ap[:, SG_BR:SG_BR + 16], ALU.add, reads=[s_.t, sg.t], writes=[bz.t])
                P.add("dve", lambda e, o=m1.ap, i_=v3(bz): e.tensor_reduce(out=o, in_=i_, axis=AX.X, op=ALU.max),
                      reads=[bz.t], writes=[m1.t])
                P.tt("dve", v3(eq), v3(bz), bc(m1), ALU.is_equal, reads=[bz.t, m1.t], writes=[eq.t])
                P.stt(msk.ap, eq.ap, -1e9, bz.ap, ALU.mult, ALU.add, reads=[eq.t, bz.t], writes=[msk.t])
                P.add("dve", lambda e, o=m2.ap, i_=v3(msk): e.tensor_reduce(out=o, in_=i_, axis=AX.X, op=ALU.max),
                      reads=[msk.t], writes=[m2.t])
                P.tt("dve", gs.ap, m1.ap, m2.ap, ALU.add, reads=[m1.t, m2.t], writes=[gs.t])
                P.add("dve", lambda e, o=gm.ap[:, 0:1], i_=gs.ap: e.tensor_reduce(out=o, in_=i_, axis=AX.X,
                                                                                 op=ALU.max),
                      reads=[gs.t], writes=[gm.t])
                P.ts("dve", gsel.ap, gs.ap, gm.ap[:, 0:1], None, ALU.is_equal, reads=[gs.t, gm.t], writes=[gsel.t])
                P.tt("dve", v3(ch), v3(bz), bc(m2), ALU.is_ge, reads=[bz.t, m2.t], writes=[ch.t])
                P.tt("dve", v3(ch), v3(ch), bc(gsel), ALU.mult, reads=[ch.t, gsel.t], writes=[ch.t])
                P.tt("dve", ws.ap, s_.ap, ch.ap, ALU.mult, reads=[s_.t, ch.t], writes=[ws.t])
                P.add("dve", lambda e, o=dn.ap[:, 0:1], i_=ws.ap: e.tensor_reduce(out=o, in_=i_, axis=AX.X,
                                                                                 op=ALU.add),
                      reads=[ws.t], writes=[dn.t])
                P.add("dve", lambda e, o=dn.ap[:, 1:2], i_=dn.ap[:, 0:1]: e.reciprocal(o, i_), reads=[dn.t],
                      writes=[dn.t])
                P.ts("dve", wts.ap, ws.ap, dn.ap[:, 1:2], None, ALU.mult, reads=[ws.t, dn.t], writes=[wts.t])
                pT = ps_next()
                P.transpose(pT.ap[0:16, 0:128], wts.ap, cst.ap[:, C_ID:C_ID + 128], reads=[wts.t, cst.t],
                            writes=[pT.t])
                P.copy("act", WT.ap[0:16, q0 + sub * 128:q0 + (sub + 1) * 128], pT.ap[0:16, 0:128], reads=[pT.t],
                       writes=[WT.t])
        P.barrier()
        A.off = M0
        ew = [[A.alloc([128, 4096], BF16, f"ew{i}_{k}") for k in range(3)] for i in range(2)]
        wbc = A.alloc([128, 512], F32, "wbc")
        sG = [A.alloc([128, 512], F32, f"sG{i}") for i in range(2)]
        tG = [A.alloc([128, 512], F32, f"tG{i}") for i in range(2)]
        hid = A.alloc([128, 4, 512], BF16, "hid")
        def load_expert(e):
            for k in range(3):
                P.dma("pool", ew[e % 2][k].ap, we_d[li][e, k], writes=[ew[e % 2][k].t])

        hidb = [hid, A.alloc([128, 4, 512], BF16, "hid2")]
        wbcb = [wbc, A.alloc([128, 512], F32, "wbc2")]
        items = [(e, ti) for e in range(16) for ti in range(len(mtiles))]

        def GU(ix):
            e, ti = items[ix]
            wg_, wu_, wd_ = ew[e % 2]
            wg3 = wg_.ap.rearrange("p (a b) -> p a b", a=8)
            wu3 = wu_.ap.rearrange("p (a b) -> p a b", a=8)
            t0, nt, mj = mtiles[ti]
            q0 = qpos(t0)
            hid_, wbc_ = hidb[ix % 2], wbcb[ix % 2]
            pw = ps_next(8)
            P.mm(pw.ap[:, 0:nt], cst.ap[0:16, C_SEL + e * 128:C_SEL + (e + 1) * 128], WT.ap[0:16, q0:q0 + nt],
                 True, True, reads=[cst.t, WT.t], writes=[pw.t])
            P.copy("act", wbc_.ap[:, 0:nt], pw.ap[:, 0:nt], reads=[pw.t], writes=[wbc_.t])
            for hc in range(4):
                hsl = slice(hc * 128, (hc + 1) * 128)
                pg, pu = ps_next(8), ps_next(8)
                for kc in range(8):
                    P.mm(pg.ap[:, 0:nt], wg3[:, kc, hsl], h2T.ap[:, kc, q0:q0 + nt], kc == 0, kc == 7,
                         reads=[wg_.t, h2T.t], writes=[pg.t])
                for kc in range(8):
                    P.mm(pu.ap[:, 0:nt], wu3[:, kc, hsl], h2T.ap[:, kc, q0:q0 + nt], kc == 0, kc == 7,
                         reads=[wu_.t, h2T.t], writes=[pu.t])
                sg_, tg_ = sG[hc % 2], tG[hc % 2]
                P.act(sg_.ap[:, 0:nt], pg.ap[:, 0:nt], AF.Silu, reads=[pg.t], writes=[sg_.t])
                P.tt("dve", tg_.ap[:, 0:nt], sg_.ap[:, 0:nt], pu.ap[:, 0:nt], ALU.mult, reads=[sg_.t, pu.t],
                     writes=[tg_.t])
                P.tt("pool", hid_.ap[:, hc, 0:nt], tg_.ap[:, 0:nt], wbc_.ap[:, 0:nt], ALU.mult,
                     reads=[tg_.t, wbc_.t], writes=[hid_.t])

        def DOWN(ix):
            e, ti = items[ix]
            wd_ = ew[e % 2][2]
            wd3 = wd_.ap.rearrange("p (a b) -> p a b", a=4)
            t0, nt, mj = mtiles[ti]
            q0 = qpos(t0)
            hid_ = hidb[ix % 2]
            for dc in range(8):
                dsl = slice(dc * 128, (dc + 1) * 128)
                py = ps_next(8)
                for hc in range(4):
                    P.mm(py.ap[:, 0:nt], wd3[:, hc, dsl], hid_.ap[:, hc, 0:nt], hc == 0, hc == 3,
                         reads=[wd_.t, hid_.t], writes=[py.t])
                P.stt(xres.ap[:, dc, q0:q0 + nt], py.ap[:, 0:nt], GT2(dc, mj), xres.ap[:, dc, q0:q0 + nt],
                      ALU.mult, ALU.add, reads=[py.t, modT.t, xres.t], writes=[xres.t])

        load_expert(0)
        load_expert(1)
        GU(0)
        for ix in range(len(items)):
            if ix + 1 < len(items):
                GU(ix + 1)
            DOWN(ix)
            e_, ti_ = items[ix]
            if ti_ == len(mtiles) - 1 and e_ + 2 < 16:
                load_expert(e_ + 2)
        if not (last and final):
            for (t0, nt, mj) in mtiles:
                q0 = qpos(t0)
                P.dma("pool", xs3[:, :, t0:t0 + nt], xres.ap[:, :, q0:q0 + nt], reads=[xres.t], writes=[txs(t0)])
        if not last:
            for i, (t0, nt, mj) in enumerate(TOK_TILES_OWN):
                P.dma("pool", xch_src[i].rearrange("(kc p) t -> p kc t", p=128), xres.ap[:, :, t0:t0 + nt],
                      reads=[xres.t], writes=[T_xsrc[i]])
            P.barrier()
            for i in range(4):
                P.add("pool", lambda e, i=i: e.collective_compute("AllGather", ALU.bypass,
                                                                  replica_groups=[[0, 1], [2, 3], [4, 5], [6, 7]],
                                                                  ins=[xch_src[i]], outs=[xch_dst[i]]),
                      reads=[T_xsrc[i]], writes=[T_xdst[i]], cc=True)
            P.barrier()
            A.off = M0
            xa = [A.alloc([128, 8, 512], F32, f"xa{i}") for i in range(2)]
            xb_ = [A.alloc([128, 8, 512], F32, f"xb{i}") for i in range(2)]
            for i, (t0, nt, mj) in enumerate(TOK_TILES_OWN):
                d3 = xch_dst[i].rearrange("(r kc p) t -> r p kc t", r=2, p=128)
                a_, b_ = xa[i % 2], xb_[i % 2]
                P.dma("sp", a_.ap, d3[0], reads=[T_xdst[i]], writes=[a_.t])
                P.dma("sp", b_.ap, d3[1], reads=[T_xdst[i]], writes=[b_.t])
                P.ts("dve", a_.ap, a_.ap, sg.ap[:, SG_FLAG + 1:SG_FLAG + 2], None, ALU.mult, reads=[a_.t, sg.t],
                     writes=[a_.t])
                P.stt(a_.ap, b_.ap, sg.ap[:, SG_FLAG:SG_FLAG + 1], a_.ap, ALU.mult, ALU.add,
                      reads=[a_.t, b_.t, sg.t], writes=[a_.t])
                P.dma("pool", xs3[:, :, NOWN + t0:NOWN + t0 + nt], a_.ap, reads=[a_.t], writes=[txs(NOWN + t0)])
        if last and not final:
            xout3 = xout.rearrange("(kc p) t -> p kc t", p=128)
            for (t0, nt, mj) in mtiles:
                q0 = qpos(t0)
                P.dma("pool", xout3[:, :, q0:q0 + nt], xres.ap[:, :, q0:q0 + nt], reads=[xres.t])
        if last and final:
            P.barrier()
            A.off = M0
            h2f = A.alloc([128, 8, 512], F32, "h2f")
            sq = A.alloc([128, 8, 512], BF16, "sq")
            rstd = A.alloc([128, 512], F32, "rstd")
            tmp = [A.alloc([128, 512], F32, f"tmp{i}") for i in range(2)]
            yout3 = yout.rearrange("(kc p) t -> p kc t", p=128)
            for (t0, nt, mj) in TOK_TILES_OWN:
                P.act(sq.ap[:, :, 0:nt], xres.ap[:, :, t0:t0 + nt], AF.Square, reads=[xres.t], writes=[sq.t])
                pb = ps_next()
                for kc in range(8):
                    P.mm(pb.ap[:, 0:nt], ones_bf.ap, sq.ap[:, kc, 0:nt], kc == 0, kc == 7, reads=[ones_bf.t, sq.t],
                         writes=[pb.t])
                rsqrt_from_psum(rstd.ap[:, 0:nt], pb.ap[:, 0:nt], 1024.0, [pb.t], [rstd.t])
                for kc in range(8):
                    tm_ = tmp[kc % 2]
                    P.tt("dve", tm_.ap[:, 0:nt], xres.ap[:, kc, t0:t0 + nt], rstd.ap[:, 0:nt], ALU.mult,
                         reads=[xres.t, rstd.t], writes=[tm_.t])
                    P.act(h2f.ap[:, kc, 0:nt], tm_.ap[:, 0:nt], AF.Identity, reads=[tm_.t, sg.t], writes=[h2f.t],
                          scale=sg.ap[:, SG_GF + kc:SG_GF + kc + 1])
                P.dma("pool", yout3[:, :, t0:t0 + nt], h2f.ap[:, :, 0:nt], reads=[h2f.t])
    P.emit()
    return nc


_PROGS = {}


def _prog(key, *args, **kw):
    if key not in _PROGS:
        _PROGS[key] = build(*args, **kw)
    return _PROGS[key]


def kernel(**inp):
    inp = {k: np.asarray(v) for k, v in inp.items()}
    x, ctx = inp["x"], inp["ctx"]
    B = x.shape[0]
    cores = [(b, h) for b in range(B) for h in range(2)]
    consts = [make_consts(h) for h in range(2)]
    tabs = [make_tabs(h) for h in range(2)]
    w0 = prep_layer_weights(inp, 0)
    w1 = prep_layer_weights(inp, 1)
    maps = []
    for (b, h) in cores:
        xo = x[b, h * NOWN:(h + 1) * NOWN].T
        xt = x[b, (1 - h) * NOWN:(2 - h) * NOWN].T
        xall = np.ascontiguousarray(np.concatenate([xo, xt, ctx[b].T], axis=1))
        maps.append(dict(xall=xall, tabs=tabs[h], consts=consts[h], sg=make_small_global(inp, b, h),
                         sm0=w0[2], wall0=w0[0], we0=w0[1], sm1=w1[2], wall1=w1[0], we1=w1[1]))
    nc = _prog("fused", [0, 1], [True, False], True)
    r = run_bass_kernel_spmd(nc, maps, core_ids=list(range(len(cores)))).results
    out = np.zeros((B, 2 * NOWN, D), np.float32)
    for i, (b, h) in enumerate(cores):
        out[b, h * NOWN:(h + 1) * NOWN] = np.asarray(r[i]["yout"]).T
    return out
```

```python
import numpy as np
import concourse.bass as bass
import concourse.mybir as mybir
from concourse.bass_utils import run_bass_kernel_spmd

F32 = mybir.dt.float32
BF16 = mybir.dt.bfloat16
AF = mybir.ActivationFunctionType
ALU = mybir.AluOpType
AX = mybir.AxisListType

NOWN, NOTH, NCTX = 2048, 2048, 256
NALL = NOWN + NOTH + NCTX
NQ = NOWN + NCTX
D = 1024
EPS = 1e-6
NSLOT = 24
SKIP = set()


class T:
    __slots__ = ("name", "w", "r", "psum")

    def __init__(self, name="", psum=False):
        self.name = name
        self.w = None
        self.r = []
        self.psum = psum


class Op:
    __slots__ = ("eng", "fn", "deps", "id", "is_dma", "slot", "val", "marked", "semval", "cc")

    def __init__(self, eng, fn, is_dma):
        self.eng = eng
        self.fn = fn
        self.deps = []
        self.is_dma = is_dma
        self.slot = None
        self.val = None
        self.marked = False
        self.semval = None
        self.cc = False


class Prog:
    ENGS = ("pe", "act", "dve", "pool", "sp")

    def __init__(self, nc):
        self.nc = nc
        self.ops = []
        self.streams = {e: [] for e in self.ENGS}
        self.ndma = {e: 0 for e in self.ENGS}
        self.dmas = {e: [] for e in self.ENGS}

    def add(self, eng, fn, reads=(), writes=(), dma=False, extra=(), cc=False):
        op = Op(eng, fn, dma or cc)
        op.cc = cc
        op.id = len(self.ops)
        deps = {}
        for t in reads:
            if t.w is not None:
                deps[t.w.id] = t.w
            if t.psum:
                for r in t.r:
                    if r.eng != eng:
                        deps[r.id] = r
        for t in writes:
            if t.w is not None:
                deps[t.w.id] = t.w
            for r in t.r:
                deps[r.id] = r
        for d in extra:
            deps[d.id] = d
        op.deps = list(deps.values())
        for t in reads:
            if not dma:
                t.r = [r for r in t.r if r.is_dma or r.eng != eng]
            t.r.append(op)
        for t in writes:
            t.w = op
            t.r = []
        if cc:
            self.ncc = getattr(self, "ncc", 0) + 1
            op.slot = ("cc", self.ncc - 1)
            op.val = 1
            self.dmas[eng].append(op)
        elif dma:
            i = self.ndma[eng]
            self.ndma[eng] += 1
            op.slot = i % NSLOT
            op.val = 16 * (i // NSLOT + 1)
            self.dmas[eng].append(op)
        self.ops.append(op)
        self.streams[eng].append(op)
        return op

    def barrier(self):
        lasts = []
        for e in self.ENGS:
            if self.streams[e]:
                lasts.append(self.streams[e][-1])
            lasts.extend(self.dmas[e][-NSLOT:])
        for e in self.ENGS:
            self.add(e, lambda eng: eng.nop(), extra=lasts)

    def dma(self, q, out, in_, reads=(), writes=()):
        return self.add(q, lambda e: e.dma_start(out=out, in_=in_), reads, writes, dma=True)

    def mm(self, out, lhsT, rhs, start, stop, reads=(), writes=()):
        return self.add("pe", lambda e: e.matmul(out, lhsT, rhs, start=start, stop=stop), reads, writes)

    def transpose(self, out, in_, ident, reads=(), writes=()):
        return self.add("pe", lambda e: e.transpose(out, in_, ident), reads, writes)

    def act(self, out, in_, func, reads=(), writes=(), bias=None, scale=None):
        kw = {}
        if bias is not None:
            kw["bias"] = bias
        if scale is not None:
            kw["scale"] = scale
        return self.add("act", lambda e: e.activation(out, in_, func, **kw), reads, writes)

    def tt(self, eng, out, in0, in1, op, reads=(), writes=()):
        return self.add(eng, lambda e: e.tensor_tensor(out, in0, in1, op), reads, writes)

    def ts(self, eng, out, in0, s1, s2, op0, op1=None, reads=(), writes=()):
        if op1 is None:
            return self.add(eng, lambda e: e.tensor_scalar(out=out, in0=in0, scalar1=s1, scalar2=None, op0=op0),
                            reads, writes)
        return self.add(eng, lambda e: e.tensor_scalar(out=out, in0=in0, scalar1=s1, scalar2=s2, op0=op0, op1=op1),
                        reads, writes)

    def stt(self, out, in0, scalar, in1, op0, op1, reads=(), writes=()):
        return self.add("dve", lambda e: e.scalar_tensor_tensor(out=out, in0=in0, scalar=scalar, in1=in1,
                                                                op0=op0, op1=op1), reads, writes)

    def copy(self, eng, out, in_, reads=(), writes=()):
        if eng == "act":
            return self.add("act", lambda e: e.copy(out, in_), reads, writes)
        return self.add(eng, lambda e: e.tensor_copy(out, in_), reads, writes)

    def emit(self):
        nc = self.nc
        for op in self.ops:
            for d in op.deps:
                if d.is_dma:
                    continue
                if d.eng == op.eng and not op.is_dma and d.eng == "pe":
                    continue
                d.marked = True
        cnt = {e: 0 for e in self.ENGS}
        for op in self.ops:
            if op.marked and not op.is_dma:
                cnt[op.eng] += 1
                op.semval = cnt[op.eng]
        sems = {e: nc.alloc_semaphore(f"s_{e}") for e in self.ENGS}
        dsems = {e: {i: nc.alloc_semaphore(f"d_{e}_{i}") for i in range(min(NSLOT, self.ndma[e]))}
                 for e in self.ENGS}
        for i in range(getattr(self, "ncc", 0)):
            dsems["pool"][("cc", i)] = nc.alloc_semaphore(f"cc_{i}")
        engobj = {"pe": nc.tensor, "act": nc.scalar, "dve": nc.vector, "pool": nc.gpsimd, "sp": nc.sync}
        with nc.Block() as block:
            def run(ename):
                eng = engobj[ename]
                waited = {}

                def wait(key, sem, val):
                    if waited.get(key, 0) >= val:
                        return
                    waited[key] = val
                    eng.wait_ge(sem, val)

                for op in self.streams[ename]:
                    for d in op.deps:
                        if d.is_dma:
                            wait(("d", d.eng, d.slot), dsems[d.eng][d.slot], d.val)
                        else:
                            if d.eng == ename and not op.is_dma and ename == "pe":
                                continue
                            wait(("c", d.eng), sems[d.eng], d.semval)
                    if op.cc:
                        ins = op.fn(eng)
                        ins.then_inc(dsems[ename][op.slot], 1)
                    elif op.is_dma:
                        if op.val > 16:
                            wait(("d", ename, op.slot), dsems[ename][op.slot], op.val - 16)
                        ins = op.fn(eng)
                        ins.then_inc(dsems[ename][op.slot], 16)
                    else:
                        ins = op.fn(eng)
                        if op.marked:
                            ins.then_inc(sems[ename], 1)
                n = self.ndma[ename]
                for i in range(max(0, n - NSLOT), n):
                    wait(("d", ename, i % NSLOT), dsems[ename][i % NSLOT], 16 * (i // NSLOT + 1))

            @block.tensor
            def _(e):
                run("pe")

            @block.scalar
            def _(e):
                run("act")

            @block.vector
            def _(e):
                run("dve")

            @block.gpsimd
            def _(e):
                run("pool")

            @block.sync
            def _(e):
                run("sp")


class Buf:
    __slots__ = ("ap", "t")

    def __init__(self, ap, name="", psum=False):
        self.ap = ap
        self.t = T(name, psum)


class Arena:
    def __init__(self, nc, nbytes):
        self.t = nc.alloc_sbuf_tensor("arena", [128, nbytes // 2], BF16)
        self.nbytes = nbytes
        self.off = 0

    def alloc(self, shape, dtype, name=""):
        n = 1
        for s in shape[1:]:
            n *= s
        es = 4 if dtype == F32 else 2
        nb = (n * es + 63) // 64 * 64
        assert self.off + nb <= self.nbytes, f"arena overflow {name} {self.off + nb}"
        v = self.t[0:shape[0], self.off // 2:(self.off + n * es) // 2]
        if dtype == F32:
            v = v.bitcast(F32)
        if len(shape) == 3:
            v = v.rearrange("p (a b) -> p a b", a=shape[1])
        self.off += nb
        return Buf(v, name)


SPL = [512, 512, 1024, 1024, 1024, 128, 128, 1024, 256, 256, 1024, 1024, 1024]
OFF = np.concatenate([[0], np.cumsum(SPL)]).astype(int)
O_QR, O_KR, O_VR, O_UR, O_QS, O_KS, O_VS, O_QA, O_KA, O_VA, O_AR, O_AS, O_AA = [int(v) for v in OFF[:13]]


def _perm128():
    f = np.arange(128)
    return np.where(f % 64 < 32, f + 32, f - 32)


def _perm64():
    f = np.arange(128)
    return np.where(f % 32 < 16, f + 16, f - 16)


def fm_units():
    u = []
    ar = np.arange(128)
    for h in range(4):
        u.append(dict(name=f"kr{h}", kind="rope128", cols=O_KR + h * 128 + ar, tok="all"))
    u.append(dict(name="ks", kind="rope64", cols=O_KS + ar, tok="all"))
    u.append(dict(name="ks2", kind="rope64", cols=O_KS + (ar + 64) % 128, tok="all"))
    for h in range(2):
        u.append(dict(name=f"ka{h}", kind="normk", cols=O_KA + h * 128 + ar, tok="all"))
    for h in range(4):
        u.append(dict(name=f"qr{h}", kind="rope128", cols=O_QR + h * 128 + ar, tok="q"))
    for c in range(8):
        u.append(dict(name=f"qs{c}", kind="rope64", cols=O_QS + c * 128 + ar, tok="q"))
    for h in range(8):
        u.append(dict(name=f"qa{h}", kind="normq", cols=O_QA + h * 128 + ar, tok="q"))
    for c in range(8):
        u.append(dict(name=f"ur{c}", kind="silu", cols=O_UR + c * 128 + ar, tok="q"))
    for nm, o in (("ar", O_AR), ("as", O_AS), ("aa", O_AA)):
        for c in range(8):
            u.append(dict(name=f"{nm}{c}", kind="sig", cols=o + c * 128 + ar, tok="q"))
    g, used = 0, 0
    for x in u:
        w = 256 if x["kind"] in ("rope128", "rope64", "normq", "normk") else 128
        if used + w > 512:
            g, used = g + 1, 0
        x["group"], x["c0"] = g, used
        x["c1"] = used + 128 if w == 256 else None
        used += w
    return u, g + 1


FM_UNITS, N_FM_GROUPS = fm_units()
TM_COLS = np.concatenate([O_VR + np.arange(1024), O_VS + np.arange(128), O_VA + np.arange(256)])
N_TM_GROUPS = 3
G_MOD = 0
G_FM = 12
G_TM = G_FM + N_FM_GROUPS
G_MERGE = G_TM + N_TM_GROUPS
N_GROUPS = G_MERGE + 8

SM_BMOD, SM_G1, SM_G2, SM_RET, SM_SINK, SM_GQ, SM_GK = 0, 48, 56, 64, 72, 88, 90
SM_L = 96
SG_C, SG_WR, SG_BR, SG_GF, SG_FLAG = 0, 16, 144, 160, 168
SG_N = 176
C_ID, C_DPOS, C_DNEG, C_MF, C_MB, C_IP1, C_IB, C_ML, C_MR, C_MLB, C_MRB = [i * 128 for i in range(11)]
C_PCF, C_PCB = 11 * 128, 11 * 128 + 1
C_SEL = 11 * 128 + 8
C_N = C_SEL + 2048


def _grp(w):
    n = w.shape[1] // 512
    return np.ascontiguousarray(w.reshape(8, 128, n, 512).transpose(2, 1, 0, 3))


def prep_layer_weights(inp, l):
    w_in = inp["w_in"][l]
    p128, p64 = _perm128(), _perm64()
    cols = np.zeros(N_FM_GROUPS * 512, dtype=np.int64)
    for u in FM_UNITS:
        base = u["group"] * 512
        cols[base + u["c0"]:base + u["c0"] + 128] = u["cols"]
        if u["c1"] is not None:
            pm = p64 if u["kind"] == "rope64" else p128
            cols[base + u["c1"]:base + u["c1"] + 128] = u["cols"][pm]
    tmc = np.concatenate([TM_COLS, np.zeros(1536 - 1408, dtype=np.int64)])
    wcat = np.concatenate([inp["w_mod"][l], w_in[:, cols], w_in[:, tmc], inp["w_br_ret"][l], inp["w_br_swa"][l],
                           inp["w_br_ga"][l], inp["w_out"][l]], axis=1)
    wall = _grp(wcat)
    assert wall.shape[0] == N_GROUPS
    wg = inp["w_gate"][l].reshape(16, 8, 128, 512).transpose(0, 2, 1, 3).reshape(16, 128, 4096)
    wu = inp["w_up"][l].reshape(16, 8, 128, 512).transpose(0, 2, 1, 3).reshape(16, 128, 4096)
    wd = inp["w_down"][l].reshape(16, 4, 128, 1024).transpose(0, 2, 1, 3).reshape(16, 128, 4096)
    we = np.ascontiguousarray(np.stack([wg, wu, wd], axis=1))
    sm = np.zeros((128, SM_L), np.float32)
    sm[:, SM_BMOD:SM_BMOD + 48] = inp["b_mod"][l].reshape(48, 128).T
    sm[:, SM_G1:SM_G1 + 8] = inp["g_norm1"][l].reshape(8, 128).T
    sm[:, SM_G2:SM_G2 + 8] = inp["g_norm2"][l].reshape(8, 128).T
    sm[:, SM_RET:SM_RET + 8] = inp["ret_decay_logit"][l].reshape(1, 8)
    sm[:, SM_SINK:SM_SINK + 16] = inp["swa_sink"][l].reshape(1, 16)
    sm[:, SM_GQ] = inp["g_qnorm"][l]
    sm[:, SM_GQ + 1] = inp["g_qnorm"][l][p128]
    sm[:, SM_GK] = inp["g_knorm"][l]
    sm[:, SM_GK + 1] = inp["g_knorm"][l][p128]
    return wall, we, sm


def make_consts(half):
    c = np.zeros((128, C_N), np.float32)
    m = np.arange(128)[:, None].astype(np.float32)
    n = np.arange(128)[None, :].astype(np.float32)
    c[:, C_ID:C_ID + 128] = np.eye(128)
    c[:, C_DPOS:C_DPOS + 128] = np.maximum(n - m, 0)
    c[:, C_DNEG:C_DNEG + 128] = np.maximum(m - n, 0)
    c[:, C_MF:C_MF + 128] = (n >= m)
    c[:, C_MB:C_MB + 128] = (m > n)
    c[:, C_IP1:C_IP1 + 128] = n + 1 + 0 * m
    c[:, C_IB:C_IB + 128] = 128 - n + 0 * m
    c[:, C_ML:C_ML + 128] = (m >= n)
    c[:, C_MR:C_MR + 128] = (m <= n)
    c[:, C_MLB:C_MLB + 128] = (m >= n) * (1.0 if half == 1 else 0.0)
    c[:, C_MRB:C_MRB + 128] = (m <= n) * (1.0 if half == 0 else 0.0)
    c[:, C_PCF] = 127 - np.arange(128)
    c[:, C_PCB] = np.arange(128)
    for e in range(16):
        c[e, C_SEL + e * 128:C_SEL + (e + 1) * 128] = 1.0
    return c


def make_tabs(half):
    pos_own = half * NOWN + np.arange(NOWN)
    pos_oth = (1 - half) * NOWN + np.arange(NOTH)
    pos = np.concatenate([pos_own, pos_oth])
    row = (pos // 64).astype(np.float32)
    col = (pos % 64).astype(np.float32)
    tabs = np.zeros((4, 128, NALL), np.float32)
    tabs[0, :, 4096:] = 1.0
    tabs[2, :, 4096:] = 1.0
    for ti, hd in ((0, 128), (2, 64)):
        half_d, quarter = hd // 2, hd // 4
        freqs = (np.float32(10000.0) ** (-np.arange(quarter, dtype=np.float32) / np.float32(quarter))).astype(np.float32)
        for f in range(128):
            fl = f % hd
            a = fl // half_d
            j = fl % half_d
            p = row if a == 0 else col
            ang = (p * freqs[j % quarter]).astype(np.float32)
            tabs[ti, f, :4096] = np.cos(ang)
            tabs[ti + 1, f, :4096] = np.sin(ang) * (-1.0 if j < quarter else 1.0)
    return tabs


def make_small_global(inp, b, half):
    sg = np.zeros((128, SG_N), np.float32)
    cT = inp["c"][b].reshape(8, 128).T
    ccT = inp["c_ctx"].reshape(8, 128).T
    sg[:, SG_C:SG_C + 16:2] = cT
    sg[:, SG_C + 1:SG_C + 16:2] = ccT
    sg[:, SG_WR:SG_WR + 128] = inp["w_router"].reshape(8, 128, 16).transpose(1, 0, 2).reshape(128, 128)
    sg[:, SG_BR:SG_BR + 16] = inp["b_router"].reshape(1, 16)
    sg[:, SG_GF:SG_GF + 8] = inp["g_final"].reshape(8, 128).T
    sg[:, SG_FLAG] = 1.0 if half == 0 else 0.0
    sg[:, SG_FLAG + 1] = 1.0 if half == 1 else 0.0
    return sg


TOK_TILES_ALL = [(i * 512, 512, 0) for i in range(8)] + [(4096, 256, 1)]
TOK_TILES_OWN = [(i * 512, 512, 0) for i in range(4)]
CTX_TILE = (4096, 256, 1)


def qpos(t0):
    return t0 if t0 < NOWN else t0 - NOTH


def build(layers, need_ctx_flags, final, dbg=(), stop=99):
    nc = bass.Bass("TRN2", target_bir_lowering=False)
    P = Prog(nc)
    nl = len(layers)

    def din(name, shape, dt=F32):
        return nc.dram_tensor(name, list(shape), dt, kind="ExternalInput").ap()

    def dscr(name, shape, dt=BF16):
        kind = "ExternalOutput" if name in dbg else "Internal"
        return nc.dram_tensor(name, list(shape), dt, kind=kind).ap()

    xall = din("xall", [D, NALL])
    tabs = din("tabs", [4, 128, NALL])
    consts_d = din("consts", [128, C_N])
    sg_d = din("sg", [128, SG_N])
    sm_d = [din(f"sm{l}", [128, SM_L]) for l in range(nl)]
    wall_d = [din(f"wall{l}", [N_GROUPS, 128, 8, 512]) for l in range(nl)]
    we_d = [din(f"we{l}", [16, 3, 128, 4096]) for l in range(nl)]
    if final:
        yout = nc.dram_tensor("yout", [D, NOWN], F32, kind="ExternalOutput").ap()
    else:
        xout = nc.dram_tensor("xout", [D, NQ], F32, kind="ExternalOutput").ap()

    xs_d = dscr("xs", [D, NALL], F32)
    fm_d = {u["name"]: dscr("fm_" + u["name"], [128, NALL]) for u in FM_UNITS}
    vall_d = dscr("vall", [NALL, 1536])
    obr_d = [dscr(f"obr{i}", [D, NQ]) for i in range(3)]
    web_d = dscr("web", [16, 3, 128, 4096])
    T_xs = {}

    def txs(t0):
        return T_xs.setdefault(t0, T(f"xs{t0}"))
    T_fm = {}

    def tfm(name, t0):
        return T_fm.setdefault((name, t0), T(f"fm{name}{t0}"))
    T_vall = {}

    def tvall(sub):
        return T_vall.setdefault(sub, T(f"vall{sub}"))
    T_obr = {}

    def tobr(i, t0):
        return T_obr.setdefault((i, t0), T(f"obr{i}_{t0}"))
    T_web = {}

    def tweb(e, k):
        return T_web.setdefault((e, k), T(f"web{e}_{k}"))

    xs3 = xs_d.rearrange("(kc p) t -> p kc t", p=128)
    if nl > 1:
        xch_src = [nc.dram_tensor(f"xch_src{i}", [D, 512], F32, kind="Internal").ap() for i in range(4)]
        xch_dst = [nc.dram_tensor(f"xch_dst{i}", [2 * D, 512], F32, kind="Internal").ap() for i in range(4)]
        T_xsrc, T_xdst = [T("xsrc") for i in range(4)], [T("xdst") for i in range(4)]
    xall3 = xall.rearrange("(kc p) t -> p kc t", p=128)

    A = Arena(nc, 206 * 1024)
    cst = A.alloc([128, C_N], F32, "cst")
    sg = A.alloc([128, SG_N], F32, "sg")
    ones_bf = A.alloc([128, 128], BF16, "ones")
    id_bf = A.alloc([128, 128], BF16, "idbf")
    mk_bf = A.alloc([128, 4, 512], BF16, "mk")
    PERS_END = None
    ps = [Buf(nc.alloc_psum_tensor(f"ps{i}", [128, 512], F32)[:], f"ps{i}", True) for i in range(8)]

    P.dma("sp", cst.ap, consts_d, writes=[cst.t])
    P.dma("sp", sg.ap, sg_d, writes=[sg.t])
    P.add("dve", lambda e: e.memset(ones_bf.ap, 1.0), writes=[ones_bf.t])
    P.copy("dve", id_bf.ap, cst.ap[:, C_ID:C_ID + 128], reads=[cst.t], writes=[id_bf.t])
    for i, co in enumerate((C_ML, C_MR, C_MLB, C_MRB)):
        for r in range(4):
            P.copy("dve", mk_bf.ap[:, i, r * 128:(r + 1) * 128], cst.ap[:, co:co + 128], reads=[cst.t],
                   writes=[mk_bf.t])
    for (t0, nt, _) in TOK_TILES_ALL:
        P.dma("sp", xs_d[:, t0:t0 + nt], xall[:, t0:t0 + nt], writes=[txs(t0)])
    PERS_END = A.off

    def rsqrt_from_psum(dst, src_ps, n, rd, wr):
        P.ts("dve", dst, src_ps, 1.0 / n, EPS, ALU.mult, ALU.add, reads=rd, writes=wr)
        P.act(dst, dst, AF.Ln, reads=wr, writes=wr)
        P.act(dst, dst, AF.Exp, reads=wr, writes=wr, scale=-0.5)

    psi = [0]

    def ps_next(k=8):
        psi[0] = (psi[0] + 1) % k
        return ps[psi[0]]

    for li, l in enumerate(layers):
        need_ctx = need_ctx_flags[li]
        last = (li == nl - 1)
        qtiles = TOK_TILES_OWN + ([CTX_TILE] if need_ctx else [])
        P.barrier()
        A.off = PERS_END
        sm = A.alloc([128, SM_L], F32, "sm")
        modT = A.alloc([128, 48, 2], F32, "modT")
        A1 = A.alloc([128, 8, 2], F32, "A1")
        A2 = A.alloc([128, 8, 2], F32, "A2")
        silc = A.alloc([128, 16], F32, "silc")
        lg = A.alloc([128, 8], F32, "lg")
        sinkx = A.alloc([128, 16], F32, "sinkx")
        P.dma("sp", sm.ap, sm_d[li], writes=[sm.t])
        P.act(silc.ap, sg.ap[:, SG_C:SG_C + 16], AF.Silu, reads=[sg.t], writes=[silc.t])
        silc3 = silc.ap.rearrange("p (k j) -> p k j", j=2)
        P.act(lg.ap, sm.ap[:, SM_RET:SM_RET + 8], AF.Exp, reads=[sm.t], writes=[lg.t], scale=-1.0)
        P.ts("dve", lg.ap, lg.ap, 1.0, None, ALU.add, reads=[lg.t], writes=[lg.t])
        P.act(lg.ap, lg.ap, AF.Ln, reads=[lg.t], writes=[lg.t])
        P.ts("dve", lg.ap, lg.ap, -1.0, None, ALU.mult, reads=[lg.t], writes=[lg.t])
        P.act(sinkx.ap, sm.ap[:, SM_SINK:SM_SINK + 16], AF.Exp, reads=[sm.t], writes=[sinkx.t])
        PH0_END = A.off
        wst = [A.alloc([128, 8, 512], F32, f"wst{i}") for i in range(2)]
        for g in range(12):
            w = wst[g % 2]
            P.dma("sp", w.ap, wall_d[li][G_MOD + g], writes=[w.t])
            for j in range(4):
                idx = g * 4 + j
                pb = ps_next()
                for kc in range(8):
                    P.mm(pb.ap[:, 0:2], w.ap[:, kc, j * 128:(j + 1) * 128], silc3[:, kc, :], kc == 0, kc == 7,
                         reads=[w.t, silc.t], writes=[pb.t])
                P.ts("dve", modT.ap[:, idx, :], pb.ap[:, 0:2], sm.ap[:, SM_BMOD + idx:SM_BMOD + idx + 1], None,
                     ALU.add, reads=[pb.t, sm.t], writes=[modT.t])
        for j in range(2):
            P.stt(A1.ap[:, :, j], modT.ap[:, 8:16, j], 1.0, sm.ap[:, SM_G1:SM_G1 + 8], ALU.add, ALU.mult,
                  reads=[modT.t, sm.t], writes=[A1.t])
            P.stt(A2.ap[:, :, j], modT.ap[:, 32:40, j], 1.0, sm.ap[:, SM_G2:SM_G2 + 8], ALU.add, ALU.mult,
                  reads=[modT.t, sm.t], writes=[A2.t])

        def SH1(kc, j): return modT.ap[:, 0 + kc, j:j + 1]
        def GT1(kc, j): return modT.ap[:, 16 + kc, j:j + 1]
        def SH2(kc, j): return modT.ap[:, 24 + kc, j:j + 1]
        def GT2(kc, j): return modT.ap[:, 40 + kc, j:j + 1]

        P.barrier()
        A.off = PH0_END
        hT = A.alloc([128, 8, NALL], BF16, "hT")
        hts = {t0: T(f"hT{t0}") for (t0, _, _) in TOK_TILES_ALL}
        xt = [A.alloc([128, 8, 512], F32, f"xt{i}") for i in range(2)]
        sq = A.alloc([128, 8, 512], BF16, "sq")
        rstd = A.alloc([128, 512], F32, "rstd")
        tmp = [A.alloc([128, 512], F32, f"tmp{i}") for i in range(2)]
        PH1_END = A.off

        def norm_tiles(tiles, Aco, SHf, out_fn, src3=xs3):
            for i, (t0, nt, mj) in enumerate(tiles):
                x_ = xt[i % 2]
                P.dma("sp", x_.ap[:, :, 0:nt], src3[:, :, t0:t0 + nt], reads=[txs(t0)], writes=[x_.t])
                P.act(sq.ap[:, :, 0:nt], x_.ap[:, :, 0:nt], AF.Square, reads=[x_.t], writes=[sq.t])
                pb = ps_next()
                for kc in range(8):
                    P.mm(pb.ap[:, 0:nt], ones_bf.ap, sq.ap[:, kc, 0:nt], kc == 0, kc == 7,
                         reads=[ones_bf.t, sq.t], writes=[pb.t])
                rsqrt_from_psum(rstd.ap[:, 0:nt], pb.ap[:, 0:nt], 1024.0, [pb.t], [rstd.t])
                for kc in range(8):
                    tm_ = tmp[kc % 2]
                    P.tt("dve" if kc % 2 == 0 else "pool", tm_.ap[:, 0:nt], x_.ap[:, kc, 0:nt], rstd.ap[:, 0:nt],
                         ALU.mult, reads=[x_.t, rstd.t], writes=[tm_.t])
                    out_fn(kc, t0, nt, mj, tm_, Aco.ap[:, kc, mj:mj + 1], SHf(kc, mj))

        def out_h1(kc, t0, nt, mj, tm_, a_, b_):
            P.act(hT.ap[:, kc, t0:t0 + nt], tm_.ap[:, 0:nt], AF.Identity, reads=[tm_.t, A1.t, modT.t],
                  writes=[hts[t0]], bias=b_, scale=a_)

        norm_tiles(TOK_TILES_ALL, A1, SH1, out_h1)

        A.off = PH1_END
        wbf = [A.alloc([128, 8, 512], BF16, f"wbf{i}") for i in range(3)]
        tabC = [A.alloc([128, 512], F32, f"tabC{i}") for i in range(2)]
        tabS = [A.alloc([128, 512], F32, f"tabS{i}") for i in range(2)]
        stage = [A.alloc([128, 512], BF16, f"stage{i}") for i in range(3)]
        t1b = [A.alloc([128, 512], F32, f"t1b{i}") for i in range(2)]
        t2b = [A.alloc([128, 512], F32, f"t2b{i}") for i in range(2)]
        sqb = A.alloc([128, 512], BF16, "sqb")
        rs2 = A.alloc([128, 512], F32, "rs2")
        cnt = [0]

        def load_group(gi):
            wb = wbf[gi % 3]
            P.dma("pool", wb.ap, wall_d[li][gi], writes=[wb.t])
            return wb

        nxt = load_group(G_FM)
        for gi in range(N_FM_GROUPS):
            wb = nxt
            nxt = load_group(G_FM + gi + 1)
            for u in [x for x in FM_UNITS if x["group"] == gi]:
                tiles = TOK_TILES_ALL if u["tok"] == "all" else qtiles
                kind = u["kind"]
                dual = u["c1"] is not None
                tsel = 2 if kind == "rope64" else 0
                for (t0, nt, mj) in tiles:
                    k = cnt[0]
                    cnt[0] += 1
                    pa = ps_next()
                    for kc in range(8):
                        P.mm(pa.ap[:, 0:nt], wb.ap[:, kc, u["c0"]:u["c0"] + 128], hT.ap[:, kc, t0:t0 + nt],
                             kc == 0, kc == 7, reads=[wb.t, hts[t0]], writes=[pa.t])
                    if dual:
                        pbk = ps_next()
                        for kc in range(8):
                            P.mm(pbk.ap[:, 0:nt], wb.ap[:, kc, u["c1"]:u["c1"] + 128], hT.ap[:, kc, t0:t0 + nt],
                                 kc == 0, kc == 7, reads=[wb.t, hts[t0]], writes=[pbk.t])
                        tc_, ts_ = tabC[k % 2], tabS[k % 2]
                        P.dma("sp", tc_.ap[:, 0:nt], tabs[tsel, :, t0:t0 + nt], writes=[tc_.t])
                        P.dma("sp", ts_.ap[:, 0:nt], tabs[tsel + 1, :, t0:t0 + nt], writes=[ts_.t])
                    st = stage[k % 3]
                    o_ = st.ap[:, 0:nt]
                    if kind in ("rope128", "rope64"):
                        a_, b_ = t1b[k % 2], t2b[k % 2]
                        P.tt("dve", a_.ap[:, 0:nt], pa.ap[:, 0:nt], tc_.ap[:, 0:nt], ALU.mult,
                             reads=[pa.t, tc_.t], writes=[a_.t])
                        P.tt("dve", b_.ap[:, 0:nt], pbk.ap[:, 0:nt], ts_.ap[:, 0:nt], ALU.mult,
                             reads=[pbk.t, ts_.t], writes=[b_.t])
                        P.tt("pool", o_, a_.ap[:, 0:nt], b_.ap[:, 0:nt], ALU.add, reads=[a_.t, b_.t], writes=[st.t])
                    elif kind in ("normq", "normk"):
                        gco = SM_GQ if kind == "normq" else SM_GK
                        a_, b_ = t1b[k % 2], t2b[k % 2]
                        P.act(sqb.ap[:, 0:nt], pa.ap[:, 0:nt], AF.Square, reads=[pa.t], writes=[sqb.t])
                        pc = ps_next()
                        P.mm(pc.ap[:, 0:nt], ones_bf.ap, sqb.ap[:, 0:nt], True, True, reads=[ones_bf.t, sqb.t],
                             writes=[pc.t])
                        rsqrt_from_psum(rs2.ap[:, 0:nt], pc.ap[:, 0:nt], 128.0, [pc.t], [rs2.t])
                        P.stt(a_.ap[:, 0:nt], pa.ap[:, 0:nt], sm.ap[:, gco:gco + 1], tc_.ap[:, 0:nt], ALU.mult,
                              ALU.mult, reads=[pa.t, tc_.t, sm.t], writes=[a_.t])
                        P.stt(b_.ap[:, 0:nt], pbk.ap[:, 0:nt], sm.ap[:, gco + 1:gco + 2], ts_.ap[:, 0:nt], ALU.mult,
                              ALU.mult, reads=[pbk.t, ts_.t, sm.t], writes=[b_.t])
                        P.tt("pool", a_.ap[:, 0:nt], a_.ap[:, 0:nt], b_.ap[:, 0:nt], ALU.add, reads=[a_.t, b_.t],
                             writes=[a_.t])
                        P.tt("pool", o_, a_.ap[:, 0:nt], rs2.ap[:, 0:nt], ALU.mult, reads=[a_.t, rs2.t],
                             writes=[st.t])
                    elif kind == "silu":
                        P.act(o_, pa.ap[:, 0:nt], AF.Silu, reads=[pa.t], writes=[st.t])
                    else:
                        P.act(o_, pa.ap[:, 0:nt], AF.Sigmoid, reads=[pa.t], writes=[st.t])
                    P.dma("pool", fm_d[u["name"]][:, t0:t0 + nt], o_, reads=[st.t], writes=[tfm(u["name"], t0)])
        for gi in range(N_TM_GROUPS):
            wb = nxt
            if gi + 1 < N_TM_GROUPS:
                nxt = load_group(G_TM + gi + 1)
            for sub in range(NALL // 128):
                t0 = sub * 128
                tile0 = (t0 // 512) * 512
                k = cnt[0]
                cnt[0] += 1
                pa = ps_next()
                for kc in range(8):
                    P.mm(pa.ap, hT.ap[:, kc, t0:t0 + 128], wb.ap[:, kc, :], kc == 0, kc == 7,
                         reads=[wb.t, hts[tile0]], writes=[pa.t])
                st = stage[k % 3]
                P.copy("act" if k % 2 == 0 else "dve", st.ap, pa.ap, reads=[pa.t], writes=[st.t])
                P.dma("pool", vall_d[t0:t0 + 128, gi * 512:(gi + 1) * 512], st.ap, reads=[st.t],
                      writes=[tvall(sub)])
        P.barrier()
        if stop <= 2:
            continue
        A.off = PH0_END
        KSCALE = 128.0 ** -0.5
        KT = A.alloc([128, NALL], BF16, "KT")
        QT = A.alloc([128, NALL], BF16, "QT")
        Vr = A.alloc([128, 34, 256], BF16, "Vr")
        Ur = A.alloc([128, 2, NALL], BF16, "Ur")
        Kf = A.alloc([128, 34, 128], BF16, "Kf")
        Kb = A.alloc([128, 34, 128], BF16, "Kb")
        SFb = A.alloc([128, 18, 256], BF16, "SFb")
        SBb = A.alloc([128, 18, 256], BF16, "SBb")
        Dm = A.alloc([128, 512], BF16, "Dm")
        qdf = A.alloc([128, 512], F32, "qdf")
        qdb = A.alloc([128, 512], F32, "qdb")
        e1 = A.alloc([128, 128], F32, "e1")
        e2 = A.alloc([128, 128], F32, "e2")
        kd = A.alloc([128, 4], F32, "kd")
        S = A.alloc([128, 256], F32, "S")
        S2 = [A.alloc([128, 256], F32, f"S2{i}") for i in range(2)]
        KVf = A.alloc([128, 34, 256], F32, "KVf")
        KVb = A.alloc([128, 34, 256], F32, "KVb")
        S0f = A.alloc([128, 256], F32, "S0f")
        S0b = A.alloc([128, 256], F32, "S0b")
        Pm = A.alloc([128, 512], BF16, "Pm")
        Qf = A.alloc([128, 512], BF16, "Qf")
        Qb = A.alloc([128, 512], BF16, "Qb")
        sq2 = A.alloc([128, 2, 512], BF16, "sq2")
        rs3 = A.alloc([128, 512], F32, "rs3")
        to_ = [A.alloc([128, 512], F32, f"to{i}") for i in range(2)]
        stg = [A.alloc([128, 512], BF16, f"stg{i}") for i in range(2)]
        flg = sg.ap[:, SG_FLAG:SG_FLAG + 2]
        for h in range(4):
            lgf, lgb = lg.ap[:, h:h + 1], lg.ap[:, 4 + h:5 + h]
            P.act(e1.ap, cst.ap[:, C_DPOS:C_DPOS + 128], AF.Exp, reads=[cst.t, lg.t], writes=[e1.t], scale=lgf)
            P.tt("dve", e1.ap, e1.ap, cst.ap[:, C_MF:C_MF + 128], ALU.mult, reads=[e1.t, cst.t], writes=[e1.t])
            P.act(e2.ap, cst.ap[:, C_DNEG:C_DNEG + 128], AF.Exp, reads=[cst.t, lg.t], writes=[e2.t], scale=lgb)
            P.tt("dve", e2.ap, e2.ap, cst.ap[:, C_MB:C_MB + 128], ALU.mult, reads=[e2.t, cst.t], writes=[e2.t])
            P.tt("dve", e1.ap, e1.ap, e2.ap, ALU.add, reads=[e1.t, e2.t], writes=[e1.t])
            for r in range(4):
                P.ts("dve", Dm.ap[:, r * 128:(r + 1) * 128], e1.ap, KSCALE, None, ALU.mult, reads=[e1.t],
                     writes=[Dm.t])
                P.act(qdf.ap[:, r * 128:(r + 1) * 128], cst.ap[:, C_IP1:C_IP1 + 128], AF.Exp, reads=[cst.t, lg.t],
                      writes=[qdf.t], scale=lgf)
                P.act(qdb.ap[:, r * 128:(r + 1) * 128], cst.ap[:, C_IB:C_IB + 128], AF.Exp, reads=[cst.t, lg.t],
                      writes=[qdb.t], scale=lgb)
            P.act(kd.ap[:, 0:1], cst.ap[:, C_PCF:C_PCF + 1], AF.Exp, reads=[cst.t, lg.t], writes=[kd.t], scale=lgf)
            P.act(kd.ap[:, 1:2], cst.ap[:, C_PCB:C_PCB + 1], AF.Exp, reads=[cst.t, lg.t], writes=[kd.t], scale=lgb)
            P.ts("dve", kd.ap[:, 0:2], kd.ap[:, 0:2], KSCALE, None, ALU.mult, reads=[kd.t], writes=[kd.t])
            P.act(kd.ap[:, 2:3], lgf, AF.Exp, reads=[lg.t], writes=[kd.t], scale=128.0)
            P.act(kd.ap[:, 3:4], lgb, AF.Exp, reads=[lg.t], writes=[kd.t], scale=128.0)
            P.dma("sp", KT.ap, fm_d[f"kr{h}"], writes=[KT.t])
            P.dma("sp", QT.ap[:, 0:NOWN], fm_d[f"qr{h}"][:, 0:NOWN], writes=[QT.t])
            if need_ctx:
                P.dma("sp", QT.ap[:, 4096:NALL], fm_d[f"qr{h}"][:, 4096:NALL], writes=[QT.t])
            vsrc = vall_d[:, h * 256:(h + 1) * 256].rearrange("(t p) c -> p t c", p=128)
            for tq in range(0, 34, 4):
                P.dma("sp", Vr.ap[:, tq:min(tq + 4, 34), :], vsrc[:, tq:min(tq + 4, 34), :], writes=[Vr.t])
            for j in range(2):
                P.dma("sp", Ur.ap[:, j, 0:NOWN], fm_d[f"ur{2 * h + j}"][:, 0:NOWN], writes=[Ur.t])
                if need_ctx:
                    P.dma("sp", Ur.ap[:, j, 4096:NALL], fm_d[f"ur{2 * h + j}"][:, 4096:NALL], writes=[Ur.t])
            for c0 in (range(0, 32 if 'trlast' in SKIP else 34, 4) if 'tr' not in SKIP else []):
                n = min(4, 34 - c0)
                pb_ = ps_next()
                for c in range(n):
                    P.mm(pb_.ap[:, c * 128:(c + 1) * 128], KT.ap[:, (c0 + c) * 128:(c0 + c + 1) * 128], id_bf.ap,
                         True, True, reads=[KT.t, id_bf.t], writes=[pb_.t])
                P.ts("dve", Kf.ap[:, c0:c0 + n, :], pb_.ap[:, 0:n * 128].rearrange("p (a b) -> p a b", a=n),
                     kd.ap[:, 0:1], None, ALU.mult, reads=[pb_.t, kd.t], writes=[Kf.t])
                P.act(Kb.ap[:, c0:c0 + n, :], pb_.ap[:, 0:n * 128].rearrange("p (a b) -> p a b", a=n), AF.Identity,
                      reads=[pb_.t, kd.t], writes=[Kb.t], scale=kd.ap[:, 1:2])

            for kvi, (Kx, KV) in enumerate(((Kf, KVf), (Kb, KVb))):
                for c0 in range(0, 34, 2):
                    pb_ = ps_next()
                    for c in range(2):
                        P.mm(pb_.ap[:, c * 256:(c + 1) * 256], Kx.ap[:, c0 + c, :], Vr.ap[:, c0 + c, :], True, True,
                             reads=[Kx.t, Vr.t], writes=[pb_.t])
                    P.copy("act" if (c0 // 2) % 2 == 0 else "dve", KV.ap[:, c0:c0 + 2, :],
                           pb_.ap.rearrange("p (a b) -> p a b", a=2), reads=[pb_.t], writes=[KV.t])
            cur = [0]

            def upd(Kx, c, cdcol, first):
                KV = KVf if Kx is Kf else KVb
                if first:
                    P.copy("dve", S2[cur[0]].ap, KV.ap[:, c, :], reads=[KV.t], writes=[S2[cur[0]].t])
                else:
                    nxt_ = 1 - cur[0]
                    P.stt(S2[nxt_].ap, S2[cur[0]].ap, kd.ap[:, cdcol:cdcol + 1], KV.ap[:, c, :], ALU.mult, ALU.add,
                          reads=[S2[cur[0]].t, kd.t, KV.t], writes=[S2[nxt_].t])
                    cur[0] = nxt_

            def snap(dst, idx):
                P.copy("dve", dst.ap[:, idx, :], S2[cur[0]].ap, reads=[S2[cur[0]].t], writes=[dst.t])

            def blend(S0, fcol):
                a, b = S2[cur[0]], S2[1 - cur[0]]
                P.tt("dve", b.ap, a.ap, S0.ap, ALU.subtract, reads=[a.t, S0.t], writes=[b.t])
                P.stt(a.ap, b.ap, flg[:, fcol:fcol + 1], S0.ap, ALU.mult, ALU.add, reads=[b.t, S0.t, sg.t],
                      writes=[a.t])

            upd(Kf, 32, 2, True)
            snap(SFb, 17)
            upd(Kf, 33, 2, False)
            P.copy("dve", S0f.ap, S2[cur[0]].ap, reads=[S2[cur[0]].t], writes=[S0f.t])
            for c in range(16, 32):
                upd(Kf, c, 2, False)
            blend(S0f, 1)
            for c in range(0, 16):
                snap(SFb, c)
                if c < 15:
                    upd(Kf, c, 2, False)
            upd(Kb, 33, 3, True)
            snap(SBb, 16)
            upd(Kb, 32, 3, False)
            P.copy("dve", S0b.ap, S2[cur[0]].ap, reads=[S2[cur[0]].t], writes=[S0b.t])
            for c in range(31, 15, -1):
                upd(Kb, c, 3, False)
            blend(S0b, 0)
            for c in range(15, -1, -1):
                snap(SBb, c)
                if c > 0:
                    upd(Kb, c, 3, False)
            for (t0, nt, mj) in (qtiles if 'out' not in SKIP else []):
                ncx = nt // 128
                pS = ps_next()
                for j in range(ncx):
                    sl = slice(t0 + j * 128, t0 + (j + 1) * 128)
                    P.mm(pS.ap[:, j * 128:(j + 1) * 128], KT.ap[:, sl], QT.ap[:, sl], True, True,
                         reads=[KT.t, QT.t], writes=[pS.t])
                P.tt("dve", Pm.ap[:, 0:nt], pS.ap[:, 0:nt], Dm.ap[:, 0:nt], ALU.mult, reads=[pS.t, Dm.t],
                     writes=[Pm.t])
                P.tt("pool", Qf.ap[:, 0:nt], QT.ap[:, t0:t0 + nt], qdf.ap[:, 0:nt], ALU.mult, reads=[QT.t, qdf.t],
                     writes=[Qf.t])
                P.tt("pool", Qb.ap[:, 0:nt], QT.ap[:, t0:t0 + nt], qdb.ap[:, 0:nt], ALU.mult, reads=[QT.t, qdb.t],
                     writes=[Qb.t])
                pO = [ps_next(), ps_next()]
                for dj in range(2):
                    dsl = slice(dj * 128, (dj + 1) * 128)
                    for j in range(ncx):
                        c = t0 // 128 + j
                        csl = slice(j * 128, (j + 1) * 128)
                        if c < 16:
                            sf, sb = c, c
                        elif c == 32:
                            sf, sb = None, 16
                        else:
                            sf, sb = 17, None
                        terms = [(Vr.ap[:, c, dsl], Pm.ap[:, csl], [Vr.t, Pm.t])]
                        if sf is not None:
                            terms.append((SFb.ap[:, sf, dsl], Qf.ap[:, csl], [SFb.t, Qf.t]))
                        if sb is not None:
                            terms.append((SBb.ap[:, sb, dsl], Qb.ap[:, csl], [SBb.t, Qb.t]))
                        for ti, (l_, r_, rd) in enumerate(terms):
                            P.mm(pO[dj].ap[:, csl], l_, r_, ti == 0, ti == len(terms) - 1, reads=rd,
                                 writes=[pO[dj].t])
                    P.act(sq2.ap[:, dj, 0:nt], pO[dj].ap[:, 0:nt], AF.Square, reads=[pO[dj].t], writes=[sq2.t])
                pN = ps_next()
                for dj in range(2):
                    P.mm(pN.ap[:, 0:nt], ones_bf.ap, sq2.ap[:, dj, 0:nt], dj == 0, dj == 1,
                         reads=[ones_bf.t, sq2.t], writes=[pN.t])
                rsqrt_from_psum(rs3.ap[:, 0:nt], pN.ap[:, 0:nt], 256.0, [pN.t], [rs3.t])
                for dj in range(2):
                    P.tt("dve", to_[dj].ap[:, 0:nt], pO[dj].ap[:, 0:nt], rs3.ap[:, 0:nt], ALU.mult,
                         reads=[pO[dj].t, rs3.t], writes=[to_[dj].t])
                    P.tt("pool", stg[dj].ap[:, 0:nt], to_[dj].ap[:, 0:nt], Ur.ap[:, dj, t0:t0 + nt], ALU.mult,
                         reads=[to_[dj].t, Ur.t], writes=[stg[dj].t])
                    r0 = (2 * h + dj) * 128
                    P.dma("sp", obr_d[0][r0:r0 + 128, qpos(t0):qpos(t0) + nt], stg[dj].ap[:, 0:nt],
                          reads=[stg[dj].t], writes=[tobr(0, (h, dj, t0))])
        P.barrier()
        if stop <= 3:
            continue
        def run_attn(groups, Pt, depth=2):
            items = [(gi, ki) for gi, g in enumerate(groups) for ki in range(g["n"])]
            pts = {}
            sidx = [0]

            def SE(idx):
                gi, ki = items[idx]
                g = groups[gi]
                pS = ps[sidx[0] % 4]
                sidx[0] += 1
                g["S"](ki, pS)
                pt = Pt[idx % len(Pt)]
                g["E"](ki, pS, pt)
                pts[idx] = pt
            for idx in range(min(depth, len(items))):
                SE(idx)
            for idx in range(len(items)):
                if idx + depth < len(items):
                    SE(idx + depth)
                gi, ki = items[idx]
                g = groups[gi]
                pO, pD = (ps[4], ps[5]) if gi % 2 == 0 else (ps[6], ps[7])
                g["PV"](ki, pts.pop(idx), pO, pD, ki == 0, ki == g["n"] - 1)
                if ki == g["n"] - 1:
                    g["epi"](pO, pD)

        A.off = PH0_END
        KSa = A.alloc([128, NALL], BF16, "KSa")
        KSb = A.alloc([128, NALL], BF16, "KSb")
        QS = A.alloc([128, 4, NALL], BF16, "QS")
        Vs = A.alloc([128, 34, 64], BF16, "Vs")
        Pt = [A.alloc([128, 512], BF16, f"Pt{i}") for i in range(6)]
        sk = [A.alloc([64, 512], F32, f"sk{i}") for i in range(2)]
        den = [A.alloc([128, 512], F32, f"den{i}") for i in range(2)]
        ostg = [A.alloc([128, 512], BF16, f"ostg{i}") for i in range(2)]
        P.dma("sp", KSa.ap, fm_d["ks"], writes=[KSa.t])
        P.dma("sp", KSb.ap, fm_d["ks2"], writes=[KSb.t])
        obr1 = obr_d[1].rearrange("(c p) t -> p c t", p=128)
        gcount = 0
        for g in range(2):
            for i in range(4):
                P.dma("sp", QS.ap[:, i, 0:NOWN], fm_d[f"qs{g * 4 + i}"][:, 0:NOWN], writes=[QS.t])
                if need_ctx:
                    P.dma("sp", QS.ap[:, i, 4096:NALL], fm_d[f"qs{g * 4 + i}"][:, 4096:NALL], writes=[QS.t])
            vsrc = vall_d[:, 1024 + g * 64:1024 + (g + 1) * 64].rearrange("(t p) c -> p t c", p=128)
            for tq in range(0, 34, 4):
                P.dma("sp", Vs.ap[:, tq:min(tq + 4, 34), :], vsrc[:, tq:min(tq + 4, 34), :], writes=[Vs.t])
            groups = []
            for par in range(2):
                pb0 = par * 64
                Ksrc = KSa if g == par else KSb
                sk_ = sk[par]
                for i in range(4):
                    hq = g * 8 + par + 2 * i
                    P.act(sk_.ap[:, i * 128:(i + 1) * 128], cst.ap[0:64, C_DPOS:C_DPOS + 128], AF.Identity,
                          reads=[cst.t, sinkx.t], writes=[sk_.t], bias=sinkx.ap[0:64, hq:hq + 1], scale=0.0)
                blocks = [(jb * 128, jb) for jb in range(16)] + ([(4096, 16), (4224, 17)] if need_ctx else [])
                for (t0, jb) in blocks:
                    if jb < 16:
                        kts = [((jb - 1) * 128, 0) if jb > 0 else (2048 + 15 * 128, 2), (jb * 128, None),
                               ((jb + 1) * 128, 1) if jb < 15 else (2048, 3), (4096, None), (4224, None)]
                    else:
                        kts = [(4096, None), (4224, None)]

                    def S(ki, pS, kts=kts, pb0=pb0, Ksrc=Ksrc, t0=t0):
                        k0 = kts[ki][0]
                        P.mm(pS.ap.rearrange("p (a b) -> p a b", a=4), Ksrc.ap[pb0:pb0 + 64, k0:k0 + 128],
                             QS.ap[pb0:pb0 + 64, :, t0:t0 + 128], True, True, reads=[Ksrc.t, QS.t], writes=[pS.t])

                    def E(ki, pS, pt, kts=kts):
                        P.act(pt.ap, pS.ap, AF.Exp, reads=[pS.t], writes=[pt.t], scale=0.125)
                        mi = kts[ki][1]
                        if mi is not None:
                            P.tt("pool", pt.ap, pt.ap, mk_bf.ap[:, mi, :], ALU.mult, reads=[pt.t, mk_bf.t],
                                 writes=[pt.t])

                    def PV(ki, pt, pO, pD, first, lastk, kts=kts):
                        k0 = kts[ki][0]
                        P.mm(pO.ap[0:64, :], Vs.ap[:, k0 // 128, :], pt.ap, first, lastk, reads=[Vs.t, pt.t],
                             writes=[pO.t])
                        P.mm(pD.ap[0:64, :], ones_bf.ap[:, 0:64], pt.ap, first, lastk, reads=[ones_bf.t, pt.t],
                             writes=[pD.t])

                    def epi(pO, pD, t0=t0, pb0=pb0, g=g, sk_=sk_, gc=gcount):
                        dn_, os_ = den[gc % 2], ostg[gc % 2]
                        P.tt("dve", dn_.ap[0:64, :], pD.ap[0:64, :], sk_.ap, ALU.add, reads=[pD.t, sk_.t],
                             writes=[dn_.t])
                        P.act(dn_.ap[0:64, :], dn_.ap[0:64, :], AF.Ln, reads=[dn_.t], writes=[dn_.t])
                        P.act(dn_.ap[0:64, :], dn_.ap[0:64, :], AF.Exp, reads=[dn_.t], writes=[dn_.t], scale=-1.0)
                        P.tt("dve", os_.ap[0:64, :], pO.ap[0:64, :], dn_.ap[0:64, :], ALU.mult,
                             reads=[pO.t, dn_.t], writes=[os_.t])
                        q0 = qpos(t0)
                        P.dma("sp", obr1[pb0:pb0 + 64, g * 4:g * 4 + 4, q0:q0 + 128],
                              os_.ap[0:64, :].rearrange("p (a b) -> p a b", a=4), reads=[os_.t],
                              writes=[tobr(1, (g, pb0, t0))])
                    groups.append(dict(n=len(kts), S=S, E=E, PV=PV, epi=epi))
                    gcount += 1
            run_attn(groups, Pt, depth=3)
        P.barrier()
        if stop <= 4:
            continue
        A.off = PH0_END
        KA = A.alloc([128, NALL], BF16, "KA")
        VA = A.alloc([128, 34, 128], BF16, "VA")
        QA = [A.alloc([128, NALL], BF16, f"QA{i}") for i in range(4)]
        Pt = [A.alloc([128, 512], BF16, f"Pt{i}") for i in range(4)]
        den = [A.alloc([128, 512], F32, f"den{i}") for i in range(2)]
        ostg = [A.alloc([128, 512], BF16, f"ostg{i}") for i in range(2)]
        GSCALE = 128.0 ** -0.5
        gcount = 0
        for g in range(2):
            P.dma("sp", KA.ap, fm_d[f"ka{g}"], writes=[KA.t])
            vsrc = vall_d[:, 1152 + g * 128:1152 + (g + 1) * 128].rearrange("(t p) c -> p t c", p=128)
            for tq in range(0, 34, 4):
                P.dma("sp", VA.ap[:, tq:min(tq + 4, 34), :], vsrc[:, tq:min(tq + 4, 34), :], writes=[VA.t])
            groups = []
            for hh in range(4):
                h = g * 4 + hh
                Q_ = QA[hh]
                P.dma("sp", Q_.ap[:, 0:NOWN], fm_d[f"qa{h}"][:, 0:NOWN], writes=[Q_.t])
                if need_ctx:
                    P.dma("sp", Q_.ap[:, 4096:NALL], fm_d[f"qa{h}"][:, 4096:NALL], writes=[Q_.t])
                for (t0, nt, mj) in qtiles:
                    kts = list(range(34)) if t0 < 4096 else [32, 33]

                    def S(ki, pS, kts=kts, Q_=Q_, t0=t0, nt=nt):
                        kt = kts[ki]
                        P.mm(pS.ap[:, 0:nt], KA.ap[:, kt * 128:(kt + 1) * 128], Q_.ap[:, t0:t0 + nt], True, True,
                             reads=[KA.t, Q_.t], writes=[pS.t])

                    def E(ki, pS, pt, nt=nt):
                        P.act(pt.ap[:, 0:nt], pS.ap[:, 0:nt], AF.Exp, reads=[pS.t], writes=[pt.t], scale=GSCALE)

                    def PV(ki, pt, pO, pD, first, lastk, kts=kts, nt=nt):
                        kt = kts[ki]
                        P.mm(pO.ap[:, 0:nt], VA.ap[:, kt, :], pt.ap[:, 0:nt], first, lastk, reads=[VA.t, pt.t],
                             writes=[pO.t])
                        P.mm(pD.ap[:, 0:nt], ones_bf.ap, pt.ap[:, 0:nt], first, lastk, reads=[ones_bf.t, pt.t],
                             writes=[pD.t])

                    def epi(pO, pD, t0=t0, nt=nt, h=h, gc=gcount):
                        dn_, os_ = den[gc % 2], ostg[gc % 2]
                        P.act(dn_.ap[:, 0:nt], pD.ap[:, 0:nt], AF.Ln, reads=[pD.t], writes=[dn_.t])
                        P.act(dn_.ap[:, 0:nt], dn_.ap[:, 0:nt], AF.Exp, reads=[dn_.t], writes=[dn_.t], scale=-1.0)
                        P.tt("dve", os_.ap[:, 0:nt], pO.ap[:, 0:nt], dn_.ap[:, 0:nt], ALU.mult,
                             reads=[pO.t, dn_.t], writes=[os_.t])
                        q0 = qpos(t0)
                        P.dma("sp", obr_d[2][h * 128:(h + 1) * 128, q0:q0 + nt], os_.ap[:, 0:nt], reads=[os_.t],
                              writes=[tobr(2, (h, t0))])
                    groups.append(dict(n=len(kts), S=S, E=E, PV=PV, epi=epi))
                    gcount += 1
            run_attn(groups, Pt)
        P.barrier()
        if stop <= 5:
            continue
        A.off = PH0_END
        wm = [A.alloc([128, 8, 1024], BF16, f"wm{i}") for i in range(4)]
        ob = [A.alloc([128, 8, 512], BF16, f"ob{i}") for i in range(3)]
        gb = [A.alloc([128, 8, 512], BF16, f"gb{i}") for i in range(3)]
        ypre = A.alloc([128, 8, 512], BF16, "ypre")
        xtl = A.alloc([128, 8, 512], F32, "xtl")
        ya = [A.alloc([128, 512], F32, f"ya{i}") for i in range(2)]
        tb = [A.alloc([128, 512], F32, f"tb{i}") for i in range(2)]
        for gi in range(8):
            dst = wm[gi // 2].ap[:, :, (gi % 2) * 512:(gi % 2 + 1) * 512]
            P.dma("pool", dst, wall_d[li][G_MERGE + gi], writes=[wm[gi // 2].t])
        gnames = ("ar", "as", "aa")
        for (t0, nt, mj) in qtiles:
            q0 = qpos(t0)
            for b_ in range(3):
                P.dma("sp", ob[b_].ap[:, :, 0:nt],
                      obr_d[b_].rearrange("(kc p) t -> p kc t", p=128)[:, :, q0:q0 + nt], writes=[ob[b_].t])
                for c in range(8):
                    P.dma("sp", gb[b_].ap[:, c, 0:nt], fm_d[f"{gnames[b_]}{c}"][:, t0:t0 + nt], writes=[gb[b_].t])
            P.dma("sp", xtl.ap[:, :, 0:nt], xs3[:, :, t0:t0 + nt], reads=[txs(t0)], writes=[xtl.t])
            for dc in range(8):
                dsl = slice(dc * 128, (dc + 1) * 128)
                y_ = ya[dc % 2]
                for b_ in range(3):
                    pa = ps_next()
                    for kc in range(8):
                        P.mm(pa.ap[:, 0:nt], wm[b_].ap[:, kc, dsl], ob[b_].ap[:, kc, 0:nt], kc == 0, kc == 7,
                             reads=[wm[b_].t, ob[b_].t], writes=[pa.t])
                    if b_ == 0:
                        P.tt("dve", y_.ap[:, 0:nt], pa.ap[:, 0:nt], gb[0].ap[:, dc, 0:nt], ALU.mult,
                             reads=[pa.t, gb[0].t], writes=[y_.t])
                    else:
                        t_ = tb[b_ % 2]
                        P.tt("dve", t_.ap[:, 0:nt], pa.ap[:, 0:nt], gb[b_].ap[:, dc, 0:nt], ALU.mult,
                             reads=[pa.t, gb[b_].t], writes=[t_.t])
                        if b_ == 1:
                            P.tt("pool", y_.ap[:, 0:nt], y_.ap[:, 0:nt], t_.ap[:, 0:nt], ALU.add,
                                 reads=[y_.t, t_.t], writes=[y_.t])
                        else:
                            P.tt("pool", ypre.ap[:, dc, 0:nt], y_.ap[:, 0:nt], t_.ap[:, 0:nt], ALU.add,
                                 reads=[y_.t, t_.t], writes=[ypre.t])
            for dc in range(8):
                dsl = slice(dc * 128, (dc + 1) * 128)
                py = ps_next()
                for kc in range(8):
                    P.mm(py.ap[:, 0:nt], wm[3].ap[:, kc, dsl], ypre.ap[:, kc, 0:nt], kc == 0, kc == 7,
                         reads=[wm[3].t, ypre.t], writes=[py.t])
                P.stt(xtl.ap[:, dc, 0:nt], py.ap[:, 0:nt], GT1(dc, mj), xtl.ap[:, dc, 0:nt], ALU.mult, ALU.add,
                      reads=[py.t, modT.t, xtl.t], writes=[xtl.t])
            P.dma("pool", xs3[:, :, t0:t0 + nt], xtl.ap[:, :, 0:nt], reads=[xtl.t], writes=[txs(t0)])
        P.barrier()
        if stop <= 6:
            continue
        A.off = PH0_END
        h2T = A.alloc([128, 8, NQ], BF16, "h2T")
        xres = A.alloc([128, 8, NQ], F32, "xres")
        WT = A.alloc([16, NQ], F32, "WT")
        M0 = A.off
        h2f = A.alloc([128, 8, 512], F32, "h2f")
        sq = A.alloc([128, 8, 512], BF16, "sq")
        rstd = A.alloc([128, 512], F32, "rstd")
        tmp = [A.alloc([128, 512], F32, f"tmp{i}") for i in range(2)]
        rt = {nm: A.alloc([128, 16], F32, "rt_" + nm) for nm in ("s", "bz", "eq", "msk", "ch", "ws", "wts")}
        rs = {nm: A.alloc([128, 4], F32, "rs_" + nm) for nm in ("m1", "m2", "gs", "gsel", "gm", "dn")}

        def v3(b_):
            return b_.ap.rearrange("p (g k) -> p g k", k=4)

        def bc(b_):
            return b_.ap[:, 0:4].unsqueeze(2).to_broadcast([128, 4, 4])
        mtiles = qtiles
        for i, (t0, nt, mj) in enumerate(mtiles):
            q0 = qpos(t0)
            P.dma("sp", xres.ap[:, :, q0:q0 + nt], xs3[:, :, t0:t0 + nt], reads=[txs(t0)], writes=[xres.t])
            P.act(sq.ap[:, :, 0:nt], xres.ap[:, :, q0:q0 + nt], AF.Square, reads=[xres.t], writes=[sq.t])
            pb = ps_next()
            for kc in range(8):
                P.mm(pb.ap[:, 0:nt], ones_bf.ap, sq.ap[:, kc, 0:nt], kc == 0, kc == 7, reads=[ones_bf.t, sq.t],
                     writes=[pb.t])
            rsqrt_from_psum(rstd.ap[:, 0:nt], pb.ap[:, 0:nt], 1024.0, [pb.t], [rstd.t])
            for kc in range(8):
                tm_ = tmp[kc % 2]
                P.tt("dve", tm_.ap[:, 0:nt], xres.ap[:, kc, q0:q0 + nt], rstd.ap[:, 0:nt], ALU.mult,
                     reads=[xres.t, rstd.t], writes=[tm_.t])
                P.act(h2f.ap[:, kc, 0:nt], tm_.ap[:, 0:nt], AF.Identity, reads=[tm_.t, A2.t, modT.t], writes=[h2f.t],
                      bias=SH2(kc, mj), scale=A2.ap[:, kc, mj:mj + 1])
                P.copy("pool", h2T.ap[:, kc, q0:q0 + nt], h2f.ap[:, kc, 0:nt], reads=[h2f.t], writes=[h2T.t])
            for sub in range(nt // 128):
                ssl = slice(sub * 128, (sub + 1) * 128)
                pr = ps_next()
                for kc in range(8):
                    P.mm(pr.ap[:, 0:16], h2f.ap[:, kc, ssl], sg.ap[:, SG_WR + kc * 16:SG_WR + (kc + 1) * 16],
                         kc == 0, kc == 7, reads=[h2f.t, sg.t], writes=[pr.t])
                s_, bz, eq, msk, ch, ws, wts = [rt[n] for n in ("s", "bz", "eq", "msk", "ch", "ws", "wts")]
                m1, m2, gs, gsel, gm, dn = [rs[n] for n in ("m1", "m2", "gs", "gsel", "gm", "dn")]
                P.act(s_.ap, pr.ap[:, 0:16], AF.Sigmoid, reads=[pr.t], writes=[s_.t])
                P.tt("dve", bz.ap, s_.ap, sg.ap[:, SG_BR:SG_BR + 16], ALU.add, reads=[s_.t, sg.t], writes=[bz.t])
                P.add("dve", lambda e, o=m1.ap, i_=v3(bz): e.tensor_reduce(out=o, in_=i_, axis=AX.X, op=ALU.max),
                      reads=[bz.t], writes=[m1.t])
                P.tt("dve", v3(eq), v3(bz), bc(m1), ALU.is_equal, reads=[bz.t, m1.t], writes=[eq.t])
                P.stt(msk.ap, eq.ap, -1e9, bz.ap, ALU.mult, ALU.add, reads=[eq.t, bz.t], writes=[msk.t])
                P.add("dve", lambda e, o=m2.ap, i_=v3(msk): e.tensor_reduce(out=o, in_=i_, axis=AX.X, op=ALU.max),
                      reads=[msk.t], writes=[m2.t])
                P.tt("dve", gs.ap, m1.ap, m2.ap, ALU.add, reads=[m1.t, m2.t], writes=[gs.t])
                P.add("dve", lambda e, o=gm.ap[:, 0:1], i_=gs.ap: e.tensor_reduce(out=o, in_=i_, axis=AX.X,
                                                                                 op=ALU.max),
                      reads=[gs.t], writes=[gm.t])
                P.ts("dve", gsel.ap, gs.ap, gm.ap[:, 0:1], None, ALU.is_equal, reads=[gs.t, gm.t], writes=[gsel.t])
                P.tt("dve", v3(ch), v3(bz), bc(m2), ALU.is_ge, reads=[bz.t, m2.t], writes=[ch.t])
                P.tt("dve", v3(ch), v3(ch), bc(gsel), ALU.mult, reads=[ch.t, gsel.t], writes=[ch.t])
                P.tt("dve", ws.ap, s_.ap, ch.ap, ALU.mult, reads=[s_.t, ch.t], writes=[ws.t])
                P.add("dve", lambda e, o=dn.ap[:, 0:1], i_=ws.ap: e.tensor_reduce(out=o, in_=i_, axis=AX.X,
                                                                                 op=ALU.add),
                      reads=[ws.t], writes=[dn.t])
                P.add("dve", lambda e, o=dn.ap[:, 1:2], i_=dn.ap[:, 0:1]: e.reciprocal(o, i_), reads=[dn.t],
                      writes=[dn.t])
                P.ts("dve", wts.ap, ws.ap, dn.ap[:, 1:2], None, ALU.mult, reads=[ws.t, dn.t], writes=[wts.t])
                pT = ps_next()
                P.transpose(pT.ap[0:16, 0:128], wts.ap, cst.ap[:, C_ID:C_ID + 128], reads=[wts.t, cst.t],
                            writes=[pT.t])
                P.copy("act", WT.ap[0:16, q0 + sub * 128:q0 + (sub + 1) * 128], pT.ap[0:16, 0:128], reads=[pT.t],
                       writes=[WT.t])
        P.barrier()
        A.off = M0
        ew = [[A.alloc([128, 4096], BF16, f"ew{i}_{k}") for k in range(3)] for i in range(2)]
        wbc = A.alloc([128, 512], F32, "wbc")
        sG = [A.alloc([128, 512], F32, f"sG{i}") for i in range(2)]
        tG = [A.alloc([128, 512], F32, f"tG{i}") for i in range(2)]
        hid = A.alloc([128, 4, 512], BF16, "hid")
        def load_expert(e):
            for k in range(3):
                P.dma("pool", ew[e % 2][k].ap, we_d[li][e, k], writes=[ew[e % 2][k].t])

        hidb = [hid, A.alloc([128, 4, 512], BF16, "hid2")]
        wbcb = [wbc, A.alloc([128, 512], F32, "wbc2")]
        items = [(e, ti) for e in range(16) for ti in range(len(mtiles))]

        def GU(ix):
            e, ti = items[ix]
            wg_, wu_, wd_ = ew[e % 2]
            wg3 = wg_.ap.rearrange("p (a b) -> p a b", a=8)
            wu3 = wu_.ap.rearrange("p (a b) -> p a b", a=8)
            t0, nt, mj = mtiles[ti]
            q0 = qpos(t0)
            hid_, wbc_ = hidb[ix % 2], wbcb[ix % 2]
            pw = ps_next(8)
            P.mm(pw.ap[:, 0:nt], cst.ap[0:16, C_SEL + e * 128:C_SEL + (e + 1) * 128], WT.ap[0:16, q0:q0 + nt],
                 True, True, reads=[cst.t, WT.t], writes=[pw.t])
            P.copy("act", wbc_.ap[:, 0:nt], pw.ap[:, 0:nt], reads=[pw.t], writes=[wbc_.t])
            for hc in range(4):
                hsl = slice(hc * 128, (hc + 1) * 128)
                pg, pu = ps_next(8), ps_next(8)
                for kc in range(8):
                    P.mm(pg.ap[:, 0:nt], wg3[:, kc, hsl], h2T.ap[:, kc, q0:q0 + nt], kc == 0, kc == 7,
                         reads=[wg_.t, h2T.t], writes=[pg.t])
                for kc in range(8):
                    P.mm(pu.ap[:, 0:nt], wu3[:, kc, hsl], h2T.ap[:, kc, q0:q0 + nt], kc == 0, kc == 7,
                         reads=[wu_.t, h2T.t], writes=[pu.t])
                sg_, tg_ = sG[hc % 2], tG[hc % 2]
                P.act(sg_.ap[:, 0:nt], pg.ap[:, 0:nt], AF.Silu, reads=[pg.t], writes=[sg_.t])
                P.tt("dve", tg_.ap[:, 0:nt], sg_.ap[:, 0:nt], pu.ap[:, 0:nt], ALU.mult, reads=[sg_.t, pu.t],
                     writes=[tg_.t])
                P.tt("pool", hid_.ap[:, hc, 0:nt], tg_.ap[:, 0:nt], wbc_.ap[:, 0:nt], ALU.mult,
                     reads=[tg_.t, wbc_.t], writes=[hid_.t])

        def DOWN(ix):
            e, ti = items[ix]
            wd_ = ew[e % 2][2]
            wd3 = wd_.ap.rearrange("p (a b) -> p a b", a=4)
            t0, nt, mj = mtiles[ti]
            q0 = qpos(t0)
            hid_ = hidb[ix % 2]
            for dc in range(8):
                dsl = slice(dc * 128, (dc + 1) * 128)
                py = ps_next(8)
                for hc in range(4):
                    P.mm(py.ap[:, 0:nt], wd3[:, hc, dsl], hid_.ap[:, hc, 0:nt], hc == 0, hc == 3,
                         reads=[wd_.t, hid_.t], writes=[py.t])
                P.stt(xres.ap[:, dc, q0:q0 + nt], py.ap[:, 0:nt], GT2(dc, mj), xres.ap[:, dc, q0:q0 + nt],
                      ALU.mult, ALU.add, reads=[py.t, modT.t, xres.t], writes=[xres.t])

        load_expert(0)
        load_expert(1)
        GU(0)
        for ix in range(len(items)):
            if ix + 1 < len(items):
                GU(ix + 1)
            DOWN(ix)
            e_, ti_ = items[ix]
            if ti_ == len(mtiles) - 1 and e_ + 2 < 16:
                load_expert(e_ + 2)
        if not (last and final):
            for (t0, nt, mj) in mtiles:
                q0 = qpos(t0)
                P.dma("pool", xs3[:, :, t0:t0 + nt], xres.ap[:, :, q0:q0 + nt], reads=[xres.t], writes=[txs(t0)])
        if not last:
            for i, (t0, nt, mj) in enumerate(TOK_TILES_OWN):
                P.dma("pool", xch_src[i].rearrange("(kc p) t -> p kc t", p=128), xres.ap[:, :, t0:t0 + nt],
                      reads=[xres.t], writes=[T_xsrc[i]])
            P.barrier()
            for i in range(4):
                P.add("pool", lambda e, i=i: e.collective_compute("AllGather", ALU.bypass,
                                                                  replica_groups=[[0, 1], [2, 3], [4, 5], [6, 7]],
                                                                  ins=[xch_src[i]], outs=[xch_dst[i]]),
                      reads=[T_xsrc[i]], writes=[T_xdst[i]], cc=True)
            P.barrier()
            A.off = M0
            xa = [A.alloc([128, 8, 512], F32, f"xa{i}") for i in range(2)]
            xb_ = [A.alloc([128, 8, 512], F32, f"xb{i}") for i in range(2)]
            for i, (t0, nt, mj) in enumerate(TOK_TILES_OWN):
                d3 = xch_dst[i].rearrange("(r kc p) t -> r p kc t", r=2, p=128)
                a_, b_ = xa[i % 2], xb_[i % 2]
                P.dma("sp", a_.ap, d3[0], reads=[T_xdst[i]], writes=[a_.t])
                P.dma("sp", b_.ap, d3[1], reads=[T_xdst[i]], writes=[b_.t])
                P.ts("dve", a_.ap, a_.ap, sg.ap[:, SG_FLAG + 1:SG_FLAG + 2], None, ALU.mult, reads=[a_.t, sg.t],
                     writes=[a_.t])
                P.stt(a_.ap, b_.ap, sg.ap[:, SG_FLAG:SG_FLAG + 1], a_.ap, ALU.mult, ALU.add,
                      reads=[a_.t, b_.t, sg.t], writes=[a_.t])
                P.dma("pool", xs3[:, :, NOWN + t0:NOWN + t0 + nt], a_.ap, reads=[a_.t], writes=[txs(NOWN + t0)])
        if last and not final:
            xout3 = xout.rearrange("(kc p) t -> p kc t", p=128)
            for (t0, nt, mj) in mtiles:
                q0 = qpos(t0)
                P.dma("pool", xout3[:, :, q0:q0 + nt], xres.ap[:, :, q0:q0 + nt], reads=[xres.t])
        if last and final:
            P.barrier()
            A.off = M0
            h2f = A.alloc([128, 8, 512], F32, "h2f")
            sq = A.alloc([128, 8, 512], BF16, "sq")
            rstd = A.alloc([128, 512], F32, "rstd")
            tmp = [A.alloc([128, 512], F32, f"tmp{i}") for i in range(2)]
            yout3 = yout.rearrange("(kc p) t -> p kc t", p=128)
            for (t0, nt, mj) in TOK_TILES_OWN:
                P.act(sq.ap[:, :, 0:nt], xres.ap[:, :, t0:t0 + nt], AF.Square, reads=[xres.t], writes=[sq.t])
                pb = ps_next()
                for kc in range(8):
                    P.mm(pb.ap[:, 0:nt], ones_bf.ap, sq.ap[:, kc, 0:nt], kc == 0, kc == 7, reads=[ones_bf.t, sq.t],
                         writes=[pb.t])
                rsqrt_from_psum(rstd.ap[:, 0:nt], pb.ap[:, 0:nt], 1024.0, [pb.t], [rstd.t])
                for kc in range(8):
                    tm_ = tmp[kc % 2]
                    P.tt("dve", tm_.ap[:, 0:nt], xres.ap[:, kc, t0:t0 + nt], rstd.ap[:, 0:nt], ALU.mult,
                         reads=[xres.t, rstd.t], writes=[tm_.t])
                    P.act(h2f.ap[:, kc, 0:nt], tm_.ap[:, 0:nt], AF.Identity, reads=[tm_.t, sg.t], writes=[h2f.t],
                          scale=sg.ap[:, SG_GF + kc:SG_GF + kc + 1])
                P.dma("pool", yout3[:, :, t0:t0 + nt], h2f.ap[:, :, 0:nt], reads=[h2f.t])
    P.emit()
    return nc


_PROGS = {}


def _prog(key, *args, **kw):
    if key not in _PROGS:
        _PROGS[key] = build(*args, **kw)
    return _PROGS[key]


def kernel(**inp):
    inp = {k: np.asarray(v) for k, v in inp.items()}
    x, ctx = inp["x"], inp["ctx"]
    B = x.shape[0]
    cores = [(b, h) for b in range(B) for h in range(2)]
    consts = [make_consts(h) for h in range(2)]
    tabs = [make_tabs(h) for h in range(2)]
    w0 = prep_layer_weights(inp, 0)
    w1 = prep_layer_weights(inp, 1)
    maps = []
    for (b, h) in cores:
        xo = x[b, h * NOWN:(h + 1) * NOWN].T
        xt = x[b, (1 - h) * NOWN:(2 - h) * NOWN].T
        xall = np.ascontiguousarray(np.concatenate([xo, xt, ctx[b].T], axis=1))
        maps.append(dict(xall=xall, tabs=tabs[h], consts=consts[h], sg=make_small_global(inp, b, h),
                         sm0=w0[2], wall0=w0[0], we0=w0[1], sm1=w1[2], wall1=w1[0], we1=w1[1]))
    nc = _prog("fused", [0, 1], [True, False], True)
    r = run_bass_kernel_spmd(nc, maps, core_ids=list(range(len(cores)))).results
    out = np.zeros((B, 2 * NOWN, D), np.float32)
    for i, (b, h) in enumerate(cores):
        out[b, h * NOWN:(h + 1) * NOWN] = np.asarray(r[i]["yout"]).T
    return out
```

```python
import numpy as np
import concourse.bass as bass
import concourse.mybir as mybir
from concourse.bass_utils import run_bass_kernel_spmd

F32 = mybir.dt.float32
BF16 = mybir.dt.bfloat16
AF = mybir.ActivationFunctionType
ALU = mybir.AluOpType
AX = mybir.AxisListType

NOWN, NOTH, NCTX = 2048, 2048, 256
NALL = NOWN + NOTH + NCTX
NQ = NOWN + NCTX
D = 1024
EPS = 1e-6
NSLOT = 24
SKIP = set()


class T:
    __slots__ = ("name", "w", "r", "psum")

    def __init__(self, name="", psum=False):
        self.name = name
        self.w = None
        self.r = []
        self.psum = psum


class Op:
    __slots__ = ("eng", "fn", "deps", "id", "is_dma", "slot", "val", "marked", "semval", "cc")

    def __init__(self, eng, fn, is_dma):
        self.eng = eng
        self.fn = fn
        self.deps = []
        self.is_dma = is_dma
        self.slot = None
        self.val = None
        self.marked = False
        self.semval = None
        self.cc = False


class Prog:
    ENGS = ("pe", "act", "dve", "pool", "sp")

    def __init__(self, nc):
        self.nc = nc
        self.ops = []
        self.streams = {e: [] for e in self.ENGS}
        self.ndma = {e: 0 for e in self.ENGS}
        self.dmas = {e: [] for e in self.ENGS}

    def add(self, eng, fn, reads=(), writes=(), dma=False, extra=(), cc=False):
        op = Op(eng, fn, dma or cc)
        op.cc = cc
        op.id = len(self.ops)
        deps = {}
        for t in reads:
            if t.w is not None:
                deps[t.w.id] = t.w
            if t.psum:
                for r in t.r:
                    if r.eng != eng:
                        deps[r.id] = r
        for t in writes:
            if t.w is not None:
                deps[t.w.id] = t.w
            for r in t.r:
                deps[r.id] = r
        for d in extra:
            deps[d.id] = d
        op.deps = list(deps.values())
        for t in reads:
            if not dma:
                t.r = [r for r in t.r if r.is_dma or r.eng != eng]
            t.r.append(op)
        for t in writes:
            t.w = op
            t.r = []
        if cc:
            self.ncc = getattr(self, "ncc", 0) + 1
            op.slot = ("cc", self.ncc - 1)
            op.val = 1
            self.dmas[eng].append(op)
        elif dma:
            i = self.ndma[eng]
            self.ndma[eng] += 1
            op.slot = i % NSLOT
            op.val = 16 * (i // NSLOT + 1)
            self.dmas[eng].append(op)
        self.ops.append(op)
        self.streams[eng].append(op)
        return op

    def barrier(self):
        lasts = []
        for e in self.ENGS:
            if self.streams[e]:
                lasts.append(self.streams[e][-1])
            lasts.extend(self.dmas[e][-NSLOT:])
        for e in self.ENGS:
            self.add(e, lambda eng: eng.nop(), extra=lasts)

    def dma(self, q, out, in_, reads=(), writes=()):
        return self.add(q, lambda e: e.dma_start(out=out, in_=in_), reads, writes, dma=True)

    def mm(self, out, lhsT, rhs, start, stop, reads=(), writes=()):
        return self.add("pe", lambda e: e.matmul(out, lhsT, rhs, start=start, stop=stop), reads, writes)

    def transpose(self, out, in_, ident, reads=(), writes=()):
        return self.add("pe", lambda e: e.transpose(out, in_, ident), reads, writes)

    def act(self, out, in_, func, reads=(), writes=(), bias=None, scale=None):
        kw = {}
        if bias is not None:
            kw["bias"] = bias
        if scale is not None:
            kw["scale"] = scale
        return self.add("act", lambda e: e.activation(out, in_, func, **kw), reads, writes)

    def tt(self, eng, out, in0, in1, op, reads=(), writes=()):
        return self.add(eng, lambda e: e.tensor_tensor(out, in0, in1, op), reads, writes)

    def ts(self, eng, out, in0, s1, s2, op0, op1=None, reads=(), writes=()):
        if op1 is None:
            return self.add(eng, lambda e: e.tensor_scalar(out=out, in0=in0, scalar1=s1, scalar2=None, op0=op0),
                            reads, writes)
        return self.add(eng, lambda e: e.tensor_scalar(out=out, in0=in0, scalar1=s1, scalar2=s2, op0=op0, op1=op1),
                        reads, writes)

    def stt(self, out, in0, scalar, in1, op0, op1, reads=(), writes=()):
        return self.add("dve", lambda e: e.scalar_tensor_tensor(out=out, in0=in0, scalar=scalar, in1=in1,
                                                                op0=op0, op1=op1), reads, writes)

    def copy(self, eng, out, in_, reads=(), writes=()):
        if eng == "act":
            return self.add("act", lambda e: e.copy(out, in_), reads, writes)
        return self.add(eng, lambda e: e.tensor_copy(out, in_), reads, writes)

    def emit(self):
        nc = self.nc
        for op in self.ops:
            for d in op.deps:
                if d.is_dma:
                    continue
                if d.eng == op.eng and not op.is_dma and d.eng == "pe":
                    continue
                d.marked = True
        cnt = {e: 0 for e in self.ENGS}
        for op in self.ops:
            if op.marked and not op.is_dma:
                cnt[op.eng] += 1
                op.semval = cnt[op.eng]
        sems = {e: nc.alloc_semaphore(f"s_{e}") for e in self.ENGS}
        dsems = {e: {i: nc.alloc_semaphore(f"d_{e}_{i}") for i in range(min(NSLOT, self.ndma[e]))}
                 for e in self.ENGS}
        for i in range(getattr(self, "ncc", 0)):
            dsems["pool"][("cc", i)] = nc.alloc_semaphore(f"cc_{i}")
        engobj = {"pe": nc.tensor, "act": nc.scalar, "dve": nc.vector, "pool": nc.gpsimd, "sp": nc.sync}
        with nc.Block() as block:
            def run(ename):
                eng = engobj[ename]
                waited = {}

                def wait(key, sem, val):
                    if waited.get(key, 0) >= val:
                        return
                    waited[key] = val
                    eng.wait_ge(sem, val)

                for op in self.streams[ename]:
                    for d in op.deps:
                        if d.is_dma:
                            wait(("d", d.eng, d.slot), dsems[d.eng][d.slot], d.val)
                        else:
                            if d.eng == ename and not op.is_dma and ename == "pe":
                                continue
                            wait(("c", d.eng), sems[d.eng], d.semval)
                    if op.cc:
                        ins = op.fn(eng)
                        ins.then_inc(dsems[ename][op.slot], 1)
                    elif op.is_dma:
                        if op.val > 16:
                            wait(("d", ename, op.slot), dsems[ename][op.slot], op.val - 16)
                        ins = op.fn(eng)
                        ins.then_inc(dsems[ename][op.slot], 16)
                    else:
                        ins = op.fn(eng)
                        if op.marked:
                            ins.then_inc(sems[ename], 1)
                n = self.ndma[ename]
                for i in range(max(0, n - NSLOT), n):
                    wait(("d", ename, i % NSLOT), dsems[ename][i % NSLOT], 16 * (i // NSLOT + 1))

            @block.tensor
            def _(e):
                run("pe")

            @block.scalar
            def _(e):
                run("act")

            @block.vector
            def _(e):
                run("dve")

            @block.gpsimd
            def _(e):
                run("pool")

            @block.sync
            def _(e):
                run("sp")


class Buf:
    __slots__ = ("ap", "t")

    def __init__(self, ap, name="", psum=False):
        self.ap = ap
        self.t = T(name, psum)


class Arena:
    def __init__(self, nc, nbytes):
        self.t = nc.alloc_sbuf_tensor("arena", [128, nbytes // 2], BF16)
        self.nbytes = nbytes
        self.off = 0

    def alloc(self, shape, dtype, name=""):
        n = 1
        for s in shape[1:]:
            n *= s
        es = 4 if dtype == F32 else 2
        nb = (n * es + 63) // 64 * 64
        assert self.off + nb <= self.nbytes, f"arena overflow {name} {self.off + nb}"
        v = self.t[0:shape[0], self.off // 2:(self.off + n * es) // 2]
        if dtype == F32:
            v = v.bitcast(F32)
        if len(shape) == 3:
            v = v.rearrange("p (a b) -> p a b", a=shape[1])
        self.off += nb
        return Buf(v, name)


SPL = [512, 512, 1024, 1024, 1024, 128, 128, 1024, 256, 256, 1024, 1024, 1024]
OFF = np.concatenate([[0], np.cumsum(SPL)]).astype(int)
O_QR, O_KR, O_VR, O_UR, O_QS, O_KS, O_VS, O_QA, O_KA, O_VA, O_AR, O_AS, O_AA = [int(v) for v in OFF[:13]]


def _perm128():
    f = np.arange(128)
    return np.where(f % 64 < 32, f + 32, f - 32)


def _perm64():
    f = np.arange(128)
    return np.where(f % 32 < 16, f + 16, f - 16)


def fm_units():
    u = []
    ar = np.arange(128)
    for h in range(4):
        u.append(dict(name=f"kr{h}", kind="rope128", cols=O_KR + h * 128 + ar, tok="all"))
    u.append(dict(name="ks", kind="rope64", cols=O_KS + ar, tok="all"))
    u.append(dict(name="ks2", kind="rope64", cols=O_KS + (ar + 64) % 128, tok="all"))
    for h in range(2):
        u.append(dict(name=f"ka{h}", kind="normk", cols=O_KA + h * 128 + ar, tok="all"))
    for h in range(4):
        u.append(dict(name=f"qr{h}", kind="rope128", cols=O_QR + h * 128 + ar, tok="q"))
    for c in range(8):
        u.append(dict(name=f"qs{c}", kind="rope64", cols=O_QS + c * 128 + ar, tok="q"))
    for h in range(8):
        u.append(dict(name=f"qa{h}", kind="normq", cols=O_QA + h * 128 + ar, tok="q"))
    for c in range(8):
        u.append(dict(name=f"ur{c}", kind="silu", cols=O_UR + c * 128 + ar, tok="q"))
    for nm, o in (("ar", O_AR), ("as", O_AS), ("aa", O_AA)):
        for c in range(8):
            u.append(dict(name=f"{nm}{c}", kind="sig", cols=o + c * 128 + ar, tok="q"))
    g, used = 0, 0
    for x in u:
        w = 128
        if used + w > 512:
            g, used = g + 1, 0
        x["group"], x["c0"] = g, used
        x["dual"] = x["kind"] in ("rope128", "rope64", "normq", "normk")
        used += w
    return u, g + 1


FM_UNITS, N_FM_GROUPS = fm_units()
TM_COLS = np.concatenate([O_VR + np.arange(1024), O_VS + np.arange(128), O_VA + np.arange(256)])
N_TM_GROUPS = 3
G_MOD = 0
G_FM = 12
G_TM = G_FM + N_FM_GROUPS
G_MERGE = G_TM + N_TM_GROUPS
N_GROUPS = G_MERGE + 8

SM_BMOD, SM_G1, SM_G2, SM_RET, SM_SINK, SM_GQ, SM_GK = 0, 48, 56, 64, 72, 88, 90
SM_L = 96
SG_C, SG_WR, SG_BR, SG_GF, SG_FLAG = 0, 16, 144, 160, 168
SG_N = 176
C_ID, C_DPOS, C_DNEG, C_MF, C_MB, C_IP1, C_IB, C_ML, C_MR, C_MLB, C_MRB = [i * 128 for i in range(11)]
C_PCF, C_PCB = 11 * 128, 11 * 128 + 1
C_SEL = 11 * 128 + 8
C_P128 = C_SEL + 2048
C_P64 = C_P128 + 128
C_N = C_P64 + 128


def _grp(w):
    n = w.shape[1] // 512
    return np.ascontiguousarray(w.reshape(8, 128, n, 512).transpose(2, 1, 0, 3))


def prep_layer_weights(inp, l):
    w_in = inp["w_in"][l]
    p128, p64 = _perm128(), _perm64()
    cols = np.zeros(N_FM_GROUPS * 512, dtype=np.int64)
    for u in FM_UNITS:
        base = u["group"] * 512
        cols[base + u["c0"]:base + u["c0"] + 128] = u["cols"]
    tmc = np.concatenate([TM_COLS, np.zeros(1536 - 1408, dtype=np.int64)])
    wcat = np.concatenate([inp["w_mod"][l], w_in[:, cols], w_in[:, tmc], inp["w_br_ret"][l], inp["w_br_swa"][l],
                           inp["w_br_ga"][l], inp["w_out"][l]], axis=1)
    wall = _grp(wcat)
    assert wall.shape[0] == N_GROUPS
    wg = inp["w_gate"][l].reshape(16, 8, 128, 512).transpose(0, 2, 1, 3).reshape(16, 128, 4096)
    wu = inp["w_up"][l].reshape(16, 8, 128, 512).transpose(0, 2, 1, 3).reshape(16, 128, 4096)
    wd = inp["w_down"][l].reshape(16, 4, 128, 1024).transpose(0, 2, 1, 3).reshape(16, 128, 4096)
    we = np.ascontiguousarray(np.stack([wg, wu, wd], axis=1))
    sm = np.zeros((128, SM_L), np.float32)
    sm[:, SM_BMOD:SM_BMOD + 48] = inp["b_mod"][l].reshape(48, 128).T
    sm[:, SM_G1:SM_G1 + 8] = inp["g_norm1"][l].reshape(8, 128).T
    sm[:, SM_G2:SM_G2 + 8] = inp["g_norm2"][l].reshape(8, 128).T
    sm[:, SM_RET:SM_RET + 8] = inp["ret_decay_logit"][l].reshape(1, 8)
    sm[:, SM_SINK:SM_SINK + 16] = inp["swa_sink"][l].reshape(1, 16)
    sm[:, SM_GQ] = inp["g_qnorm"][l]
    sm[:, SM_GQ + 1] = inp["g_qnorm"][l][p128]
    sm[:, SM_GK] = inp["g_knorm"][l]
    sm[:, SM_GK + 1] = inp["g_knorm"][l][p128]
    return wall, we, sm


def make_consts(half):
    c = np.zeros((128, C_N), np.float32)
    m = np.arange(128)[:, None].astype(np.float32)
    n = np.arange(128)[None, :].astype(np.float32)
    c[:, C_ID:C_ID + 128] = np.eye(128)
    c[:, C_DPOS:C_DPOS + 128] = np.maximum(n - m, 0)
    c[:, C_DNEG:C_DNEG + 128] = np.maximum(m - n, 0)
    c[:, C_MF:C_MF + 128] = (n >= m)
    c[:, C_MB:C_MB + 128] = (m > n)
    c[:, C_IP1:C_IP1 + 128] = n + 1 + 0 * m
    c[:, C_IB:C_IB + 128] = 128 - n + 0 * m
    c[:, C_ML:C_ML + 128] = (m >= n)
    c[:, C_MR:C_MR + 128] = (m <= n)
    c[:, C_MLB:C_MLB + 128] = (m >= n) * (1.0 if half == 1 else 0.0)
    c[:, C_MRB:C_MRB + 128] = (m <= n) * (1.0 if half == 0 else 0.0)
    c[:, C_PCF] = 127 - np.arange(128)
    c[:, C_PCB] = np.arange(128)
    for e in range(16):
        c[e, C_SEL + e * 128:C_SEL + (e + 1) * 128] = 1.0
    for co, pm in ((C_P128, _perm128()), (C_P64, _perm64())):
        for f in range(128):
            c[pm[f], co + f] = 1.0
    return c


def make_tabs(half):
    pos_own = half * NOWN + np.arange(NOWN)
    pos_oth = (1 - half) * NOWN + np.arange(NOTH)
    pos = np.concatenate([pos_own, pos_oth])
    row = (pos // 64).astype(np.float32)
    col = (pos % 64).astype(np.float32)
    tabs = np.zeros((4, 128, NALL), np.float32)
    tabs[0, :, 4096:] = 1.0
    tabs[2, :, 4096:] = 1.0
    for ti, hd in ((0, 128), (2, 64)):
        half_d, quarter = hd // 2, hd // 4
        freqs = (np.float32(10000.0) ** (-np.arange(quarter, dtype=np.float32) / np.float32(quarter))).astype(np.float32)
        for f in range(128):
            fl = f % hd
            a = fl // half_d
            j = fl % half_d
            p = row if a == 0 else col
            ang = (p * freqs[j % quarter]).astype(np.float32)
            tabs[ti, f, :4096] = np.cos(ang)
            tabs[ti + 1, f, :4096] = np.sin(ang) * (-1.0 if j < quarter else 1.0)
    return tabs


def make_small_global(inp, b, half):
    sg = np.zeros((128, SG_N), np.float32)
    cT = inp["c"][b].reshape(8, 128).T
    ccT = inp["c_ctx"].reshape(8, 128).T
    sg[:, SG_C:SG_C + 16:2] = cT
    sg[:, SG_C + 1:SG_C + 16:2] = ccT
    sg[:, SG_WR:SG_WR + 128] = inp["w_router"].reshape(8, 128, 16).transpose(1, 0, 2).reshape(128, 128)
    sg[:, SG_BR:SG_BR + 16] = inp["b_router"].reshape(1, 16)
    sg[:, SG_GF:SG_GF + 8] = inp["g_final"].reshape(8, 128).T
    sg[:, SG_FLAG] = 1.0 if half == 0 else 0.0
    sg[:, SG_FLAG + 1] = 1.0 if half == 1 else 0.0
    return sg


TOK_TILES_ALL = [(i * 512, 512, 0) for i in range(8)] + [(4096, 256, 1)]
TOK_TILES_OWN = [(i * 512, 512, 0) for i in range(4)]
CTX_TILE = (4096, 256, 1)


def qpos(t0):
    return t0 if t0 < NOWN else t0 - NOTH


def build(layers, need_ctx_flags, final, dbg=(), stop=99):
    nc = bass.Bass("TRN2", target_bir_lowering=False)
    P = Prog(nc)
    nl = len(layers)

    def din(name, shape, dt=F32):
        return nc.dram_tensor(name, list(shape), dt, kind="ExternalInput").ap()

    def dscr(name, shape, dt=BF16):
        kind = "ExternalOutput" if name in dbg else "Internal"
        return nc.dram_tensor(name, list(shape), dt, kind=kind).ap()

    xall = din("xall", [D, NALL])
    tabs = din("tabs", [4, 128, NALL])
    consts_d = din("consts", [128, C_N])
    sg_d = din("sg", [128, SG_N])
    sm_d = [din(f"sm{l}", [128, SM_L]) for l in range(nl)]
    wall_d = [din(f"wall{l}", [N_GROUPS, 128, 8, 512]) for l in range(nl)]
    we_d = [din(f"we{l}", [16, 3, 128, 4096]) for l in range(nl)]
    if final:
        yout = nc.dram_tensor("yout", [D, NOWN], F32, kind="ExternalOutput").ap()
    else:
        xout = nc.dram_tensor("xout", [D, NQ], F32, kind="ExternalOutput").ap()

    xs_d = dscr("xs", [D, NALL], F32)
    fm_d = {u["name"]: dscr("fm_" + u["name"], [128, NALL]) for u in FM_UNITS}
    vall_d = dscr("vall", [NALL, 1536])
    obr_d = [dscr(f"obr{i}", [D, NQ]) for i in range(3)]
    web_d = dscr("web", [16, 3, 128, 4096])
    T_xs = {}

    def txs(t0):
        return T_xs.setdefault(t0, T(f"xs{t0}"))
    T_fm = {}

    def tfm(name, t0):
        return T_fm.setdefault((name, t0), T(f"fm{name}{t0}"))
    T_vall = {}

    def tvall(sub):
        return T_vall.setdefault(sub, T(f"vall{sub}"))
    T_obr = {}

    def tobr(i, t0):
        return T_obr.setdefault((i, t0), T(f"obr{i}_{t0}"))
    T_web = {}

    def tweb(e, k):
        return T_web.setdefault((e, k), T(f"web{e}_{k}"))

    xs3 = xs_d.rearrange("(kc p) t -> p kc t", p=128)
    if nl > 1:
        xch_src = [nc.dram_tensor(f"xch_src{i}", [D, 512], F32, kind="Internal").ap() for i in range(4)]
        xch_dst = [nc.dram_tensor(f"xch_dst{i}", [2 * D, 512], F32, kind="Internal").ap() for i in range(4)]
        T_xsrc, T_xdst = [T("xsrc") for i in range(4)], [T("xdst") for i in range(4)]
    xall3 = xall.rearrange("(kc p) t -> p kc t", p=128)

    A = Arena(nc, 211968)
    cst = A.alloc([128, C_N], F32, "cst")
    sg = A.alloc([128, SG_N], F32, "sg")
    ones_bf = A.alloc([128, 128], BF16, "ones")
    id_bf = A.alloc([128, 128], BF16, "idbf")
    mk_bf = A.alloc([128, 4, 512], BF16, "mk")
    pm_bf = A.alloc([128, 2, 128], BF16, "pm")
    PERS_END = None
    ps = [Buf(nc.alloc_psum_tensor(f"ps{i}", [128, 512], F32)[:], f"ps{i}", True) for i in range(8)]

    P.dma("sp", cst.ap, consts_d, writes=[cst.t])
    P.dma("sp", sg.ap, sg_d, writes=[sg.t])
    P.add("dve", lambda e: e.memset(ones_bf.ap, 1.0), writes=[ones_bf.t])
    P.copy("dve", id_bf.ap, cst.ap[:, C_ID:C_ID + 128], reads=[cst.t], writes=[id_bf.t])
    for i, co in enumerate((C_P128, C_P64)):
        P.copy("dve", pm_bf.ap[:, i, :], cst.ap[:, co:co + 128], reads=[cst.t], writes=[pm_bf.t])
    for i, co in enumerate((C_ML, C_MR, C_MLB, C_MRB)):
        for r in range(4):
            P.copy("dve", mk_bf.ap[:, i, r * 128:(r + 1) * 128], cst.ap[:, co:co + 128], reads=[cst.t],
                   writes=[mk_bf.t])
    for (t0, nt, _) in TOK_TILES_ALL:
        P.dma("sp", xs_d[:, t0:t0 + nt], xall[:, t0:t0 + nt], writes=[txs(t0)])
    PERS_END = A.off

    def rsqrt_from_psum(dst, src_ps, n, rd, wr):
        P.ts("dve", dst, src_ps, 1.0 / n, EPS, ALU.mult, ALU.add, reads=rd, writes=wr)
        P.act(dst, dst, AF.Ln, reads=wr, writes=wr)
        P.act(dst, dst, AF.Exp, reads=wr, writes=wr, scale=-0.5)

    psi = [0]

    def ps_next(k=8):
        psi[0] = (psi[0] + 1) % k
        return ps[psi[0]]

    for li, l in enumerate(layers):
        need_ctx = need_ctx_flags[li]
        last = (li == nl - 1)
        qtiles = TOK_TILES_OWN + ([CTX_TILE] if need_ctx else [])
        P.barrier()
        A.off = PERS_END
        sm = A.alloc([128, SM_L], F32, "sm")
        modT = A.alloc([128, 48, 2], F32, "modT")
        A1 = A.alloc([128, 8, 2], F32, "A1")
        A2 = A.alloc([128, 8, 2], F32, "A2")
        silc = A.alloc([128, 16], F32, "silc")
        lg = A.alloc([128, 8], F32, "lg")
        sinkx = A.alloc([128, 16], F32, "sinkx")
        P.dma("sp", sm.ap, sm_d[li], writes=[sm.t])
        P.act(silc.ap, sg.ap[:, SG_C:SG_C + 16], AF.Silu, reads=[sg.t], writes=[silc.t])
        silc3 = silc.ap.rearrange("p (k j) -> p k j", j=2)
        P.act(lg.ap, sm.ap[:, SM_RET:SM_RET + 8], AF.Exp, reads=[sm.t], writes=[lg.t], scale=-1.0)
        P.ts("dve", lg.ap, lg.ap, 1.0, None, ALU.add, reads=[lg.t], writes=[lg.t])
        P.act(lg.ap, lg.ap, AF.Ln, reads=[lg.t], writes=[lg.t])
        P.ts("dve", lg.ap, lg.ap, -1.0, None, ALU.mult, reads=[lg.t], writes=[lg.t])
        P.act(sinkx.ap, sm.ap[:, SM_SINK:SM_SINK + 16], AF.Exp, reads=[sm.t], writes=[sinkx.t])
        PH0_END = A.off
        wst = [A.alloc([128, 8, 512], F32, f"wst{i}") for i in range(2)]
        for g in range(12):
            w = wst[g % 2]
            P.dma("sp", w.ap, wall_d[li][G_MOD + g], writes=[w.t])
            for j in range(4):
                idx = g * 4 + j
                pb = ps_next()
                for kc in range(8):
                    P.mm(pb.ap[:, 0:2], w.ap[:, kc, j * 128:(j + 1) * 128], silc3[:, kc, :], kc == 0, kc == 7,
                         reads=[w.t, silc.t], writes=[pb.t])
                P.ts("dve", modT.ap[:, idx, :], pb.ap[:, 0:2], sm.ap[:, SM_BMOD + idx:SM_BMOD + idx + 1], None,
                     ALU.add, reads=[pb.t, sm.t], writes=[modT.t])
        for j in range(2):
            P.stt(A1.ap[:, :, j], modT.ap[:, 8:16, j], 1.0, sm.ap[:, SM_G1:SM_G1 + 8], ALU.add, ALU.mult,
                  reads=[modT.t, sm.t], writes=[A1.t])
            P.stt(A2.ap[:, :, j], modT.ap[:, 32:40, j], 1.0, sm.ap[:, SM_G2:SM_G2 + 8], ALU.add, ALU.mult,
                  reads=[modT.t, sm.t], writes=[A2.t])

        def SH1(kc, j): return modT.ap[:, 0 + kc, j:j + 1]
        def GT1(kc, j): return modT.ap[:, 16 + kc, j:j + 1]
        def SH2(kc, j): return modT.ap[:, 24 + kc, j:j + 1]
        def GT2(kc, j): return modT.ap[:, 40 + kc, j:j + 1]

        P.barrier()
        A.off = PH0_END
        hT = A.alloc([128, 8, NALL], BF16, "hT")
        hts = {t0: T(f"hT{t0}") for (t0, _, _) in TOK_TILES_ALL}
        xt = [A.alloc([128, 8, 512], F32, f"xt{i}") for i in range(2)]
        sq = A.alloc([128, 8, 512], BF16, "sq")
        rstd = A.alloc([128, 512], F32, "rstd")
        tmp = [A.alloc([128, 512], F32, f"tmp{i}") for i in range(2)]
        PH1_END = A.off

        def norm_tiles(tiles, Aco, SHf, out_fn, src3=xs3):
            for i, (t0, nt, mj) in enumerate(tiles):
                x_ = xt[i % 2]
                P.dma("sp", x_.ap[:, :, 0:nt], src3[:, :, t0:t0 + nt], reads=[txs(t0)], writes=[x_.t])
                P.act(sq.ap[:, :, 0:nt], x_.ap[:, :, 0:nt], AF.Square, reads=[x_.t], writes=[sq.t])
                pb = ps_next()
                for kc in range(8):
                    P.mm(pb.ap[:, 0:nt], ones_bf.ap, sq.ap[:, kc, 0:nt], kc == 0, kc == 7,
                         reads=[ones_bf.t, sq.t], writes=[pb.t])
                rsqrt_from_psum(rstd.ap[:, 0:nt], pb.ap[:, 0:nt], 1024.0, [pb.t], [rstd.t])
                for kc in range(8):
                    tm_ = tmp[kc % 2]
                    P.tt("dve" if kc % 2 == 0 else "pool", tm_.ap[:, 0:nt], x_.ap[:, kc, 0:nt], rstd.ap[:, 0:nt],
                         ALU.mult, reads=[x_.t, rstd.t], writes=[tm_.t])
                    out_fn(kc, t0, nt, mj, tm_, Aco.ap[:, kc, mj:mj + 1], SHf(kc, mj))

        def out_h1(kc, t0, nt, mj, tm_, a_, b_):
            P.act(hT.ap[:, kc, t0:t0 + nt], tm_.ap[:, 0:nt], AF.Identity, reads=[tm_.t, A1.t, modT.t],
                  writes=[hts[t0]], bias=b_, scale=a_)

        norm_tiles(TOK_TILES_ALL, A1, SH1, out_h1)

        A.off = PH1_END
        wbf = [A.alloc([128, 8, 512], BF16, f"wbf{i}") for i in range(3)]
        tabC = [A.alloc([128, 512], F32, f"tabC{i}") for i in range(2)]
        tabS = [A.alloc([128, 512], F32, f"tabS{i}") for i in range(2)]
        stage = [A.alloc([128, 512], BF16, f"stage{i}") for i in range(3)]
        t1b = [A.alloc([128, 512], F32, f"t1b{i}") for i in range(2)]
        t2b = [A.alloc([128, 512], F32, f"t2b{i}") for i in range(2)]
        sqb = A.alloc([128, 512], BF16, "sqb")
        rs2 = A.alloc([128, 512], F32, "rs2")
        cnt = [0]

        def load_group(gi):
            wb = wbf[gi % 3]
            P.dma("pool", wb.ap, wall_d[li][gi], writes=[wb.t])
            return wb

        pabf = [A.alloc([128, 512], BF16, f"pabf{i}") for i in range(2)]
        work = []
        for gi in range(N_FM_GROUPS):
            for u in [x for x in FM_UNITS if x["group"] == gi]:
                for tile in (TOK_TILES_ALL if u["tok"] == "all" else qtiles):
                    work.append((gi, u, tile))
        wbs = {}
        loaded = [G_FM - 1]

        def ensure(gi):
            while loaded[0] < G_FM + gi:
                loaded[0] += 1
                wbs[loaded[0] - G_FM] = load_group(loaded[0])

        def MAIN(k):
            gi, u, (t0, nt, mj) = work[k]
            ensure(gi + 1)
            wb = wbs[gi]
            pa = ps_next()
            for kc in range(8):
                P.mm(pa.ap[:, 0:nt], wb.ap[:, kc, u["c0"]:u["c0"] + 128], hT.ap[:, kc, t0:t0 + nt],
                     kc == 0, kc == 7, reads=[wb.t, hts[t0]], writes=[pa.t])
            if u["dual"]:
                pb_ = pabf[k % 2]
                P.copy("act", pb_.ap[:, 0:nt], pa.ap[:, 0:nt], reads=[pa.t], writes=[pb_.t])
                tsel = 2 if u["kind"] == "rope64" else 0
                tc_, ts_ = tabC[k % 2], tabS[k % 2]
                P.dma("sp", tc_.ap[:, 0:nt], tabs[tsel, :, t0:t0 + nt], writes=[tc_.t])
                P.dma("sp", ts_.ap[:, 0:nt], tabs[tsel + 1, :, t0:t0 + nt], writes=[ts_.t])
            return pa

        def REST(k, pa):
            gi, u, (t0, nt, mj) = work[k]
            kind = u["kind"]
            if u["dual"]:
                pbk = ps_next()
                P.mm(pbk.ap[:, 0:nt], pm_bf.ap[:, 1 if kind == "rope64" else 0, :], pabf[k % 2].ap[:, 0:nt],
                     True, True, reads=[pm_bf.t, pabf[k % 2].t], writes=[pbk.t])
                tc_, ts_ = tabC[k % 2], tabS[k % 2]
            st = stage[k % 3]
            o_ = st.ap[:, 0:nt]
            if kind in ("rope128", "rope64"):
                a_, b_ = t1b[k % 2], t2b[k % 2]
                P.tt("dve", a_.ap[:, 0:nt], pa.ap[:, 0:nt], tc_.ap[:, 0:nt], ALU.mult,
                     reads=[pa.t, tc_.t], writes=[a_.t])
                P.tt("dve", b_.ap[:, 0:nt], pbk.ap[:, 0:nt], ts_.ap[:, 0:nt], ALU.mult,
                     reads=[pbk.t, ts_.t], writes=[b_.t])
                P.tt("pool", o_, a_.ap[:, 0:nt], b_.ap[:, 0:nt], ALU.add, reads=[a_.t, b_.t], writes=[st.t])
            elif kind in ("normq", "normk"):
                gco = SM_GQ if kind == "normq" else SM_GK
                a_, b_ = t1b[k % 2], t2b[k % 2]
                P.act(sqb.ap[:, 0:nt], pa.ap[:, 0:nt], AF.Square, reads=[pa.t], writes=[sqb.t])
                pc = ps_next()
                P.mm(pc.ap[:, 0:nt], ones_bf.ap, sqb.ap[:, 0:nt], True, True, reads=[ones_bf.t, sqb.t],
                     writes=[pc.t])
                rsqrt_from_psum(rs2.ap[:, 0:nt], pc.ap[:, 0:nt], 128.0, [pc.t], [rs2.t])
                P.stt(a_.ap[:, 0:nt], pa.ap[:, 0:nt], sm.ap[:, gco:gco + 1], tc_.ap[:, 0:nt], ALU.mult,
                      ALU.mult, reads=[pa.t, tc_.t, sm.t], writes=[a_.t])
                P.stt(b_.ap[:, 0:nt], pbk.ap[:, 0:nt], sm.ap[:, gco + 1:gco + 2], ts_.ap[:, 0:nt], ALU.mult,
                      ALU.mult, reads=[pbk.t, ts_.t, sm.t], writes=[b_.t])
                P.tt("pool", a_.ap[:, 0:nt], a_.ap[:, 0:nt], b_.ap[:, 0:nt], ALU.add, reads=[a_.t, b_.t],
                     writes=[a_.t])
                P.tt("pool", o_, a_.ap[:, 0:nt], rs2.ap[:, 0:nt], ALU.mult, reads=[a_.t, rs2.t],
                     writes=[st.t])
            elif kind == "silu":
                P.act(o_, pa.ap[:, 0:nt], AF.Silu, reads=[pa.t], writes=[st.t])
            else:
                P.act(o_, pa.ap[:, 0:nt], AF.Sigmoid, reads=[pa.t], writes=[st.t])
            P.dma("pool", fm_d[u["name"]][:, t0:t0 + nt], o_, reads=[st.t], writes=[tfm(u["name"], t0)])

        ensure(0)
        pas = {0: MAIN(0)}
        for k in range(len(work)):
            if k + 1 < len(work):
                pas[k + 1] = MAIN(k + 1)
            REST(k, pas.pop(k))
        cnt[0] = len(work)
        ensure(N_FM_GROUPS)
        nxt = wbs[N_FM_GROUPS]
        for gi in range(N_TM_GROUPS):
            wb = nxt
            if gi + 1 < N_TM_GROUPS:
                nxt = load_group(G_TM + gi + 1)
            for sub in range(NALL // 128):
                t0 = sub * 128
                tile0 = (t0 // 512) * 512
                k = cnt[0]
                cnt[0] += 1
                pa = ps_next()
                for kc in range(8):
                    P.mm(pa.ap, hT.ap[:, kc, t0:t0 + 128], wb.ap[:, kc, :], kc == 0, kc == 7,
                         reads=[wb.t, hts[tile0]], writes=[pa.t])
                st = stage[k % 3]
                P.copy("act" if k % 2 == 0 else "dve", st.ap, pa.ap, reads=[pa.t], writes=[st.t])
                P.dma("pool", vall_d[t0:t0 + 128, gi * 512:(gi + 1) * 512], st.ap, reads=[st.t],
                      writes=[tvall(sub)])
        P.barrier()
        if stop <= 2:
            continue
        A.off = PH0_END
        KSCALE = 128.0 ** -0.5
        KT = A.alloc([128, NALL], BF16, "KT")
        QT = A.alloc([128, NALL], BF16, "QT")
        Vr = A.alloc([128, 34, 256], BF16, "Vr")
        Ur = A.alloc([128, 2, NALL], BF16, "Ur")
        Kf = A.alloc([128, 34, 128], BF16, "Kf")
        Kb = A.alloc([128, 34, 128], BF16, "Kb")
        SFb = A.alloc([128, 18, 256], BF16, "SFb")
        SBb = A.alloc([128, 18, 256], BF16, "SBb")
        Dm = A.alloc([128, 512], BF16, "Dm")
        qdf = A.alloc([128, 512], F32, "qdf")
        qdb = A.alloc([128, 512], F32, "qdb")
        e1 = A.alloc([128, 128], F32, "e1")
        e2 = A.alloc([128, 128], F32, "e2")
        kd = A.alloc([128, 4], F32, "kd")
        S = A.alloc([128, 256], F32, "S")
        S2 = [A.alloc([128, 256], F32, f"S2{i}") for i in range(2)]
        S3 = [A.alloc([128, 256], F32, f"S3{i}") for i in range(2)]
        KVf = A.alloc([128, 34, 256], F32, "KVf")
        KVb = A.alloc([128, 34, 256], F32, "KVb")
        S0f = A.alloc([128, 256], F32, "S0f")
        S0b = A.alloc([128, 256], F32, "S0b")
        Pm = A.alloc([128, 512], BF16, "Pm")
        Qf = A.alloc([128, 512], BF16, "Qf")
        Qb = A.alloc([128, 512], BF16, "Qb")
        sq2 = A.alloc([128, 2, 512], BF16, "sq2")
        rs3 = A.alloc([128, 512], F32, "rs3")
        to_ = [A.alloc([128, 512], F32, f"to{i}") for i in range(2)]
        stg = [A.alloc([128, 512], BF16, f"stg{i}") for i in range(2)]
        flg = sg.ap[:, SG_FLAG:SG_FLAG + 2]
        for h in range(4):
            lgf, lgb = lg.ap[:, h:h + 1], lg.ap[:, 4 + h:5 + h]
            P.act(e1.ap, cst.ap[:, C_DPOS:C_DPOS + 128], AF.Exp, reads=[cst.t, lg.t], writes=[e1.t], scale=lgf)
            P.tt("dve", e1.ap, e1.ap, cst.ap[:, C_MF:C_MF + 128], ALU.mult, reads=[e1.t, cst.t], writes=[e1.t])
            P.act(e2.ap, cst.ap[:, C_DNEG:C_DNEG + 128], AF.Exp, reads=[cst.t, lg.t], writes=[e2.t], scale=lgb)
            P.tt("dve", e2.ap, e2.ap, cst.ap[:, C_MB:C_MB + 128], ALU.mult, reads=[e2.t, cst.t], writes=[e2.t])
            P.tt("dve", e1.ap, e1.ap, e2.ap, ALU.add, reads=[e1.t, e2.t], writes=[e1.t])
            for r in range(4):
                P.ts("dve", Dm.ap[:, r * 128:(r + 1) * 128], e1.ap, KSCALE, None, ALU.mult, reads=[e1.t],
                     writes=[Dm.t])
                P.act(qdf.ap[:, r * 128:(r + 1) * 128], cst.ap[:, C_IP1:C_IP1 + 128], AF.Exp, reads=[cst.t, lg.t],
                      writes=[qdf.t], scale=lgf)
                P.act(qdb.ap[:, r * 128:(r + 1) * 128], cst.ap[:, C_IB:C_IB + 128], AF.Exp, reads=[cst.t, lg.t],
                      writes=[qdb.t], scale=lgb)
            P.act(kd.ap[:, 0:1], cst.ap[:, C_PCF:C_PCF + 1], AF.Exp, reads=[cst.t, lg.t], writes=[kd.t], scale=lgf)
            P.act(kd.ap[:, 1:2], cst.ap[:, C_PCB:C_PCB + 1], AF.Exp, reads=[cst.t, lg.t], writes=[kd.t], scale=lgb)
            P.ts("dve", kd.ap[:, 0:2], kd.ap[:, 0:2], KSCALE, None, ALU.mult, reads=[kd.t], writes=[kd.t])
            P.act(kd.ap[:, 2:3], lgf, AF.Exp, reads=[lg.t], writes=[kd.t], scale=128.0)
            P.act(kd.ap[:, 3:4], lgb, AF.Exp, reads=[lg.t], writes=[kd.t], scale=128.0)
            P.dma("sp", KT.ap, fm_d[f"kr{h}"], writes=[KT.t])
            P.dma("sp", QT.ap[:, 0:NOWN], fm_d[f"qr{h}"][:, 0:NOWN], writes=[QT.t])
            if need_ctx:
                P.dma("sp", QT.ap[:, 4096:NALL], fm_d[f"qr{h}"][:, 4096:NALL], writes=[QT.t])
            vsrc = vall_d[:, h * 256:(h + 1) * 256].rearrange("(t p) c -> p t c", p=128)
            for tq in range(0, 34, 4):
                P.dma("sp", Vr.ap[:, tq:min(tq + 4, 34), :], vsrc[:, tq:min(tq + 4, 34), :], writes=[Vr.t])
            for j in range(2):
                P.dma("sp", Ur.ap[:, j, 0:NOWN], fm_d[f"ur{2 * h + j}"][:, 0:NOWN], writes=[Ur.t])
                if need_ctx:
                    P.dma("sp", Ur.ap[:, j, 4096:NALL], fm_d[f"ur{2 * h + j}"][:, 4096:NALL], writes=[Ur.t])
            for c0 in (range(0, 32 if 'trlast' in SKIP else 34, 4) if 'tr' not in SKIP else []):
                n = min(4, 34 - c0)
                pb_ = ps_next()
                for c in range(n):
                    P.mm(pb_.ap[:, c * 128:(c + 1) * 128], KT.ap[:, (c0 + c) * 128:(c0 + c + 1) * 128], id_bf.ap,
                         True, True, reads=[KT.t, id_bf.t], writes=[pb_.t])
                P.ts("dve", Kf.ap[:, c0:c0 + n, :], pb_.ap[:, 0:n * 128].rearrange("p (a b) -> p a b", a=n),
                     kd.ap[:, 0:1], None, ALU.mult, reads=[pb_.t, kd.t], writes=[Kf.t])
                P.act(Kb.ap[:, c0:c0 + n, :], pb_.ap[:, 0:n * 128].rearrange("p (a b) -> p a b", a=n), AF.Identity,
                      reads=[pb_.t, kd.t], writes=[Kb.t], scale=kd.ap[:, 1:2])

            for kvi, (Kx, KV) in enumerate(((Kf, KVf), (Kb, KVb))):
                for c0 in range(0, 34, 2):
                    pb_ = ps_next()
                    for c in range(2):
                        P.mm(pb_.ap[:, c * 256:(c + 1) * 256], Kx.ap[:, c0 + c, :], Vr.ap[:, c0 + c, :], True, True,
                             reads=[Kx.t, Vr.t], writes=[pb_.t])
                    P.copy("act" if (c0 // 2) % 2 == 0 else "dve", KV.ap[:, c0:c0 + 2, :],
                           pb_.ap.rearrange("p (a b) -> p a b", a=2), reads=[pb_.t], writes=[KV.t])
            class Chain:
                def __init__(self, bufs):
                    self.b = bufs
                    self.cur = 0
                    self.steps = []

                def upd(self, Kx, c, cdcol, first):
                    KV = KVf if Kx is Kf else KVb
                    if first:
                        d = self.b[self.cur]
                        self.steps.append(lambda d=d, KV=KV, c=c: P.copy("dve", d.ap, KV.ap[:, c, :], reads=[KV.t],
                                                                     writes=[d.t]))
                    else:
                        a, d = self.b[self.cur], self.b[1 - self.cur]
                        self.steps.append(lambda a=a, d=d, KV=KV, c=c, cdcol=cdcol: P.stt(
                            d.ap, a.ap, kd.ap[:, cdcol:cdcol + 1], KV.ap[:, c, :], ALU.mult, ALU.add,
                            reads=[a.t, kd.t, KV.t], writes=[d.t]))
                        self.cur = 1 - self.cur

                def snap(self, dst, idx):
                    a = self.b[self.cur]
                    self.steps.append(lambda a=a, dst=dst, idx=idx: P.copy("act", dst.ap[:, idx, :], a.ap,
                                                                          reads=[a.t], writes=[dst.t]))

                def save(self, S0):
                    a = self.b[self.cur]
                    self.steps.append(lambda a=a, S0=S0: P.copy("dve", S0.ap, a.ap, reads=[a.t], writes=[S0.t]))

                def blend(self, S0, fcol):
                    a, b = self.b[self.cur], self.b[1 - self.cur]

                    def f(a=a, b=b, S0=S0, fcol=fcol):
                        P.tt("dve", b.ap, a.ap, S0.ap, ALU.subtract, reads=[a.t, S0.t], writes=[b.t])
                        P.stt(a.ap, b.ap, flg[:, fcol:fcol + 1], S0.ap, ALU.mult, ALU.add, reads=[b.t, S0.t, sg.t],
                              writes=[a.t])
                    self.steps.append(f)

            cf, cb = Chain(S2), Chain(S3)
            cf.upd(Kf, 32, 2, True)
            cf.snap(SFb, 17)
            cf.upd(Kf, 33, 2, False)
            cf.save(S0f)
            for c in range(16, 32):
                cf.upd(Kf, c, 2, False)
            cf.blend(S0f, 1)
            for c in range(0, 16):
                cf.snap(SFb, c)
                if c < 15:
                    cf.upd(Kf, c, 2, False)
            cb.upd(Kb, 33, 3, True)
            cb.snap(SBb, 16)
            cb.upd(Kb, 32, 3, False)
            cb.save(S0b)
            for c in range(31, 15, -1):
                cb.upd(Kb, c, 3, False)
            cb.blend(S0b, 0)
            for c in range(15, -1, -1):
                cb.snap(SBb, c)
                if c > 0:
                    cb.upd(Kb, c, 3, False)
            for i in range(max(len(cf.steps), len(cb.steps))):
                if i < len(cf.steps):
                    cf.steps[i]()
                if i < len(cb.steps):
                    cb.steps[i]()
            for (t0, nt, mj) in (qtiles if 'out' not in SKIP else []):
                ncx = nt // 128
                pS = ps_next()
                for j in range(ncx):
                    sl = slice(t0 + j * 128, t0 + (j + 1) * 128)
                    P.mm(pS.ap[:, j * 128:(j + 1) * 128], KT.ap[:, sl], QT.ap[:, sl], True, True,
                         reads=[KT.t, QT.t], writes=[pS.t])
                P.tt("dve", Pm.ap[:, 0:nt], pS.ap[:, 0:nt], Dm.ap[:, 0:nt], ALU.mult, reads=[pS.t, Dm.t],
                     writes=[Pm.t])
                P.tt("pool", Qf.ap[:, 0:nt], QT.ap[:, t0:t0 + nt], qdf.ap[:, 0:nt], ALU.mult, reads=[QT.t, qdf.t],
                     writes=[Qf.t])
                P.tt("pool", Qb.ap[:, 0:nt], QT.ap[:, t0:t0 + nt], qdb.ap[:, 0:nt], ALU.mult, reads=[QT.t, qdb.t],
                     writes=[Qb.t])
                pO = [ps_next(), ps_next()]
                for dj in range(2):
                    dsl = slice(dj * 128, (dj + 1) * 128)
                    for j in range(ncx):
                        c = t0 // 128 + j
                        csl = slice(j * 128, (j + 1) * 128)
                        if c < 16:
                            sf, sb = c, c
                        elif c == 32:
                            sf, sb = None, 16
                        else:
                            sf, sb = 17, None
                        terms = [(Vr.ap[:, c, dsl], Pm.ap[:, csl], [Vr.t, Pm.t])]
                        if sf is not None:
                            terms.append((SFb.ap[:, sf, dsl], Qf.ap[:, csl], [SFb.t, Qf.t]))
                        if sb is not None:
                            terms.append((SBb.ap[:, sb, dsl], Qb.ap[:, csl], [SBb.t, Qb.t]))
                        for ti, (l_, r_, rd) in enumerate(terms):
                            P.mm(pO[dj].ap[:, csl], l_, r_, ti == 0, ti == len(terms) - 1, reads=rd,
                                 writes=[pO[dj].t])
                    P.act(sq2.ap[:, dj, 0:nt], pO[dj].ap[:, 0:nt], AF.Square, reads=[pO[dj].t], writes=[sq2.t])
                pN = ps_next()
                for dj in range(2):
                    P.mm(pN.ap[:, 0:nt], ones_bf.ap, sq2.ap[:, dj, 0:nt], dj == 0, dj == 1,
                         reads=[ones_bf.t, sq2.t], writes=[pN.t])
                rsqrt_from_psum(rs3.ap[:, 0:nt], pN.ap[:, 0:nt], 256.0, [pN.t], [rs3.t])
                for dj in range(2):
                    P.tt("dve", to_[dj].ap[:, 0:nt], pO[dj].ap[:, 0:nt], rs3.ap[:, 0:nt], ALU.mult,
                         reads=[pO[dj].t, rs3.t], writes=[to_[dj].t])
                    P.tt("pool", stg[dj].ap[:, 0:nt], to_[dj].ap[:, 0:nt], Ur.ap[:, dj, t0:t0 + nt], ALU.mult,
                         reads=[to_[dj].t, Ur.t], writes=[stg[dj].t])
                    r0 = (2 * h + dj) * 128
                    P.dma("sp", obr_d[0][r0:r0 + 128, qpos(t0):qpos(t0) + nt], stg[dj].ap[:, 0:nt],
                          reads=[stg[dj].t], writes=[tobr(0, (h, dj, t0))])
        P.barrier()
        if stop <= 3:
            continue
        def run_attn(groups, Pt, depth=2):
            items = [(gi, ki) for gi, g in enumerate(groups) for ki in range(g["n"])]
            pts = {}
            sidx = [0]

            def SE(idx):
                gi, ki = items[idx]
                g = groups[gi]
                pS = ps[sidx[0] % 4]
                sidx[0] += 1
                g["S"](ki, pS)
                pt = Pt[idx % len(Pt)]
                g["E"](ki, pS, pt)
                pts[idx] = pt
            for idx in range(min(depth, len(items))):
                SE(idx)
            for idx in range(len(items)):
                if idx + depth < len(items):
                    SE(idx + depth)
                gi, ki = items[idx]
                g = groups[gi]
                pO, pD = (ps[4], ps[5]) if gi % 2 == 0 else (ps[6], ps[7])
                g["PV"](ki, pts.pop(idx), pO, pD, ki == 0, ki == g["n"] - 1)
                if ki == g["n"] - 1:
                    g["epi"](pO, pD)

        A.off = PH0_END
        KSa = A.alloc([128, NALL], BF16, "KSa")
        KSb = A.alloc([128, NALL], BF16, "KSb")
        QS = A.alloc([128, 4, NALL], BF16, "QS")
        Vs = A.alloc([128, 34, 64], BF16, "Vs")
        Pt = [A.alloc([128, 512], BF16, f"Pt{i}") for i in range(6)]
        sk = [A.alloc([64, 512], F32, f"sk{i}") for i in range(2)]
        den = [A.alloc([128, 512], F32, f"den{i}") for i in range(2)]
        ostg = [A.alloc([128, 512], BF16, f"ostg{i}") for i in range(2)]
        P.dma("sp", KSa.ap, fm_d["ks"], writes=[KSa.t])
        P.dma("sp", KSb.ap, fm_d["ks2"], writes=[KSb.t])
        obr1 = obr_d[1].rearrange("(c p) t -> p c t", p=128)
        gcount = 0
        for g in range(2):
            for i in range(4):
                P.dma("sp", QS.ap[:, i, 0:NOWN], fm_d[f"qs{g * 4 + i}"][:, 0:NOWN], writes=[QS.t])
                if need_ctx:
                    P.dma("sp", QS.ap[:, i, 4096:NALL], fm_d[f"qs{g * 4 + i}"][:, 4096:NALL], writes=[QS.t])
            vsrc = vall_d[:, 1024 + g * 64:1024 + (g + 1) * 64].rearrange("(t p) c -> p t c", p=128)
            for tq in range(0, 34, 4):
                P.dma("sp", Vs.ap[:, tq:min(tq + 4, 34), :], vsrc[:, tq:min(tq + 4, 34), :], writes=[Vs.t])
            groups = []
            for par in range(2):
                pb0 = par * 64
                Ksrc = KSa if g == par else KSb
                sk_ = sk[par]
                for i in range(4):
                    hq = g * 8 + par + 2 * i
                    P.act(sk_.ap[:, i * 128:(i + 1) * 128], cst.ap[0:64, C_DPOS:C_DPOS + 128], AF.Identity,
                          reads=[cst.t, sinkx.t], writes=[sk_.t], bias=sinkx.ap[0:64, hq:hq + 1], scale=0.0)
                blocks = [(jb * 128, jb) for jb in range(16)] + ([(4096, 16), (4224, 17)] if need_ctx else [])
                for (t0, jb) in blocks:
                    if jb < 16:
                        kts = [((jb - 1) * 128, 0) if jb > 0 else (2048 + 15 * 128, 2), (jb * 128, None),
                               ((jb + 1) * 128, 1) if jb < 15 else (2048, 3), (4096, None), (4224, None)]
                    else:
                        kts = [(4096, None), (4224, None)]

                    def S(ki, pS, kts=kts, pb0=pb0, Ksrc=Ksrc, t0=t0):
                        k0 = kts[ki][0]
                        P.mm(pS.ap.rearrange("p (a b) -> p a b", a=4), Ksrc.ap[pb0:pb0 + 64, k0:k0 + 128],
                             QS.ap[pb0:pb0 + 64, :, t0:t0 + 128], True, True, reads=[Ksrc.t, QS.t], writes=[pS.t])

                    def E(ki, pS, pt, kts=kts):
                        P.act(pt.ap, pS.ap, AF.Exp, reads=[pS.t], writes=[pt.t], scale=0.125)
                        mi = kts[ki][1]
                        if mi is not None:
                            P.tt("pool", pt.ap, pt.ap, mk_bf.ap[:, mi, :], ALU.mult, reads=[pt.t, mk_bf.t],
                                 writes=[pt.t])

                    def PV(ki, pt, pO, pD, first, lastk, kts=kts):
                        k0 = kts[ki][0]
                        P.mm(pO.ap[0:64, :], Vs.ap[:, k0 // 128, :], pt.ap, first, lastk, reads=[Vs.t, pt.t],
                             writes=[pO.t])
                        P.mm(pD.ap[0:64, :], ones_bf.ap[:, 0:64], pt.ap, first, lastk, reads=[ones_bf.t, pt.t],
                             writes=[pD.t])

                    def epi(pO, pD, t0=t0, pb0=pb0, g=g, sk_=sk_, gc=gcount):
                        dn_, os_ = den[gc % 2], ostg[gc % 2]
                        P.tt("dve", dn_.ap[0:64, :], pD.ap[0:64, :], sk_.ap, ALU.add, reads=[pD.t, sk_.t],
                             writes=[dn_.t])
                        P.act(dn_.ap[0:64, :], dn_.ap[0:64, :], AF.Ln, reads=[dn_.t], writes=[dn_.t])
                        P.act(dn_.ap[0:64, :], dn_.ap[0:64, :], AF.Exp, reads=[dn_.t], writes=[dn_.t], scale=-1.0)
                        P.tt("dve", os_.ap[0:64, :], pO.ap[0:64, :], dn_.ap[0:64, :], ALU.mult,
                             reads=[pO.t, dn_.t], writes=[os_.t])
                        q0 = qpos(t0)
                        P.dma("sp", obr1[pb0:pb0 + 64, g * 4:g * 4 + 4, q0:q0 + 128],
                              os_.ap[0:64, :].rearrange("p (a b) -> p a b", a=4), reads=[os_.t],
                              writes=[tobr(1, (g, pb0, t0))])
                    groups.append(dict(n=len(kts), S=S, E=E, PV=PV, epi=epi))
                    gcount += 1
            run_attn(groups, Pt, depth=3)
        P.barrier()
        if stop <= 4:
            continue
        A.off = PH0_END
        KA = A.alloc([128, NALL], BF16, "KA")
        VA = A.alloc([128, 34, 128], BF16, "VA")
        QA = [A.alloc([128, NALL], BF16, f"QA{i}") for i in range(4)]
        Pt = [A.alloc([128, 512], BF16, f"Pt{i}") for i in range(4)]
        den = [A.alloc([128, 512], F32, f"den{i}") for i in range(2)]
        ostg = [A.alloc([128, 512], BF16, f"ostg{i}") for i in range(2)]
        GSCALE = 128.0 ** -0.5
        gcount = 0
        for g in range(2):
            P.dma("sp", KA.ap, fm_d[f"ka{g}"], writes=[KA.t])
            vsrc = vall_d[:, 1152 + g * 128:1152 + (g + 1) * 128].rearrange("(t p) c -> p t c", p=128)
            for tq in range(0, 34, 4):
                P.dma("sp", VA.ap[:, tq:min(tq + 4, 34), :], vsrc[:, tq:min(tq + 4, 34), :], writes=[VA.t])
            groups = []
            for hh in range(4):
                h = g * 4 + hh
                Q_ = QA[hh]
                P.dma("sp", Q_.ap[:, 0:NOWN], fm_d[f"qa{h}"][:, 0:NOWN], writes=[Q_.t])
                if need_ctx:
                    P.dma("sp", Q_.ap[:, 4096:NALL], fm_d[f"qa{h}"][:, 4096:NALL], writes=[Q_.t])
                for (t0, nt, mj) in qtiles:
                    kts = list(range(34)) if t0 < 4096 else [32, 33]

                    def S(ki, pS, kts=kts, Q_=Q_, t0=t0, nt=nt):
                        kt = kts[ki]
                        P.mm(pS.ap[:, 0:nt], KA.ap[:, kt * 128:(kt + 1) * 128], Q_.ap[:, t0:t0 + nt], True, True,
                             reads=[KA.t, Q_.t], writes=[pS.t])

                    def E(ki, pS, pt, nt=nt):
                        P.act(pt.ap[:, 0:nt], pS.ap[:, 0:nt], AF.Exp, reads=[pS.t], writes=[pt.t], scale=GSCALE)

                    def PV(ki, pt, pO, pD, first, lastk, kts=kts, nt=nt):
                        kt = kts[ki]
                        P.mm(pO.ap[:, 0:nt], VA.ap[:, kt, :], pt.ap[:, 0:nt], first, lastk, reads=[VA.t, pt.t],
                             writes=[pO.t])
                        P.mm(pD.ap[:, 0:nt], ones_bf.ap, pt.ap[:, 0:nt], first, lastk, reads=[ones_bf.t, pt.t],
                             writes=[pD.t])

                    def epi(pO, pD, t0=t0, nt=nt, h=h, gc=gcount):
                        dn_, os_ = den[gc % 2], ostg[gc % 2]
                        P.act(dn_.ap[:, 0:nt], pD.ap[:, 0:nt], AF.Ln, reads=[pD.t], writes=[dn_.t])
                        P.act(dn_.ap[:, 0:nt], dn_.ap[:, 0:nt], AF.Exp, reads=[dn_.t], writes=[dn_.t], scale=-1.0)
                        P.tt("dve", os_.ap[:, 0:nt], pO.ap[:, 0:nt], dn_.ap[:, 0:nt], ALU.mult,
                             reads=[pO.t, dn_.t], writes=[os_.t])
                        q0 = qpos(t0)
                        P.dma("sp", obr_d[2][h * 128:(h + 1) * 128, q0:q0 + nt], os_.ap[:, 0:nt], reads=[os_.t],
                              writes=[tobr(2, (h, t0))])
                    groups.append(dict(n=len(kts), S=S, E=E, PV=PV, epi=epi))
                    gcount += 1
            run_attn(groups, Pt)
        P.barrier()
        if stop <= 5:
            continue
        A.off = PH0_END
        wm = [A.alloc([128, 8, 1024], BF16, f"wm{i}") for i in range(4)]
        ob = [A.alloc([128, 8, 512], BF16, f"ob{i}") for i in range(3)]
        gb = [A.alloc([128, 8, 512], BF16, f"gb{i}") for i in range(3)]
        ypre = A.alloc([128, 8, 512], BF16, "ypre")
        xtl = A.alloc([128, 8, 512], F32, "xtl")
        ya = [A.alloc([128, 512], F32, f"ya{i}") for i in range(2)]
        tb = [A.alloc([128, 512], F32, f"tb{i}") for i in range(2)]
        for gi in range(8):
            dst = wm[gi // 2].ap[:, :, (gi % 2) * 512:(gi % 2 + 1) * 512]
            P.dma("pool", dst, wall_d[li][G_MERGE + gi], writes=[wm[gi // 2].t])
        gnames = ("ar", "as", "aa")
        for (t0, nt, mj) in qtiles:
            q0 = qpos(t0)
            for b_ in range(3):
                P.dma("sp", ob[b_].ap[:, :, 0:nt],
                      obr_d[b_].rearrange("(kc p) t -> p kc t", p=128)[:, :, q0:q0 + nt], writes=[ob[b_].t])
                for c in range(8):
                    P.dma("sp", gb[b_].ap[:, c, 0:nt], fm_d[f"{gnames[b_]}{c}"][:, t0:t0 + nt], writes=[gb[b_].t])
            P.dma("sp", xtl.ap[:, :, 0:nt], xs3[:, :, t0:t0 + nt], reads=[txs(t0)], writes=[xtl.t])
            for dc in range(8):
                dsl = slice(dc * 128, (dc + 1) * 128)
                y_ = ya[dc % 2]
                for b_ in range(3):
                    pa = ps_next()
                    for kc in range(8):
                        P.mm(pa.ap[:, 0:nt], wm[b_].ap[:, kc, dsl], ob[b_].ap[:, kc, 0:nt], kc == 0, kc == 7,
                             reads=[wm[b_].t, ob[b_].t], writes=[pa.t])
                    if b_ == 0:
                        P.tt("dve", y_.ap[:, 0:nt], pa.ap[:, 0:nt], gb[0].ap[:, dc, 0:nt], ALU.mult,
                             reads=[pa.t, gb[0].t], writes=[y_.t])
                    else:
                        t_ = tb[b_ % 2]
                        P.tt("dve", t_.ap[:, 0:nt], pa.ap[:, 0:nt], gb[b_].ap[:, dc, 0:nt], ALU.mult,
                             reads=[pa.t, gb[b_].t], writes=[t_.t])
                        if b_ == 1:
                            P.tt("pool", y_.ap[:, 0:nt], y_.ap[:, 0:nt], t_.ap[:, 0:nt], ALU.add,
                                 reads=[y_.t, t_.t], writes=[y_.t])
                        else:
                            P.tt("pool", ypre.ap[:, dc, 0:nt], y_.ap[:, 0:nt], t_.ap[:, 0:nt], ALU.add,
                                 reads=[y_.t, t_.t], writes=[ypre.t])
            for dc in range(8):
                dsl = slice(dc * 128, (dc + 1) * 128)
                py = ps_next()
                for kc in range(8):
                    P.mm(py.ap[:, 0:nt], wm[3].ap[:, kc, dsl], ypre.ap[:, kc, 0:nt], kc == 0, kc == 7,
                         reads=[wm[3].t, ypre.t], writes=[py.t])
                P.stt(xtl.ap[:, dc, 0:nt], py.ap[:, 0:nt], GT1(dc, mj), xtl.ap[:, dc, 0:nt], ALU.mult, ALU.add,
                      reads=[py.t, modT.t, xtl.t], writes=[xtl.t])
            P.dma("pool", xs3[:, :, t0:t0 + nt], xtl.ap[:, :, 0:nt], reads=[xtl.t], writes=[txs(t0)])
        P.barrier()
        if stop <= 6:
            continue
        A.off = PH0_END
        h2T = A.alloc([128, 8, NQ], BF16, "h2T")
        xres = A.alloc([128, 8, NQ], F32, "xres")
        WT = A.alloc([16, NQ], F32, "WT")
        M0 = A.off
        h2f = A.alloc([128, 8, 512], F32, "h2f")
        sq = A.alloc([128, 8, 512], BF16, "sq")
        rstd = A.alloc([128, 512], F32, "rstd")
        tmp = [A.alloc([128, 512], F32, f"tmp{i}") for i in range(2)]
        rt = {nm: A.alloc([128, 16], F32, "rt_" + nm) for nm in ("s", "bz", "eq", "msk", "ch", "ws", "wts")}
        rs = {nm: A.alloc([128, 4], F32, "rs_" + nm) for nm in ("m1", "m2", "gs", "gsel", "gm", "dn")}

        def v3(b_):
            return b_.ap.rearrange("p (g k) -> p g k", k=4)

        def bc(b_):
            return b_.ap[:, 0:4].unsqueeze(2).to_broadcast([128, 4, 4])
        mtiles = qtiles
        for i, (t0, nt, mj) in enumerate(mtiles):
            q0 = qpos(t0)
            P.dma("sp", xres.ap[:, :, q0:q0 + nt], xs3[:, :, t0:t0 + nt], reads=[txs(t0)], writes=[xres.t])
            P.act(sq.ap[:, :, 0:nt], xres.ap[:, :, q0:q0 + nt], AF.Square, reads=[xres.t], writes=[sq.t])
            pb = ps_next()
            for kc in range(8):
                P.mm(pb.ap[:, 0:nt], ones_bf.ap, sq.ap[:, kc, 0:nt], kc == 0, kc == 7, reads=[ones_bf.t, sq.t],
                     writes=[pb.t])
            rsqrt_from_psum(rstd.ap[:, 0:nt], pb.ap[:, 0:nt], 1024.0, [pb.t], [rstd.t])
            for kc in range(8):
                tm_ = tmp[kc % 2]
                P.tt("dve", tm_.ap[:, 0:nt], xres.ap[:, kc, q0:q0 + nt], rstd.ap[:, 0:nt], ALU.mult,
                     reads=[xres.t, rstd.t], writes=[tm_.t])
                P.act(h2f.ap[:, kc, 0:nt], tm_.ap[:, 0:nt], AF.Identity, reads=[tm_.t, A2.t, modT.t], writes=[h2f.t],
                      bias=SH2(kc, mj), scale=A2.ap[:, kc, mj:mj + 1])
                P.copy("pool", h2T.ap[:, kc, q0:q0 + nt], h2f.ap[:, kc, 0:nt], reads=[h2f.t], writes=[h2T.t])
            for sub in range(nt // 128):
                ssl = slice(sub * 128, (sub + 1) * 128)
                pr = ps_next()
                for kc in range(8):
                    P.mm(pr.ap[:, 0:16], h2f.ap[:, kc, ssl], sg.ap[:, SG_WR + kc * 16:SG_WR + (kc + 1) * 16],
                         kc == 0, kc == 7, reads=[h2f.t, sg.t], writes=[pr.t])
                s_, bz, eq, msk, ch, ws, wts = [rt[n] for n in ("s", "bz", "eq", "msk", "ch", "ws", "wts")]
                m1, m2, gs, gsel, gm, dn = [rs[n] for n in ("m1", "m2", "gs", "gsel", "gm", "dn")]
                P.act(s_.ap, pr.ap[:, 0:16], AF.Sigmoid, reads=[pr.t], writes=[s_.t])
                P.tt("dve", bz.ap, s_.ap, sg.ap[:, SG_BR:SG_BR + 16], ALU.add, reads=[s_.t, sg.t], writes=[bz.t])
                P.add("dve", lambda e, o=m1.ap, i_=v3(bz): e.tensor_reduce(out=o, in_=i_, axis=AX.X, op=ALU.max),
                      reads=[bz.t], writes=[m1.t])
                P.tt("dve", v3(eq), v3(bz), bc(m1), ALU.is_equal, reads=[bz.t, m1.t], writes=[eq.t])
                P.stt(msk.ap, eq.ap, -1e9, bz.ap, ALU.mult, ALU.add, reads=[eq.t, bz.t], writes=[msk.t])
                P.add("dve", lambda e, o=m2.ap, i_=v3(msk): e.tensor_reduce(out=o, in_=i_, axis=AX.X, op=ALU.max),
                      reads=[msk.t], writes=[m2.t])
                P.tt("dve", gs.ap, m1.ap, m2.ap, ALU.add, reads=[m1.t, m2.t], writes=[gs.t])
                P.add("dve", lambda e, o=gm.ap[:, 0:1], i_=gs.ap: e.tensor_reduce(out=o, in_=i_, axis=AX.X,
                                                                                 op=ALU.max),
                      reads=[gs.t], writes=[gm.t])
                P.ts("dve", gsel.ap, gs.ap, gm.ap[:, 0:1], None, ALU.is_equal, reads=[gs.t, gm.t], writes=[gsel.t])
                P.tt("dve", v3(ch), v3(bz), bc(m2), ALU.is_ge, reads=[bz.t, m2.t], writes=[ch.t])
                P.tt("dve", v3(ch), v3(ch), bc(gsel), ALU.mult, reads=[ch.t, gsel.t], writes=[ch.t])
                P.tt("dve", ws.ap, s_.ap, ch.ap, ALU.mult, reads=[s_.t, ch.t], writes=[ws.t])
                P.add("dve", lambda e, o=dn.ap[:, 0:1], i_=ws.ap: e.tensor_reduce(out=o, in_=i_, axis=AX.X,
                                                                                 op=ALU.add),
                      reads=[ws.t], writes=[dn.t])
                P.add("dve", lambda e, o=dn.ap[:, 1:2], i_=dn.ap[:, 0:1]: e.reciprocal(o, i_), reads=[dn.t],
                      writes=[dn.t])
                P.ts("dve", wts.ap, ws.ap, dn.ap[:, 1:2], None, ALU.mult, reads=[ws.t, dn.t], writes=[wts.t])
                pT = ps_next()
                P.transpose(pT.ap[0:16, 0:128], wts.ap, cst.ap[:, C_ID:C_ID + 128], reads=[wts.t, cst.t],
                            writes=[pT.t])
                P.copy("act", WT.ap[0:16, q0 + sub * 128:q0 + (sub + 1) * 128], pT.ap[0:16, 0:128], reads=[pT.t],
                       writes=[WT.t])
        P.barrier()
        A.off = M0
        ew = [[A.alloc([128, 4096], BF16, f"ew{i}_{k}") for k in range(3)] for i in range(2)]
        wbc = A.alloc([128, 512], F32, "wbc")
        sG = [A.alloc([128, 512], F32, f"sG{i}") for i in range(2)]
        tG = [A.alloc([128, 512], F32, f"tG{i}") for i in range(2)]
        hid = A.alloc([128, 4, 512], BF16, "hid")
        def load_expert(e):
            for k in range(3):
                P.dma("pool", ew[e % 2][k].ap, we_d[li][e, k], writes=[ew[e % 2][k].t])

        hidb = [hid, A.alloc([128, 4, 512], BF16, "hid2")]
        wbcb = [wbc, A.alloc([128, 512], F32, "wbc2")]
        items = [(e, ti) for e in range(16) for ti in range(len(mtiles))]

        def GU(ix):
            e, ti = items[ix]
            wg_, wu_, wd_ = ew[e % 2]
            wg3 = wg_.ap.rearrange("p (a b) -> p a b", a=8)
            wu3 = wu_.ap.rearrange("p (a b) -> p a b", a=8)
            t0, nt, mj = mtiles[ti]
            q0 = qpos(t0)
            hid_, wbc_ = hidb[ix % 2], wbcb[ix % 2]
            pw = ps_next(8)
            P.mm(pw.ap[:, 0:nt], cst.ap[0:16, C_SEL + e * 128:C_SEL + (e + 1) * 128], WT.ap[0:16, q0:q0 + nt],
                 True, True, reads=[cst.t, WT.t], writes=[pw.t])
            P.copy("act", wbc_.ap[:, 0:nt], pw.ap[:, 0:nt], reads=[pw.t], writes=[wbc_.t])
            for hc in range(4):
                hsl = slice(hc * 128, (hc + 1) * 128)
                pg, pu = ps_next(8), ps_next(8)
                for kc in range(8):
                    P.mm(pg.ap[:, 0:nt], wg3[:, kc, hsl], h2T.ap[:, kc, q0:q0 + nt], kc == 0, kc == 7,
                         reads=[wg_.t, h2T.t], writes=[pg.t])
                for kc in range(8):
                    P.mm(pu.ap[:, 0:nt], wu3[:, kc, hsl], h2T.ap[:, kc, q0:q0 + nt], kc == 0, kc == 7,
                         reads=[wu_.t, h2T.t], writes=[pu.t])
                sg_, tg_ = sG[hc % 2], tG[hc % 2]
                P.act(sg_.ap[:, 0:nt], pg.ap[:, 0:nt], AF.Silu, reads=[pg.t], writes=[sg_.t])
                P.tt("dve", tg_.ap[:, 0:nt], sg_.ap[:, 0:nt], pu.ap[:, 0:nt], ALU.mult, reads=[sg_.t, pu.t],
                     writes=[tg_.t])
                P.tt("pool", hid_.ap[:, hc, 0:nt], tg_.ap[:, 0:nt], wbc_.ap[:, 0:nt], ALU.mult,
                     reads=[tg_.t, wbc_.t], writes=[hid_.t])

        def DOWN(ix):
            e, ti = items[ix]
            wd_ = ew[e % 2][2]
            wd3 = wd_.ap.rearrange("p (a b) -> p a b", a=4)
            t0, nt, mj = mtiles[ti]
            q0 = qpos(t0)
            hid_ = hidb[ix % 2]
            for dc in range(8):
                dsl = slice(dc * 128, (dc + 1) * 128)
                py = ps_next(8)
                for hc in range(4):
                    P.mm(py.ap[:, 0:nt], wd3[:, hc, dsl], hid_.ap[:, hc, 0:nt], hc == 0, hc == 3,
                         reads=[wd_.t, hid_.t], writes=[py.t])
                P.stt(xres.ap[:, dc, q0:q0 + nt], py.ap[:, 0:nt], GT2(dc, mj), xres.ap[:, dc, q0:q0 + nt],
                      ALU.mult, ALU.add, reads=[py.t, modT.t, xres.t], writes=[xres.t])

        load_expert(0)
        load_expert(1)
        GU(0)
        for ix in range(len(items)):
            if ix + 1 < len(items):
                GU(ix + 1)
            DOWN(ix)
            e_, ti_ = items[ix]
            if ti_ == len(mtiles) - 1 and e_ + 2 < 16:
                load_expert(e_ + 2)
        if not (last and final):
            for (t0, nt, mj) in mtiles:
                q0 = qpos(t0)
                P.dma("pool", xs3[:, :, t0:t0 + nt], xres.ap[:, :, q0:q0 + nt], reads=[xres.t], writes=[txs(t0)])
        if not last:
            for i, (t0, nt, mj) in enumerate(TOK_TILES_OWN):
                P.dma("pool", xch_src[i].rearrange("(kc p) t -> p kc t", p=128), xres.ap[:, :, t0:t0 + nt],
                      reads=[xres.t], writes=[T_xsrc[i]])
            P.barrier()
            for i in range(4):
                P.add("pool", lambda e, i=i: e.collective_compute("AllGather", ALU.bypass,
                                                                  replica_groups=[[0, 1], [2, 3], [4, 5], [6, 7]],
                                                                  ins=[xch_src[i]], outs=[xch_dst[i]]),
                      reads=[T_xsrc[i]], writes=[T_xdst[i]], cc=True)
            P.barrier()
            A.off = M0
            xa = [A.alloc([128, 8, 512], F32, f"xa{i}") for i in range(2)]
            xb_ = [A.alloc([128, 8, 512], F32, f"xb{i}") for i in range(2)]
            for i, (t0, nt, mj) in enumerate(TOK_TILES_OWN):
                d3 = xch_dst[i].rearrange("(r kc p) t -> r p kc t", r=2, p=128)
                a_, b_ = xa[i % 2], xb_[i % 2]
                P.dma("sp", a_.ap, d3[0], reads=[T_xdst[i]], writes=[a_.t])
                P.dma("sp", b_.ap, d3[1], reads=[T_xdst[i]], writes=[b_.t])
                P.ts("dve", a_.ap, a_.ap, sg.ap[:, SG_FLAG + 1:SG_FLAG + 2], None, ALU.mult, reads=[a_.t, sg.t],
                     writes=[a_.t])
                P.stt(a_.ap, b_.ap, sg.ap[:, SG_FLAG:SG_FLAG + 1], a_.ap, ALU.mult, ALU.add,
                      reads=[a_.t, b_.t, sg.t], writes=[a_.t])
                P.dma("pool", xs3[:, :, NOWN + t0:NOWN + t0 + nt], a_.ap, reads=[a_.t], writes=[txs(NOWN + t0)])
        if last and not final:
            xout3 = xout.rearrange("(kc p) t -> p kc t", p=128)
            for (t0, nt, mj) in mtiles:
                q0 = qpos(t0)
                P.dma("pool", xout3[:, :, q0:q0 + nt], xres.ap[:, :, q0:q0 + nt], reads=[xres.t])
        if last and final:
            P.barrier()
            A.off = M0
            h2f = A.alloc([128, 8, 512], F32, "h2f")
            sq = A.alloc([128, 8, 512], BF16, "sq")
            rstd = A.alloc([128, 512], F32, "rstd")
            tmp = [A.alloc([128, 512], F32, f"tmp{i}") for i in range(2)]
            yout3 = yout.rearrange("(kc p) t -> p kc t", p=128)
            for (t0, nt, mj) in TOK_TILES_OWN:
                P.act(sq.ap[:, :, 0:nt], xres.ap[:, :, t0:t0 + nt], AF.Square, reads=[xres.t], writes=[sq.t])
                pb = ps_next()
                for kc in range(8):
                    P.mm(pb.ap[:, 0:nt], ones_bf.ap, sq.ap[:, kc, 0:nt], kc == 0, kc == 7, reads=[ones_bf.t, sq.t],
                         writes=[pb.t])
                rsqrt_from_psum(rstd.ap[:, 0:nt], pb.ap[:, 0:nt], 1024.0, [pb.t], [rstd.t])
                for kc in range(8):
                    tm_ = tmp[kc % 2]
                    P.tt("dve", tm_.ap[:, 0:nt], xres.ap[:, kc, t0:t0 + nt], rstd.ap[:, 0:nt], ALU.mult,
                         reads=[xres.t, rstd.t], writes=[tm_.t])
                    P.act(h2f.ap[:, kc, 0:nt], tm_.ap[:, 0:nt], AF.Identity, reads=[tm_.t, sg.t], writes=[h2f.t],
                          scale=sg.ap[:, SG_GF + kc:SG_GF + kc + 1])
                P.dma("pool", yout3[:, :, t0:t0 + nt], h2f.ap[:, :, 0:nt], reads=[h2f.t])
    P.emit()
    return nc


_PROGS = {}


def _prog(key, *args, **kw):
    if key not in _PROGS:
        _PROGS[key] = build(*args, **kw)
    return _PROGS[key]


def kernel(**inp):
    inp = {k: np.asarray(v) for k, v in inp.items()}
    x, ctx = inp["x"], inp["ctx"]
    B = x.shape[0]
    cores = [(b, h) for b in range(B) for h in range(2)]
    consts = [make_consts(h) for h in range(2)]
    tabs = [make_tabs(h) for h in range(2)]
    w0 = prep_layer_weights(inp, 0)
    w1 = prep_layer_weights(inp, 1)
    maps = []
    for (b, h) in cores:
        xo = x[b, h * NOWN:(h + 1) * NOWN].T
        xt = x[b, (1 - h) * NOWN:(2 - h) * NOWN].T
        xall = np.ascontiguousarray(np.concatenate([xo, xt, ctx[b].T], axis=1))
        maps.append(dict(xall=xall, tabs=tabs[h], consts=consts[h], sg=make_small_global(inp, b, h),
                         sm0=w0[2], wall0=w0[0], we0=w0[1], sm1=w1[2], wall1=w1[0], we1=w1[1]))
    nc = _prog("fused", [0, 1], [True, False], True)
    r = run_bass_kernel_spmd(nc, maps, core_ids=list(range(len(cores)))).results
    out = np.zeros((B, 2 * NOWN, D), np.float32)
    for i, (b, h) in enumerate(cores):
        out[b, h * NOWN:(h + 1) * NOWN] = np.asarray(r[i]["yout"]).T
    return out
```

```python
import numpy as np
import concourse.bass as bass
import concourse.mybir as mybir
from concourse.bass_utils import run_bass_kernel_spmd

F32 = mybir.dt.float32
BF16 = mybir.dt.bfloat16
AF = mybir.ActivationFunctionType
ALU = mybir.AluOpType
AX = mybir.AxisListType

NOWN, NOTH, NCTX = 2048, 2048, 256
NALL = NOWN + NOTH + NCTX
NQ = NOWN + NCTX
D = 1024
EPS = 1e-6
NSLOT = 24
SKIP = set()


class T:
    __slots__ = ("name", "w", "r", "psum")

    def __init__(self, name="", psum=False):
        self.name = name
        self.w = None
        self.r = []
        self.psum = psum


class Op:
    __slots__ = ("eng", "fn", "deps", "id", "is_dma", "slot", "val", "marked", "semval", "cc")

    def __init__(self, eng, fn, is_dma):
        self.eng = eng
        self.fn = fn
        self.deps = []
        self.is_dma = is_dma
        self.slot = None
        self.val = None
        self.marked = False
        self.semval = None
        self.cc = False


class Prog:
    ENGS = ("pe", "act", "dve", "pool", "sp")

    def __init__(self, nc):
        self.nc = nc
        self.ops = []
        self.streams = {e: [] for e in self.ENGS}
        self.ndma = {e: 0 for e in self.ENGS}
        self.dmas = {e: [] for e in self.ENGS}

    def add(self, eng, fn, reads=(), writes=(), dma=False, extra=(), cc=False):
        op = Op(eng, fn, dma or cc)
        op.cc = cc
        op.id = len(self.ops)
        deps = {}
        for t in reads:
            if t.w is not None:
                deps[t.w.id] = t.w
            if t.psum:
                for r in t.r:
                    if r.eng != eng:
                        deps[r.id] = r
        for t in writes:
            if t.w is not None:
                deps[t.w.id] = t.w
            for r in t.r:
                deps[r.id] = r
        for d in extra:
            deps[d.id] = d
        op.deps = list(deps.values())
        for t in reads:
            if not dma:
                t.r = [r for r in t.r if r.is_dma or r.eng != eng]
            t.r.append(op)
        for t in writes:
            t.w = op
            t.r = []
        if cc:
            self.ncc = getattr(self, "ncc", 0) + 1
            op.slot = ("cc", self.ncc - 1)
            op.val = 1
            self.dmas[eng].append(op)
        elif dma:
            i = self.ndma[eng]
            self.ndma[eng] += 1
            op.slot = i % NSLOT
            op.val = 16 * (i // NSLOT + 1)
            self.dmas[eng].append(op)
        self.ops.append(op)
        self.streams[eng].append(op)
        return op

    def barrier(self):
        lasts = []
        for e in self.ENGS:
            if self.streams[e]:
                lasts.append(self.streams[e][-1])
            lasts.extend(self.dmas[e][-NSLOT:])
        for e in self.ENGS:
            self.add(e, lambda eng: eng.nop(), extra=lasts)

    def dma(self, q, out, in_, reads=(), writes=()):
        return self.add(q, lambda e: e.dma_start(out=out, in_=in_), reads, writes, dma=True)

    def mm(self, out, lhsT, rhs, start, stop, reads=(), writes=()):
        return self.add("pe", lambda e: e.matmul(out, lhsT, rhs, start=start, stop=stop), reads, writes)

    def transpose(self, out, in_, ident, reads=(), writes=()):
        return self.add("pe", lambda e: e.transpose(out, in_, ident), reads, writes)

    def act(self, out, in_, func, reads=(), writes=(), bias=None, scale=None):
        kw = {}
        if bias is not None:
            kw["bias"] = bias
        if scale is not None:
            kw["scale"] = scale
        return self.add("act", lambda e: e.activation(out, in_, func, **kw), reads, writes)

    def tt(self, eng, out, in0, in1, op, reads=(), writes=()):
        return self.add(eng, lambda e: e.tensor_tensor(out, in0, in1, op), reads, writes)

    def ts(self, eng, out, in0, s1, s2, op0, op1=None, reads=(), writes=()):
        if op1 is None:
            return self.add(eng, lambda e: e.tensor_scalar(out=out, in0=in0, scalar1=s1, scalar2=None, op0=op0),
                            reads, writes)
        return self.add(eng, lambda e: e.tensor_scalar(out=out, in0=in0, scalar1=s1, scalar2=s2, op0=op0, op1=op1),
                        reads, writes)

    def stt(self, out, in0, scalar, in1, op0, op1, reads=(), writes=()):
        return self.add("dve", lambda e: e.scalar_tensor_tensor(out=out, in0=in0, scalar=scalar, in1=in1,
                                                                op0=op0, op1=op1), reads, writes)

    def copy(self, eng, out, in_, reads=(), writes=()):
        if eng == "act":
            return self.add("act", lambda e: e.copy(out, in_), reads, writes)
        return self.add(eng, lambda e: e.tensor_copy(out, in_), reads, writes)

    def emit(self):
        nc = self.nc
        for op in self.ops:
            for d in op.deps:
                if d.is_dma:
                    continue
                if d.eng == op.eng and not op.is_dma and d.eng == "pe":
                    continue
                d.marked = True
        cnt = {e: 0 for e in self.ENGS}
        for op in self.ops:
            if op.marked and not op.is_dma:
                cnt[op.eng] += 1
                op.semval = cnt[op.eng]
        sems = {e: nc.alloc_semaphore(f"s_{e}") for e in self.ENGS}
        dsems = {e: {i: nc.alloc_semaphore(f"d_{e}_{i}") for i in range(min(NSLOT, self.ndma[e]))}
                 for e in self.ENGS}
        for i in range(getattr(self, "ncc", 0)):
            dsems["pool"][("cc", i)] = nc.alloc_semaphore(f"cc_{i}")
        engobj = {"pe": nc.tensor, "act": nc.scalar, "dve": nc.vector, "pool": nc.gpsimd, "sp": nc.sync}
        with nc.Block() as block:
            def run(ename):
                eng = engobj[ename]
                waited = {}

                def wait(key, sem, val):
                    if waited.get(key, 0) >= val:
                        return
                    waited[key] = val
                    eng.wait_ge(sem, val)

                for op in self.streams[ename]:
                    for d in op.deps:
                        if d.is_dma:
                            wait(("d", d.eng, d.slot), dsems[d.eng][d.slot], d.val)
                        else:
                            if d.eng == ename and not op.is_dma and ename == "pe":
                                continue
                            wait(("c", d.eng), sems[d.eng], d.semval)
                    if op.cc:
                        ins = op.fn(eng)
                        ins.then_inc(dsems[ename][op.slot], 1)
                    elif op.is_dma:
                        if op.val > 16:
                            wait(("d", ename, op.slot), dsems[ename][op.slot], op.val - 16)
                        ins = op.fn(eng)
                        ins.then_inc(dsems[ename][op.slot], 16)
                    else:
                        ins = op.fn(eng)
                        if op.marked:
                            ins.then_inc(sems[ename], 1)
                n = self.ndma[ename]
                for i in range(max(0, n - NSLOT), n):
                    wait(("d", ename, i % NSLOT), dsems[ename][i % NSLOT], 16 * (i // NSLOT + 1))

            @block.tensor
            def _(e):
                run("pe")

            @block.scalar
            def _(e):
                run("act")

            @block.vector
            def _(e):
                run("dve")

            @block.gpsimd
            def _(e):
                run("pool")

            @block.sync
            def _(e):
                run("sp")


class Buf:
    __slots__ = ("ap", "t")

    def __init__(self, ap, name="", psum=False):
        self.ap = ap
        self.t = T(name, psum)


class Arena:
    def __init__(self, nc, nbytes):
        self.t = nc.alloc_sbuf_tensor("arena", [128, nbytes // 2], BF16)
        self.nbytes = nbytes
        self.off = 0

    def alloc(self, shape, dtype, name=""):
        n = 1
        for s in shape[1:]:
            n *= s
        es = 4 if dtype == F32 else 2
        nb = (n * es + 63) // 64 * 64
        assert self.off + nb <= self.nbytes, f"arena overflow {name} {self.off + nb}"
        v = self.t[0:shape[0], self.off // 2:(self.off + n * es) // 2]
        if dtype == F32:
            v = v.bitcast(F32)
        if len(shape) == 3:
            v = v.rearrange("p (a b) -> p a b", a=shape[1])
        self.off += nb
        return Buf(v, name)


SPL = [512, 512, 1024, 1024, 1024, 128, 128, 1024, 256, 256, 1024, 1024, 1024]
OFF = np.concatenate([[0], np.cumsum(SPL)]).astype(int)
O_QR, O_KR, O_VR, O_UR, O_QS, O_KS, O_VS, O_QA, O_KA, O_VA, O_AR, O_AS, O_AA = [int(v) for v in OFF[:13]]


def _perm128():
    f = np.arange(128)
    return np.where(f % 64 < 32, f + 32, f - 32)


def _perm64():
    f = np.arange(128)
    return np.where(f % 32 < 16, f + 16, f - 16)


def fm_units():
    u = []
    ar = np.arange(128)
    for h in range(4):
        u.append(dict(name=f"kr{h}", kind="rope128", cols=O_KR + h * 128 + ar, tok="all"))
    u.append(dict(name="ks", kind="rope64", cols=O_KS + ar, tok="all"))
    u.append(dict(name="ks2", kind="rope64", cols=O_KS + (ar + 64) % 128, tok="all"))
    for h in range(2):
        u.append(dict(name=f"ka{h}", kind="normk", cols=O_KA + h * 128 + ar, tok="all"))
    for h in range(4):
        u.append(dict(name=f"qr{h}", kind="rope128", cols=O_QR + h * 128 + ar, tok="q"))
    for c in range(8):
        u.append(dict(name=f"qs{c}", kind="rope64", cols=O_QS + c * 128 + ar, tok="q"))
    for h in range(8):
        u.append(dict(name=f"qa{h}", kind="normq", cols=O_QA + h * 128 + ar, tok="q"))
    for c in range(8):
        u.append(dict(name=f"ur{c}", kind="silu", cols=O_UR + c * 128 + ar, tok="q"))
    for nm, o in (("ar", O_AR), ("as", O_AS), ("aa", O_AA)):
        for c in range(8):
            u.append(dict(name=f"{nm}{c}", kind="sig", cols=o + c * 128 + ar, tok="q"))
    g, used = 0, 0
    for x in u:
        w = 128
        if used + w > 512:
            g, used = g + 1, 0
        x["group"], x["c0"] = g, used
        x["dual"] = x["kind"] in ("rope128", "rope64", "normq", "normk")
        used += w
    return u, g + 1


FM_UNITS, N_FM_GROUPS = fm_units()
TM_COLS = np.concatenate([O_VR + np.arange(1024), O_VS + np.arange(128), O_VA + np.arange(256)])
N_TM_GROUPS = 3
G_MOD = 0
G_FM = 12
G_TM = G_FM + N_FM_GROUPS
G_MERGE = G_TM + N_TM_GROUPS
N_GROUPS = G_MERGE + 8

SM_BMOD, SM_G1, SM_G2, SM_RET, SM_SINK, SM_GQ, SM_GK = 0, 48, 56, 64, 72, 88, 90
SM_L = 96
SG_C, SG_WR, SG_BR, SG_GF, SG_FLAG = 0, 16, 144, 160, 168
SG_N = 176
C_ID, C_DPOS, C_DNEG, C_MF, C_MB, C_IP1, C_IB, C_ML, C_MR, C_MLB, C_MRB = [i * 128 for i in range(11)]
C_PCF, C_PCB = 11 * 128, 11 * 128 + 1
C_SEL = 11 * 128 + 8
C_P128 = C_SEL + 2048
C_P64 = C_P128 + 128
C_N = C_P64 + 128


def _grp(w):
    n = w.shape[1] // 512
    return np.ascontiguousarray(w.reshape(8, 128, n, 512).transpose(2, 1, 0, 3))


def prep_layer_weights(inp, l):
    w_in = inp["w_in"][l]
    p128, p64 = _perm128(), _perm64()
    cols = np.zeros(N_FM_GROUPS * 512, dtype=np.int64)
    for u in FM_UNITS:
        base = u["group"] * 512
        cols[base + u["c0"]:base + u["c0"] + 128] = u["cols"]
    tmc = np.concatenate([TM_COLS, np.zeros(1536 - 1408, dtype=np.int64)])
    wcat = np.concatenate([inp["w_mod"][l], w_in[:, cols], w_in[:, tmc], inp["w_br_ret"][l], inp["w_br_swa"][l],
                           inp["w_br_ga"][l], inp["w_out"][l]], axis=1)
    wall = _grp(wcat)
    assert wall.shape[0] == N_GROUPS
    wg = inp["w_gate"][l].reshape(16, 8, 128, 512).transpose(0, 2, 1, 3).reshape(16, 128, 4096)
    wu = inp["w_up"][l].reshape(16, 8, 128, 512).transpose(0, 2, 1, 3).reshape(16, 128, 4096)
    wd = inp["w_down"][l].reshape(16, 4, 128, 1024).transpose(0, 2, 1, 3).reshape(16, 128, 4096)
    we = np.ascontiguousarray(np.stack([wg, wu, wd], axis=1))
    sm = np.zeros((128, SM_L), np.float32)
    sm[:, SM_BMOD:SM_BMOD + 48] = inp["b_mod"][l].reshape(48, 128).T
    sm[:, SM_G1:SM_G1 + 8] = inp["g_norm1"][l].reshape(8, 128).T
    sm[:, SM_G2:SM_G2 + 8] = inp["g_norm2"][l].reshape(8, 128).T
    sm[:, SM_RET:SM_RET + 8] = inp["ret_decay_logit"][l].reshape(1, 8)
    sm[:, SM_SINK:SM_SINK + 16] = inp["swa_sink"][l].reshape(1, 16)
    sm[:, SM_GQ] = inp["g_qnorm"][l]
    sm[:, SM_GQ + 1] = inp["g_qnorm"][l][p128]
    sm[:, SM_GK] = inp["g_knorm"][l]
    sm[:, SM_GK + 1] = inp["g_knorm"][l][p128]
    return wall, we, sm


def make_consts(half):
    c = np.zeros((128, C_N), np.float32)
    m = np.arange(128)[:, None].astype(np.float32)
    n = np.arange(128)[None, :].astype(np.float32)
    c[:, C_ID:C_ID + 128] = np.eye(128)
    c[:, C_DPOS:C_DPOS + 128] = np.maximum(n - m, 0)
    c[:, C_DNEG:C_DNEG + 128] = np.maximum(m - n, 0)
    c[:, C_MF:C_MF + 128] = (n >= m)
    c[:, C_MB:C_MB + 128] = (m > n)
    c[:, C_IP1:C_IP1 + 128] = n + 1 + 0 * m
    c[:, C_IB:C_IB + 128] = 128 - n + 0 * m
    c[:, C_ML:C_ML + 128] = (m >= n)
    c[:, C_MR:C_MR + 128] = (m <= n)
    c[:, C_MLB:C_MLB + 128] = (m >= n) * (1.0 if half == 1 else 0.0)
    c[:, C_MRB:C_MRB + 128] = (m <= n) * (1.0 if half == 0 else 0.0)
    c[:, C_PCF] = 127 - np.arange(128)
    c[:, C_PCB] = np.arange(128)
    for e in range(16):
        c[e, C_SEL + e * 128:C_SEL + (e + 1) * 128] = 1.0
    for co, pm in ((C_P128, _perm128()), (C_P64, _perm64())):
        for f in range(128):
            c[pm[f], co + f] = 1.0
    return c


def make_tabs(half):
    pos_own = half * NOWN + np.arange(NOWN)
    pos_oth = (1 - half) * NOWN + np.arange(NOTH)
    pos = np.concatenate([pos_own, pos_oth])
    row = (pos // 64).astype(np.float32)
    col = (pos % 64).astype(np.float32)
    tabs = np.zeros((4, 128, NALL), np.float32)
    tabs[0, :, 4096:] = 1.0
    tabs[2, :, 4096:] = 1.0
    for ti, hd in ((0, 128), (2, 64)):
        half_d, quarter = hd // 2, hd // 4
        freqs = (np.float32(10000.0) ** (-np.arange(quarter, dtype=np.float32) / np.float32(quarter))).astype(np.float32)
        for f in range(128):
            fl = f % hd
            a = fl // half_d
            j = fl % half_d
            p = row if a == 0 else col
            ang = (p * freqs[j % quarter]).astype(np.float32)
            tabs[ti, f, :4096] = np.cos(ang)
            tabs[ti + 1, f, :4096] = np.sin(ang) * (-1.0 if j < quarter else 1.0)
    return tabs


def make_small_global(inp, b, half):
    sg = np.zeros((128, SG_N), np.float32)
    cT = inp["c"][b].reshape(8, 128).T
    ccT = inp["c_ctx"].reshape(8, 128).T
    sg[:, SG_C:SG_C + 16:2] = cT
    sg[:, SG_C + 1:SG_C + 16:2] = ccT
    sg[:, SG_WR:SG_WR + 128] = inp["w_router"].reshape(8, 128, 16).transpose(1, 0, 2).reshape(128, 128)
    sg[:, SG_BR:SG_BR + 16] = inp["b_router"].reshape(1, 16)
    sg[:, SG_GF:SG_GF + 8] = inp["g_final"].reshape(8, 128).T
    sg[:, SG_FLAG] = 1.0 if half == 0 else 0.0
    sg[:, SG_FLAG + 1] = 1.0 if half == 1 else 0.0
    return sg


TOK_TILES_ALL = [(i * 512, 512, 0) for i in range(8)] + [(4096, 256, 1)]
TOK_TILES_OWN = [(i * 512, 512, 0) for i in range(4)]
CTX_TILE = (4096, 256, 1)


def qpos(t0):
    return t0 if t0 < NOWN else t0 - NOTH


def build(layers, need_ctx_flags, final, dbg=(), stop=99):
    nc = bass.Bass("TRN2", target_bir_lowering=False)
    P = Prog(nc)
    nl = len(layers)

    def din(name, shape, dt=F32):
        return nc.dram_tensor(name, list(shape), dt, kind="ExternalInput").ap()

    def dscr(name, shape, dt=BF16):
        kind = "ExternalOutput" if name in dbg else "Internal"
        return nc.dram_tensor(name, list(shape), dt, kind=kind).ap()

    xall = din("xall", [D, NALL])
    tabs = din("tabs", [4, 128, NALL])
    consts_d = din("consts", [128, C_N])
    sg_d = din("sg", [128, SG_N])
    sm_d = [din(f"sm{l}", [128, SM_L]) for l in range(nl)]
    wall_d = [din(f"wall{l}", [N_GROUPS, 128, 8, 512]) for l in range(nl)]
    we_d = [din(f"we{l}", [16, 3, 128, 4096]) for l in range(nl)]
    if final:
        yout = nc.dram_tensor("yout", [D, NOWN], F32, kind="ExternalOutput").ap()
    else:
        xout = nc.dram_tensor("xout", [D, NQ], F32, kind="ExternalOutput").ap()

    xs_d = dscr("xs", [D, NALL], F32)
    fm_d = {u["name"]: dscr("fm_" + u["name"], [128, NALL]) for u in FM_UNITS}
    vall_d = dscr("vall", [NALL, 1536])
    obr_d = [dscr(f"obr{i}", [D, NQ]) for i in range(3)]
    web_d = dscr("web", [16, 3, 128, 4096])
    T_xs = {}

    def txs(t0):
        return T_xs.setdefault(t0, T(f"xs{t0}"))
    T_fm = {}

    def tfm(name, t0):
        return T_fm.setdefault((name, t0), T(f"fm{name}{t0}"))
    T_vall = {}

    def tvall(sub):
        return T_vall.setdefault(sub, T(f"vall{sub}"))
    T_obr = {}

    def tobr(i, t0):
        return T_obr.setdefault((i, t0), T(f"obr{i}_{t0}"))
    T_web = {}

    def tweb(e, k):
        return T_web.setdefault((e, k), T(f"web{e}_{k}"))

    xs3 = xs_d.rearrange("(kc p) t -> p kc t", p=128)
    if nl > 1:
        xch_src = [nc.dram_tensor(f"xch_src{i}", [D, 512], F32, kind="Internal").ap() for i in range(4)]
        xch_dst = [nc.dram_tensor(f"xch_dst{i}", [2 * D, 512], F32, kind="Internal").ap() for i in range(4)]
        T_xsrc, T_xdst = [T("xsrc") for i in range(4)], [T("xdst") for i in range(4)]
    xall3 = xall.rearrange("(kc p) t -> p kc t", p=128)

    A = Arena(nc, 211968)
    cst = A.alloc([128, C_N], F32, "cst")
    sg = A.alloc([128, SG_N], F32, "sg")
    ones_bf = A.alloc([128, 128], BF16, "ones")
    id_bf = A.alloc([128, 128], BF16, "idbf")
    mk_bf = A.alloc([128, 4, 512], BF16, "mk")
    pm_bf = A.alloc([128, 2, 128], BF16, "pm")
    PERS_END = None
    ps = [Buf(nc.alloc_psum_tensor(f"ps{i}", [128, 512], F32)[:], f"ps{i}", True) for i in range(8)]

    P.dma("sp", cst.ap, consts_d, writes=[cst.t])
    P.dma("sp", sg.ap, sg_d, writes=[sg.t])
    P.add("dve", lambda e: e.memset(ones_bf.ap, 1.0), writes=[ones_bf.t])
    P.copy("dve", id_bf.ap, cst.ap[:, C_ID:C_ID + 128], reads=[cst.t], writes=[id_bf.t])
    for i, co in enumerate((C_P128, C_P64)):
        P.copy("dve", pm_bf.ap[:, i, :], cst.ap[:, co:co + 128], reads=[cst.t], writes=[pm_bf.t])
    for i, co in enumerate((C_ML, C_MR, C_MLB, C_MRB)):
        for r in range(4):
            P.copy("dve", mk_bf.ap[:, i, r * 128:(r + 1) * 128], cst.ap[:, co:co + 128], reads=[cst.t],
                   writes=[mk_bf.t])
    for (t0, nt, _) in TOK_TILES_ALL:
        P.dma("sp", xs_d[:, t0:t0 + nt], xall[:, t0:t0 + nt], writes=[txs(t0)])
    PERS_END = A.off

    def rsqrt_from_psum(dst, src_ps, n, rd, wr):
        P.ts("dve", dst, src_ps, 1.0 / n, EPS, ALU.mult, ALU.add, reads=rd, writes=wr)
        P.act(dst, dst, AF.Ln, reads=wr, writes=wr)
        P.act(dst, dst, AF.Exp, reads=wr, writes=wr, scale=-0.5)

    psi = [0]

    def ps_next(k=8):
        psi[0] = (psi[0] + 1) % k
        return ps[psi[0]]

    for li, l in enumerate(layers):
        need_ctx = need_ctx_flags[li]
        last = (li == nl - 1)
        qtiles = TOK_TILES_OWN + ([CTX_TILE] if need_ctx else [])
        P.barrier()
        A.off = PERS_END
        sm = A.alloc([128, SM_L], F32, "sm")
        modT = A.alloc([128, 48, 2], F32, "modT")
        A1 = A.alloc([128, 8, 2], F32, "A1")
        A2 = A.alloc([128, 8, 2], F32, "A2")
        silc = A.alloc([128, 16], F32, "silc")
        lg = A.alloc([128, 8], F32, "lg")
        sinkx = A.alloc([128, 16], F32, "sinkx")
        P.dma("sp", sm.ap, sm_d[li], writes=[sm.t])
        P.act(silc.ap, sg.ap[:, SG_C:SG_C + 16], AF.Silu, reads=[sg.t], writes=[silc.t])
        silc3 = silc.ap.rearrange("p (k j) -> p k j", j=2)
        P.act(lg.ap, sm.ap[:, SM_RET:SM_RET + 8], AF.Exp, reads=[sm.t], writes=[lg.t], scale=-1.0)
        P.ts("dve", lg.ap, lg.ap, 1.0, None, ALU.add, reads=[lg.t], writes=[lg.t])
        P.act(lg.ap, lg.ap, AF.Ln, reads=[lg.t], writes=[lg.t])
        P.ts("dve", lg.ap, lg.ap, -1.0, None, ALU.mult, reads=[lg.t], writes=[lg.t])
        P.act(sinkx.ap, sm.ap[:, SM_SINK:SM_SINK + 16], AF.Exp, reads=[sm.t], writes=[sinkx.t])
        PH0_END = A.off
        wst = [A.alloc([128, 8, 512], F32, f"wst{i}") for i in range(2)]
        for g in range(12):
            w = wst[g % 2]
            P.dma("sp", w.ap, wall_d[li][G_MOD + g], writes=[w.t])
            for j in range(4):
                idx = g * 4 + j
                pb = ps_next()
                for kc in range(8):
                    P.mm(pb.ap[:, 0:2], w.ap[:, kc, j * 128:(j + 1) * 128], silc3[:, kc, :], kc == 0, kc == 7,
                         reads=[w.t, silc.t], writes=[pb.t])
                P.ts("dve", modT.ap[:, idx, :], pb.ap[:, 0:2], sm.ap[:, SM_BMOD + idx:SM_BMOD + idx + 1], None,
                     ALU.add, reads=[pb.t, sm.t], writes=[modT.t])
        for j in range(2):
            P.stt(A1.ap[:, :, j], modT.ap[:, 8:16, j], 1.0, sm.ap[:, SM_G1:SM_G1 + 8], ALU.add, ALU.mult,
                  reads=[modT.t, sm.t], writes=[A1.t])
            P.stt(A2.ap[:, :, j], modT.ap[:, 32:40, j], 1.0, sm.ap[:, SM_G2:SM_G2 + 8], ALU.add, ALU.mult,
                  reads=[modT.t, sm.t], writes=[A2.t])

        def SH1(kc, j): return modT.ap[:, 0 + kc, j:j + 1]
        def GT1(kc, j): return modT.ap[:, 16 + kc, j:j + 1]
        def SH2(kc, j): return modT.ap[:, 24 + kc, j:j + 1]
        def GT2(kc, j): return modT.ap[:, 40 + kc, j:j + 1]

        P.barrier()
        A.off = PH0_END
        hT = A.alloc([128, 8, NALL], BF16, "hT")
        hts = {t0: T(f"hT{t0}") for (t0, _, _) in TOK_TILES_ALL}
        xt = [A.alloc([128, 8, 512], F32, f"xt{i}") for i in range(2)]
        sq = A.alloc([128, 8, 512], BF16, "sq")
        rstd = A.alloc([128, 512], F32, "rstd")
        tmp = [A.alloc([128, 512], F32, f"tmp{i}") for i in range(2)]
        PH1_END = A.off

        def norm_tiles(tiles, Aco, SHf, out_fn, src3=xs3):
            for i, (t0, nt, mj) in enumerate(tiles):
                x_ = xt[i % 2]
                P.dma("sp", x_.ap[:, :, 0:nt], src3[:, :, t0:t0 + nt], reads=[txs(t0)], writes=[x_.t])
                P.act(sq.ap[:, :, 0:nt], x_.ap[:, :, 0:nt], AF.Square, reads=[x_.t], writes=[sq.t])
                pb = ps_next()
                for kc in range(8):
                    P.mm(pb.ap[:, 0:nt], ones_bf.ap, sq.ap[:, kc, 0:nt], kc == 0, kc == 7,
                         reads=[ones_bf.t, sq.t], writes=[pb.t])
                rsqrt_from_psum(rstd.ap[:, 0:nt], pb.ap[:, 0:nt], 1024.0, [pb.t], [rstd.t])
                for kc in range(8):
                    tm_ = tmp[kc % 2]
                    P.tt("dve" if kc % 2 == 0 else "pool", tm_.ap[:, 0:nt], x_.ap[:, kc, 0:nt], rstd.ap[:, 0:nt],
                         ALU.mult, reads=[x_.t, rstd.t], writes=[tm_.t])
                    out_fn(kc, t0, nt, mj, tm_, Aco.ap[:, kc, mj:mj + 1], SHf(kc, mj))

        def out_h1(kc, t0, nt, mj, tm_, a_, b_):
            P.act(hT.ap[:, kc, t0:t0 + nt], tm_.ap[:, 0:nt], AF.Identity, reads=[tm_.t, A1.t, modT.t],
                  writes=[hts[t0]], bias=b_, scale=a_)

        norm_tiles(TOK_TILES_ALL, A1, SH1, out_h1)

        A.off = PH1_END
        wbf = [A.alloc([128, 8, 512], BF16, f"wbf{i}") for i in range(3)]
        tabC = [A.alloc([128, 512], F32, f"tabC{i}") for i in range(2)]
        tabS = [A.alloc([128, 512], F32, f"tabS{i}") for i in range(2)]
        stage = [A.alloc([128, 512], BF16, f"stage{i}") for i in range(3)]
        t1b = [A.alloc([128, 512], F32, f"t1b{i}") for i in range(2)]
        t2b = [A.alloc([128, 512], F32, f"t2b{i}") for i in range(2)]
        sqb = A.alloc([128, 512], BF16, "sqb")
        rs2 = A.alloc([128, 512], F32, "rs2")
        cnt = [0]

        def load_group(gi):
            wb = wbf[gi % 3]
            P.dma("pool", wb.ap, wall_d[li][gi], writes=[wb.t])
            return wb

        pabf = [A.alloc([128, 512], BF16, f"pabf{i}") for i in range(2)]
        work = []
        for gi in range(N_FM_GROUPS):
            for u in [x for x in FM_UNITS if x["group"] == gi]:
                for tile in (TOK_TILES_ALL if u["tok"] == "all" else qtiles):
                    work.append((gi, u, tile))
        wbs = {}
        loaded = [G_FM - 1]

        def ensure(gi):
            while loaded[0] < G_FM + gi:
                loaded[0] += 1
                wbs[loaded[0] - G_FM] = load_group(loaded[0])

        def MAIN(k):
            gi, u, (t0, nt, mj) = work[k]
            ensure(gi + 1)
            wb = wbs[gi]
            pa = ps_next()
            for kc in range(8):
                P.mm(pa.ap[:, 0:nt], wb.ap[:, kc, u["c0"]:u["c0"] + 128], hT.ap[:, kc, t0:t0 + nt],
                     kc == 0, kc == 7, reads=[wb.t, hts[t0]], writes=[pa.t])
            if u["dual"]:
                pb_ = pabf[k % 2]
                P.copy("act", pb_.ap[:, 0:nt], pa.ap[:, 0:nt], reads=[pa.t], writes=[pb_.t])
                tsel = 2 if u["kind"] == "rope64" else 0
                tc_, ts_ = tabC[k % 2], tabS[k % 2]
                P.dma("sp", tc_.ap[:, 0:nt], tabs[tsel, :, t0:t0 + nt], writes=[tc_.t])
                P.dma("sp", ts_.ap[:, 0:nt], tabs[tsel + 1, :, t0:t0 + nt], writes=[ts_.t])
            return pa

        def REST(k, pa):
            gi, u, (t0, nt, mj) = work[k]
            kind = u["kind"]
            if u["dual"]:
                pbk = ps_next()
                P.mm(pbk.ap[:, 0:nt], pm_bf.ap[:, 1 if kind == "rope64" else 0, :], pabf[k % 2].ap[:, 0:nt],
                     True, True, reads=[pm_bf.t, pabf[k % 2].t], writes=[pbk.t])
                tc_, ts_ = tabC[k % 2], tabS[k % 2]
            st = stage[k % 3]
            o_ = st.ap[:, 0:nt]
            if kind in ("rope128", "rope64"):
                a_, b_ = t1b[k % 2], t2b[k % 2]
                P.tt("dve", a_.ap[:, 0:nt], pa.ap[:, 0:nt], tc_.ap[:, 0:nt], ALU.mult,
                     reads=[pa.t, tc_.t], writes=[a_.t])
                P.tt("dve", b_.ap[:, 0:nt], pbk.ap[:, 0:nt], ts_.ap[:, 0:nt], ALU.mult,
                     reads=[pbk.t, ts_.t], writes=[b_.t])
                P.tt("pool", o_, a_.ap[:, 0:nt], b_.ap[:, 0:nt], ALU.add, reads=[a_.t, b_.t], writes=[st.t])
            elif kind in ("normq", "normk"):
                gco = SM_GQ if kind == "normq" else SM_GK
                a_, b_ = t1b[k % 2], t2b[k % 2]
                P.act(sqb.ap[:, 0:nt], pa.ap[:, 0:nt], AF.Square, reads=[pa.t], writes=[sqb.t])
                pc = ps_next()
                P.mm(pc.ap[:, 0:nt], ones_bf.ap, sqb.ap[:, 0:nt], True, True, reads=[ones_bf.t, sqb.t],
                     writes=[pc.t])
                rsqrt_from_psum(rs2.ap[:, 0:nt], pc.ap[:, 0:nt], 128.0, [pc.t], [rs2.t])
                P.stt(a_.ap[:, 0:nt], pa.ap[:, 0:nt], sm.ap[:, gco:gco + 1], tc_.ap[:, 0:nt], ALU.mult,
                      ALU.mult, reads=[pa.t, tc_.t, sm.t], writes=[a_.t])
                P.stt(b_.ap[:, 0:nt], pbk.ap[:, 0:nt], sm.ap[:, gco + 1:gco + 2], ts_.ap[:, 0:nt], ALU.mult,
                      ALU.mult, reads=[pbk.t, ts_.t, sm.t], writes=[b_.t])
                P.tt("pool", a_.ap[:, 0:nt], a_.ap[:, 0:nt], b_.ap[:, 0:nt], ALU.add, reads=[a_.t, b_.t],
                     writes=[a_.t])
                P.tt("pool", o_, a_.ap[:, 0:nt], rs2.ap[:, 0:nt], ALU.mult, reads=[a_.t, rs2.t],
                     writes=[st.t])
            elif kind == "silu":
                P.act(o_, pa.ap[:, 0:nt], AF.Silu, reads=[pa.t], writes=[st.t])
            else:
                P.act(o_, pa.ap[:, 0:nt], AF.Sigmoid, reads=[pa.t], writes=[st.t])
            P.dma("pool", fm_d[u["name"]][:, t0:t0 + nt], o_, reads=[st.t], writes=[tfm(u["name"], t0)])

        ensure(0)
        pas = {0: MAIN(0)}
        for k in range(len(work)):
            if k + 1 < len(work):
                pas[k + 1] = MAIN(k + 1)
            REST(k, pas.pop(k))
        cnt[0] = len(work)
        ensure(N_FM_GROUPS)
        nxt = wbs[N_FM_GROUPS]
        for gi in range(N_TM_GROUPS):
            wb = nxt
            if gi + 1 < N_TM_GROUPS:
                nxt = load_group(G_TM + gi + 1)
            for sub in range(NALL // 128):
                t0 = sub * 128
                tile0 = (t0 // 512) * 512
                k = cnt[0]
                cnt[0] += 1
                pa = ps_next()
                for kc in range(8):
                    P.mm(pa.ap, hT.ap[:, kc, t0:t0 + 128], wb.ap[:, kc, :], kc == 0, kc == 7,
                         reads=[wb.t, hts[tile0]], writes=[pa.t])
                st = stage[k % 3]
                P.copy("act" if k % 2 == 0 else "dve", st.ap, pa.ap, reads=[pa.t], writes=[st.t])
                P.dma("pool", vall_d[t0:t0 + 128, gi * 512:(gi + 1) * 512], st.ap, reads=[st.t],
                      writes=[tvall(sub)])
        P.barrier()
        if stop <= 2:
            continue
        A.off = PH0_END
        KSCALE = 128.0 ** -0.5
        KT = A.alloc([128, NALL], BF16, "KT")
        QT = A.alloc([128, NALL], BF16, "QT")
        Vr = A.alloc([128, 34, 256], BF16, "Vr")
        Ur = A.alloc([128, 2, NALL], BF16, "Ur")
        Kf = A.alloc([128, 34, 128], BF16, "Kf")
        Kb = A.alloc([128, 34, 128], BF16, "Kb")
        SFb = A.alloc([128, 18, 256], BF16, "SFb")
        SBb = A.alloc([128, 18, 256], BF16, "SBb")
        Dm = A.alloc([128, 512], BF16, "Dm")
        qdf = A.alloc([128, 512], F32, "qdf")
        qdb = A.alloc([128, 512], F32, "qdb")
        e1 = A.alloc([128, 128], F32, "e1")
        e2 = A.alloc([128, 128], F32, "e2")
        kd = A.alloc([128, 4], F32, "kd")
        S = A.alloc([128, 256], F32, "S")
        S2 = [A.alloc([128, 256], F32, f"S2{i}") for i in range(2)]
        S3 = [A.alloc([128, 256], F32, f"S3{i}") for i in range(2)]
        KVf = A.alloc([128, 34, 256], F32, "KVf")
        KVb = A.alloc([128, 34, 256], F32, "KVb")
        S0f = A.alloc([128, 256], F32, "S0f")
        S0b = A.alloc([128, 256], F32, "S0b")
        Pm = A.alloc([128, 512], BF16, "Pm")
        Qf = A.alloc([128, 512], BF16, "Qf")
        Qb = A.alloc([128, 512], BF16, "Qb")
        sq2 = A.alloc([128, 2, 512], BF16, "sq2")
        rs3 = A.alloc([128, 512], F32, "rs3")
        to_ = [A.alloc([128, 512], F32, f"to{i}") for i in range(2)]
        stg = [A.alloc([128, 512], BF16, f"stg{i}") for i in range(2)]
        flg = sg.ap[:, SG_FLAG:SG_FLAG + 2]
        for h in range(4):
            lgf, lgb = lg.ap[:, h:h + 1], lg.ap[:, 4 + h:5 + h]
            P.act(e1.ap, cst.ap[:, C_DPOS:C_DPOS + 128], AF.Exp, reads=[cst.t, lg.t], writes=[e1.t], scale=lgf)
            P.tt("dve", e1.ap, e1.ap, cst.ap[:, C_MF:C_MF + 128], ALU.mult, reads=[e1.t, cst.t], writes=[e1.t])
            P.act(e2.ap, cst.ap[:, C_DNEG:C_DNEG + 128], AF.Exp, reads=[cst.t, lg.t], writes=[e2.t], scale=lgb)
            P.tt("dve", e2.ap, e2.ap, cst.ap[:, C_MB:C_MB + 128], ALU.mult, reads=[e2.t, cst.t], writes=[e2.t])
            P.tt("dve", e1.ap, e1.ap, e2.ap, ALU.add, reads=[e1.t, e2.t], writes=[e1.t])
            for r in range(4):
                P.ts("dve", Dm.ap[:, r * 128:(r + 1) * 128], e1.ap, KSCALE, None, ALU.mult, reads=[e1.t],
                     writes=[Dm.t])
                P.act(qdf.ap[:, r * 128:(r + 1) * 128], cst.ap[:, C_IP1:C_IP1 + 128], AF.Exp, reads=[cst.t, lg.t],
                      writes=[qdf.t], scale=lgf)
                P.act(qdb.ap[:, r * 128:(r + 1) * 128], cst.ap[:, C_IB:C_IB + 128], AF.Exp, reads=[cst.t, lg.t],
                      writes=[qdb.t], scale=lgb)
            P.act(kd.ap[:, 0:1], cst.ap[:, C_PCF:C_PCF + 1], AF.Exp, reads=[cst.t, lg.t], writes=[kd.t], scale=lgf)
            P.act(kd.ap[:, 1:2], cst.ap[:, C_PCB:C_PCB + 1], AF.Exp, reads=[cst.t, lg.t], writes=[kd.t], scale=lgb)
            P.ts("dve", kd.ap[:, 0:2], kd.ap[:, 0:2], KSCALE, None, ALU.mult, reads=[kd.t], writes=[kd.t])
            P.act(kd.ap[:, 2:3], lgf, AF.Exp, reads=[lg.t], writes=[kd.t], scale=128.0)
            P.act(kd.ap[:, 3:4], lgb, AF.Exp, reads=[lg.t], writes=[kd.t], scale=128.0)
            P.dma("sp", KT.ap, fm_d[f"kr{h}"], writes=[KT.t])
            P.dma("sp", QT.ap[:, 0:NOWN], fm_d[f"qr{h}"][:, 0:NOWN], writes=[QT.t])
            if need_ctx:
                P.dma("sp", QT.ap[:, 4096:NALL], fm_d[f"qr{h}"][:, 4096:NALL], writes=[QT.t])
            vsrc = vall_d[:, h * 256:(h + 1) * 256].rearrange("(t p) c -> p t c", p=128)
            for tq in range(0, 34, 4):
                P.dma("sp", Vr.ap[:, tq:min(tq + 4, 34), :], vsrc[:, tq:min(tq + 4, 34), :], writes=[Vr.t])
            for j in range(2):
                P.dma("sp", Ur.ap[:, j, 0:NOWN], fm_d[f"ur{2 * h + j}"][:, 0:NOWN], writes=[Ur.t])
                if need_ctx:
                    P.dma("sp", Ur.ap[:, j, 4096:NALL], fm_d[f"ur{2 * h + j}"][:, 4096:NALL], writes=[Ur.t])
            for c0 in (range(0, 32 if 'trlast' in SKIP else 34, 4) if 'tr' not in SKIP else []):
                n = min(4, 34 - c0)
                pb_ = ps_next()
                for c in range(n):
                    P.mm(pb_.ap[:, c * 128:(c + 1) * 128], KT.ap[:, (c0 + c) * 128:(c0 + c + 1) * 128], id_bf.ap,
                         True, True, reads=[KT.t, id_bf.t], writes=[pb_.t])
                P.ts("dve", Kf.ap[:, c0:c0 + n, :], pb_.ap[:, 0:n * 128].rearrange("p (a b) -> p a b", a=n),
                     kd.ap[:, 0:1], None, ALU.mult, reads=[pb_.t, kd.t], writes=[Kf.t])
                P.act(Kb.ap[:, c0:c0 + n, :], pb_.ap[:, 0:n * 128].rearrange("p (a b) -> p a b", a=n), AF.Identity,
                      reads=[pb_.t, kd.t], writes=[Kb.t], scale=kd.ap[:, 1:2])

            for kvi, (Kx, KV) in enumerate(((Kf, KVf), (Kb, KVb))):
                for c0 in range(0, 34, 2):
                    pb_ = ps_next()
                    for c in range(2):
                        P.mm(pb_.ap[:, c * 256:(c + 1) * 256], Kx.ap[:, c0 + c, :], Vr.ap[:, c0 + c, :], True, True,
                             reads=[Kx.t, Vr.t], writes=[pb_.t])
                    P.copy("act" if (c0 // 2) % 2 == 0 else "dve", KV.ap[:, c0:c0 + 2, :],
                           pb_.ap.rearrange("p (a b) -> p a b", a=2), reads=[pb_.t], writes=[KV.t])
            class Chain:
                def __init__(self, bufs):
                    self.b = bufs
                    self.cur = 0
                    self.steps = []

                def upd(self, Kx, c, cdcol, first):
                    KV = KVf if Kx is Kf else KVb
                    if first:
                        d = self.b[self.cur]
                        self.steps.append(lambda d=d, KV=KV, c=c: P.copy("dve", d.ap, KV.ap[:, c, :], reads=[KV.t],
                                                                     writes=[d.t]))
                    else:
                        a, d = self.b[self.cur], self.b[1 - self.cur]
                        self.steps.append(lambda a=a, d=d, KV=KV, c=c, cdcol=cdcol: P.stt(
                            d.ap, a.ap, kd.ap[:, cdcol:cdcol + 1], KV.ap[:, c, :], ALU.mult, ALU.add,
                            reads=[a.t, kd.t, KV.t], writes=[d.t]))
                        self.cur = 1 - self.cur

                def snap(self, dst, idx):
                    a = self.b[self.cur]
                    self.steps.append(lambda a=a, dst=dst, idx=idx: P.copy("act", dst.ap[:, idx, :], a.ap,
                                                                          reads=[a.t], writes=[dst.t]))

                def save(self, S0):
                    a = self.b[self.cur]
                    self.steps.append(lambda a=a, S0=S0: P.copy("dve", S0.ap, a.ap, reads=[a.t], writes=[S0.t]))

                def blend(self, S0, fcol):
                    a, b = self.b[self.cur], self.b[1 - self.cur]

                    def f(a=a, b=b, S0=S0, fcol=fcol):
                        P.tt("dve", b.ap, a.ap, S0.ap, ALU.subtract, reads=[a.t, S0.t], writes=[b.t])
                        P.stt(a.ap, b.ap, flg[:, fcol:fcol + 1], S0.ap, ALU.mult, ALU.add, reads=[b.t, S0.t, sg.t],
                              writes=[a.t])
                    self.steps.append(f)

            cf, cb = Chain(S2), Chain(S3)
            cf.upd(Kf, 32, 2, True)
            cf.snap(SFb, 17)
            cf.upd(Kf, 33, 2, False)
            cf.save(S0f)
            for c in range(16, 32):
                cf.upd(Kf, c, 2, False)
            cf.blend(S0f, 1)
            for c in range(0, 16):
                cf.snap(SFb, c)
                if c < 15:
                    cf.upd(Kf, c, 2, False)
            cb.upd(Kb, 33, 3, True)
            cb.snap(SBb, 16)
            cb.upd(Kb, 32, 3, False)
            cb.save(S0b)
            for c in range(31, 15, -1):
                cb.upd(Kb, c, 3, False)
            cb.blend(S0b, 0)
            for c in range(15, -1, -1):
                cb.snap(SBb, c)
                if c > 0:
                    cb.upd(Kb, c, 3, False)
            for i in range(max(len(cf.steps), len(cb.steps))):
                if i < len(cf.steps):
                    cf.steps[i]()
                if i < len(cb.steps):
                    cb.steps[i]()
            for (t0, nt, mj) in (qtiles if 'out' not in SKIP else []):
                ncx = nt // 128
                pS = ps_next()
                for j in range(ncx):
                    sl = slice(t0 + j * 128, t0 + (j + 1) * 128)
                    P.mm(pS.ap[:, j * 128:(j + 1) * 128], KT.ap[:, sl], QT.ap[:, sl], True, True,
                         reads=[KT.t, QT.t], writes=[pS.t])
                P.tt("dve", Pm.ap[:, 0:nt], pS.ap[:, 0:nt], Dm.ap[:, 0:nt], ALU.mult, reads=[pS.t, Dm.t],
                     writes=[Pm.t])
                P.tt("pool", Qf.ap[:, 0:nt], QT.ap[:, t0:t0 + nt], qdf.ap[:, 0:nt], ALU.mult, reads=[QT.t, qdf.t],
                     writes=[Qf.t])
                P.tt("pool", Qb.ap[:, 0:nt], QT.ap[:, t0:t0 + nt], qdb.ap[:, 0:nt], ALU.mult, reads=[QT.t, qdb.t],
                     writes=[Qb.t])
                pO = [ps_next(), ps_next()]
                for dj in range(2):
                    dsl = slice(dj * 128, (dj + 1) * 128)
                    for j in range(ncx):
                        c = t0 // 128 + j
                        csl = slice(j * 128, (j + 1) * 128)
                        if c < 16:
                            sf, sb = c, c
                        elif c == 32:
                            sf, sb = None, 16
                        else:
                            sf, sb = 17, None
                        terms = [(Vr.ap[:, c, dsl], Pm.ap[:, csl], [Vr.t, Pm.t])]
                        if sf is not None:
                            terms.append((SFb.ap[:, sf, dsl], Qf.ap[:, csl], [SFb.t, Qf.t]))
                        if sb is not None:
                            terms.append((SBb.ap[:, sb, dsl], Qb.ap[:, csl], [SBb.t, Qb.t]))
                        for ti, (l_, r_, rd) in enumerate(terms):
                            P.mm(pO[dj].ap[:, csl], l_, r_, ti == 0, ti == len(terms) - 1, reads=rd,
                                 writes=[pO[dj].t])
                    P.act(sq2.ap[:, dj, 0:nt], pO[dj].ap[:, 0:nt], AF.Square, reads=[pO[dj].t], writes=[sq2.t])
                pN = ps_next()
                for dj in range(2):
                    P.mm(pN.ap[:, 0:nt], ones_bf.ap, sq2.ap[:, dj, 0:nt], dj == 0, dj == 1,
                         reads=[ones_bf.t, sq2.t], writes=[pN.t])
                rsqrt_from_psum(rs3.ap[:, 0:nt], pN.ap[:, 0:nt], 256.0, [pN.t], [rs3.t])
                for dj in range(2):
                    P.tt("dve", to_[dj].ap[:, 0:nt], pO[dj].ap[:, 0:nt], rs3.ap[:, 0:nt], ALU.mult,
                         reads=[pO[dj].t, rs3.t], writes=[to_[dj].t])
                    P.tt("pool", stg[dj].ap[:, 0:nt], to_[dj].ap[:, 0:nt], Ur.ap[:, dj, t0:t0 + nt], ALU.mult,
                         reads=[to_[dj].t, Ur.t], writes=[stg[dj].t])
                    r0 = (2 * h + dj) * 128
                    P.dma("sp", obr_d[0][r0:r0 + 128, qpos(t0):qpos(t0) + nt], stg[dj].ap[:, 0:nt],
                          reads=[stg[dj].t], writes=[tobr(0, (h, dj, t0))])
        P.barrier()
        if stop <= 3:
            continue
        def run_attn(groups, Pt, depth=2):
            items = [(gi, ki) for gi, g in enumerate(groups) for ki in range(g["n"])]
            pts = {}
            sidx = [0]

            def SE(idx):
                gi, ki = items[idx]
                g = groups[gi]
                pS = ps[sidx[0] % 4]
                sidx[0] += 1
                g["S"](ki, pS)
                pt = Pt[idx % len(Pt)]
                g["E"](ki, pS, pt)
                pts[idx] = pt
            for idx in range(min(depth, len(items))):
                SE(idx)
            for idx in range(len(items)):
                if idx + depth < len(items):
                    SE(idx + depth)
                gi, ki = items[idx]
                g = groups[gi]
                pO, pD = (ps[4], ps[5]) if gi % 2 == 0 else (ps[6], ps[7])
                g["PV"](ki, pts.pop(idx), pO, pD, ki == 0, ki == g["n"] - 1)
                if ki == g["n"] - 1:
                    g["epi"](pO, pD)

        A.off = PH0_END
        KSa = A.alloc([128, NALL], BF16, "KSa")
        KSb = A.alloc([128, NALL], BF16, "KSb")
        QS = A.alloc([128, 4, NALL], BF16, "QS")
        Vs = A.alloc([128, 34, 64], BF16, "Vs")
        Pt = [A.alloc([128, 512], BF16, f"Pt{i}") for i in range(6)]
        sk = [A.alloc([64, 512], F32, f"sk{i}") for i in range(2)]
        den = [A.alloc([128, 512], F32, f"den{i}") for i in range(2)]
        ostg = [A.alloc([128, 512], BF16, f"ostg{i}") for i in range(2)]
        P.dma("sp", KSa.ap, fm_d["ks"], writes=[KSa.t])
        P.dma("sp", KSb.ap, fm_d["ks2"], writes=[KSb.t])
        obr1 = obr_d[1].rearrange("(c p) t -> p c t", p=128)
        gcount = 0
        for g in range(2):
            for i in range(4):
                P.dma("sp", QS.ap[:, i, 0:NOWN], fm_d[f"qs{g * 4 + i}"][:, 0:NOWN], writes=[QS.t])
                if need_ctx:
                    P.dma("sp", QS.ap[:, i, 4096:NALL], fm_d[f"qs{g * 4 + i}"][:, 4096:NALL], writes=[QS.t])
            vsrc = vall_d[:, 1024 + g * 64:1024 + (g + 1) * 64].rearrange("(t p) c -> p t c", p=128)
            for tq in range(0, 34, 4):
                P.dma("sp", Vs.ap[:, tq:min(tq + 4, 34), :], vsrc[:, tq:min(tq + 4, 34), :], writes=[Vs.t])
            groups = []
            for par in range(2):
                pb0 = par * 64
                Ksrc = KSa if g == par else KSb
                sk_ = sk[par]
                for i in range(4):
                    hq = g * 8 + par + 2 * i
                    P.act(sk_.ap[:, i * 128:(i + 1) * 128], cst.ap[0:64, C_DPOS:C_DPOS + 128], AF.Identity,
                          reads=[cst.t, sinkx.t], writes=[sk_.t], bias=sinkx.ap[0:64, hq:hq + 1], scale=0.0)
                blocks = [(jb * 128, jb) for jb in range(16)] + ([(4096, 16), (4224, 17)] if need_ctx else [])
                for (t0, jb) in blocks:
                    if jb < 16:
                        kts = [((jb - 1) * 128, 0) if jb > 0 else (2048 + 15 * 128, 2), (jb * 128, None),
                               ((jb + 1) * 128, 1) if jb < 15 else (2048, 3), (4096, None), (4224, None)]
                    else:
                        kts = [(4096, None), (4224, None)]

                    def S(ki, pS, kts=kts, pb0=pb0, Ksrc=Ksrc, t0=t0):
                        k0 = kts[ki][0]
                        P.mm(pS.ap.rearrange("p (a b) -> p a b", a=4), Ksrc.ap[pb0:pb0 + 64, k0:k0 + 128],
                             QS.ap[pb0:pb0 + 64, :, t0:t0 + 128], True, True, reads=[Ksrc.t, QS.t], writes=[pS.t])

                    def E(ki, pS, pt, kts=kts):
                        P.act(pt.ap, pS.ap, AF.Exp, reads=[pS.t], writes=[pt.t], scale=0.125)
                        mi = kts[ki][1]
                        if mi is not None:
                            P.tt("pool", pt.ap, pt.ap, mk_bf.ap[:, mi, :], ALU.mult, reads=[pt.t, mk_bf.t],
                                 writes=[pt.t])

                    def PV(ki, pt, pO, pD, first, lastk, kts=kts):
                        k0 = kts[ki][0]
                        P.mm(pO.ap[0:64, :], Vs.ap[:, k0 // 128, :], pt.ap, first, lastk, reads=[Vs.t, pt.t],
                             writes=[pO.t])
                        P.mm(pD.ap[0:64, :], ones_bf.ap[:, 0:64], pt.ap, first, lastk, reads=[ones_bf.t, pt.t],
                             writes=[pD.t])

                    def epi(pO, pD, t0=t0, pb0=pb0, g=g, sk_=sk_, gc=gcount):
                        dn_, os_ = den[gc % 2], ostg[gc % 2]
                        P.tt("dve", dn_.ap[0:64, :], pD.ap[0:64, :], sk_.ap, ALU.add, reads=[pD.t, sk_.t],
                             writes=[dn_.t])
                        P.act(dn_.ap[0:64, :], dn_.ap[0:64, :], AF.Ln, reads=[dn_.t], writes=[dn_.t])
                        P.act(dn_.ap[0:64, :], dn_.ap[0:64, :], AF.Exp, reads=[dn_.t], writes=[dn_.t], scale=-1.0)
                        P.tt("dve", os_.ap[0:64, :], pO.ap[0:64, :], dn_.ap[0:64, :], ALU.mult,
                             reads=[pO.t, dn_.t], writes=[os_.t])
                        q0 = qpos(t0)
                        P.dma("sp", obr1[pb0:pb0 + 64, g * 4:g * 4 + 4, q0:q0 + 128],
                              os_.ap[0:64, :].rearrange("p (a b) -> p a b", a=4), reads=[os_.t],
                              writes=[tobr(1, (g, pb0, t0))])
                    groups.append(dict(n=len(kts), S=S, E=E, PV=PV, epi=epi))
                    gcount += 1
            run_attn(groups, Pt, depth=3)
        P.barrier()
        if stop <= 4:
            continue
        A.off = PH0_END
        KA = A.alloc([128, NALL], BF16, "KA")
        VA = A.alloc([128, 34, 128], BF16, "VA")
        QA = [A.alloc([128, NALL], BF16, f"QA{i}") for i in range(4)]
        Pt = [A.alloc([128, 512], BF16, f"Pt{i}") for i in range(4)]
        den = [A.alloc([128, 512], F32, f"den{i}") for i in range(2)]
        ostg = [A.alloc([128, 512], BF16, f"ostg{i}") for i in range(2)]
        GSCALE = 128.0 ** -0.5
        gcount = 0
        for g in range(2):
            P.dma("sp", KA.ap, fm_d[f"ka{g}"], writes=[KA.t])
            vsrc = vall_d[:, 1152 + g * 128:1152 + (g + 1) * 128].rearrange("(t p) c -> p t c", p=128)
            for tq in range(0, 34, 4):
                P.dma("sp", VA.ap[:, tq:min(tq + 4, 34), :], vsrc[:, tq:min(tq + 4, 34), :], writes=[VA.t])
            groups = []
            for hh in range(4):
                h = g * 4 + hh
                Q_ = QA[hh]
                P.dma("sp", Q_.ap[:, 0:NOWN], fm_d[f"qa{h}"][:, 0:NOWN], writes=[Q_.t])
                if need_ctx:
                    P.dma("sp", Q_.ap[:, 4096:NALL], fm_d[f"qa{h}"][:, 4096:NALL], writes=[Q_.t])
                for (t0, nt, mj) in qtiles:
                    kts = list(range(34)) if t0 < 4096 else [32, 33]

                    def S(ki, pS, kts=kts, Q_=Q_, t0=t0, nt=nt):
                        kt = kts[ki]
                        P.mm(pS.ap[:, 0:nt], KA.ap[:, kt * 128:(kt + 1) * 128], Q_.ap[:, t0:t0 + nt], True, True,
                             reads=[KA.t, Q_.t], writes=[pS.t])

                    def E(ki, pS, pt, nt=nt):
                        P.act(pt.ap[:, 0:nt], pS.ap[:, 0:nt], AF.Exp, reads=[pS.t], writes=[pt.t], scale=GSCALE)

                    def PV(ki, pt, pO, pD, first, lastk, kts=kts, nt=nt):
                        kt = kts[ki]
                        P.mm(pO.ap[:, 0:nt], VA.ap[:, kt, :], pt.ap[:, 0:nt], first, lastk, reads=[VA.t, pt.t],
                             writes=[pO.t])
                        P.mm(pD.ap[:, 0:nt], ones_bf.ap, pt.ap[:, 0:nt], first, lastk, reads=[ones_bf.t, pt.t],
                             writes=[pD.t])

                    def epi(pO, pD, t0=t0, nt=nt, h=h, gc=gcount):
                        dn_, os_ = den[gc % 2], ostg[gc % 2]
                        P.act(dn_.ap[:, 0:nt], pD.ap[:, 0:nt], AF.Ln, reads=[pD.t], writes=[dn_.t])
                        P.act(dn_.ap[:, 0:nt], dn_.ap[:, 0:nt], AF.Exp, reads=[dn_.t], writes=[dn_.t], scale=-1.0)
                        P.tt("dve", os_.ap[:, 0:nt], pO.ap[:, 0:nt], dn_.ap[:, 0:nt], ALU.mult,
                             reads=[pO.t, dn_.t], writes=[os_.t])
                        q0 = qpos(t0)
                        P.dma("sp", obr_d[2][h * 128:(h + 1) * 128, q0:q0 + nt], os_.ap[:, 0:nt], reads=[os_.t],
                              writes=[tobr(2, (h, t0))])
                    groups.append(dict(n=len(kts), S=S, E=E, PV=PV, epi=epi))
                    gcount += 1
            run_attn(groups, Pt)
        P.barrier()
        if stop <= 5:
            continue
        A.off = PH0_END
        wm = [A.alloc([128, 8, 1024], BF16, f"wm{i}") for i in range(4)]
        ob = [[A.alloc([128, 8, 512], BF16, f"ob{j}{i}") for i in range(3)] for j in range(2)]
        gbr = [A.alloc([128, 512], BF16, f"gbr{i}") for i in range(8)]
        ypre = A.alloc([128, 8, 512], BF16, "ypre")
        xtl2 = [A.alloc([128, 8, 512], F32, f"xtl{i}") for i in range(2)]
        ya = [A.alloc([128, 512], F32, f"ya{i}") for i in range(2)]
        tb = [A.alloc([128, 512], F32, f"tb{i}") for i in range(2)]
        for gi in range(8):
            dst = wm[gi // 2].ap[:, :, (gi % 2) * 512:(gi % 2 + 1) * 512]
            P.dma("pool", dst, wall_d[li][G_MERGE + gi], writes=[wm[gi // 2].t])
        gnames = ("ar", "as", "aa")

        def merge_loads(ti):
            t0, nt, mj = qtiles[ti]
            q0 = qpos(t0)
            for b_ in range(3):
                o_ = ob[ti % 2][b_]
                P.dma("sp", o_.ap[:, :, 0:nt], obr_d[b_].rearrange("(kc p) t -> p kc t", p=128)[:, :, q0:q0 + nt],
                      writes=[o_.t])
            x_ = xtl2[ti % 2]
            P.dma("sp", x_.ap[:, :, 0:nt], xs3[:, :, t0:t0 + nt], reads=[txs(t0)], writes=[x_.t])

        gcnt = [0]
        merge_loads(0)
        for ti, (t0, nt, mj) in enumerate(qtiles):
            if ti + 1 < len(qtiles):
                merge_loads(ti + 1)
            xtl = xtl2[ti % 2]
            for dc in range(8):
                dsl = slice(dc * 128, (dc + 1) * 128)
                y_ = ya[dc % 2]
                for b_ in range(3):
                    g_ = gbr[gcnt[0] % 8]
                    gcnt[0] += 1
                    P.dma("sp", g_.ap[:, 0:nt], fm_d[f"{gnames[b_]}{dc}"][:, t0:t0 + nt], writes=[g_.t])
                    pa = ps_next()
                    o_ = ob[ti % 2][b_]
                    for kc in range(8):
                        P.mm(pa.ap[:, 0:nt], wm[b_].ap[:, kc, dsl], o_.ap[:, kc, 0:nt], kc == 0, kc == 7,
                             reads=[wm[b_].t, o_.t], writes=[pa.t])
                    if b_ == 0:
                        P.tt("dve", y_.ap[:, 0:nt], pa.ap[:, 0:nt], g_.ap[:, 0:nt], ALU.mult,
                             reads=[pa.t, g_.t], writes=[y_.t])
                    else:
                        t_ = tb[b_ % 2]
                        P.tt("dve", t_.ap[:, 0:nt], pa.ap[:, 0:nt], g_.ap[:, 0:nt], ALU.mult,
                             reads=[pa.t, g_.t], writes=[t_.t])
                        if b_ == 1:
                            P.tt("pool", y_.ap[:, 0:nt], y_.ap[:, 0:nt], t_.ap[:, 0:nt], ALU.add,
                                 reads=[y_.t, t_.t], writes=[y_.t])
                        else:
                            P.tt("pool", ypre.ap[:, dc, 0:nt], y_.ap[:, 0:nt], t_.ap[:, 0:nt], ALU.add,
                                 reads=[y_.t, t_.t], writes=[ypre.t])
            for dc in range(8):
                dsl = slice(dc * 128, (dc + 1) * 128)
                py = ps_next()
                for kc in range(8):
                    P.mm(py.ap[:, 0:nt], wm[3].ap[:, kc, dsl], ypre.ap[:, kc, 0:nt], kc == 0, kc == 7,
                         reads=[wm[3].t, ypre.t], writes=[py.t])
                P.stt(xtl.ap[:, dc, 0:nt], py.ap[:, 0:nt], GT1(dc, mj), xtl.ap[:, dc, 0:nt], ALU.mult, ALU.add,
                      reads=[py.t, modT.t, xtl.t], writes=[xtl.t])
            P.dma("pool", xs3[:, :, t0:t0 + nt], xtl.ap[:, :, 0:nt], reads=[xtl.t], writes=[txs(t0)])
        P.barrier()
        if stop <= 6:
            continue
        A.off = PH0_END
        h2T = A.alloc([128, 8, NQ], BF16, "h2T")
        xres = A.alloc([128, 8, NQ], F32, "xres")
        WT = A.alloc([16, NQ], F32, "WT")
        M0 = A.off
        h2f = A.alloc([128, 8, 512], F32, "h2f")
        sq = A.alloc([128, 8, 512], BF16, "sq")
        rstd = A.alloc([128, 512], F32, "rstd")
        tmp = [A.alloc([128, 512], F32, f"tmp{i}") for i in range(2)]
        rt = {nm: A.alloc([128, 16], F32, "rt_" + nm) for nm in ("s", "bz", "eq", "msk", "ch", "ws", "wts")}
        rs = {nm: A.alloc([128, 4], F32, "rs_" + nm) for nm in ("m1", "m2", "gs", "gsel", "gm", "dn")}

        def v3(b_):
            return b_.ap.rearrange("p (g k) -> p g k", k=4)

        def bc(b_):
            return b_.ap[:, 0:4].unsqueeze(2).to_broadcast([128, 4, 4])
        mtiles = qtiles
        for i, (t0, nt, mj) in enumerate(mtiles):
            q0 = qpos(t0)
            P.dma("sp", xres.ap[:, :, q0:q0 + nt], xs3[:, :, t0:t0 + nt], reads=[txs(t0)], writes=[xres.t])
            P.act(sq.ap[:, :, 0:nt], xres.ap[:, :, q0:q0 + nt], AF.Square, reads=[xres.t], writes=[sq.t])
            pb = ps_next()
            for kc in range(8):
                P.mm(pb.ap[:, 0:nt], ones_bf.ap, sq.ap[:, kc, 0:nt], kc == 0, kc == 7, reads=[ones_bf.t, sq.t],
                     writes=[pb.t])
            rsqrt_from_psum(rstd.ap[:, 0:nt], pb.ap[:, 0:nt], 1024.0, [pb.t], [rstd.t])
            for kc in range(8):
                tm_ = tmp[kc % 2]
                P.tt("dve", tm_.ap[:, 0:nt], xres.ap[:, kc, q0:q0 + nt], rstd.ap[:, 0:nt], ALU.mult,
                     reads=[xres.t, rstd.t], writes=[tm_.t])
                P.act(h2f.ap[:, kc, 0:nt], tm_.ap[:, 0:nt], AF.Identity, reads=[tm_.t, A2.t, modT.t], writes=[h2f.t],
                      bias=SH2(kc, mj), scale=A2.ap[:, kc, mj:mj + 1])
                P.copy("pool", h2T.ap[:, kc, q0:q0 + nt], h2f.ap[:, kc, 0:nt], reads=[h2f.t], writes=[h2T.t])
            for sub in range(nt // 128):
                ssl = slice(sub * 128, (sub + 1) * 128)
                pr = ps_next()
                for kc in range(8):
                    P.mm(pr.ap[:, 0:16], h2f.ap[:, kc, ssl], sg.ap[:, SG_WR + kc * 16:SG_WR + (kc + 1) * 16],
                         kc == 0, kc == 7, reads=[h2f.t, sg.t], writes=[pr.t])
                s_, bz, eq, msk, ch, ws, wts = [rt[n] for n in ("s", "bz", "eq", "msk", "ch", "ws", "wts")]
                m1, m2, gs, gsel, gm, dn = [rs[n] for n in ("m1", "m2", "gs", "gsel", "gm", "dn")]
                P.act(s_.ap, pr.ap[:, 0:16], AF.Sigmoid, reads=[pr.t], writes=[s_.t])
                P.tt("dve", bz.ap, s_.ap, sg.ap[:, SG_BR:SG_BR + 16], ALU.add, reads=[s_.t, sg.t], writes=[bz.t])
                P.add("dve", lambda e, o=m1.ap, i_=v3(bz): e.tensor_reduce(out=o, in_=i_, axis=AX.X, op=ALU.max),
                      reads=[bz.t], writes=[m1.t])
                P.tt("dve", v3(eq), v3(bz), bc(m1), ALU.is_equal, reads=[bz.t, m1.t], writes=[eq.t])
                P.stt(msk.ap, eq.ap, -1e9, bz.ap, ALU.mult, ALU.add, reads=[eq.t, bz.t], writes=[msk.t])
                P.add("dve", lambda e, o=m2.ap, i_=v3(msk): e.tensor_reduce(out=o, in_=i_, axis=AX.X, op=ALU.max),
                      reads=[msk.t], writes=[m2.t])
                P.tt("dve", gs.ap, m1.ap, m2.ap, ALU.add, reads=[m1.t, m2.t], writes=[gs.t])
                P.add("dve", lambda e, o=gm.ap[:, 0:1], i_=gs.ap: e.tensor_reduce(out=o, in_=i_, axis=AX.X,
                                                                                 op=ALU.max),
                      reads=[gs.t], writes=[gm.t])
                P.ts("dve", gsel.ap, gs.ap, gm.ap[:, 0:1], None, ALU.is_equal, reads=[gs.t, gm.t], writes=[gsel.t])
                P.tt("dve", v3(ch), v3(bz), bc(m2), ALU.is_ge, reads=[bz.t, m2.t], writes=[ch.t])
                P.tt("dve", v3(ch), v3(ch), bc(gsel), ALU.mult, reads=[ch.t, gsel.t], writes=[ch.t])
                P.tt("dve", ws.ap, s_.ap, ch.ap, ALU.mult, reads=[s_.t, ch.t], writes=[ws.t])
                P.add("dve", lambda e, o=dn.ap[:, 0:1], i_=ws.ap: e.tensor_reduce(out=o, in_=i_, axis=AX.X,
                                                                                 op=ALU.add),
                      reads=[ws.t], writes=[dn.t])
                P.add("dve", lambda e, o=dn.ap[:, 1:2], i_=dn.ap[:, 0:1]: e.reciprocal(o, i_), reads=[dn.t],
                      writes=[dn.t])
                P.ts("dve", wts.ap, ws.ap, dn.ap[:, 1:2], None, ALU.mult, reads=[ws.t, dn.t], writes=[wts.t])
                pT = ps_next()
                P.transpose(pT.ap[0:16, 0:128], wts.ap, cst.ap[:, C_ID:C_ID + 128], reads=[wts.t, cst.t],
                            writes=[pT.t])
                P.copy("act", WT.ap[0:16, q0 + sub * 128:q0 + (sub + 1) * 128], pT.ap[0:16, 0:128], reads=[pT.t],
                       writes=[WT.t])
        P.barrier()
        A.off = M0
        ew = [[A.alloc([128, 4096], BF16, f"ew{i}_{k}") for k in range(3)] for i in range(2)]
        wbc = A.alloc([128, 512], F32, "wbc")
        sG = [A.alloc([128, 512], F32, f"sG{i}") for i in range(2)]
        tG = [A.alloc([128, 512], F32, f"tG{i}") for i in range(2)]
        hid = A.alloc([128, 4, 512], BF16, "hid")
        def load_expert(e):
            for k in range(3):
                P.dma("pool", ew[e % 2][k].ap, we_d[li][e, k], writes=[ew[e % 2][k].t])

        hidb = [hid, A.alloc([128, 4, 512], BF16, "hid2")]
        wbcb = [wbc, A.alloc([128, 512], F32, "wbc2")]
        items = [(e, ti) for e in range(16) for ti in range(len(mtiles))]

        def GU(ix):
            e, ti = items[ix]
            wg_, wu_, wd_ = ew[e % 2]
            wg3 = wg_.ap.rearrange("p (a b) -> p a b", a=8)
            wu3 = wu_.ap.rearrange("p (a b) -> p a b", a=8)
            t0, nt, mj = mtiles[ti]
            q0 = qpos(t0)
            hid_, wbc_ = hidb[ix % 2], wbcb[ix % 2]
            pw = ps_next(8)
            P.mm(pw.ap[:, 0:nt], cst.ap[0:16, C_SEL + e * 128:C_SEL + (e + 1) * 128], WT.ap[0:16, q0:q0 + nt],
                 True, True, reads=[cst.t, WT.t], writes=[pw.t])
            P.copy("act", wbc_.ap[:, 0:nt], pw.ap[:, 0:nt], reads=[pw.t], writes=[wbc_.t])
            for hc in range(4):
                hsl = slice(hc * 128, (hc + 1) * 128)
                pg, pu = ps_next(8), ps_next(8)
                for kc in range(8):
                    P.mm(pg.ap[:, 0:nt], wg3[:, kc, hsl], h2T.ap[:, kc, q0:q0 + nt], kc == 0, kc == 7,
                         reads=[wg_.t, h2T.t], writes=[pg.t])
                for kc in range(8):
                    P.mm(pu.ap[:, 0:nt], wu3[:, kc, hsl], h2T.ap[:, kc, q0:q0 + nt], kc == 0, kc == 7,
                         reads=[wu_.t, h2T.t], writes=[pu.t])
                sg_, tg_ = sG[hc % 2], tG[hc % 2]
                P.act(sg_.ap[:, 0:nt], pg.ap[:, 0:nt], AF.Silu, reads=[pg.t], writes=[sg_.t])
                P.tt("dve", tg_.ap[:, 0:nt], sg_.ap[:, 0:nt], pu.ap[:, 0:nt], ALU.mult, reads=[sg_.t, pu.t],
                     writes=[tg_.t])
                P.tt("pool", hid_.ap[:, hc, 0:nt], tg_.ap[:, 0:nt], wbc_.ap[:, 0:nt], ALU.mult,
                     reads=[tg_.t, wbc_.t], writes=[hid_.t])

        def DOWN(ix):
            e, ti = items[ix]
            wd_ = ew[e % 2][2]
            wd3 = wd_.ap.rearrange("p (a b) -> p a b", a=4)
            t0, nt, mj = mtiles[ti]
            q0 = qpos(t0)
            hid_ = hidb[ix % 2]
            for dc in range(8):
                dsl = slice(dc * 128, (dc + 1) * 128)
                py = ps_next(8)
                for hc in range(4):
                    P.mm(py.ap[:, 0:nt], wd3[:, hc, dsl], hid_.ap[:, hc, 0:nt], hc == 0, hc == 3,
                         reads=[wd_.t, hid_.t], writes=[py.t])
                P.stt(xres.ap[:, dc, q0:q0 + nt], py.ap[:, 0:nt], GT2(dc, mj), xres.ap[:, dc, q0:q0 + nt],
                      ALU.mult, ALU.add, reads=[py.t, modT.t, xres.t], writes=[xres.t])

        load_expert(0)
        load_expert(1)
        GU(0)
        for ix in range(len(items)):
            if ix + 1 < len(items):
                GU(ix + 1)
            DOWN(ix)
            e_, ti_ = items[ix]
            if ti_ == len(mtiles) - 1 and e_ + 2 < 16:
                load_expert(e_ + 2)
        if not (last and final):
            for (t0, nt, mj) in mtiles:
                q0 = qpos(t0)
                P.dma("pool", xs3[:, :, t0:t0 + nt], xres.ap[:, :, q0:q0 + nt], reads=[xres.t], writes=[txs(t0)])
        if not last:
            for i, (t0, nt, mj) in enumerate(TOK_TILES_OWN):
                P.dma("pool", xch_src[i].rearrange("(kc p) t -> p kc t", p=128), xres.ap[:, :, t0:t0 + nt],
                      reads=[xres.t], writes=[T_xsrc[i]])
            P.barrier()
            for i in range(4):
                P.add("pool", lambda e, i=i: e.collective_compute("AllGather", ALU.bypass,
                                                                  replica_groups=[[0, 1], [2, 3], [4, 5], [6, 7]],
                                                                  ins=[xch_src[i]], outs=[xch_dst[i]]),
                      reads=[T_xsrc[i]], writes=[T_xdst[i]], cc=True)
            P.barrier()
            A.off = M0
            xa = [A.alloc([128, 8, 512], F32, f"xa{i}") for i in range(2)]
            xb_ = [A.alloc([128, 8, 512], F32, f"xb{i}") for i in range(2)]
            for i, (t0, nt, mj) in enumerate(TOK_TILES_OWN):
                d3 = xch_dst[i].rearrange("(r kc p) t -> r p kc t", r=2, p=128)
                a_, b_ = xa[i % 2], xb_[i % 2]
                P.dma("sp", a_.ap, d3[0], reads=[T_xdst[i]], writes=[a_.t])
                P.dma("sp", b_.ap, d3[1], reads=[T_xdst[i]], writes=[b_.t])
                P.ts("dve", a_.ap, a_.ap, sg.ap[:, SG_FLAG + 1:SG_FLAG + 2], None, ALU.mult, reads=[a_.t, sg.t],
                     writes=[a_.t])
                P.stt(a_.ap, b_.ap, sg.ap[:, SG_FLAG:SG_FLAG + 1], a_.ap, ALU.mult, ALU.add,
                      reads=[a_.t, b_.t, sg.t], writes=[a_.t])
                P.dma("pool", xs3[:, :, NOWN + t0:NOWN + t0 + nt], a_.ap, reads=[a_.t], writes=[txs(NOWN + t0)])
        if last and not final:
            xout3 = xout.rearrange("(kc p) t -> p kc t", p=128)
            for (t0, nt, mj) in mtiles:
                q0 = qpos(t0)
                P.dma("pool", xout3[:, :, q0:q0 + nt], xres.ap[:, :, q0:q0 + nt], reads=[xres.t])
        if last and final:
            P.barrier()
            A.off = M0
            h2f = A.alloc([128, 8, 512], F32, "h2f")
            sq = A.alloc([128, 8, 512], BF16, "sq")
            rstd = A.alloc([128, 512], F32, "rstd")
            tmp = [A.alloc([128, 512], F32, f"tmp{i}") for i in range(2)]
            yout3 = yout.rearrange("(kc p) t -> p kc t", p=128)
            for (t0, nt, mj) in TOK_TILES_OWN:
                P.act(sq.ap[:, :, 0:nt], xres.ap[:, :, t0:t0 + nt], AF.Square, reads=[xres.t], writes=[sq.t])
                pb = ps_next()
                for kc in range(8):
                    P.mm(pb.ap[:, 0:nt], ones_bf.ap, sq.ap[:, kc, 0:nt], kc == 0, kc == 7, reads=[ones_bf.t, sq.t],
                         writes=[pb.t])
                rsqrt_from_psum(rstd.ap[:, 0:nt], pb.ap[:, 0:nt], 1024.0, [pb.t], [rstd.t])
                for kc in range(8):
                    tm_ = tmp[kc % 2]
                    P.tt("dve", tm_.ap[:, 0:nt], xres.ap[:, kc, t0:t0 + nt], rstd.ap[:, 0:nt], ALU.mult,
                         reads=[xres.t, rstd.t], writes=[tm_.t])
                    P.act(h2f.ap[:, kc, 0:nt], tm_.ap[:, 0:nt], AF.Identity, reads=[tm_.t, sg.t], writes=[h2f.t],
                          scale=sg.ap[:, SG_GF + kc:SG_GF + kc + 1])
                P.dma("pool", yout3[:, :, t0:t0 + nt], h2f.ap[:, :, 0:nt], reads=[h2f.t])
    P.emit()
    return nc


_PROGS = {}


def _prog(key, *args, **kw):
    if key not in _PROGS:
        _PROGS[key] = build(*args, **kw)
    return _PROGS[key]


def kernel(**inp):
    inp = {k: np.asarray(v) for k, v in inp.items()}
    x, ctx = inp["x"], inp["ctx"]
    B = x.shape[0]
    cores = [(b, h) for b in range(B) for h in range(2)]
    consts = [make_consts(h) for h in range(2)]
    tabs = [make_tabs(h) for h in range(2)]
    w0 = prep_layer_weights(inp, 0)
    w1 = prep_layer_weights(inp, 1)
    maps = []
    for (b, h) in cores:
        xo = x[b, h * NOWN:(h + 1) * NOWN].T
        xt = x[b, (1 - h) * NOWN:(2 - h) * NOWN].T
        xall = np.ascontiguousarray(np.concatenate([xo, xt, ctx[b].T], axis=1))
        maps.append(dict(xall=xall, tabs=tabs[h], consts=consts[h], sg=make_small_global(inp, b, h),
                         sm0=w0[2], wall0=w0[0], we0=w0[1], sm1=w1[2], wall1=w1[0], we1=w1[1]))
    nc = _prog("fused", [0, 1], [True, False], True)
    r = run_bass_kernel_spmd(nc, maps, core_ids=list(range(len(cores)))).results
    out = np.zeros((B, 2 * NOWN, D), np.float32)
    for i, (b, h) in enumerate(cores):
        out[b, h * NOWN:(h + 1) * NOWN] = np.asarray(r[i]["yout"]).T
    return out
```

```python
import numpy as np
import concourse.bass as bass
import concourse.mybir as mybir
from concourse.bass_utils import run_bass_kernel_spmd

F32 = mybir.dt.float32
BF16 = mybir.dt.bfloat16
AF = mybir.ActivationFunctionType
ALU = mybir.AluOpType
AX = mybir.AxisListType

NOWN, NOTH, NCTX = 2048, 2048, 256
NALL = NOWN + NOTH + NCTX
NQ = NOWN + NCTX
D = 1024
EPS = 1e-6
NSLOT = 24
SKIP = set()


class T:
    __slots__ = ("name", "w", "r", "psum")

    def __init__(self, name="", psum=False):
        self.name = name
        self.w = None
        self.r = []
        self.psum = psum


class Op:
    __slots__ = ("eng", "fn", "deps", "id", "is_dma", "slot", "val", "marked", "semval", "cc")

    def __init__(self, eng, fn, is_dma):
        self.eng = eng
        self.fn = fn
        self.deps = []
        self.is_dma = is_dma
        self.slot = None
        self.val = None
        self.marked = False
        self.semval = None
        self.cc = False


class Prog:
    ENGS = ("pe", "act", "dve", "pool", "sp")

    def __init__(self, nc):
        self.nc = nc
        self.ops = []
        self.streams = {e: [] for e in self.ENGS}
        self.ndma = {e: 0 for e in self.ENGS}
        self.dmas = {e: [] for e in self.ENGS}

    def add(self, eng, fn, reads=(), writes=(), dma=False, extra=(), cc=False):
        op = Op(eng, fn, dma or cc)
        op.cc = cc
        op.id = len(self.ops)
        deps = {}
        for t in reads:
            if t.w is not None:
                deps[t.w.id] = t.w
            if t.psum:
                for r in t.r:
                    if r.eng != eng:
                        deps[r.id] = r
        for t in writes:
            if t.w is not None:
                deps[t.w.id] = t.w
            for r in t.r:
                deps[r.id] = r
        for d in extra:
            deps[d.id] = d
        op.deps = list(deps.values())
        for t in reads:
            if not dma:
                t.r = [r for r in t.r if r.is_dma or r.eng != eng]
            t.r.append(op)
        for t in writes:
            t.w = op
            t.r = []
        if cc:
            self.ncc = getattr(self, "ncc", 0) + 1
            op.slot = ("cc", self.ncc - 1)
            op.val = 1
            self.dmas[eng].append(op)
        elif dma:
            i = self.ndma[eng]
            self.ndma[eng] += 1
            op.slot = i % NSLOT
            op.val = 16 * (i // NSLOT + 1)
            self.dmas[eng].append(op)
        self.ops.append(op)
        self.streams[eng].append(op)
        return op

    def barrier(self):
        lasts = []
        for e in self.ENGS:
            if self.streams[e]:
                lasts.append(self.streams[e][-1])
            lasts.extend(self.dmas[e][-NSLOT:])
        for e in self.ENGS:
            self.add(e, lambda eng: eng.nop(), extra=lasts)

    def dma(self, q, out, in_, reads=(), writes=()):
        return self.add(q, lambda e: e.dma_start(out=out, in_=in_), reads, writes, dma=True)

    def mm(self, out, lhsT, rhs, start, stop, reads=(), writes=()):
        return self.add("pe", lambda e: e.matmul(out, lhsT, rhs, start=start, stop=stop), reads, writes)

    def transpose(self, out, in_, ident, reads=(), writes=()):
        return self.add("pe", lambda e: e.transpose(out, in_, ident), reads, writes)

    def act(self, out, in_, func, reads=(), writes=(), bias=None, scale=None):
        kw = {}
        if bias is not None:
            kw["bias"] = bias
        if scale is not None:
            kw["scale"] = scale
        return self.add("act", lambda e: e.activation(out, in_, func, **kw), reads, writes)

    def tt(self, eng, out, in0, in1, op, reads=(), writes=()):
        return self.add(eng, lambda e: e.tensor_tensor(out, in0, in1, op), reads, writes)

    def ts(self, eng, out, in0, s1, s2, op0, op1=None, reads=(), writes=()):
        if op1 is None:
            return self.add(eng, lambda e: e.tensor_scalar(out=out, in0=in0, scalar1=s1, scalar2=None, op0=op0),
                            reads, writes)
        return self.add(eng, lambda e: e.tensor_scalar(out=out, in0=in0, scalar1=s1, scalar2=s2, op0=op0, op1=op1),
                        reads, writes)

    def stt(self, out, in0, scalar, in1, op0, op1, reads=(), writes=()):
        return self.add("dve", lambda e: e.scalar_tensor_tensor(out=out, in0=in0, scalar=scalar, in1=in1,
                                                                op0=op0, op1=op1), reads, writes)

    def copy(self, eng, out, in_, reads=(), writes=()):
        if eng == "act":
            return self.add("act", lambda e: e.copy(out, in_), reads, writes)
        return self.add(eng, lambda e: e.tensor_copy(out, in_), reads, writes)

    def emit(self):
        nc = self.nc
        for op in self.ops:
            for d in op.deps:
                if d.is_dma:
                    continue
                if d.eng == op.eng and not op.is_dma and d.eng == "pe":
                    continue
                d.marked = True
        cnt = {e: 0 for e in self.ENGS}
        for op in self.ops:
            if op.marked and not op.is_dma:
                cnt[op.eng] += 1
                op.semval = cnt[op.eng]
        sems = {e: nc.alloc_semaphore(f"s_{e}") for e in self.ENGS}
        dsems = {e: {i: nc.alloc_semaphore(f"d_{e}_{i}") for i in range(min(NSLOT, self.ndma[e]))}
                 for e in self.ENGS}
        for i in range(getattr(self, "ncc", 0)):
            dsems["pool"][("cc", i)] = nc.alloc_semaphore(f"cc_{i}")
        engobj = {"pe": nc.tensor, "act": nc.scalar, "dve": nc.vector, "pool": nc.gpsimd, "sp": nc.sync}
        with nc.Block() as block:
            def run(ename):
                eng = engobj[ename]
                waited = {}

                def wait(key, sem, val):
                    if waited.get(key, 0) >= val:
                        return
                    waited[key] = val
                    eng.wait_ge(sem, val)

                for op in self.streams[ename]:
                    for d in op.deps:
                        if d.is_dma:
                            wait(("d", d.eng, d.slot), dsems[d.eng][d.slot], d.val)
                        else:
                            if d.eng == ename and not op.is_dma and ename == "pe":
                                continue
                            wait(("c", d.eng), sems[d.eng], d.semval)
                    if op.cc:
                        ins = op.fn(eng)
                        ins.then_inc(dsems[ename][op.slot], 1)
                    elif op.is_dma:
                        if op.val > 16:
                            wait(("d", ename, op.slot), dsems[ename][op.slot], op.val - 16)
                        ins = op.fn(eng)
                        ins.then_inc(dsems[ename][op.slot], 16)
                    else:
                        ins = op.fn(eng)
                        if op.marked:
                            ins.then_inc(sems[ename], 1)
                n = self.ndma[ename]
                for i in range(max(0, n - NSLOT), n):
                    wait(("d", ename, i % NSLOT), dsems[ename][i % NSLOT], 16 * (i // NSLOT + 1))

            @block.tensor
            def _(e):
                run("pe")

            @block.scalar
            def _(e):
                run("act")

            @block.vector
            def _(e):
                run("dve")

            @block.gpsimd
            def _(e):
                run("pool")

            @block.sync
            def _(e):
                run("sp")


class Buf:
    __slots__ = ("ap", "t")

    def __init__(self, ap, name="", psum=False):
        self.ap = ap
        self.t = T(name, psum)


class Arena:
    def __init__(self, nc, nbytes):
        self.t = nc.alloc_sbuf_tensor("arena", [128, nbytes // 2], BF16)
        self.nbytes = nbytes
        self.off = 0

    def alloc(self, shape, dtype, name=""):
        n = 1
        for s in shape[1:]:
            n *= s
        es = 4 if dtype == F32 else 2
        nb = (n * es + 63) // 64 * 64
        assert self.off + nb <= self.nbytes, f"arena overflow {name} {self.off + nb}"
        v = self.t[0:shape[0], self.off // 2:(self.off + n * es) // 2]
        if dtype == F32:
            v = v.bitcast(F32)
        if len(shape) == 3:
            v = v.rearrange("p (a b) -> p a b", a=shape[1])
        self.off += nb
        return Buf(v, name)


SPL = [512, 512, 1024, 1024, 1024, 128, 128, 1024, 256, 256, 1024, 1024, 1024]
OFF = np.concatenate([[0], np.cumsum(SPL)]).astype(int)
O_QR, O_KR, O_VR, O_UR, O_QS, O_KS, O_VS, O_QA, O_KA, O_VA, O_AR, O_AS, O_AA = [int(v) for v in OFF[:13]]


def _perm128():
    f = np.arange(128)
    return np.where(f % 64 < 32, f + 32, f - 32)


def _perm64():
    f = np.arange(128)
    return np.where(f % 32 < 16, f + 16, f - 16)


def fm_units():
    u = []
    ar = np.arange(128)
    for h in range(4):
        u.append(dict(name=f"kr{h}", kind="rope128", cols=O_KR + h * 128 + ar, tok="all"))
    u.append(dict(name="ks", kind="rope64", cols=O_KS + ar, tok="all"))
    u.append(dict(name="ks2", kind="rope64", cols=O_KS + (ar + 64) % 128, tok="all"))
    for h in range(2):
        u.append(dict(name=f"ka{h}", kind="normk", cols=O_KA + h * 128 + ar, tok="all"))
    for h in range(4):
        u.append(dict(name=f"qr{h}", kind="rope128", cols=O_QR + h * 128 + ar, tok="q"))
    for c in range(8):
        u.append(dict(name=f"qs{c}", kind="rope64", cols=O_QS + c * 128 + ar, tok="q"))
    for h in range(8):
        u.append(dict(name=f"qa{h}", kind="normq", cols=O_QA + h * 128 + ar, tok="q"))
    for c in range(8):
        u.append(dict(name=f"ur{c}", kind="silu", cols=O_UR + c * 128 + ar, tok="q"))
    for nm, o in (("ar", O_AR), ("as", O_AS), ("aa", O_AA)):
        for c in range(8):
            u.append(dict(name=f"{nm}{c}", kind="sig", cols=o + c * 128 + ar, tok="q"))
    g, used = 0, 0
    for x in u:
        w = 128
        if used + w > 512:
            g, used = g + 1, 0
        x["group"], x["c0"] = g, used
        x["dual"] = x["kind"] in ("rope128", "rope64", "normq", "normk")
        used += w
    return u, g + 1


FM_UNITS, N_FM_GROUPS = fm_units()
TM_COLS = np.concatenate([O_VR + np.arange(1024), O_VS + np.arange(128), O_VA + np.arange(256)])
N_TM_GROUPS = 3
G_MOD = 0
G_FM = 12
G_TM = G_FM + N_FM_GROUPS
G_MERGE = G_TM + N_TM_GROUPS
N_GROUPS = G_MERGE + 8

SM_BMOD, SM_G1, SM_G2, SM_RET, SM_SINK, SM_GQ, SM_GK = 0, 48, 56, 64, 72, 88, 90
SM_L = 96
SG_C, SG_WR, SG_BR, SG_GF, SG_FLAG = 0, 16, 144, 160, 168
SG_N = 176
C_ID, C_DPOS, C_DNEG, C_MF, C_MB, C_IP1, C_IB, C_ML, C_MR, C_MLB, C_MRB = [i * 128 for i in range(11)]
C_PCF, C_PCB = 11 * 128, 11 * 128 + 1
C_SEL = 11 * 128 + 8
C_P128 = C_SEL + 2048
C_P64 = C_P128 + 128
C_N = C_P64 + 128


def _grp(w):
    n = w.shape[1] // 512
    return np.ascontiguousarray(w.reshape(8, 128, n, 512).transpose(2, 1, 0, 3))


def prep_layer_weights(inp, l):
    w_in = inp["w_in"][l]
    p128, p64 = _perm128(), _perm64()
    cols = np.zeros(N_FM_GROUPS * 512, dtype=np.int64)
    for u in FM_UNITS:
        base = u["group"] * 512
        cols[base + u["c0"]:base + u["c0"] + 128] = u["cols"]
    tmc = np.concatenate([TM_COLS, np.zeros(1536 - 1408, dtype=np.int64)])
    wcat = np.concatenate([inp["w_mod"][l], w_in[:, cols], w_in[:, tmc], inp["w_br_ret"][l], inp["w_br_swa"][l],
                           inp["w_br_ga"][l], inp["w_out"][l]], axis=1)
    wall = _grp(wcat)
    assert wall.shape[0] == N_GROUPS
    wg = inp["w_gate"][l].reshape(16, 8, 128, 512).transpose(0, 2, 1, 3).reshape(16, 128, 4096)
    wu = inp["w_up"][l].reshape(16, 8, 128, 512).transpose(0, 2, 1, 3).reshape(16, 128, 4096)
    wd = inp["w_down"][l].reshape(16, 4, 128, 1024).transpose(0, 2, 1, 3).reshape(16, 128, 4096)
    we = np.ascontiguousarray(np.stack([wg, wu, wd], axis=1))
    sm = np.zeros((128, SM_L), np.float32)
    sm[:, SM_BMOD:SM_BMOD + 48] = inp["b_mod"][l].reshape(48, 128).T
    sm[:, SM_G1:SM_G1 + 8] = inp["g_norm1"][l].reshape(8, 128).T
    sm[:, SM_G2:SM_G2 + 8] = inp["g_norm2"][l].reshape(8, 128).T
    sm[:, SM_RET:SM_RET + 8] = inp["ret_decay_logit"][l].reshape(1, 8)
    sm[:, SM_SINK:SM_SINK + 16] = inp["swa_sink"][l].reshape(1, 16)
    sm[:, SM_GQ] = inp["g_qnorm"][l]
    sm[:, SM_GQ + 1] = inp["g_qnorm"][l][p128]
    sm[:, SM_GK] = inp["g_knorm"][l]
    sm[:, SM_GK + 1] = inp["g_knorm"][l][p128]
    return wall, we, sm


def make_consts(half):
    c = np.zeros((128, C_N), np.float32)
    m = np.arange(128)[:, None].astype(np.float32)
    n = np.arange(128)[None, :].astype(np.float32)
    c[:, C_ID:C_ID + 128] = np.eye(128)
    c[:, C_DPOS:C_DPOS + 128] = np.maximum(n - m, 0)
    c[:, C_DNEG:C_DNEG + 128] = np.maximum(m - n, 0)
    c[:, C_MF:C_MF + 128] = (n >= m)
    c[:, C_MB:C_MB + 128] = (m > n)
    c[:, C_IP1:C_IP1 + 128] = n + 1 + 0 * m
    c[:, C_IB:C_IB + 128] = 128 - n + 0 * m
    c[:, C_ML:C_ML + 128] = (m >= n)
    c[:, C_MR:C_MR + 128] = (m <= n)
    c[:, C_MLB:C_MLB + 128] = (m >= n) * (1.0 if half == 1 else 0.0)
    c[:, C_MRB:C_MRB + 128] = (m <= n) * (1.0 if half == 0 else 0.0)
    c[:, C_PCF] = 127 - np.arange(128)
    c[:, C_PCB] = np.arange(128)
    for e in range(16):
        c[e, C_SEL + e * 128:C_SEL + (e + 1) * 128] = 1.0
    for co, pm in ((C_P128, _perm128()), (C_P64, _perm64())):
        for f in range(128):
            c[pm[f], co + f] = 1.0
    return c


def make_tabs(half):
    pos_own = half * NOWN + np.arange(NOWN)
    pos_oth = (1 - half) * NOWN + np.arange(NOTH)
    pos = np.concatenate([pos_own, pos_oth])
    row = (pos // 64).astype(np.float32)
    col = (pos % 64).astype(np.float32)
    tabs = np.zeros((4, 128, NALL), np.float32)
    tabs[0, :, 4096:] = 1.0
    tabs[2, :, 4096:] = 1.0
    for ti, hd in ((0, 128), (2, 64)):
        half_d, quarter = hd // 2, hd // 4
        freqs = (np.float32(10000.0) ** (-np.arange(quarter, dtype=np.float32) / np.float32(quarter))).astype(np.float32)
        for f in range(128):
            fl = f % hd
            a = fl // half_d
            j = fl % half_d
            p = row if a == 0 else col
            ang = (p * freqs[j % quarter]).astype(np.float32)
            tabs[ti, f, :4096] = np.cos(ang)
            tabs[ti + 1, f, :4096] = np.sin(ang) * (-1.0 if j < quarter else 1.0)
    return tabs


def make_small_global(inp, b, half):
    sg = np.zeros((128, SG_N), np.float32)
    cT = inp["c"][b].reshape(8, 128).T
    ccT = inp["c_ctx"].reshape(8, 128).T
    sg[:, SG_C:SG_C + 16:2] = cT
    sg[:, SG_C + 1:SG_C + 16:2] = ccT
    sg[:, SG_WR:SG_WR + 128] = inp["w_router"].reshape(8, 128, 16).transpose(1, 0, 2).reshape(128, 128)
    sg[:, SG_BR:SG_BR + 16] = inp["b_router"].reshape(1, 16)
    sg[:, SG_GF:SG_GF + 8] = inp["g_final"].reshape(8, 128).T
    sg[:, SG_FLAG] = 1.0 if half == 0 else 0.0
    sg[:, SG_FLAG + 1] = 1.0 if half == 1 else 0.0
    return sg


TOK_TILES_ALL = [(i * 512, 512, 0) for i in range(8)] + [(4096, 256, 1)]
TOK_TILES_OWN = [(i * 512, 512, 0) for i in range(4)]
CTX_TILE = (4096, 256, 1)


def qpos(t0):
    return t0 if t0 < NOWN else t0 - NOTH


def build(layers, need_ctx_flags, final, dbg=(), stop=99):
    nc = bass.Bass("TRN2", target_bir_lowering=False)
    P = Prog(nc)
    nl = len(layers)

    def din(name, shape, dt=F32):
        return nc.dram_tensor(name, list(shape), dt, kind="ExternalInput").ap()

    def dscr(name, shape, dt=BF16):
        kind = "ExternalOutput" if name in dbg else "Internal"
        return nc.dram_tensor(name, list(shape), dt, kind=kind).ap()

    xall = din("xall", [D, NALL])
    tabs = din("tabs", [4, 128, NALL])
    consts_d = din("consts", [128, C_N])
    sg_d = din("sg", [128, SG_N])
    sm_d = [din(f"sm{l}", [128, SM_L]) for l in range(nl)]
    wall_d = [din(f"wall{l}", [N_GROUPS, 128, 8, 512]) for l in range(nl)]
    we_d = [din(f"we{l}", [16, 3, 128, 4096]) for l in range(nl)]
    if final:
        yout = nc.dram_tensor("yout", [D, NOWN], F32, kind="ExternalOutput").ap()
    else:
        xout = nc.dram_tensor("xout", [D, NQ], F32, kind="ExternalOutput").ap()

    xs_d = dscr("xs", [D, NALL], F32)
    fm_d = {u["name"]: dscr("fm_" + u["name"], [128, NALL]) for u in FM_UNITS}
    vall_d = dscr("vall", [NALL, 1536])
    obr_d = [dscr(f"obr{i}", [D, NQ]) for i in range(3)]
    web_d = dscr("web", [16, 3, 128, 4096])
    T_xs = {}

    def txs(t0):
        return T_xs.setdefault(t0, T(f"xs{t0}"))
    T_fm = {}

    def tfm(name, t0):
        return T_fm.setdefault((name, t0), T(f"fm{name}{t0}"))
    T_vall = {}

    def tvall(sub):
        return T_vall.setdefault(sub, T(f"vall{sub}"))
    T_obr = {}

    def tobr(i, t0):
        return T_obr.setdefault((i, t0), T(f"obr{i}_{t0}"))
    T_web = {}

    def tweb(e, k):
        return T_web.setdefault((e, k), T(f"web{e}_{k}"))

    xs3 = xs_d.rearrange("(kc p) t -> p kc t", p=128)
    if nl > 1:
        xch_src = [nc.dram_tensor(f"xch_src{i}", [D, 512], F32, kind="Internal").ap() for i in range(4)]
        xch_dst = [nc.dram_tensor(f"xch_dst{i}", [2 * D, 512], F32, kind="Internal").ap() for i in range(4)]
        T_xsrc, T_xdst = [T("xsrc") for i in range(4)], [T("xdst") for i in range(4)]
    xall3 = xall.rearrange("(kc p) t -> p kc t", p=128)

    A = Arena(nc, 211968)
    cst = A.alloc([128, C_N], F32, "cst")
    sg = A.alloc([128, SG_N], F32, "sg")
    ones_bf = A.alloc([128, 128], BF16, "ones")
    id_bf = A.alloc([128, 128], BF16, "idbf")
    mk_bf = A.alloc([128, 4, 512], BF16, "mk")
    pm_bf = A.alloc([128, 2, 128], BF16, "pm")
    PERS_END = None
    ps = [Buf(nc.alloc_psum_tensor(f"ps{i}", [128, 512], F32)[:], f"ps{i}", True) for i in range(8)]

    P.dma("sp", cst.ap, consts_d, writes=[cst.t])
    P.dma("sp", sg.ap, sg_d, writes=[sg.t])
    P.add("dve", lambda e: e.memset(ones_bf.ap, 1.0), writes=[ones_bf.t])
    P.copy("dve", id_bf.ap, cst.ap[:, C_ID:C_ID + 128], reads=[cst.t], writes=[id_bf.t])
    for i, co in enumerate((C_P128, C_P64)):
        P.copy("dve", pm_bf.ap[:, i, :], cst.ap[:, co:co + 128], reads=[cst.t], writes=[pm_bf.t])
    for i, co in enumerate((C_ML, C_MR, C_MLB, C_MRB)):
        for r in range(4):
            P.copy("dve", mk_bf.ap[:, i, r * 128:(r + 1) * 128], cst.ap[:, co:co + 128], reads=[cst.t],
                   writes=[mk_bf.t])
    for (t0, nt, _) in TOK_TILES_ALL:
        P.dma("sp", xs_d[:, t0:t0 + nt], xall[:, t0:t0 + nt], writes=[txs(t0)])
    PERS_END = A.off

    def rsqrt_from_psum(dst, src_ps, n, rd, wr):
        P.ts("dve", dst, src_ps, 1.0 / n, EPS, ALU.mult, ALU.add, reads=rd, writes=wr)
        P.act(dst, dst, AF.Ln, reads=wr, writes=wr)
        P.act(dst, dst, AF.Exp, reads=wr, writes=wr, scale=-0.5)

    psi = [0]

    def ps_next(k=8):
        psi[0] = (psi[0] + 1) % k
        return ps[psi[0]]

    for li, l in enumerate(layers):
        need_ctx = need_ctx_flags[li]
        last = (li == nl - 1)
        qtiles = TOK_TILES_OWN + ([CTX_TILE] if need_ctx else [])
        P.barrier()
        A.off = PERS_END
        sm = A.alloc([128, SM_L], F32, "sm")
        modT = A.alloc([128, 48, 2], F32, "modT")
        A1 = A.alloc([128, 8, 2], F32, "A1")
        A2 = A.alloc([128, 8, 2], F32, "A2")
        silc = A.alloc([128, 16], F32, "silc")
        lg = A.alloc([128, 8], F32, "lg")
        sinkx = A.alloc([128, 16], F32, "sinkx")
        P.dma("sp", sm.ap, sm_d[li], writes=[sm.t])
        P.act(silc.ap, sg.ap[:, SG_C:SG_C + 16], AF.Silu, reads=[sg.t], writes=[silc.t])
        silc3 = silc.ap.rearrange("p (k j) -> p k j", j=2)
        P.act(lg.ap, sm.ap[:, SM_RET:SM_RET + 8], AF.Exp, reads=[sm.t], writes=[lg.t], scale=-1.0)
        P.ts("dve", lg.ap, lg.ap, 1.0, None, ALU.add, reads=[lg.t], writes=[lg.t])
        P.act(lg.ap, lg.ap, AF.Ln, reads=[lg.t], writes=[lg.t])
        P.ts("dve", lg.ap, lg.ap, -1.0, None, ALU.mult, reads=[lg.t], writes=[lg.t])
        P.act(sinkx.ap, sm.ap[:, SM_SINK:SM_SINK + 16], AF.Exp, reads=[sm.t], writes=[sinkx.t])
        PH0_END = A.off
        wst = [A.alloc([128, 8, 512], BF16, f"wst{i}") for i in range(4)]
        silb = A.alloc([128, 16], BF16, "silb")
        P.copy("dve", silb.ap, silc.ap, reads=[silc.t], writes=[silb.t])
        silc3 = silb.ap.rearrange("p (k j) -> p k j", j=2)
        for g in range(12):
            w = wst[g % 4]
            P.dma("pool", w.ap, wall_d[li][G_MOD + g], writes=[w.t])
            for j in range(4):
                idx = g * 4 + j
                pb = ps_next()
                for kc in range(8):
                    P.mm(pb.ap[:, 0:2], w.ap[:, kc, j * 128:(j + 1) * 128], silc3[:, kc, :], kc == 0, kc == 7,
                         reads=[w.t, silb.t], writes=[pb.t])
                P.ts("dve", modT.ap[:, idx, :], pb.ap[:, 0:2], sm.ap[:, SM_BMOD + idx:SM_BMOD + idx + 1], None,
                     ALU.add, reads=[pb.t, sm.t], writes=[modT.t])
        for j in range(2):
            P.stt(A1.ap[:, :, j], modT.ap[:, 8:16, j], 1.0, sm.ap[:, SM_G1:SM_G1 + 8], ALU.add, ALU.mult,
                  reads=[modT.t, sm.t], writes=[A1.t])
            P.stt(A2.ap[:, :, j], modT.ap[:, 32:40, j], 1.0, sm.ap[:, SM_G2:SM_G2 + 8], ALU.add, ALU.mult,
                  reads=[modT.t, sm.t], writes=[A2.t])

        def SH1(kc, j): return modT.ap[:, 0 + kc, j:j + 1]
        def GT1(kc, j): return modT.ap[:, 16 + kc, j:j + 1]
        def SH2(kc, j): return modT.ap[:, 24 + kc, j:j + 1]
        def GT2(kc, j): return modT.ap[:, 40 + kc, j:j + 1]

        P.barrier()
        A.off = PH0_END
        hT = A.alloc([128, 8, NALL], BF16, "hT")
        hts = {t0: T(f"hT{t0}") for (t0, _, _) in TOK_TILES_ALL}
        xt = [A.alloc([128, 8, 512], F32, f"xt{i}") for i in range(2)]
        sq = A.alloc([128, 8, 512], BF16, "sq")
        rstd = A.alloc([128, 512], F32, "rstd")
        tmp = [A.alloc([128, 512], F32, f"tmp{i}") for i in range(2)]
        PH1_END = A.off

        def norm_tiles(tiles, Aco, SHf, out_fn, src3=xs3):
            for i, (t0, nt, mj) in enumerate(tiles):
                x_ = xt[i % 2]
                P.dma("sp", x_.ap[:, :, 0:nt], src3[:, :, t0:t0 + nt], reads=[txs(t0)], writes=[x_.t])
                P.act(sq.ap[:, :, 0:nt], x_.ap[:, :, 0:nt], AF.Square, reads=[x_.t], writes=[sq.t])
                pb = ps_next()
                for kc in range(8):
                    P.mm(pb.ap[:, 0:nt], ones_bf.ap, sq.ap[:, kc, 0:nt], kc == 0, kc == 7,
                         reads=[ones_bf.t, sq.t], writes=[pb.t])
                rsqrt_from_psum(rstd.ap[:, 0:nt], pb.ap[:, 0:nt], 1024.0, [pb.t], [rstd.t])
                for kc in range(8):
                    tm_ = tmp[kc % 2]
                    P.tt("dve" if kc % 2 == 0 else "pool", tm_.ap[:, 0:nt], x_.ap[:, kc, 0:nt], rstd.ap[:, 0:nt],
                         ALU.mult, reads=[x_.t, rstd.t], writes=[tm_.t])
                    out_fn(kc, t0, nt, mj, tm_, Aco.ap[:, kc, mj:mj + 1], SHf(kc, mj))

        def out_h1(kc, t0, nt, mj, tm_, a_, b_):
            P.act(hT.ap[:, kc, t0:t0 + nt], tm_.ap[:, 0:nt], AF.Identity, reads=[tm_.t, A1.t, modT.t],
                  writes=[hts[t0]], bias=b_, scale=a_)

        if stop <= 0:
            P.barrier()
            continue
        norm_tiles(TOK_TILES_ALL, A1, SH1, out_h1)

        A.off = PH1_END
        wbf = [A.alloc([128, 8, 512], BF16, f"wbf{i}") for i in range(3)]
        tabC = [A.alloc([128, 512], F32, f"tabC{i}") for i in range(2)]
        tabS = [A.alloc([128, 512], F32, f"tabS{i}") for i in range(2)]
        stage = [A.alloc([128, 512], BF16, f"stage{i}") for i in range(3)]
        t1b = [A.alloc([128, 512], F32, f"t1b{i}") for i in range(2)]
        t2b = [A.alloc([128, 512], F32, f"t2b{i}") for i in range(2)]
        sqb = A.alloc([128, 512], BF16, "sqb")
        rs2 = A.alloc([128, 512], F32, "rs2")
        cnt = [0]

        def load_group(gi):
            wb = wbf[gi % 3]
            P.dma("pool", wb.ap, wall_d[li][gi], writes=[wb.t])
            return wb

        pabf = [A.alloc([128, 512], BF16, f"pabf{i}") for i in range(2)]
        work = []
        for gi in range(N_FM_GROUPS):
            for u in [x for x in FM_UNITS if x["group"] == gi]:
                for tile in (TOK_TILES_ALL if u["tok"] == "all" else qtiles):
                    work.append((gi, u, tile))
        wbs = {}
        loaded = [G_FM - 1]

        def ensure(gi):
            while loaded[0] < G_FM + gi:
                loaded[0] += 1
                wbs[loaded[0] - G_FM] = load_group(loaded[0])

        def MAIN(k):
            gi, u, (t0, nt, mj) = work[k]
            ensure(gi + 1)
            wb = wbs[gi]
            pa = ps_next()
            for kc in range(8):
                P.mm(pa.ap[:, 0:nt], wb.ap[:, kc, u["c0"]:u["c0"] + 128], hT.ap[:, kc, t0:t0 + nt],
                     kc == 0, kc == 7, reads=[wb.t, hts[t0]], writes=[pa.t])
            if u["dual"]:
                pb_ = pabf[k % 2]
                P.copy("act", pb_.ap[:, 0:nt], pa.ap[:, 0:nt], reads=[pa.t], writes=[pb_.t])
                tsel = 2 if u["kind"] == "rope64" else 0
                tc_, ts_ = tabC[k % 2], tabS[k % 2]
                P.dma("sp", tc_.ap[:, 0:nt], tabs[tsel, :, t0:t0 + nt], writes=[tc_.t])
                P.dma("sp", ts_.ap[:, 0:nt], tabs[tsel + 1, :, t0:t0 + nt], writes=[ts_.t])
            return pa

        def REST(k, pa):
            gi, u, (t0, nt, mj) = work[k]
            kind = u["kind"]
            if u["dual"]:
                pbk = ps_next()
                P.mm(pbk.ap[:, 0:nt], pm_bf.ap[:, 1 if kind == "rope64" else 0, :], pabf[k % 2].ap[:, 0:nt],
                     True, True, reads=[pm_bf.t, pabf[k % 2].t], writes=[pbk.t])
                tc_, ts_ = tabC[k % 2], tabS[k % 2]
            st = stage[k % 3]
            o_ = st.ap[:, 0:nt]
            if kind in ("rope128", "rope64"):
                a_, b_ = t1b[k % 2], t2b[k % 2]
                P.tt("dve", a_.ap[:, 0:nt], pa.ap[:, 0:nt], tc_.ap[:, 0:nt], ALU.mult,
                     reads=[pa.t, tc_.t], writes=[a_.t])
                P.tt("dve", b_.ap[:, 0:nt], pbk.ap[:, 0:nt], ts_.ap[:, 0:nt], ALU.mult,
                     reads=[pbk.t, ts_.t], writes=[b_.t])
                P.tt("pool", o_, a_.ap[:, 0:nt], b_.ap[:, 0:nt], ALU.add, reads=[a_.t, b_.t], writes=[st.t])
            elif kind in ("normq", "normk"):
                gco = SM_GQ if kind == "normq" else SM_GK
                a_, b_ = t1b[k % 2], t2b[k % 2]
                P.act(sqb.ap[:, 0:nt], pa.ap[:, 0:nt], AF.Square, reads=[pa.t], writes=[sqb.t])
                pc = ps_next()
                P.mm(pc.ap[:, 0:nt], ones_bf.ap, sqb.ap[:, 0:nt], True, True, reads=[ones_bf.t, sqb.t],
                     writes=[pc.t])
                rsqrt_from_psum(rs2.ap[:, 0:nt], pc.ap[:, 0:nt], 128.0, [pc.t], [rs2.t])
                P.stt(a_.ap[:, 0:nt], pa.ap[:, 0:nt], sm.ap[:, gco:gco + 1], tc_.ap[:, 0:nt], ALU.mult,
                      ALU.mult, reads=[pa.t, tc_.t, sm.t], writes=[a_.t])
                P.stt(b_.ap[:, 0:nt], pbk.ap[:, 0:nt], sm.ap[:, gco + 1:gco + 2], ts_.ap[:, 0:nt], ALU.mult,
                      ALU.mult, reads=[pbk.t, ts_.t, sm.t], writes=[b_.t])
                P.tt("pool", a_.ap[:, 0:nt], a_.ap[:, 0:nt], b_.ap[:, 0:nt], ALU.add, reads=[a_.t, b_.t],
                     writes=[a_.t])
                P.tt("pool", o_, a_.ap[:, 0:nt], rs2.ap[:, 0:nt], ALU.mult, reads=[a_.t, rs2.t],
                     writes=[st.t])
            elif kind == "silu":
                P.act(o_, pa.ap[:, 0:nt], AF.Silu, reads=[pa.t], writes=[st.t])
            else:
                P.act(o_, pa.ap[:, 0:nt], AF.Sigmoid, reads=[pa.t], writes=[st.t])
            P.dma("pool", fm_d[u["name"]][:, t0:t0 + nt], o_, reads=[st.t], writes=[tfm(u["name"], t0)])

        ensure(0)
        pas = {0: MAIN(0)}
        for k in range(len(work)):
            if k + 1 < len(work):
                pas[k + 1] = MAIN(k + 1)
            REST(k, pas.pop(k))
        cnt[0] = len(work)
        ensure(N_FM_GROUPS)
        nxt = wbs[N_FM_GROUPS]
        for gi in range(N_TM_GROUPS):
            wb = nxt
            if gi + 1 < N_TM_GROUPS:
                nxt = load_group(G_TM + gi + 1)
            for sub in range(NALL // 128):
                t0 = sub * 128
                tile0 = (t0 // 512) * 512
                k = cnt[0]
                cnt[0] += 1
                pa = ps_next()
                for kc in range(8):
                    P.mm(pa.ap, hT.ap[:, kc, t0:t0 + 128], wb.ap[:, kc, :], kc == 0, kc == 7,
                         reads=[wb.t, hts[tile0]], writes=[pa.t])
                st = stage[k % 3]
                P.copy("act" if k % 2 == 0 else "dve", st.ap, pa.ap, reads=[pa.t], writes=[st.t])
                P.dma("pool", vall_d[t0:t0 + 128, gi * 512:(gi + 1) * 512], st.ap, reads=[st.t],
                      writes=[tvall(sub)])
        P.barrier()
        if stop <= 2:
            continue
        A.off = PH0_END
        KSCALE = 128.0 ** -0.5
        KT = A.alloc([128, NALL], BF16, "KT")
        QT = A.alloc([128, NALL], BF16, "QT")
        Vr = A.alloc([128, 34, 256], BF16, "Vr")
        Ur = A.alloc([128, 2, NALL], BF16, "Ur")
        Kf = A.alloc([128, 34, 128], BF16, "Kf")
        Kb = A.alloc([128, 34, 128], BF16, "Kb")
        SFb = A.alloc([128, 18, 256], BF16, "SFb")
        SBb = A.alloc([128, 18, 256], BF16, "SBb")
        Dm = A.alloc([128, 512], BF16, "Dm")
        qdf = A.alloc([128, 512], F32, "qdf")
        qdb = A.alloc([128, 512], F32, "qdb")
        e1 = A.alloc([128, 128], F32, "e1")
        e2 = A.alloc([128, 128], F32, "e2")
        kd = A.alloc([128, 4], F32, "kd")
        S = A.alloc([128, 256], F32, "S")
        S2 = [A.alloc([128, 256], F32, f"S2{i}") for i in range(2)]
        S3 = [A.alloc([128, 256], F32, f"S3{i}") for i in range(2)]
        KVf = A.alloc([128, 34, 256], F32, "KVf")
        KVb = A.alloc([128, 34, 256], F32, "KVb")
        S0f = A.alloc([128, 256], F32, "S0f")
        S0b = A.alloc([128, 256], F32, "S0b")
        Pm = A.alloc([128, 512], BF16, "Pm")
        Qf = A.alloc([128, 512], BF16, "Qf")
        Qb = A.alloc([128, 512], BF16, "Qb")
        sq2 = A.alloc([128, 2, 512], BF16, "sq2")
        rs3 = A.alloc([128, 512], F32, "rs3")
        to_ = [A.alloc([128, 512], F32, f"to{i}") for i in range(2)]
        stg = [A.alloc([128, 512], BF16, f"stg{i}") for i in range(2)]
        flg = sg.ap[:, SG_FLAG:SG_FLAG + 2]
        for h in range(4):
            lgf, lgb = lg.ap[:, h:h + 1], lg.ap[:, 4 + h:5 + h]
            P.act(e1.ap, cst.ap[:, C_DPOS:C_DPOS + 128], AF.Exp, reads=[cst.t, lg.t], writes=[e1.t], scale=lgf)
            P.tt("dve", e1.ap, e1.ap, cst.ap[:, C_MF:C_MF + 128], ALU.mult, reads=[e1.t, cst.t], writes=[e1.t])
            P.act(e2.ap, cst.ap[:, C_DNEG:C_DNEG + 128], AF.Exp, reads=[cst.t, lg.t], writes=[e2.t], scale=lgb)
            P.tt("dve", e2.ap, e2.ap, cst.ap[:, C_MB:C_MB + 128], ALU.mult, reads=[e2.t, cst.t], writes=[e2.t])
            P.tt("dve", e1.ap, e1.ap, e2.ap, ALU.add, reads=[e1.t, e2.t], writes=[e1.t])
            for r in range(4):
                P.ts("dve", Dm.ap[:, r * 128:(r + 1) * 128], e1.ap, KSCALE, None, ALU.mult, reads=[e1.t],
                     writes=[Dm.t])
                P.act(qdf.ap[:, r * 128:(r + 1) * 128], cst.ap[:, C_IP1:C_IP1 + 128], AF.Exp, reads=[cst.t, lg.t],
                      writes=[qdf.t], scale=lgf)
                P.act(qdb.ap[:, r * 128:(r + 1) * 128], cst.ap[:, C_IB:C_IB + 128], AF.Exp, reads=[cst.t, lg.t],
                      writes=[qdb.t], scale=lgb)
            P.act(kd.ap[:, 0:1], cst.ap[:, C_PCF:C_PCF + 1], AF.Exp, reads=[cst.t, lg.t], writes=[kd.t], scale=lgf)
            P.act(kd.ap[:, 1:2], cst.ap[:, C_PCB:C_PCB + 1], AF.Exp, reads=[cst.t, lg.t], writes=[kd.t], scale=lgb)
            P.ts("dve", kd.ap[:, 0:2], kd.ap[:, 0:2], KSCALE, None, ALU.mult, reads=[kd.t], writes=[kd.t])
            P.act(kd.ap[:, 2:3], lgf, AF.Exp, reads=[lg.t], writes=[kd.t], scale=128.0)
            P.act(kd.ap[:, 3:4], lgb, AF.Exp, reads=[lg.t], writes=[kd.t], scale=128.0)
            P.dma("sp", KT.ap, fm_d[f"kr{h}"], writes=[KT.t])
            P.dma("sp", QT.ap[:, 0:NOWN], fm_d[f"qr{h}"][:, 0:NOWN], writes=[QT.t])
            if need_ctx:
                P.dma("sp", QT.ap[:, 4096:NALL], fm_d[f"qr{h}"][:, 4096:NALL], writes=[QT.t])
            vsrc = vall_d[:, h * 256:(h + 1) * 256].rearrange("(t p) c -> p t c", p=128)
            for tq in range(0, 34, 4):
                P.dma("sp", Vr.ap[:, tq:min(tq + 4, 34), :], vsrc[:, tq:min(tq + 4, 34), :], writes=[Vr.t])
            for j in range(2):
                P.dma("sp", Ur.ap[:, j, 0:NOWN], fm_d[f"ur{2 * h + j}"][:, 0:NOWN], writes=[Ur.t])
                if need_ctx:
                    P.dma("sp", Ur.ap[:, j, 4096:NALL], fm_d[f"ur{2 * h + j}"][:, 4096:NALL], writes=[Ur.t])
            for c0 in (range(0, 32 if 'trlast' in SKIP else 34, 4) if 'tr' not in SKIP else []):
                n = min(4, 34 - c0)
                pb_ = ps_next()
                for c in range(n):
                    P.mm(pb_.ap[:, c * 128:(c + 1) * 128], KT.ap[:, (c0 + c) * 128:(c0 + c + 1) * 128], id_bf.ap,
                         True, True, reads=[KT.t, id_bf.t], writes=[pb_.t])
                P.ts("dve", Kf.ap[:, c0:c0 + n, :], pb_.ap[:, 0:n * 128].rearrange("p (a b) -> p a b", a=n),
                     kd.ap[:, 0:1], None, ALU.mult, reads=[pb_.t, kd.t], writes=[Kf.t])
                P.act(Kb.ap[:, c0:c0 + n, :], pb_.ap[:, 0:n * 128].rearrange("p (a b) -> p a b", a=n), AF.Identity,
                      reads=[pb_.t, kd.t], writes=[Kb.t], scale=kd.ap[:, 1:2])

            for kvi, (Kx, KV) in enumerate(((Kf, KVf), (Kb, KVb))):
                for c0 in range(0, 34, 2):
                    pb_ = ps_next()
                    for c in range(2):
                        P.mm(pb_.ap[:, c * 256:(c + 1) * 256], Kx.ap[:, c0 + c, :], Vr.ap[:, c0 + c, :], True, True,
                             reads=[Kx.t, Vr.t], writes=[pb_.t])
                    P.copy("act" if (c0 // 2) % 2 == 0 else "dve", KV.ap[:, c0:c0 + 2, :],
                           pb_.ap.rearrange("p (a b) -> p a b", a=2), reads=[pb_.t], writes=[KV.t])
            class Chain:
                def __init__(self, bufs):
                    self.b = bufs
                    self.cur = 0
                    self.steps = []

                def upd(self, Kx, c, cdcol, first):
                    KV = KVf if Kx is Kf else KVb
                    if first:
                        d = self.b[self.cur]
                        self.steps.append(lambda d=d, KV=KV, c=c: P.copy("dve", d.ap, KV.ap[:, c, :], reads=[KV.t],
                                                                     writes=[d.t]))
                    else:
                        a, d = self.b[self.cur], self.b[1 - self.cur]
                        self.steps.append(lambda a=a, d=d, KV=KV, c=c, cdcol=cdcol: P.stt(
                            d.ap, a.ap, kd.ap[:, cdcol:cdcol + 1], KV.ap[:, c, :], ALU.mult, ALU.add,
                            reads=[a.t, kd.t, KV.t], writes=[d.t]))
                        self.cur = 1 - self.cur

                def snap(self, dst, idx):
                    a = self.b[self.cur]
                    self.steps.append(lambda a=a, dst=dst, idx=idx: P.copy("act", dst.ap[:, idx, :], a.ap,
                                                                          reads=[a.t], writes=[dst.t]))

                def save(self, S0):
                    a = self.b[self.cur]
                    self.steps.append(lambda a=a, S0=S0: P.copy("dve", S0.ap, a.ap, reads=[a.t], writes=[S0.t]))

                def blend(self, S0, fcol):
                    a, b = self.b[self.cur], self.b[1 - self.cur]

                    def f(a=a, b=b, S0=S0, fcol=fcol):
                        P.tt("dve", b.ap, a.ap, S0.ap, ALU.subtract, reads=[a.t, S0.t], writes=[b.t])
                        P.stt(a.ap, b.ap, flg[:, fcol:fcol + 1], S0.ap, ALU.mult, ALU.add, reads=[b.t, S0.t, sg.t],
                              writes=[a.t])
                    self.steps.append(f)

            cf, cb = Chain(S2), Chain(S3)
            cf.upd(Kf, 32, 2, True)
            cf.snap(SFb, 17)
            cf.upd(Kf, 33, 2, False)
            cf.save(S0f)
            for c in range(16, 32):
                cf.upd(Kf, c, 2, False)
            cf.blend(S0f, 1)
            for c in range(0, 16):
                cf.snap(SFb, c)
                if c < 15:
                    cf.upd(Kf, c, 2, False)
            cb.upd(Kb, 33, 3, True)
            cb.snap(SBb, 16)
            cb.upd(Kb, 32, 3, False)
            cb.save(S0b)
            for c in range(31, 15, -1):
                cb.upd(Kb, c, 3, False)
            cb.blend(S0b, 0)
            for c in range(15, -1, -1):
                cb.snap(SBb, c)
                if c > 0:
                    cb.upd(Kb, c, 3, False)
            for i in range(max(len(cf.steps), len(cb.steps))):
                if i < len(cf.steps):
                    cf.steps[i]()
                if i < len(cb.steps):
                    cb.steps[i]()
            for (t0, nt, mj) in (qtiles if 'out' not in SKIP else []):
                ncx = nt // 128
                pS = ps_next()
                for j in range(ncx):
                    sl = slice(t0 + j * 128, t0 + (j + 1) * 128)
                    P.mm(pS.ap[:, j * 128:(j + 1) * 128], KT.ap[:, sl], QT.ap[:, sl], True, True,
                         reads=[KT.t, QT.t], writes=[pS.t])
                P.tt("dve", Pm.ap[:, 0:nt], pS.ap[:, 0:nt], Dm.ap[:, 0:nt], ALU.mult, reads=[pS.t, Dm.t],
                     writes=[Pm.t])
                P.tt("pool", Qf.ap[:, 0:nt], QT.ap[:, t0:t0 + nt], qdf.ap[:, 0:nt], ALU.mult, reads=[QT.t, qdf.t],
                     writes=[Qf.t])
                P.tt("pool", Qb.ap[:, 0:nt], QT.ap[:, t0:t0 + nt], qdb.ap[:, 0:nt], ALU.mult, reads=[QT.t, qdb.t],
                     writes=[Qb.t])
                pO = [ps_next(), ps_next()]
                for dj in range(2):
                    dsl = slice(dj * 128, (dj + 1) * 128)
                    for j in range(ncx):
                        c = t0 // 128 + j
                        csl = slice(j * 128, (j + 1) * 128)
                        if c < 16:
                            sf, sb = c, c
                        elif c == 32:
                            sf, sb = None, 16
                        else:
                            sf, sb = 17, None
                        terms = [(Vr.ap[:, c, dsl], Pm.ap[:, csl], [Vr.t, Pm.t])]
                        if sf is not None:
                            terms.append((SFb.ap[:, sf, dsl], Qf.ap[:, csl], [SFb.t, Qf.t]))
                        if sb is not None:
                            terms.append((SBb.ap[:, sb, dsl], Qb.ap[:, csl], [SBb.t, Qb.t]))
                        for ti, (l_, r_, rd) in enumerate(terms):
                            P.mm(pO[dj].ap[:, csl], l_, r_, ti == 0, ti == len(terms) - 1, reads=rd,
                                 writes=[pO[dj].t])
                    P.act(sq2.ap[:, dj, 0:nt], pO[dj].ap[:, 0:nt], AF.Square, reads=[pO[dj].t], writes=[sq2.t])
                pN = ps_next()
                for dj in range(2):
                    P.mm(pN.ap[:, 0:nt], ones_bf.ap, sq2.ap[:, dj, 0:nt], dj == 0, dj == 1,
                         reads=[ones_bf.t, sq2.t], writes=[pN.t])
                rsqrt_from_psum(rs3.ap[:, 0:nt], pN.ap[:, 0:nt], 256.0, [pN.t], [rs3.t])
                for dj in range(2):
                    P.tt("dve", to_[dj].ap[:, 0:nt], pO[dj].ap[:, 0:nt], rs3.ap[:, 0:nt], ALU.mult,
                         reads=[pO[dj].t, rs3.t], writes=[to_[dj].t])
                    P.tt("pool", stg[dj].ap[:, 0:nt], to_[dj].ap[:, 0:nt], Ur.ap[:, dj, t0:t0 + nt], ALU.mult,
                         reads=[to_[dj].t, Ur.t], writes=[stg[dj].t])
                    r0 = (2 * h + dj) * 128
                    P.dma("sp", obr_d[0][r0:r0 + 128, qpos(t0):qpos(t0) + nt], stg[dj].ap[:, 0:nt],
                          reads=[stg[dj].t], writes=[tobr(0, (h, dj, t0))])
        P.barrier()
        if stop <= 3:
            continue
        def run_attn(groups, Pt, depth=2):
            items = [(gi, ki) for gi, g in enumerate(groups) for ki in range(g["n"])]
            pts = {}
            sidx = [0]

            def SE(idx):
                gi, ki = items[idx]
                g = groups[gi]
                pS = ps[sidx[0] % 4]
                sidx[0] += 1
                g["S"](ki, pS)
                pt = Pt[idx % len(Pt)]
                g["E"](ki, pS, pt)
                pts[idx] = pt
            for idx in range(min(depth, len(items))):
                SE(idx)
            for idx in range(len(items)):
                if idx + depth < len(items):
                    SE(idx + depth)
                gi, ki = items[idx]
                g = groups[gi]
                pO, pD = (ps[4], ps[5]) if gi % 2 == 0 else (ps[6], ps[7])
                g["PV"](ki, pts.pop(idx), pO, pD, ki == 0, ki == g["n"] - 1)
                if ki == g["n"] - 1:
                    g["epi"](pO, pD)

        A.off = PH0_END
        KSa = A.alloc([128, NALL], BF16, "KSa")
        KSb = A.alloc([128, NALL], BF16, "KSb")
        QS = A.alloc([128, 4, NALL], BF16, "QS")
        Vs = A.alloc([128, 34, 64], BF16, "Vs")
        Pt = [A.alloc([128, 512], BF16, f"Pt{i}") for i in range(6)]
        sk = [A.alloc([64, 512], F32, f"sk{i}") for i in range(2)]
        den = [A.alloc([128, 512], F32, f"den{i}") for i in range(2)]
        ostg = [A.alloc([128, 512], BF16, f"ostg{i}") for i in range(2)]
        P.dma("sp", KSa.ap, fm_d["ks"], writes=[KSa.t])
        P.dma("sp", KSb.ap, fm_d["ks2"], writes=[KSb.t])
        obr1 = obr_d[1].rearrange("(c p) t -> p c t", p=128)
        gcount = 0
        for g in range(2):
            for i in range(4):
                P.dma("sp", QS.ap[:, i, 0:NOWN], fm_d[f"qs{g * 4 + i}"][:, 0:NOWN], writes=[QS.t])
                if need_ctx:
                    P.dma("sp", QS.ap[:, i, 4096:NALL], fm_d[f"qs{g * 4 + i}"][:, 4096:NALL], writes=[QS.t])
            vsrc = vall_d[:, 1024 + g * 64:1024 + (g + 1) * 64].rearrange("(t p) c -> p t c", p=128)
            for tq in range(0, 34, 4):
                P.dma("sp", Vs.ap[:, tq:min(tq + 4, 34), :], vsrc[:, tq:min(tq + 4, 34), :], writes=[Vs.t])
            groups = []
            for par in range(2):
                pb0 = par * 64
                Ksrc = KSa if g == par else KSb
                sk_ = sk[par]
                for i in range(4):
                    hq = g * 8 + par + 2 * i
                    P.act(sk_.ap[:, i * 128:(i + 1) * 128], cst.ap[0:64, C_DPOS:C_DPOS + 128], AF.Identity,
                          reads=[cst.t, sinkx.t], writes=[sk_.t], bias=sinkx.ap[0:64, hq:hq + 1], scale=0.0)
                blocks = [(jb * 128, jb) for jb in range(16)] + ([(4096, 16), (4224, 17)] if need_ctx else [])
                for (t0, jb) in blocks:
                    if jb < 16:
                        kts = [((jb - 1) * 128, 0) if jb > 0 else (2048 + 15 * 128, 2), (jb * 128, None),
                               ((jb + 1) * 128, 1) if jb < 15 else (2048, 3), (4096, None), (4224, None)]
                    else:
                        kts = [(4096, None), (4224, None)]

                    def S(ki, pS, kts=kts, pb0=pb0, Ksrc=Ksrc, t0=t0):
                        k0 = kts[ki][0]
                        P.mm(pS.ap.rearrange("p (a b) -> p a b", a=4), Ksrc.ap[pb0:pb0 + 64, k0:k0 + 128],
                             QS.ap[pb0:pb0 + 64, :, t0:t0 + 128], True, True, reads=[Ksrc.t, QS.t], writes=[pS.t])

                    def E(ki, pS, pt, kts=kts):
                        P.act(pt.ap, pS.ap, AF.Exp, reads=[pS.t], writes=[pt.t], scale=0.125)
                        mi = kts[ki][1]
                        if mi is not None:
                            P.tt("pool", pt.ap, pt.ap, mk_bf.ap[:, mi, :], ALU.mult, reads=[pt.t, mk_bf.t],
                                 writes=[pt.t])

                    def PV(ki, pt, pO, pD, first, lastk, kts=kts):
                        k0 = kts[ki][0]
                        P.mm(pO.ap[0:64, :], Vs.ap[:, k0 // 128, :], pt.ap, first, lastk, reads=[Vs.t, pt.t],
                             writes=[pO.t])
                        P.mm(pD.ap[0:64, :], ones_bf.ap[:, 0:64], pt.ap, first, lastk, reads=[ones_bf.t, pt.t],
                             writes=[pD.t])

                    def epi(pO, pD, t0=t0, pb0=pb0, g=g, sk_=sk_, gc=gcount):
                        dn_, os_ = den[gc % 2], ostg[gc % 2]
                        P.tt("dve", dn_.ap[0:64, :], pD.ap[0:64, :], sk_.ap, ALU.add, reads=[pD.t, sk_.t],
                             writes=[dn_.t])
                        P.act(dn_.ap[0:64, :], dn_.ap[0:64, :], AF.Ln, reads=[dn_.t], writes=[dn_.t])
                        P.act(dn_.ap[0:64, :], dn_.ap[0:64, :], AF.Exp, reads=[dn_.t], writes=[dn_.t], scale=-1.0)
                        P.tt("dve", os_.ap[0:64, :], pO.ap[0:64, :], dn_.ap[0:64, :], ALU.mult,
                             reads=[pO.t, dn_.t], writes=[os_.t])
                        q0 = qpos(t0)
                        P.dma("sp", obr1[pb0:pb0 + 64, g * 4:g * 4 + 4, q0:q0 + 128],
                              os_.ap[0:64, :].rearrange("p (a b) -> p a b", a=4), reads=[os_.t],
                              writes=[tobr(1, (g, pb0, t0))])
                    groups.append(dict(n=len(kts), S=S, E=E, PV=PV, epi=epi))
                    gcount += 1
            run_attn(groups, Pt, depth=3)
        P.barrier()
        if stop <= 4:
            continue
        A.off = PH0_END
        KA = A.alloc([128, NALL], BF16, "KA")
        VA = A.alloc([128, 34, 128], BF16, "VA")
        QA = [A.alloc([128, NALL], BF16, f"QA{i}") for i in range(4)]
        Pt = [A.alloc([128, 512], BF16, f"Pt{i}") for i in range(4)]
        den = [A.alloc([128, 512], F32, f"den{i}") for i in range(2)]
        ostg = [A.alloc([128, 512], BF16, f"ostg{i}") for i in range(2)]
        GSCALE = 128.0 ** -0.5
        gcount = 0
        for g in range(2):
            P.dma("sp", KA.ap, fm_d[f"ka{g}"], writes=[KA.t])
            vsrc = vall_d[:, 1152 + g * 128:1152 + (g + 1) * 128].rearrange("(t p) c -> p t c", p=128)
            for tq in range(0, 34, 4):
                P.dma("sp", VA.ap[:, tq:min(tq + 4, 34), :], vsrc[:, tq:min(tq + 4, 34), :], writes=[VA.t])
            groups = []
            for hh in range(4):
                h = g * 4 + hh
                Q_ = QA[hh]
                P.dma("sp", Q_.ap[:, 0:NOWN], fm_d[f"qa{h}"][:, 0:NOWN], writes=[Q_.t])
                if need_ctx:
                    P.dma("sp", Q_.ap[:, 4096:NALL], fm_d[f"qa{h}"][:, 4096:NALL], writes=[Q_.t])
                for (t0, nt, mj) in qtiles:
                    kts = list(range(34)) if t0 < 4096 else [32, 33]

                    def S(ki, pS, kts=kts, Q_=Q_, t0=t0, nt=nt):
                        kt = kts[ki]
                        P.mm(pS.ap[:, 0:nt], KA.ap[:, kt * 128:(kt + 1) * 128], Q_.ap[:, t0:t0 + nt], True, True,
                             reads=[KA.t, Q_.t], writes=[pS.t])

                    def E(ki, pS, pt, nt=nt):
                        P.act(pt.ap[:, 0:nt], pS.ap[:, 0:nt], AF.Exp, reads=[pS.t], writes=[pt.t], scale=GSCALE)

                    def PV(ki, pt, pO, pD, first, lastk, kts=kts, nt=nt):
                        kt = kts[ki]
                        P.mm(pO.ap[:, 0:nt], VA.ap[:, kt, :], pt.ap[:, 0:nt], first, lastk, reads=[VA.t, pt.t],
                             writes=[pO.t])
                        P.mm(pD.ap[:, 0:nt], ones_bf.ap, pt.ap[:, 0:nt], first, lastk, reads=[ones_bf.t, pt.t],
                             writes=[pD.t])

                    def epi(pO, pD, t0=t0, nt=nt, h=h, gc=gcount):
                        dn_, os_ = den[gc % 2], ostg[gc % 2]
                        P.act(dn_.ap[:, 0:nt], pD.ap[:, 0:nt], AF.Ln, reads=[pD.t], writes=[dn_.t])
                        P.act(dn_.ap[:, 0:nt], dn_.ap[:, 0:nt], AF.Exp, reads=[dn_.t], writes=[dn_.t], scale=-1.0)
                        P.tt("dve", os_.ap[:, 0:nt], pO.ap[:, 0:nt], dn_.ap[:, 0:nt], ALU.mult,
                             reads=[pO.t, dn_.t], writes=[os_.t])
                        q0 = qpos(t0)
                        P.dma("sp", obr_d[2][h * 128:(h + 1) * 128, q0:q0 + nt], os_.ap[:, 0:nt], reads=[os_.t],
                              writes=[tobr(2, (h, t0))])
                    groups.append(dict(n=len(kts), S=S, E=E, PV=PV, epi=epi))
                    gcount += 1
            run_attn(groups, Pt)
        P.barrier()
        if stop <= 5:
            continue
        A.off = PH0_END
        wm = [A.alloc([128, 8, 1024], BF16, f"wm{i}") for i in range(4)]
        ob = [[A.alloc([128, 8, 512], BF16, f"ob{j}{i}") for i in range(3)] for j in range(2)]
        gbr = [A.alloc([128, 512], BF16, f"gbr{i}") for i in range(8)]
        ypre = A.alloc([128, 8, 512], BF16, "ypre")
        xtl2 = [A.alloc([128, 8, 512], F32, f"xtl{i}") for i in range(2)]
        ya = [A.alloc([128, 512], F32, f"ya{i}") for i in range(2)]
        tb = [A.alloc([128, 512], F32, f"tb{i}") for i in range(2)]
        for gi in range(8):
            dst = wm[gi // 2].ap[:, :, (gi % 2) * 512:(gi % 2 + 1) * 512]
            P.dma("pool", dst, wall_d[li][G_MERGE + gi], writes=[wm[gi // 2].t])
        gnames = ("ar", "as", "aa")

        def merge_loads(ti):
            t0, nt, mj = qtiles[ti]
            q0 = qpos(t0)
            for b_ in range(3):
                o_ = ob[ti % 2][b_]
                P.dma("sp", o_.ap[:, :, 0:nt], obr_d[b_].rearrange("(kc p) t -> p kc t", p=128)[:, :, q0:q0 + nt],
                      writes=[o_.t])
            x_ = xtl2[ti % 2]
            P.dma("sp", x_.ap[:, :, 0:nt], xs3[:, :, t0:t0 + nt], reads=[txs(t0)], writes=[x_.t])

        gcnt = [0]
        merge_loads(0)
        for ti, (t0, nt, mj) in enumerate(qtiles):
            if ti + 1 < len(qtiles):
                merge_loads(ti + 1)
            xtl = xtl2[ti % 2]
            for dc in range(8):
                dsl = slice(dc * 128, (dc + 1) * 128)
                y_ = ya[dc % 2]
                for b_ in range(3):
                    g_ = gbr[gcnt[0] % 8]
                    gcnt[0] += 1
                    P.dma("sp", g_.ap[:, 0:nt], fm_d[f"{gnames[b_]}{dc}"][:, t0:t0 + nt], writes=[g_.t])
                    pa = ps_next()
                    o_ = ob[ti % 2][b_]
                    for kc in range(8):
                        P.mm(pa.ap[:, 0:nt], wm[b_].ap[:, kc, dsl], o_.ap[:, kc, 0:nt], kc == 0, kc == 7,
                             reads=[wm[b_].t, o_.t], writes=[pa.t])
                    if b_ == 0:
                        P.tt("dve", y_.ap[:, 0:nt], pa.ap[:, 0:nt], g_.ap[:, 0:nt], ALU.mult,
                             reads=[pa.t, g_.t], writes=[y_.t])
                    else:
                        t_ = tb[b_ % 2]
                        P.tt("dve", t_.ap[:, 0:nt], pa.ap[:, 0:nt], g_.ap[:, 0:nt], ALU.mult,
                             reads=[pa.t, g_.t], writes=[t_.t])
                        if b_ == 1:
                            P.tt("pool", y_.ap[:, 0:nt], y_.ap[:, 0:nt], t_.ap[:, 0:nt], ALU.add,
                                 reads=[y_.t, t_.t], writes=[y_.t])
                        else:
                            P.tt("pool", ypre.ap[:, dc, 0:nt], y_.ap[:, 0:nt], t_.ap[:, 0:nt], ALU.add,
                                 reads=[y_.t, t_.t], writes=[ypre.t])
            for dc in range(8):
                dsl = slice(dc * 128, (dc + 1) * 128)
                py = ps_next()
                for kc in range(8):
                    P.mm(py.ap[:, 0:nt], wm[3].ap[:, kc, dsl], ypre.ap[:, kc, 0:nt], kc == 0, kc == 7,
                         reads=[wm[3].t, ypre.t], writes=[py.t])
                P.stt(xtl.ap[:, dc, 0:nt], py.ap[:, 0:nt], GT1(dc, mj), xtl.ap[:, dc, 0:nt], ALU.mult, ALU.add,
                      reads=[py.t, modT.t, xtl.t], writes=[xtl.t])
            P.dma("pool", xs3[:, :, t0:t0 + nt], xtl.ap[:, :, 0:nt], reads=[xtl.t], writes=[txs(t0)])
        P.barrier()
        if stop <= 6:
            continue
        A.off = PH0_END
        h2T = A.alloc([128, 8, NQ], BF16, "h2T")
        xres = A.alloc([128, 8, NQ], F32, "xres")
        WT = A.alloc([16, NQ], F32, "WT")
        M0 = A.off
        h2f = A.alloc([128, 8, 512], F32, "h2f")
        sq = A.alloc([128, 8, 512], BF16, "sq")
        rstd = A.alloc([128, 512], F32, "rstd")
        tmp = [A.alloc([128, 512], F32, f"tmp{i}") for i in range(2)]
        rt = {nm: A.alloc([128, 16], F32, "rt_" + nm) for nm in ("s", "bz", "eq", "msk", "ch", "ws", "wts")}
        rs = {nm: A.alloc([128, 4], F32, "rs_" + nm) for nm in ("m1", "m2", "gs", "gsel", "gm", "dn")}

        def v3(b_):
            return b_.ap.rearrange("p (g k) -> p g k", k=4)

        def bc(b_):
            return b_.ap[:, 0:4].unsqueeze(2).to_broadcast([128, 4, 4])
        mtiles = qtiles
        for i, (t0, nt, mj) in enumerate(mtiles):
            q0 = qpos(t0)
            P.dma("sp", xres.ap[:, :, q0:q0 + nt], xs3[:, :, t0:t0 + nt], reads=[txs(t0)], writes=[xres.t])
            P.act(sq.ap[:, :, 0:nt], xres.ap[:, :, q0:q0 + nt], AF.Square, reads=[xres.t], writes=[sq.t])
            pb = ps_next()
            for kc in range(8):
                P.mm(pb.ap[:, 0:nt], ones_bf.ap, sq.ap[:, kc, 0:nt], kc == 0, kc == 7, reads=[ones_bf.t, sq.t],
                     writes=[pb.t])
            rsqrt_from_psum(rstd.ap[:, 0:nt], pb.ap[:, 0:nt], 1024.0, [pb.t], [rstd.t])
            for kc in range(8):
                tm_ = tmp[kc % 2]
                P.tt("dve", tm_.ap[:, 0:nt], xres.ap[:, kc, q0:q0 + nt], rstd.ap[:, 0:nt], ALU.mult,
                     reads=[xres.t, rstd.t], writes=[tm_.t])
                P.act(h2f.ap[:, kc, 0:nt], tm_.ap[:, 0:nt], AF.Identity, reads=[tm_.t, A2.t, modT.t], writes=[h2f.t],
                      bias=SH2(kc, mj), scale=A2.ap[:, kc, mj:mj + 1])
                P.copy("pool", h2T.ap[:, kc, q0:q0 + nt], h2f.ap[:, kc, 0:nt], reads=[h2f.t], writes=[h2T.t])
            for sub in range(nt // 128):
                ssl = slice(sub * 128, (sub + 1) * 128)
                pr = ps_next()
                for kc in range(8):
                    P.mm(pr.ap[:, 0:16], h2f.ap[:, kc, ssl], sg.ap[:, SG_WR + kc * 16:SG_WR + (kc + 1) * 16],
                         kc == 0, kc == 7, reads=[h2f.t, sg.t], writes=[pr.t])
                s_, bz, eq, msk, ch, ws, wts = [rt[n] for n in ("s", "bz", "eq", "msk", "ch", "ws", "wts")]
                m1, m2, gs, gsel, gm, dn = [rs[n] for n in ("m1", "m2", "gs", "gsel", "gm", "dn")]
                P.act(s_.ap, pr.ap[:, 0:16], AF.Sigmoid, reads=[pr.t], writes=[s_.t])
                P.tt("dve", bz.ap, s_.ap, sg.ap[:, SG_BR:SG_BR + 16], ALU.add, reads=[s_.t, sg.t], writes=[bz.t])
                P.add("dve", lambda e, o=m1.ap, i_=v3(bz): e.tensor_reduce(out=o, in_=i_, axis=AX.X, op=ALU.max),
                      reads=[bz.t], writes=[m1.t])
                P.tt("dve", v3(eq), v3(bz), bc(m1), ALU.is_equal, reads=[bz.t, m1.t], writes=[eq.t])
                P.stt(msk.ap, eq.ap, -1e9, bz.ap, ALU.mult, ALU.add, reads=[eq.t, bz.t], writes=[msk.t])
                P.add("dve", lambda e, o=m2.ap, i_=v3(msk): e.tensor_reduce(out=o, in_=i_, axis=AX.X, op=ALU.max),
                      reads=[msk.t], writes=[m2.t])
                P.tt("dve", gs.ap, m1.ap, m2.ap, ALU.add, reads=[m1.t, m2.t], writes=[gs.t])
                P.add("dve", lambda e, o=gm.ap[:, 0:1], i_=gs.ap: e.tensor_reduce(out=o, in_=i_, axis=AX.X,
                                                                                 op=ALU.max),
                      reads=[gs.t], writes=[gm.t])
                P.ts("dve", gsel.ap, gs.ap, gm.ap[:, 0:1], None, ALU.is_equal, reads=[gs.t, gm.t], writes=[gsel.t])
                P.tt("dve", v3(ch), v3(bz), bc(m2), ALU.is_ge, reads=[bz.t, m2.t], writes=[ch.t])
                P.tt("dve", v3(ch), v3(ch), bc(gsel), ALU.mult, reads=[ch.t, gsel.t], writes=[ch.t])
                P.tt("dve", ws.ap, s_.ap, ch.ap, ALU.mult, reads=[s_.t, ch.t], writes=[ws.t])
                P.add("dve", lambda e, o=dn.ap[:, 0:1], i_=ws.ap: e.tensor_reduce(out=o, in_=i_, axis=AX.X,
                                                                                 op=ALU.add),
                      reads=[ws.t], writes=[dn.t])
                P.add("dve", lambda e, o=dn.ap[:, 1:2], i_=dn.ap[:, 0:1]: e.reciprocal(o, i_), reads=[dn.t],
                      writes=[dn.t])
                P.ts("dve", wts.ap, ws.ap, dn.ap[:, 1:2], None, ALU.mult, reads=[ws.t, dn.t], writes=[wts.t])
                pT = ps_next()
                P.transpose(pT.ap[0:16, 0:128], wts.ap, cst.ap[:, C_ID:C_ID + 128], reads=[wts.t, cst.t],
                            writes=[pT.t])
                P.copy("act", WT.ap[0:16, q0 + sub * 128:q0 + (sub + 1) * 128], pT.ap[0:16, 0:128], reads=[pT.t],
                       writes=[WT.t])
        P.barrier()
        A.off = M0
        ew = [[A.alloc([128, 4096], BF16, f"ew{i}_{k}") for k in range(3)] for i in range(2)]
        wbc = A.alloc([128, 512], F32, "wbc")
        sG = [A.alloc([128, 512], F32, f"sG{i}") for i in range(2)]
        tG = [A.alloc([128, 512], F32, f"tG{i}") for i in range(2)]
        hid = A.alloc([128, 4, 512], BF16, "hid")
        def load_expert(e):
            for k in range(3):
                P.dma("pool", ew[e % 2][k].ap, we_d[li][e, k], writes=[ew[e % 2][k].t])

        hidb = [hid, A.alloc([128, 4, 512], BF16, "hid2")]
        wbcb = [wbc, A.alloc([128, 512], F32, "wbc2")]
        items = [(e, ti) for e in range(16) for ti in range(len(mtiles))]

        def GU(ix):
            e, ti = items[ix]
            wg_, wu_, wd_ = ew[e % 2]
            wg3 = wg_.ap.rearrange("p (a b) -> p a b", a=8)
            wu3 = wu_.ap.rearrange("p (a b) -> p a b", a=8)
            t0, nt, mj = mtiles[ti]
            q0 = qpos(t0)
            hid_, wbc_ = hidb[ix % 2], wbcb[ix % 2]
            pw = ps_next(8)
            P.mm(pw.ap[:, 0:nt], cst.ap[0:16, C_SEL + e * 128:C_SEL + (e + 1) * 128], WT.ap[0:16, q0:q0 + nt],
                 True, True, reads=[cst.t, WT.t], writes=[pw.t])
            P.copy("act", wbc_.ap[:, 0:nt], pw.ap[:, 0:nt], reads=[pw.t], writes=[wbc_.t])
            for hc in range(4):
                hsl = slice(hc * 128, (hc + 1) * 128)
                pg, pu = ps_next(8), ps_next(8)
                for kc in range(8):
                    P.mm(pg.ap[:, 0:nt], wg3[:, kc, hsl], h2T.ap[:, kc, q0:q0 + nt], kc == 0, kc == 7,
                         reads=[wg_.t, h2T.t], writes=[pg.t])
                for kc in range(8):
                    P.mm(pu.ap[:, 0:nt], wu3[:, kc, hsl], h2T.ap[:, kc, q0:q0 + nt], kc == 0, kc == 7,
                         reads=[wu_.t, h2T.t], writes=[pu.t])
                sg_, tg_ = sG[hc % 2], tG[hc % 2]
                P.act(sg_.ap[:, 0:nt], pg.ap[:, 0:nt], AF.Silu, reads=[pg.t], writes=[sg_.t])
                P.tt("dve", tg_.ap[:, 0:nt], sg_.ap[:, 0:nt], pu.ap[:, 0:nt], ALU.mult, reads=[sg_.t, pu.t],
                     writes=[tg_.t])
                P.tt("pool", hid_.ap[:, hc, 0:nt], tg_.ap[:, 0:nt], wbc_.ap[:, 0:nt], ALU.mult,
                     reads=[tg_.t, wbc_.t], writes=[hid_.t])

        def DOWN(ix):
            e, ti = items[ix]
            wd_ = ew[e % 2][2]
            wd3 = wd_.ap.rearrange("p (a b) -> p a b", a=4)
            t0, nt, mj = mtiles[ti]
            q0 = qpos(t0)
            hid_ = hidb[ix % 2]
            for dc in range(8):
                dsl = slice(dc * 128, (dc + 1) * 128)
                py = ps_next(8)
                for hc in range(4):
                    P.mm(py.ap[:, 0:nt], wd3[:, hc, dsl], hid_.ap[:, hc, 0:nt], hc == 0, hc == 3,
                         reads=[wd_.t, hid_.t], writes=[py.t])
                P.stt(xres.ap[:, dc, q0:q0 + nt], py.ap[:, 0:nt], GT2(dc, mj), xres.ap[:, dc, q0:q0 + nt],
                      ALU.mult, ALU.add, reads=[py.t, modT.t, xres.t], writes=[xres.t])

        load_expert(0)
        load_expert(1)
        GU(0)
        for ix in range(len(items)):
            if ix + 1 < len(items):
                GU(ix + 1)
            DOWN(ix)
            e_, ti_ = items[ix]
            if ti_ == len(mtiles) - 1 and e_ + 2 < 16:
                load_expert(e_ + 2)
        if not (last and final):
            for (t0, nt, mj) in mtiles:
                q0 = qpos(t0)
                P.dma("pool", xs3[:, :, t0:t0 + nt], xres.ap[:, :, q0:q0 + nt], reads=[xres.t], writes=[txs(t0)])
        if not last:
            for i, (t0, nt, mj) in enumerate(TOK_TILES_OWN):
                P.dma("pool", xch_src[i].rearrange("(kc p) t -> p kc t", p=128), xres.ap[:, :, t0:t0 + nt],
                      reads=[xres.t], writes=[T_xsrc[i]])
            P.barrier()
            for i in range(4):
                P.add("pool", lambda e, i=i: e.collective_compute("AllGather", ALU.bypass,
                                                                  replica_groups=[[0, 1], [2, 3], [4, 5], [6, 7]],
                                                                  ins=[xch_src[i]], outs=[xch_dst[i]]),
                      reads=[T_xsrc[i]], writes=[T_xdst[i]], cc=True)
            P.barrier()
            A.off = M0
            xa = [A.alloc([128, 8, 512], F32, f"xa{i}") for i in range(2)]
            xb_ = [A.alloc([128, 8, 512], F32, f"xb{i}") for i in range(2)]
            for i, (t0, nt, mj) in enumerate(TOK_TILES_OWN):
                d3 = xch_dst[i].rearrange("(r kc p) t -> r p kc t", r=2, p=128)
                a_, b_ = xa[i % 2], xb_[i % 2]
                P.dma("sp", a_.ap, d3[0], reads=[T_xdst[i]], writes=[a_.t])
                P.dma("sp", b_.ap, d3[1], reads=[T_xdst[i]], writes=[b_.t])
                P.ts("dve", a_.ap, a_.ap, sg.ap[:, SG_FLAG + 1:SG_FLAG + 2], None, ALU.mult, reads=[a_.t, sg.t],
                     writes=[a_.t])
                P.stt(a_.ap, b_.ap, sg.ap[:, SG_FLAG:SG_FLAG + 1], a_.ap, ALU.mult, ALU.add,
                      reads=[a_.t, b_.t, sg.t], writes=[a_.t])
                P.dma("pool", xs3[:, :, NOWN + t0:NOWN + t0 + nt], a_.ap, reads=[a_.t], writes=[txs(NOWN + t0)])
        if last and not final:
            xout3 = xout.rearrange("(kc p) t -> p kc t", p=128)
            for (t0, nt, mj) in mtiles:
                q0 = qpos(t0)
                P.dma("pool", xout3[:, :, q0:q0 + nt], xres.ap[:, :, q0:q0 + nt], reads=[xres.t])
        if last and final:
            P.barrier()
            A.off = M0
            h2f = A.alloc([128, 8, 512], F32, "h2f")
            sq = A.alloc([128, 8, 512], BF16, "sq")
            rstd = A.alloc([128, 512], F32, "rstd")
            tmp = [A.alloc([128, 512], F32, f"tmp{i}") for i in range(2)]
            yout3 = yout.rearrange("(kc p) t -> p kc t", p=128)
            for (t0, nt, mj) in TOK_TILES_OWN:
                P.act(sq.ap[:, :, 0:nt], xres.ap[:, :, t0:t0 + nt], AF.Square, reads=[xres.t], writes=[sq.t])
                pb = ps_next()
                for kc in range(8):
                    P.mm(pb.ap[:, 0:nt], ones_bf.ap, sq.ap[:, kc, 0:nt], kc == 0, kc == 7, reads=[ones_bf.t, sq.t],
                         writes=[pb.t])
                rsqrt_from_psum(rstd.ap[:, 0:nt], pb.ap[:, 0:nt], 1024.0, [pb.t], [rstd.t])
                for kc in range(8):
                    tm_ = tmp[kc % 2]
                    P.tt("dve", tm_.ap[:, 0:nt], xres.ap[:, kc, t0:t0 + nt], rstd.ap[:, 0:nt], ALU.mult,
                         reads=[xres.t, rstd.t], writes=[tm_.t])
                    P.act(h2f.ap[:, kc, 0:nt], tm_.ap[:, 0:nt], AF.Identity, reads=[tm_.t, sg.t], writes=[h2f.t],
                          scale=sg.ap[:, SG_GF + kc:SG_GF + kc + 1])
                P.dma("pool", yout3[:, :, t0:t0 + nt], h2f.ap[:, :, 0:nt], reads=[h2f.t])
    P.emit()
    return nc


_PROGS = {}


def _prog(key, *args, **kw):
    if key not in _PROGS:
        _PROGS[key] = build(*args, **kw)
    return _PROGS[key]


def kernel(**inp):
    inp = {k: np.asarray(v) for k, v in inp.items()}
    x, ctx = inp["x"], inp["ctx"]
    B = x.shape[0]
    cores = [(b, h) for b in range(B) for h in range(2)]
    consts = [make_consts(h) for h in range(2)]
    tabs = [make_tabs(h) for h in range(2)]
    w0 = prep_layer_weights(inp, 0)
    w1 = prep_layer_weights(inp, 1)
    maps = []
    for (b, h) in cores:
        xo = x[b, h * NOWN:(h + 1) * NOWN].T
        xt = x[b, (1 - h) * NOWN:(2 - h) * NOWN].T
        xall = np.ascontiguousarray(np.concatenate([xo, xt, ctx[b].T], axis=1))
        maps.append(dict(xall=xall, tabs=tabs[h], consts=consts[h], sg=make_small_global(inp, b, h),
                         sm0=w0[2], wall0=w0[0], we0=w0[1], sm1=w1[2], wall1=w1[0], we1=w1[1]))
    nc = _prog("fused", [0, 1], [True, False], True)
    r = run_bass_kernel_spmd(nc, maps, core_ids=list(range(len(cores)))).results
    out = np.zeros((B, 2 * NOWN, D), np.float32)
    for i, (b, h) in enumerate(cores):
        out[b, h * NOWN:(h + 1) * NOWN] = np.asarray(r[i]["yout"]).T
    return out
```
